# Optimizing a Trainium2 kernel written in Bass

```python
import math
import jax, jax.numpy as jnp
from jax import lax
import numpy as np

D_MODEL = 1024
BATCH = 1
SEQ = 16384
DEPTH = 4

GRID_W = 64
CTX_LEN = 256
EPS = 1e-6

N_BRANCH = 3
BRANCH_WIDTH = 512

S5_WIDTH = BRANCH_WIDTH
S5_GROUP_CH = 16
S5_GROUPS = S5_WIDTH // S5_GROUP_CH
S5_STATE = 64
S5_DT_MIN = 1e-3
S5_DT_MAX = 1e-1

SSD_WIDTH = BRANCH_WIDTH
SSD_HEAD_DIM = 64
SSD_HEADS = SSD_WIDTH // SSD_HEAD_DIM
SSD_GROUPS = 2
SSD_STATE = 128
SSD_CONV = 5
SSD_CHUNK = 128
SSD_CONV_CH = SSD_WIDTH + 2 * SSD_GROUPS * SSD_STATE

ATTN_HEADS = 8
ATTN_KV_HEADS = 2
ATTN_HEAD_DIM = 64
ATTN_WIDTH = ATTN_HEADS * ATTN_HEAD_DIM
ATTN_WINDOW = 128
ATTN_BLOCK = 128
ROPE_BASE = 10000.0

IN_SIZES = (S5_WIDTH, SSD_WIDTH, SSD_CONV_CH, 2 * SSD_HEADS, ATTN_WIDTH,
            ATTN_KV_HEADS * ATTN_HEAD_DIM, ATTN_KV_HEADS * ATTN_HEAD_DIM, N_BRANCH * D_MODEL)
IN_WIDTH = sum(IN_SIZES)

N_EXPERTS = 16
EXPERT_FF = 1024
EC_CAPACITY_FACTOR = 2

kernel_name = "hybrid_s5_ssd_swa_ec_dit"

F32 = jnp.float32


def rmsnorm(x, g):
    xf = x.astype(F32)
    y = xf * lax.rsqrt(jnp.mean(xf * xf, axis=-1, keepdims=True) + EPS)
    return (y * g.astype(F32)).astype(x.dtype)


def split_cols(t):
    out, start = [], 0
    for s in IN_SIZES:
        out.append(t[..., start:start + s])
        start += s
    return out


def cmul(ar, ai, br, bi):
    return ar * br - ai * bi, ar * bi + ai * br


def complex_affine_combine(e1, e2):
    a1r, a1i, b1r, b1i = e1
    a2r, a2i, b2r, b2i = e2
    ar, ai = cmul(a2r, a2i, a1r, a1i)
    tr, ti = cmul(a2r, a2i, b1r, b1i)
    return ar, ai, tr + b2r, ti + b2i


def s5_discretise(lam_re, lam_im, log_dt, b_re, b_im):
    lr, li = lam_re.astype(F32), lam_im.astype(F32)
    dt = jnp.exp(log_dt.astype(F32))[:, None]
    mag = jnp.exp(lr * dt)
    abr, abi = mag * jnp.cos(li * dt), mag * jnp.sin(li * dt)
    den = lr * lr + li * li
    fr = ((abr - 1.0) * lr + abi * li) / den
    fi = (abi * lr - (abr - 1.0) * li) / den
    bbr, bbi = cmul(fr[..., None], fi[..., None], b_re.astype(F32), b_im.astype(F32))
    return abr, abi, bbr, bbi


def s5_scan(u, abr, abi, bbr, bbi, h0, reverse):
    if reverse:
        u = jnp.flip(u, 1)
    bur = jnp.einsum('gpk,bngk->bngp', bbr, u)
    bui = jnp.einsum('gpk,bngk->bngp', bbi, u)
    ar = jnp.broadcast_to(abr, bur.shape)
    ai = jnp.broadcast_to(abi, bui.shape)
    acr, aci, hr, hi = lax.associative_scan(complex_affine_combine, (ar, ai, bur, bui), axis=1)
    if h0 is not None:
        tr, ti = cmul(acr, aci, h0[0][:, None], h0[1][:, None])
        hr, hi = hr + tr, hi + ti
    return hr, hi


def s5_readout(hr, hi, c_re, c_im, reverse):
    y = (jnp.einsum('gkp,bngp->bngk', c_re.astype(F32), hr)
         - jnp.einsum('gkp,bngp->bngk', c_im.astype(F32), hi))
    return jnp.flip(y, 1) if reverse else y


def s5_branch(u, uc, lam_re, lam_im, log_dt, b_re, b_im, c_re, c_im, d_skip, w_glu, b_glu, need_ctx_out):
    b, n, _ = u.shape
    nc = uc.shape[1]
    uf, ucf = u.astype(F32), uc.astype(F32)
    ug = uf.reshape(b, n, S5_GROUPS, S5_GROUP_CH)
    ugc = ucf.reshape(b, nc, S5_GROUPS, S5_GROUP_CH)
    ys, ycs = [], []
    for direction in range(2):
        rev = direction == 1
        abr, abi, bbr, bbi = s5_discretise(lam_re[direction], lam_im[direction], log_dt[direction],
                                           b_re[direction], b_im[direction])
        hcr, hci = s5_scan(ugc, abr, abi, bbr, bbi, None, rev)
        hr, hi = s5_scan(ug, abr, abi, bbr, bbi, (hcr[:, -1], hci[:, -1]), rev)
        ys.append(s5_readout(hr, hi, c_re[direction], c_im[direction], rev))
        if need_ctx_out:
            ycs.append(s5_readout(hcr, hci, c_re[direction], c_im[direction], rev))
    d = d_skip.astype(F32)

    def glu(yg, uu):
        yy = yg.reshape(uu.shape) + d * uu
        a = jax.nn.gelu(yy)
        return (a * jax.nn.sigmoid(a @ w_glu.astype(F32) + b_glu.astype(F32))).astype(u.dtype)

    y = glu(ys[0] + ys[1], uf)
    yc = glu(ycs[0] + ycs[1], ucf) if need_ctx_out else None
    return y, yc


def dwconv_centred(x, w, bias):
    k = w.shape[0]
    y = lax.conv_general_dilated(x, w[:, None, :], window_strides=(1,), padding=[(k // 2, k // 2)],
                                 dimension_numbers=('NWC', 'WIO', 'NWC'), feature_group_count=x.shape[-1])
    return y + bias


def ssd_scan(x, dt, a, bm, cm, h0, reverse, need_y):
    if reverse:
        x, dt, bm, cm = (jnp.flip(t, 1) for t in (x, dt, bm, cm))
    b, n, h, p = x.shape
    g, nst = bm.shape[2], bm.shape[3]
    r = h // g
    q = SSD_CHUNK
    nch = n // q
    xc = x.reshape(b, nch, q, g, r, p)
    dtc = dt.reshape(b, nch, q, g, r)
    bc = bm.reshape(b, nch, q, g, nst)
    cc = cm.reshape(b, nch, q, g, nst)
    acum = jnp.cumsum(dtc * a.reshape(g, r), axis=2)
    decay_to_end = jnp.exp(acum[:, :, -1:] - acum)
    states = jnp.einsum('bcjgn,bcjgrp->bcgrpn', bc, (decay_to_end * dtc)[..., None] * xc)
    chunk_decay = jnp.exp(acum[:, :, -1])
    if h0 is None:
        h0 = jnp.zeros((b, g, r, p, nst), F32)

    def step(hc, inp):
        s, dec = inp
        return hc * dec[..., None, None] + s, hc

    h_final, h_in = lax.scan(step, h0, (jnp.moveaxis(states, 1, 0), jnp.moveaxis(chunk_decay, 1, 0)))
    if not need_y:
        return None, h_final
    h_in = jnp.moveaxis(h_in, 0, 1)
    causal = jnp.tril(jnp.ones((q, q), bool))
    seg = acum[:, :, :, None] - acum[:, :, None, :]
    lmat = jnp.exp(jnp.where(causal[:, :, None, None], seg, -jnp.inf))
    cb = jnp.einsum('bcign,bcjgn->bcijg', cc, bc)
    wts = cb[..., None] * lmat * dtc[:, :, None]
    y = jnp.einsum('bcijgr,bcjgrp->bcigrp', wts, xc)
    y = y + jnp.einsum('bcign,bcgrpn->bcigrp', cc, h_in) * jnp.exp(acum)[..., None]
    y = y.reshape(b, n, h, p)
    if reverse:
        y = jnp.flip(y, 1)
    return y, h_final


def ssd_branch(z, xbc, dtr, zc, xbcc, dtrc, conv_w, conv_b, a_log, dt_bias, d_skip, norm_g, need_ctx_out):
    a = -jnp.exp(a_log.astype(F32))

    def prep(xbc_, dtr_):
        t = jax.nn.silu(dwconv_centred(xbc_, conv_w, conv_b)).astype(F32)
        b, n, _ = t.shape
        o1 = SSD_WIDTH
        o2 = SSD_WIDTH + SSD_GROUPS * SSD_STATE
        xs = t[..., :o1].reshape(b, n, SSD_HEADS, SSD_HEAD_DIM)
        bm = t[..., o1:o2].reshape(b, n, SSD_GROUPS, SSD_STATE)
        cm = t[..., o2:].reshape(b, n, SSD_GROUPS, SSD_STATE)
        dt = jax.nn.softplus(dtr_.astype(F32).reshape(b, n, 2, SSD_HEADS) + dt_bias.astype(F32))
        return xs, bm, cm, dt

    xs, bm, cm, dt = prep(xbc, dtr)
    xsc, bmc, cmc, dtc = prep(xbcc, dtrc)
    d = d_skip.astype(F32)[:, None]
    y = d * xs
    yc = d * xsc if need_ctx_out else None
    for direction in range(2):
        rev = direction == 1
        y_ctx, h_ctx = ssd_scan(xsc, dtc[:, :, direction], a[direction], bmc, cmc, None, rev, need_ctx_out)
        y_lat, _ = ssd_scan(xs, dt[:, :, direction], a[direction], bm, cm, h_ctx, rev, True)
        y = y + y_lat
        if need_ctx_out:
            yc = yc + y_ctx

    def gated_norm(yy, zz):
        b, n = yy.shape[:2]
        return rmsnorm(yy.reshape(b, n, SSD_WIDTH) * jax.nn.silu(zz.astype(F32)), norm_g).astype(zz.dtype)

    return gated_norm(y, z), (gated_norm(yc, zc) if need_ctx_out else None)


def axial_rope_angles(n):
    rows = n // GRID_W
    row = jnp.repeat(jnp.arange(rows, dtype=F32), GRID_W)
    col = jnp.tile(jnp.arange(GRID_W, dtype=F32), rows)
    m = ATTN_HEAD_DIM // 4
    inv_freq = ROPE_BASE ** (-jnp.arange(m, dtype=F32) / m)
    return row[:, None] * inv_freq, col[:, None] * inv_freq


def rope_axis(x, ang):
    m = ang.shape[-1]
    cos, sin = jnp.cos(ang)[:, None, :], jnp.sin(ang)[:, None, :]
    x1, x2 = x[..., :m], x[..., m:]
    return jnp.concatenate([x1 * cos - x2 * sin, x2 * cos + x1 * sin], axis=-1)


def axial_rope(x, ang_r, ang_c):
    half = x.shape[-1] // 2
    return jnp.concatenate([rope_axis(x[..., :half], ang_r), rope_axis(x[..., half:], ang_c)], axis=-1)


def window_attention(q, k, v, kc, vc, sink):
    b, n, hk, r, dh = q.shape
    blk = ATTN_BLOCK
    nb = n // blk
    scale = dh ** -0.5
    qb = q.reshape(b, nb, blk, hk, r, dh)

    def band(t):
        tp = jnp.pad(t, ((0, 0), (blk, blk), (0, 0), (0, 0))).reshape(b, nb + 2, blk, hk, dh)
        return jnp.concatenate([tp[:, :-2], tp[:, 1:-1], tp[:, 2:]], axis=2)

    kw, vw = band(k), band(v)
    s_loc = jnp.einsum('bnqhrd,bnkhd->bnhrqk', qb, kw) * scale
    qi = jnp.arange(blk)
    kj = jnp.arange(3 * blk) - blk
    rel = jnp.abs(qi[:, None] - kj[None, :]) <= ATTN_WINDOW
    kpos = jnp.arange(nb)[:, None] * blk + kj[None, :]
    valid = (kpos >= 0) & (kpos < n)
    mask = rel[None] & valid[:, None, :]
    s_loc = jnp.where(mask[None, :, None, None], s_loc, -jnp.inf)
    s_ctx = jnp.einsum('bnqhrd,bkhd->bnhrqk', qb, kc) * scale
    s_sink = jnp.broadcast_to(sink[None, None, :, :, None, None], s_loc.shape[:-1] + (1,))
    p = jax.nn.softmax(jnp.concatenate([s_loc, s_ctx, s_sink], axis=-1), axis=-1)
    nk = 3 * blk
    o = (jnp.einsum('bnhrqk,bnkhd->bnqhrd', p[..., :nk], vw)
         + jnp.einsum('bnhrqk,bkhd->bnqhrd', p[..., nk:nk + kc.shape[1]], vc))
    return o.reshape(b, n, hk * r * dh)


def context_attention(qc, kc, vc, sink):
    b, nc, hk, r, dh = qc.shape
    s = jnp.einsum('bqhrd,bkhd->bhrqk', qc, kc) * dh ** -0.5
    s_sink = jnp.broadcast_to(sink[None, :, :, None, None], s.shape[:-1] + (1,))
    p = jax.nn.softmax(jnp.concatenate([s, s_sink], axis=-1), axis=-1)[..., :kc.shape[1]]
    return jnp.einsum('bhrqk,bkhd->bqhrd', p, vc).reshape(b, nc, hk * r * dh)


def attention_branch(q, k, v, qc, kc, vc, sink, need_ctx_out):
    b, n, _ = q.shape
    nc = kc.shape[1]
    hk, r, dh = ATTN_KV_HEADS, ATTN_HEADS // ATTN_KV_HEADS, ATTN_HEAD_DIM
    ang_r, ang_c = axial_rope_angles(n)
    qh = axial_rope(q.astype(F32).reshape(b, n, ATTN_HEADS, dh), ang_r, ang_c).reshape(b, n, hk, r, dh)
    kh = axial_rope(k.astype(F32).reshape(b, n, hk, dh), ang_r, ang_c)
    vh = v.astype(F32).reshape(b, n, hk, dh)
    kch = kc.astype(F32).reshape(b, nc, hk, dh)
    vch = vc.astype(F32).reshape(b, nc, hk, dh)
    sink_hr = sink.astype(F32).reshape(hk, r)
    y = window_attention(qh, kh, vh, kch, vch, sink_hr).astype(q.dtype)
    yc = None
    if need_ctx_out:
        qch = qc.astype(F32).reshape(b, nc, hk, r, dh)
        yc = context_attention(qch, kch, vch, sink_hr).astype(q.dtype)
    return y, yc


def mixing_sublayer(h, hc, w_in, lam_re, lam_im, log_dt, b_re, b_im, c_re, c_im, s5_d, w_glu, b_glu,
                    conv_w, conv_b, a_log, dt_bias, ssd_d, ssd_norm_g, sink, w_branch, w_out, need_ctx_out):
    u, z, xbc, dtr, q, k, v, gates = split_cols(h @ w_in)
    uc, zc, xbcc, dtrc, qc, kc, vc, gatesc = split_cols(hc @ w_in)
    ya, yac = s5_branch(u, uc, lam_re, lam_im, log_dt, b_re, b_im, c_re, c_im, s5_d, w_glu, b_glu, need_ctx_out)
    yb, ybc = ssd_branch(z, xbc, dtr, zc, xbcc, dtrc, conv_w, conv_b, a_log, dt_bias, ssd_d, ssd_norm_g,
                         need_ctx_out)
    yc_, ycc = attention_branch(q, k, v, qc, kc, vc, sink, need_ctx_out)

    def merge(y1, y2, y3, g):
        b, n = g.shape[:2]
        br = jnp.einsum('bnkc,kcd->bnkd', jnp.stack([y1, y2, y3], axis=2), w_branch)
        gate = jax.nn.sigmoid(g.reshape(b, n, N_BRANCH, D_MODEL).astype(F32)).astype(br.dtype)
        return jnp.einsum('bnkd,bnkd->bnd', gate, br) @ w_out

    out = merge(ya, yb, yc_, gates)
    outc = merge(yac, ybc, ycc, gatesc) if need_ctx_out else None
    return out, outc


def expert_choice_ffn(h, w_router, w_gate, w_up, w_down):
    b, n, d = h.shape
    cap = EC_CAPACITY_FACTOR * n // N_EXPERTS
    aff = jax.nn.softmax(jnp.einsum('bnd,de->bne', h, w_router).astype(F32), axis=-1)
    g, idx = lax.top_k(jnp.swapaxes(aff, 1, 2), cap)
    xe = jax.vmap(lambda hb, ib: hb[ib])(h, idx)
    hid = jax.nn.silu(jnp.einsum('becd,edf->becf', xe, w_gate)) * jnp.einsum('becd,edf->becf', xe, w_up)
    ye = jnp.einsum('becf,efd->becd', hid, w_down) * g[..., None].astype(h.dtype)

    def combine(ib, yb):
        return jnp.zeros((n, d), yb.dtype).at[ib.reshape(-1)].add(yb.reshape(-1, d))

    return jax.vmap(combine)(idx, ye)


def setup_inputs(seed: int = 0) -> dict:
    key = jax.random.key(seed)
    keys = iter(jax.random.split(key, 64))
    D = D_MODEL

    def normal(shape, scale):
        return jax.random.normal(next(keys), shape, F32) * scale

    def uniform(shape, lo, hi):
        return jax.random.uniform(next(keys), shape, F32, lo, hi)

    n_idx = jnp.arange(S5_STATE, dtype=F32)
    ssd_dt = jnp.exp(uniform((DEPTH, 2, SSD_HEADS), math.log(1e-3), math.log(1e-1)))
    s5_shape = (DEPTH, 2, S5_GROUPS, S5_STATE)
    return {
        "x": normal((BATCH, SEQ, D), 1.0),
        "c": normal((BATCH, D), 1.0),
        "ctx": normal((BATCH, CTX_LEN, D), 1.0),
        "c_ctx": normal((D,), 1.0),
        "w_mod": normal((DEPTH, D, 6 * D), 0.5 * D ** -0.5),
        "b_mod": normal((DEPTH, 6 * D), 0.01),
        "norm1_g": 1.0 + normal((DEPTH, D), 0.02),
        "norm2_g": 1.0 + normal((DEPTH, D), 0.02),
        "w_in": normal((DEPTH, D, IN_WIDTH), D ** -0.5),
        "s5_lam_re": -0.5 + normal(s5_shape, 0.01),
        "s5_lam_im": math.pi * n_idx + normal(s5_shape, 0.01),
        "s5_log_dt": uniform((DEPTH, 2, S5_GROUPS), math.log(S5_DT_MIN), math.log(S5_DT_MAX)),
        "s5_b_re": normal((DEPTH, 2, S5_GROUPS, S5_STATE, S5_GROUP_CH), (2 * S5_GROUP_CH) ** -0.5),
        "s5_b_im": normal((DEPTH, 2, S5_GROUPS, S5_STATE, S5_GROUP_CH), (2 * S5_GROUP_CH) ** -0.5),
        "s5_c_re": normal((DEPTH, 2, S5_GROUPS, S5_GROUP_CH, S5_STATE), S5_STATE ** -0.5),
        "s5_c_im": normal((DEPTH, 2, S5_GROUPS, S5_GROUP_CH, S5_STATE), S5_STATE ** -0.5),
        "s5_d": normal((DEPTH, S5_WIDTH), 0.5),
        "s5_w_glu": normal((DEPTH, S5_WIDTH, S5_WIDTH), S5_WIDTH ** -0.5),
        "s5_b_glu": normal((DEPTH, S5_WIDTH), 0.01),
        "ssd_conv_w": normal((DEPTH, SSD_CONV, SSD_CONV_CH), SSD_CONV ** -0.5),
        "ssd_conv_b": normal((DEPTH, SSD_CONV_CH), 0.01),
        "ssd_a_log": jnp.log(uniform((DEPTH, 2, SSD_HEADS), 1.0, 16.0)),
        "ssd_dt_bias": ssd_dt + jnp.log(-jnp.expm1(-ssd_dt)),
        "ssd_d": 1.0 + normal((DEPTH, SSD_HEADS), 0.1),
        "ssd_norm_g": 1.0 + normal((DEPTH, SSD_WIDTH), 0.02),
        "attn_sink": normal((DEPTH, ATTN_HEADS), 0.5),
        "w_branch": normal((DEPTH, N_BRANCH, BRANCH_WIDTH, D), BRANCH_WIDTH ** -0.5),
        "w_out": normal((DEPTH, D, D), D ** -0.5),
        "w_router": normal((DEPTH, D, N_EXPERTS), D ** -0.5),
        "w_e_gate": normal((DEPTH, N_EXPERTS, D, EXPERT_FF), D ** -0.5),
        "w_e_up": normal((DEPTH, N_EXPERTS, D, EXPERT_FF), D ** -0.5),
        "w_e_down": normal((DEPTH, N_EXPERTS, EXPERT_FF, D), EXPERT_FF ** -0.5),
        "final_norm_g": 1.0 + normal((D,), 0.02),
    }


def reference(x, c, ctx, c_ctx, w_mod, b_mod, norm1_g, norm2_g, w_in, s5_lam_re, s5_lam_im, s5_log_dt,
              s5_b_re, s5_b_im, s5_c_re, s5_c_im, s5_d, s5_w_glu, s5_b_glu, ssd_conv_w, ssd_conv_b,
              ssd_a_log, ssd_dt_bias, ssd_d, ssd_norm_g, attn_sink, w_branch, w_out, w_router,
              w_e_gate, w_e_up, w_e_down, final_norm_g):
    D = D_MODEL
    xc = ctx
    silu_c = jax.nn.silu(c)
    silu_cc = jax.nn.silu(c_ctx)
    for i in range(DEPTH):
        ctx_out = i < DEPTH - 1
        mod = silu_c @ w_mod[i] + b_mod[i]
        modc = silu_cc @ w_mod[i] + b_mod[i]
        sh1, sc1, g1, sh2, sc2, g2 = [mod[:, None, j * D:(j + 1) * D] for j in range(6)]
        csh1, csc1, cg1, csh2, csc2, cg2 = [modc[j * D:(j + 1) * D] for j in range(6)]

        h = rmsnorm(x, norm1_g[i]) * (1.0 + sc1) + sh1
        hc = rmsnorm(xc, norm1_g[i]) * (1.0 + csc1) + csh1
        mix, mixc = mixing_sublayer(
            h, hc, w_in[i], s5_lam_re[i], s5_lam_im[i], s5_log_dt[i], s5_b_re[i], s5_b_im[i],
            s5_c_re[i], s5_c_im[i], s5_d[i], s5_w_glu[i], s5_b_glu[i], ssd_conv_w[i], ssd_conv_b[i],
            ssd_a_log[i], ssd_dt_bias[i], ssd_d[i], ssd_norm_g[i], attn_sink[i], w_branch[i], w_out[i],
            ctx_out)
        x = x + g1 * mix
        h2 = rmsnorm(x, norm2_g[i]) * (1.0 + sc2) + sh2
        x = x + g2 * expert_choice_ffn(h2, w_router[i], w_e_gate[i], w_e_up[i], w_e_down[i])
        if ctx_out:
            xc = xc + cg1 * mixc
            hc2 = rmsnorm(xc, norm2_g[i]) * (1.0 + csc2) + csh2
            xc = xc + cg2 * expert_choice_ffn(hc2, w_router[i], w_e_gate[i], w_e_up[i], w_e_down[i])
    return rmsnorm(x, final_norm_g)
```

```python
import ml_dtypes
import contextlib
import numpy as np
import concourse.bass as bass
import concourse.mybir as mybir
from concourse.bass_utils import run_bass_kernel_spmd

F32 = mybir.dt.float32
BF16 = mybir.dt.bfloat16
I32 = mybir.dt.int32
AF = mybir.ActivationFunctionType
ALU = mybir.AluOpType
AX = mybir.AxisListType

ENGS = ("pe", "dve", "act", "pool", "sp")
EPOCH = 20000
N_DMA_SEMS = 12


def _region(ap):
    t = ap.tensor
    shape = list(t.shape)
    space = str(ap.space) if hasattr(ap, "space") else ""
    dims = list(ap.ap)
    off = int(ap.offset)
    if "DRAM" in space.upper() or "HBM" in space.upper() or type(t).__name__.startswith("DRam"):
        lo = off
        hi = off + sum((c - 1) * abs(s) for s, c in dims) + 1
        return (t.name, 0, 1, lo, hi)
    row = 1
    for s in shape[1:]:
        row *= int(s)
    if type(t).__name__.startswith("PSum"):
        return (t.name, 0, 128, 0, row)
    p_lo = off // row
    f_lo = off % row
    p_hi = p_lo + int(dims[0][1])
    f_hi = f_lo + sum((c - 1) * abs(s) for s, c in dims[1:]) + 1
    return (t.name, p_lo, p_hi, f_lo, f_hi)


def _ovl(a, b):
    return a[1] < b[2] and b[1] < a[2] and a[3] < b[4] and b[3] < a[4]


def _covers(a, b):
    return a[1] <= b[1] and a[2] >= b[2] and a[3] <= b[3] and a[4] >= b[4]


class Prog:
    def __init__(self, nc, same_engine_sync=True):
        self.nc = nc
        self.es = contextlib.ExitStack()
        self.ops = {e: [] for e in ENGS}
        self.nops = {e: 0 for e in ENGS}
        self.writes = {}
        self.reads = {}
        self.same_engine_sync = same_engine_sync
        self.dma_tot = [0] * N_DMA_SEMS
        self.dma_rr = 0
        self.known = {e: {} for e in ENGS}
        self._names = 0

    def init_arenas(self, n_f32, n_bf16):
        self.arena = {F32: self.es.enter_context(self.nc.sbuf_tensor("arena_f32", [128, n_f32], F32)),
                      BF16: self.es.enter_context(self.nc.sbuf_tensor("arena_bf16", [128, n_bf16], BF16))}
        self.asize = {F32: n_f32, BF16: n_bf16}
        self.atop = {F32: 0, BF16: 0}
        self.amax = {F32: 0, BF16: 0}
        self.astack = []

    def push(self):
        self.astack.append(dict(self.atop))

    def pop(self):
        self.atop = self.astack.pop()

    def sb(self, shape, dtype=F32, name=None):
        n = 1
        for s_ in shape[1:]:
            n *= int(s_)
        n = (n + 15) // 16 * 16
        off = self.atop[dtype]
        assert off + n <= self.asize[dtype], f"arena {dtype} overflow: {off}+{n} > {self.asize[dtype]} ({name})"
        self.atop[dtype] = off + n
        self.amax[dtype] = max(self.amax[dtype], off + n)
        nn = 1
        for s_ in shape[1:]:
            nn *= int(s_)
        v = self.arena[dtype][:, off:off + nn]
        if len(shape) > 2:
            names = [f"d{i}" for i in range(len(shape) - 1)]
            pat = "p (" + " ".join(names) + ") -> p " + " ".join(names)
            v = v.rearrange(pat, **{nm: int(sz) for nm, sz in zip(names[1:], shape[2:])})
        if shape[0] < 128:
            v = v[0:shape[0]]
        return v

    def ps(self, shape, dtype=F32, name=None):
        self._names += 1
        name = name or f"ps{self._names}"
        return self.es.enter_context(self.nc.psum_tensor(name, list(shape), dtype))

    def _deps(self, reads, writes):
        deps = set()
        rr = [_region(a) for a in reads]
        wr = [_region(a) for a in writes]
        for r in rr:
            for (reg, ev) in self.writes.get(r[0], ()):
                if _ovl(reg, r):
                    deps.add(ev)
            if r[0].startswith("ps"):
                for (reg, ev) in self.reads.get(r[0], ()):
                    deps.add(ev)
        for w in wr:
            for (reg, ev) in self.writes.get(w[0], ()):
                if _ovl(reg, w):
                    deps.add(ev)
            for (reg, ev) in self.reads.get(w[0], ()):
                if _ovl(reg, w):
                    deps.add(ev)
        return deps, rr, wr

    def _record(self, rr, wr, ev):
        for w in wr:
            lst = self.writes.setdefault(w[0], [])
            lst[:] = [(reg, e) for (reg, e) in lst if not _covers(w, reg)]
            lst.append((w, ev))
            rl = self.reads.get(w[0])
            if rl:
                rl[:] = [(reg, e) for (reg, e) in rl if not _covers(w, reg)]
        for r in rr:
            lst = self.reads.setdefault(r[0], [])
            lst[:] = [(reg, e) for (reg, e) in lst if not (e[0] == ev[0] and _covers(r, reg))]
            lst.append((r, ev))

    def op(self, eng, fn, reads, writes, pe_accum=False):
        deps, rr, wr = self._deps(reads, writes)
        idx = self.nops[eng]
        self.nops[eng] += 1
        ev = ((eng, idx // EPOCH), idx % EPOCH + 1)
        waits = self._filter(eng, deps, pe_accum)
        self.ops[eng].append((fn, waits, ev, False))
        self._record(rr, wr, ev)
        return ev

    def _filter(self, eng, deps, pe_accum=False):
        best = {}
        for (s, v) in deps:
            if s[0] == eng:
                if eng == "pe" or not self.same_engine_sync:
                    continue
            if best.get(s, 0) < v:
                best[s] = v
        out = []
        kn = self.known[eng]
        for s, v in best.items():
            if kn.get(s, 0) >= v:
                continue
            kn[s] = v
            out.append((s, v))
        return out

    def dma(self, out, in_, q="sp", **kw):
        deps, rr, wr = self._deps([in_], [out])
        k = self.dma_rr
        self.dma_rr = (self.dma_rr + 1) % N_DMA_SEMS
        sem = ("dma", k)
        prev = self.dma_tot[k]
        if prev:
            deps.add((sem, prev))
        self.dma_tot[k] += 16
        ev = (sem, self.dma_tot[k])
        waits = self._filter(q, deps)
        self.nops[q] += 0
        self.ops[q].append((lambda e, o=out, i=in_, kw=kw: e.dma_start(out=o, in_=i, **kw), waits, ev, True))
        self._record(rr, wr, ev)
        return ev

    def allgather(self, out, in_, n=8):
        deps, rr, wr = self._deps([in_], [out])
        self.cc_tot = getattr(self, "cc_tot", 0) + 1
        ev = (("cc", 0), self.cc_tot)
        if self.cc_tot > 1:
            deps.add((("cc", 0), self.cc_tot - 1))
        waits = self._filter("pool", deps)
        self.ops["pool"].append((lambda e, o=out, i=in_: e.collective_compute(
            "AllGather", ALU.bypass, replica_groups=[list(range(n))], ins=[i], outs=[o]), waits, ev, "cc"))
        self._record(rr, wr, ev)
        return ev

    def wait_all(self, eng="sp"):
        deps = set()
        for lst in self.writes.values():
            for (_, ev) in lst:
                deps.add(ev)
        waits = self._filter(eng, deps)
        self.ops[eng].append((None, waits, None, False))

    def mm(self, out, lhsT, rhs, start=True, stop=True):
        rd = [lhsT, rhs] + ([] if start else [])
        return self.op("pe", lambda e: e.matmul(out, lhsT, rhs, start=start, stop=stop), rd, [out])

    def transpose(self, out, in_, ident):
        return self.op("pe", lambda e: e.transpose(out, in_, ident), [in_, ident], [out])

    def act(self, out, in_, func, bias=0.0, scale=1.0, accum_out=None):
        rd = [in_] + [a for a in (bias, scale) if not isinstance(a, (int, float))]
        wr = [out] + ([accum_out] if accum_out is not None else [])
        kw = {}
        if accum_out is not None:
            kw["accum_out"] = accum_out
        return self.op("act", lambda e: e.activation(out, in_, func, bias=bias, scale=scale, **kw), rd, wr)

    def tt(self, out, in0, in1, op, eng="dve"):
        return self.op(eng, lambda e: e.tensor_tensor(out, in0, in1, op), [in0, in1], [out])

    def ts(self, out, in0, s1, s2=None, op0=ALU.mult, op1=None, eng="dve", accum_out=None):
        rd = [in0] + [a for a in (s1, s2) if a is not None and not isinstance(a, (int, float))]
        wr = [out] + ([accum_out] if accum_out is not None else [])
        kw = {}
        if op1 is not None:
            kw["op1"] = op1
        if accum_out is not None:
            kw["accum_out"] = accum_out
        return self.op(eng, lambda e: e.tensor_scalar(out, in0, s1, s2, op0, **kw), rd, wr)

    def stt(self, out, in0, scalar, in1, op0, op1, eng="dve"):
        rd = [in0, in1] + ([] if isinstance(scalar, (int, float)) else [scalar])
        return self.op(eng, lambda e: e.scalar_tensor_tensor(out, in0, scalar, in1, op0, op1), rd, [out])

    def copy(self, out, in_, eng="dve"):
        if eng == "act":
            return self.op("act", lambda e: e.copy(out, in_), [in_], [out])
        return self.op(eng, lambda e: e.tensor_copy(out, in_), [in_], [out])

    def memset(self, ap, val, eng="dve"):
        return self.op(eng, lambda e: e.memset(ap, val), [], [ap])

    def reduce(self, out, in_, op=ALU.add, axis=AX.X, eng="dve"):
        return self.op(eng, lambda e: e.tensor_reduce(out, in_, axis, op), [in_], [out])

    def recip(self, out, in_):
        return self.op("dve", lambda e: e.reciprocal(out, in_), [in_], [out])

    def scan(self, out, d0, d1, initial, op0=ALU.mult, op1=ALU.add):
        rd = [d0, d1] + ([] if isinstance(initial, (int, float)) else [initial])
        return self.op("dve", lambda e: e.tensor_tensor_scan(out, d0, d1, initial, op0, op1), rd, [out])

    def emit(self):
        nc = self.nc
        sems = {}
        for e in ("pe", "dve", "act", "pool"):
            n_ep = (self.nops[e] + EPOCH - 1) // EPOCH
            for k in range(max(n_ep, 1)):
                sems[(e, k)] = self.es.enter_context(nc.semaphore(f"s_{e}{k}"))
        sems[("cc", 0)] = self.es.enter_context(nc.semaphore("s_cc"))
        for k in range(N_DMA_SEMS):
            sems[("dma", k)] = self.es.enter_context(nc.semaphore(f"s_dma{k}"))
        block = self.es.enter_context(nc.Block())

        def run(engobj, lst):
            for (fn, waits, ev, is_dma) in lst:
                for (s, v) in waits:
                    engobj.wait_ge(sems[s], v)
                if fn is None:
                    continue
                ins = fn(engobj)
                if is_dma == "cc":
                    ins.then_inc(sems[ev[0]])
                elif is_dma:
                    ins.then_inc(sems[ev[0]], 16)
                else:
                    ins.then_inc(sems[ev[0]], 1)

        ops = self.ops

        @block.tensor
        def _(t):
            run(t, ops["pe"])

        @block.vector
        def _(v):
            run(v, ops["dve"])

        @block.scalar
        def _(s):
            run(s, ops["act"])

        @block.gpsimd
        def _(g):
            run(g, ops["pool"])

        @block.sync
        def _(sy):
            run(sy, ops["sp"])

    def close(self):
        self.es.close()


import math
import numpy as np

PI = math.pi
D = 1024
KD = 8
NCTX = 256
HALO = 128
IN_W = 5904
C_U, C_Z, C_XBC, C_DT, C_Q, C_K, C_V, C_G = 0, 512, 1024, 2048, 2064, 2576, 2704, 2832


def V(ap, free_dims, off=0):
    return bass.AP(ap.tensor, ap.offset + off, [list(ap.ap[0])] + [list(d) for d in free_dims])


def DV(t, off, dims):
    return bass.AP(t.tensor, t.offset + off, [list(d) for d in dims])


class G:
    pass


def host_consts():
    c = {}
    c["ident"] = np.eye(128, dtype=np.float32)
    c["ut"] = np.triu(np.ones((128, 128), np.float32))
    c["lt"] = np.tril(np.ones((128, 128), np.float32))
    d = np.arange(128, dtype=np.float32)
    exl = np.tile(d[None, :], (128, 1))
    exr = np.concatenate([np.tile((d + 1)[None, :], (64, 1)), np.tile((128 - d)[None, :], (64, 1))], 0)
    c["exl"] = exl.astype(np.float32)
    c["exr"] = exr.astype(np.float32)
    exv = np.stack([127 - d, d], 1)
    c["exv"] = exv.astype(np.float32)
    m = np.zeros((128, 8), np.float32)
    for p in range(128):
        m[p, p // 16] = 1.0
    c["maskbd"] = m
    return c


CONST_SHAPES = {"ident": [128, 128], "ut": [128, 128], "lt": [128, 128], "exl": [128, 128], "exr": [128, 128],
                "exv": [128, 2], "maskbd": [128, 8]}


def load_consts(g):
    P = g.P
    g.c = {}
    for k, shp in CONST_SHAPES.items():
        t = P.sb(shp, F32)
        P.dma(t, g.dram[k])
        g.c[k] = t
    g.ident_bf = P.sb([128, 128], BF16)
    P.copy(g.ident_bf, g.c["ident"])
    g.ones_bf = P.sb([128, 128], BF16)
    P.memset(g.ones_bf, 1.0)
    g.c["ones_f"] = P.sb([128, 128], F32)
    P.memset(g.c["ones_f"], 1.0)


def sincos(P, out_cos, out_sin, ang, tmp):
    n = 1
    for d_ in ang.shape[1:]:
        n *= int(d_)
    if not hasattr(P, "_kint"):
        P._kint = P.es.enter_context(P.nc.sbuf_tensor("kint", [128, 2048], I32))
        P._halfpi = P.sb([128, 1], F32)
        P.memset(P._halfpi, PI / 2)
    ki = bass.AP(P._kint[:, 0:n].tensor, P._kint[:, 0:n].offset, [list(ang.ap[0])[:1] + [ang.ap[0][1]]] and [[P._kint[:, 0:n].ap[0][0], ang.ap[0][1]], [1, n]])
    angf = bass.AP(ang.tensor, ang.offset, [list(ang.ap[0]), [1, n]])
    tmpf = bass.AP(tmp.tensor, tmp.offset, [list(tmp.ap[0]), [1, n]])
    cosf = bass.AP(out_cos.tensor, out_cos.offset, [list(out_cos.ap[0]), [1, n]])
    sinf = bass.AP(out_sin.tensor, out_sin.offset, [list(out_sin.ap[0]), [1, n]])
    pp = slice(0, 128)
    P.ts(ki, angf, 1.0 / (2 * PI), 0.25, op0=ALU.mult, op1=ALU.add)
    P.stt(tmpf, ki, -2 * PI, angf, ALU.mult, ALU.add)
    P.act(cosf, tmpf, AF.Sin, bias=P._halfpi[0:ang.ap[0][1]] if ang.ap[0][1] < 128 else P._halfpi, scale=1.0)
    P.ts(ki, angf, 1.0 / (2 * PI), None, op0=ALU.mult)
    P.stt(tmpf, ki, -2 * PI, angf, ALU.mult, ALU.add)
    P.act(sinf, tmpf, AF.Sin)


def s5_params(g):
    P = g.P
    dr = g.dram
    s = G()
    g.s5 = s
    s.lrc = P.sb([128, 32]); s.lic = P.sb([128, 32]); s.dtc = P.sb([128, 32])
    for d in range(2):
        P.dma(s.lrc[d * 64:(d + 1) * 64], DV(dr["s5_lam_re"], d * 2048, [[1, 64], [64, 32]]), allow_slow_non_contiguous=True)
        P.dma(s.lic[d * 64:(d + 1) * 64], DV(dr["s5_lam_im"], d * 2048, [[1, 64], [64, 32]]), allow_slow_non_contiguous=True)
        P.dma(s.dtc[d * 64:(d + 1) * 64], DV(dr["s5_log_dt"], d * 32, [[0, 64], [1, 32]]))
    P.act(s.dtc, s.dtc, AF.Exp)
    s.thc = P.sb([128, 32]); s.lrdtc = P.sb([128, 32])
    P.tt(s.thc, s.lic, s.dtc, ALU.mult)
    P.tt(s.lrdtc, s.lrc, s.dtc, ALU.mult)
    mag = P.sb([128, 32]); co = P.sb([128, 32]); si = P.sb([128, 32]); tmp = P.sb([128, 32])
    P.act(mag, s.lrdtc, AF.Exp)
    sincos(P, co, si, s.thc, tmp)
    abr = P.sb([128, 32]); abi = P.sb([128, 32])
    P.tt(abr, mag, co, ALU.mult)
    P.tt(abi, mag, si, ALU.mult)
    den = P.sb([128, 32]); t2 = P.sb([128, 32])
    P.tt(den, s.lrc, s.lrc, ALU.mult)
    P.tt(t2, s.lic, s.lic, ALU.mult)
    P.tt(den, den, t2, ALU.add)
    P.recip(den, den)
    am1 = P.sb([128, 32])
    P.ts(am1, abr, -1.0, None, op0=ALU.add)
    fr = P.sb([128, 32]); fi = P.sb([128, 32])
    P.tt(fr, am1, s.lrc, ALU.mult); P.tt(t2, abi, s.lic, ALU.mult); P.tt(fr, fr, t2, ALU.add); P.tt(fr, fr, den, ALU.mult)
    P.tt(fi, abi, s.lrc, ALU.mult); P.tt(t2, am1, s.lic, ALU.mult); P.tt(fi, fi, t2, ALU.subtract); P.tt(fi, fi, den, ALU.mult)
    bre = P.sb([128, 32, 16]); bim = P.sb([128, 32, 16])
    s.cre = P.sb([128, 32, 16]); s.cim = P.sb([128, 32, 16])
    for d in range(2):
        sl = slice(d * 64, (d + 1) * 64)
        P.dma(bre[sl], DV(dr["s5_b_re"], d * 32768, [[16, 64], [1024, 32], [1, 16]]))
        P.dma(bim[sl], DV(dr["s5_b_im"], d * 32768, [[16, 64], [1024, 32], [1, 16]]))
        P.dma(s.cre[sl], DV(dr["s5_c_re"], d * 32768, [[1, 64], [1024, 32], [64, 16]]), allow_slow_non_contiguous=True)
        P.dma(s.cim[sl], DV(dr["s5_c_im"], d * 32768, [[1, 64], [1024, 32], [64, 16]]), allow_slow_non_contiguous=True)
    s.bbr = P.sb([128, 32, 16]); s.bbi = P.sb([128, 32, 16])
    frb = V(fr, [[1, 32], [0, 16]]); fib = V(fi, [[1, 32], [0, 16]])
    t3 = P.sb([128, 32, 16])
    P.tt(s.bbr, bre, frb, ALU.mult); P.tt(t3, bim, fib, ALU.mult); P.tt(s.bbr, s.bbr, t3, ALU.subtract)
    P.tt(s.bbi, bim, frb, ALU.mult); P.tt(t3, bre, fib, ALU.mult); P.tt(s.bbi, s.bbi, t3, ALU.add)
    return s


def s5_gen_E(g, ex, out_re, out_im, neg_im=False):
    P = g.P
    s = g.s5
    P.push()
    GH = 16
    ang = P.sb([128, GH, 128]); tmp = P.sb([128, GH, 128]); mag = P.sb([128, GH, 128]); co = P.sb([128, GH, 128])
    exb = V(ex, [[0, GH], [1, 128]])
    for h in range(32 // GH):
        gs = slice(h * GH, (h + 1) * GH)
        P.tt(ang, V(s.thc[:, gs], [[1, GH], [0, 128]]), exb, ALU.mult)
        P.tt(mag, V(s.lrdtc[:, gs], [[1, GH], [0, 128]]), exb, ALU.mult, eng="pool")
        P.act(mag, mag, AF.Exp)
        sincos(P, co, ang, ang, tmp)
        P.tt(out_re[:, gs, :], mag, co, ALU.mult)
        if neg_im:
            P.stt(out_im[:, gs, :], mag, -1.0, ang, ALU.mult, ALU.mult)
        else:
            P.tt(out_im[:, gs, :], mag, ang, ALU.mult)
    P.pop()


def s5_gen_V(g, vr, vi):
    P = g.P
    dr = g.dram
    P.push()
    GH = 16
    lib = P.sb([128, GH, 2, 64]); lrb = P.sb([128, GH, 2, 64]); dtb = P.sb([128, GH, 2])
    tmp = P.sb([128, GH, 2, 64]); co = P.sb([128, GH, 2, 64])
    exvb = V(g.c["exv"], [[0, GH], [1, 2], [0, 64]])
    for h in range(32 // GH):
        g0 = h * GH
        for d_ in range(2):
            P.dma(lib[:, :, d_, :], DV(dr["s5_lam_im"], g0 * 64 + d_ * 2048, [[0, 128], [64, GH], [1, 64]]))
            P.dma(lrb[:, :, d_, :], DV(dr["s5_lam_re"], g0 * 64 + d_ * 2048, [[0, 128], [64, GH], [1, 64]]))
            P.dma(dtb[:, :, d_], DV(dr["s5_log_dt"], g0 + d_ * 32, [[0, 128], [1, GH]]), allow_slow_non_contiguous=True)
        P.act(dtb, dtb, AF.Exp)
        dtbb = V(dtb, [[2, GH], [1, 2], [0, 64]])
        P.tt(lib, lib, dtbb, ALU.mult)
        P.tt(lrb, lrb, dtbb, ALU.mult, eng="pool")
        P.tt(lib, lib, exvb, ALU.mult)
        P.tt(lrb, lrb, exvb, ALU.mult, eng="pool")
        P.act(lrb, lrb, AF.Exp)
        sincos(P, co, lib, lib, tmp)
        gs = slice(g0, g0 + GH)
        P.tt(vr[:, gs, :], lrb, co, ALU.mult)
        P.tt(vi[:, gs, :], lrb, lib, ALU.mult)
    P.pop()


def cmul_acc(P, out_re, out_im, ar, ai, hr, hi, sr, si, t1, t2, eng="dve"):
    P.tt(t1, ar, hr, ALU.mult, eng=eng)
    P.tt(t2, ai, hi, ALU.mult, eng=eng)
    P.tt(t1, t1, t2, ALU.subtract, eng=eng)
    if sr is not None:
        P.tt(out_re, t1, sr, ALU.add, eng=eng)
    else:
        P.copy(out_re, t1, eng=eng)
    P.tt(t1, ar, hi, ALU.mult, eng=eng)
    P.tt(t2, ai, hr, ALU.mult, eng=eng)
    P.tt(t1, t1, t2, ALU.add, eng=eng)
    if si is not None:
        P.tt(out_im, t1, si, ALU.add, eng=eng)
    else:
        P.copy(out_im, t1, eng=eng)


def s5_states(g, uT, NCH):
    P = g.P
    s = g.s5
    s.sre = P.sb([128, 32, NCH]); s.sim = P.sb([128, 32, NCH])
    P.push()
    vr = P.sb([128, 32, 128], BF16); vi = P.sb([128, 32, 128], BF16)
    s5_gen_V(g, V(vr, [[128, 32], [64, 2], [1, 64]]), V(vi, [[128, 32], [64, 2], [1, 64]]))
    utok = P.sb([128, NCH, 512], BF16)
    for c in range(NCH):
        pt = g.pbank_bf()
        for b in range(4):
            P.transpose(pt[:, b * 128:(b + 1) * 128], uT[:, b, c * 128:(c + 1) * 128], g.ident_bf)
        P.copy(utok[:, c, :], pt[:, 0:512], eng="act" if c % 2 else "dve")
    zr = P.sb([128, NCH, 16]); zi = P.sb([128, NCH, 16]); t1 = P.sb([128, NCH, 16]); t2 = P.sb([128, NCH, 16])
    t3 = P.sb([128, NCH, 16]); t4 = P.sb([128, NCH, 16])
    N = NCH * 16
    for gi in range(32):
        p1 = g.pbank(); p2 = g.pbank()
        rhs = V(utok, [[512, NCH], [1, 16]], off=gi * 16)
        P.mm(V(p1, [[16, NCH], [1, 16]]), vr[:, gi, :], rhs)
        P.mm(V(p2, [[16, NCH], [1, 16]]), vi[:, gi, :], rhs)
        P.copy(zr, V(p1, [[16, NCH], [1, 16]]), eng="act")
        P.copy(zi, V(p2, [[16, NCH], [1, 16]]), eng="act")
        bb_r = V(s.bbr[:, gi, :], [[0, NCH], [1, 16]]); bb_i = V(s.bbi[:, gi, :], [[0, NCH], [1, 16]])
        P.tt(t1, zr, bb_r, ALU.mult); P.tt(t2, zi, bb_i, ALU.mult); P.tt(t1, t1, t2, ALU.subtract)
        P.reduce(s.sre[:, gi, :], t1)
        P.tt(t3, zi, bb_r, ALU.mult, eng="pool"); P.tt(t4, zr, bb_i, ALU.mult, eng="pool"); P.tt(t3, t3, t4, ALU.add, eng="pool")
        P.reduce(s.sim[:, gi, :], t3)
    P.pop()


def s5_local_scan(g, NCX, NC):
    P = g.P
    s = g.s5
    NCH = NCX + NC
    s.pre_re = P.sb([128, 32, NCH]); s.pre_im = P.sb([128, 32, NCH])
    s.fin_re = P.sb([128, 32, 2]); s.fin_im = P.sb([128, 32, 2])
    s.apow_re = P.sb([128, 32, NCH]); s.apow_im = P.sb([128, 32, NCH])
    t1 = P.sb([128, 32]); t2 = P.sb([128, 32])
    P.memset(s.pre_re, 0.0); P.memset(s.pre_im, 0.0)
    for (c0, n, fi) in ((0, NCX, 0), (NCX, NC, 1)):
        for half, order in ((slice(0, 64), list(range(c0, c0 + n))), (slice(64, 128), list(range(c0 + n - 1, c0 - 1, -1)))):
            eng = "dve" if half.start == 0 else "pool"
            aqr = s.aqr[half]; aqi = s.aqi[half]
            P.memset(s.apow_re[half, :, order[0]], 1.0, eng=eng); P.memset(s.apow_im[half, :, order[0]], 0.0, eng=eng)
            for k in range(n):
                c = order[k]
                if k + 1 < n:
                    cn = order[k + 1]
                    ore, oim = s.pre_re[half, :, cn], s.pre_im[half, :, cn]
                    cmul_acc(P, s.apow_re[half, :, cn], s.apow_im[half, :, cn], aqr, aqi, s.apow_re[half, :, c], s.apow_im[half, :, c],
                             None, None, t1[half], t2[half], eng=eng)
                else:
                    ore, oim = s.fin_re[half, :, fi], s.fin_im[half, :, fi]
                cmul_acc(P, ore, oim, aqr, aqi, s.pre_re[half, :, c], s.pre_im[half, :, c], s.sre[half, :, c], s.sim[half, :, c],
                         t1[half], t2[half], eng=eng)


def s5_tables_ro(g):
    P = g.P
    s = g.s5
    s.w2re = P.sb([128, 32, 128], BF16); s.nw2im = P.sb([128, 32, 128], BF16)
    s.aqr = P.sb([128, 32]); s.aqi = P.sb([128, 32])
    s5_gen_E(g, g.c["exr"], s.w2re, s.nw2im, neg_im=True)
    P.push()
    ang = P.sb([128, 32]); mag = P.sb([128, 32]); tmp = P.sb([128, 32]); co = P.sb([128, 32])
    P.ts(ang, s.thc, 128.0, None, op0=ALU.mult)
    P.act(mag, s.lrdtc, AF.Exp, scale=128.0)
    sincos(P, co, ang, ang, tmp)
    P.tt(s.aqr, mag, co, ALU.mult)
    P.tt(s.aqi, mag, ang, ALU.mult)
    P.pop()


def s5_readout(g, NCX, NC, carry_re, carry_im):
    P = g.P
    s = g.s5
    NCH = NCX + NC
    hre = P.sb([128, 32, NCH]); him = P.sb([128, 32, NCH])
    P.copy(hre, s.pre_re); P.copy(him, s.pre_im, eng="pool")
    own = slice(NCX, NCH)
    t1 = P.sb([128, 32, NC]); t2 = P.sb([128, 32, NC])
    crb = V(carry_re, [[carry_re.ap[1][0], 32], [0, NC]]); cib = V(carry_im, [[carry_im.ap[1][0], 32], [0, NC]])
    P.tt(t1, s.apow_re[:, :, own], crb, ALU.mult); P.tt(t2, s.apow_im[:, :, own], cib, ALU.mult)
    P.tt(t1, t1, t2, ALU.subtract); P.tt(hre[:, :, own], hre[:, :, own], t1, ALU.add)
    P.tt(t1, s.apow_re[:, :, own], cib, ALU.mult); P.tt(t2, s.apow_im[:, :, own], crb, ALU.mult)
    P.tt(t1, t1, t2, ALU.add); P.tt(him[:, :, own], him[:, :, own], t1, ALU.add)
    P.push()
    gre = P.sb([128, 32, NCH, 16], BF16); gim = P.sb([128, 32, NCH, 16], BF16)
    GQ = 8
    a1 = P.sb([128, GQ, NCH, 16]); a2 = P.sb([128, GQ, NCH, 16])
    for q in range(32 // GQ):
        gs = slice(q * GQ, (q + 1) * GQ)
        crb_ = V(s.cre[:, gs, :], [[16, GQ], [0, NCH], [1, 16]]); cib_ = V(s.cim[:, gs, :], [[16, GQ], [0, NCH], [1, 16]])
        hrb = V(hre[:, gs, :], [[NCH, GQ], [1, NCH], [0, 16]]); hib = V(him[:, gs, :], [[NCH, GQ], [1, NCH], [0, 16]])
        P.tt(a1, crb_, hrb, ALU.mult); P.tt(a2, cib_, hib, ALU.mult, eng="pool"); P.tt(gre[:, gs], a1, a2, ALU.subtract)
        P.tt(a1, crb_, hib, ALU.mult); P.tt(a2, cib_, hrb, ALU.mult, eng="pool"); P.tt(gim[:, gs], a1, a2, ALU.add)
    N = NCH * 16
    for gi in range(32):
        ps = g.pbank()
        o = V(ps, [[16, NCH], [1, 16]])
        P.mm(o, s.w2re[:, gi, :], V(gre[:, gi], [[16, NCH], [1, 16]]), start=True, stop=False)
        P.mm(o, s.nw2im[:, gi, :], V(gim[:, gi], [[16, NCH], [1, 16]]), start=False, stop=True)
        P.copy(V(s.ystok, [[512, NCH], [1, 16]], off=gi * 16), o, eng="act" if gi % 2 else "dve")
    P.pop()


def s5_lags(g, uT, NCH, ya_acc_cb):
    P = g.P
    s = g.s5
    P.push()
    elr = P.sb([128, 32, 128], BF16); eli = P.sb([128, 32, 128], BF16)
    s5_gen_E(g, g.c["exl"], elr, eli)
    brpad = P.sb([128, 32, 128], BF16); nbipad = P.sb([128, 32, 128], BF16)
    P.memset(brpad, 0.0); P.memset(nbipad, 0.0, eng="pool")
    padv = lambda t: V(t, [[1024, 4], [144, 8], [1, 16]])
    P.copy(padv(brpad), V(s.bbr, [[128, 4], [16, 8], [1, 16]]))
    P.ts(padv(nbipad), V(s.bbi, [[128, 4], [16, 8], [1, 16]]), -1.0, None, op0=ALU.mult)
    bd = P.sb([128, 2, 64, 128], BF16)
    care = P.sb([128, 32, 16], BF16); caim = P.sb([128, 32, 16], BF16)
    a1 = P.sb([128, 32, 16]); a2 = P.sb([128, 32, 16])
    NS = NCH * 128
    yacc = P.sb([128, NS])
    mb = V(g.c["maskbd"], [[0, 32], [1, 8], [0, 16]])
    cgs = [(c0, min(4, NCH - c0)) for c0 in range(0, NCH, 4)]
    for b in range(4):
        for lh in range(2):
            for sl in range(2):
                d0 = lh * 64 + sl * 32
                pk = [g.pbank(), g.pbank()]
                for gl in range(8):
                    gi = 8 * b + gl
                    crb = V(s.cre[:, gi, :], [[0, 32], [1, 16]]); cib = V(s.cim[:, gi, :], [[0, 32], [1, 16]])
                    erb = V(elr[:, gi, d0:d0 + 32], [[1, 32], [0, 16]]); eib = V(eli[:, gi, d0:d0 + 32], [[1, 32], [0, 16]])
                    P.tt(a1, crb, erb, ALU.mult); P.tt(a2, cib, eib, ALU.mult, eng="pool"); P.tt(care, a1, a2, ALU.subtract)
                    P.tt(a1, crb, eib, ALU.mult); P.tt(a2, cib, erb, ALU.mult, eng="pool"); P.tt(caim, a1, a2, ALU.add)
                    for dr_ in range(2):
                        h = slice(dr_ * 64, (dr_ + 1) * 64)
                        P.mm(pk[dr_], brpad[h, gi, :], V(care[h], [[1, 512]]), start=(gl == 0), stop=False)
                        P.mm(pk[dr_], nbipad[h, gi, :], V(caim[h], [[1, 512]]), start=False, stop=(gl == 7))
                for dr_ in range(2):
                    P.tt(V(bd[:, dr_, sl * 32:(sl + 1) * 32, :], [[128, 32], [16, 8], [1, 16]]),
                         V(pk[dr_], [[16, 32], [0, 8], [1, 16]]), mb, ALU.mult)
            for (c0, ncg) in cgs:
                ps = g.pbank()
                first = True
                if lh == 1:
                    P.mm(ps[:, 0:ncg * 128], g.zeros_bf, V(uT[:, b, :], [[1, ncg * 128]], off=c0 * 128), start=True, stop=False)
                    for c in range(c0, c0 + ncg):
                        P.mm(ps[:, (c - c0) * 128:(c - c0 + 1) * 128], s.ystok[:, c, b * 128:(b + 1) * 128], g.ident_bf,
                             start=False, stop=False)
                    first = False
                for d in range(64):
                    dd = lh * 64 + d
                    w = 128 - dd
                    last = (d == 63)
                    P.mm(V(ps, [[128, ncg], [1, w]], off=dd), bd[:, 0, d, :], V(uT[:, b, :], [[128, ncg], [1, w]], off=c0 * 128),
                         start=first, stop=False)
                    first = False
                    P.mm(V(ps, [[128, ncg], [1, w]]), bd[:, 1, d, :], V(uT[:, b, :], [[128, ncg], [1, w]], off=c0 * 128 + dd),
                         start=False, stop=last)
                dst = yacc[:, c0 * 128:(c0 + ncg) * 128]
                if lh == 0:
                    P.copy(dst, ps[:, 0:ncg * 128], eng="act")
                else:
                    P.tt(dst, dst, ps[:, 0:ncg * 128], ALU.add)
        ya_acc_cb(b, yacc)
    P.pop()


def setup_psum(g):
    P = g.P
    g._pb = [P.ps([128, 512], F32) for _ in range(4)]
    g._pb_o = P.ps([128, 512], F32)[:, :]
    g._pb_d = P.ps([128, 512], F32)[:, :]
    g._pbf = [P.ps([128, 1024], BF16) for _ in range(2)]
    g._pi = 0
    g._pbi = 0

    def pbank():
        t = g._pb[g._pi % 4]
        g._pi += 1
        return t[:, :]

    def pbank_bf():
        h = g._pbi % 2
        g._pbi += 1
        return g._pbf[h][:, 0:512]
    g.pbank = pbank
    g.pbank_bf = pbank_bf


def s5_core(g, uT, NCX, NC, carry_fn, ya_cb, states_cb=None, states_only=False):
    P = g.P
    NCH = NCX + NC
    s5_params(g)
    s = g.s5
    s.ystok = P.sb([128, NCH, 512], BF16)
    P.push()
    s5_tables_ro(g)
    s5_states(g, uT, NCH)
    s5_local_scan(g, NCX, NC)
    if states_cb is not None:
        states_cb()
    if states_only:
        P.pop()
        return
    cr, ci = carry_fn()
    s5_readout(g, NCX, NC, cr, ci)
    P.pop()
    s5_lags(g, uT, NCH, ya_cb)


def load_w(g, name, r0, nrows, c0, ncols, dtype=BF16, q="sp"):
    P = g.P
    kk = nrows // 128
    t = P.sb([128, kk, ncols], dtype)
    d = g.dram[name]
    ncol_total = d.shape[-1]
    P.dma(t, DV(d, r0 * ncol_total + c0, [[ncol_total, 128], [128 * ncol_total, kk], [1, ncols]]), q=q)
    return t


def load_h(g, c0, n):
    P = g.P
    t = P.sb([128, 8, n], BF16)
    P.dma(t, DV(g.hT_d, c0, [[g.E, 128], [128 * g.E, 8], [1, n]]))
    return t


def proj(g, ps_out, w, j0, hT, n, wcols=128):
    P = g.P
    for k in range(8):
        P.mm(ps_out, w[:, k, j0:j0 + wcols], hT[:, k, 0:n], start=(k == 0), stop=(k == 7))


def col_tiles(c0, n, step=512):
    out = []
    c = c0
    while c < c0 + n:
        m = min(step, c0 + n - c)
        out.append((c, m))
        c += m
    return out


def ssd_prep(g, NCX, NC):
    P = g.P
    dr = g.dram
    s = G(); g.ssd = s
    T = NC * 128; NS = (NCX + NC) * 128
    s.xsT = P.sb([128, 4, NS], BF16); s.bmT = P.sb([128, 2, NS], BF16); s.cmT = P.sb([128, 2, NS], BF16)
    s.gz = P.sb([128, 4, NS], BF16)
    s.dt = P.sb([128, NCX + NC, 16]); s.dta = P.sb([128, NCX + NC, 16])
    s.cw = P.sb([128, 8, 5]); s.cb = P.sb([128, 8])
    for k_ in range(5):
        P.dma(s.cw[:, :, k_], DV(dr["ssd_conv_w"], k_ * 1024, [[1, 128], [128, 8]]), allow_slow_non_contiguous=True)
    P.dma(s.cb, DV(dr["ssd_conv_b"], 0, [[1, 128], [128, 8]]), allow_slow_non_contiguous=True)
    s.dtb = P.sb([128, 16]); s.ab = P.sb([128, 16])
    P.dma(s.dtb, DV(dr["ssd_dt_bias"], 0, [[0, 128], [1, 16]]))
    P.dma(s.ab, DV(dr["ssd_a_log"], 0, [[0, 128], [1, 16]]))
    P.act(s.ab, s.ab, AF.Exp)
    P.ts(s.ab, s.ab, -1.0, None, op0=ALU.mult)
    s.dcol = P.sb([128, 4])
    for hh in range(2):
        P.dma(s.dcol[hh * 64:(hh + 1) * 64, :], DV(dr["ssd_d"], hh, [[0, 64], [2, 4]]), allow_slow_non_contiguous=True)
    s.ng = P.sb([128, 4])
    P.dma(s.ng, DV(dr["ssd_norm_g"], 0, [[1, 128], [128, 4]]), allow_slow_non_contiguous=True)
    P.push()
    wx = load_w(g, "w_in", 0, 1024, C_XBC, 1024)
    wz = load_w(g, "w_in", 0, 1024, C_Z, 512)
    wdt = load_w(g, "w_in", 0, 1024, C_DT, 16)
    regions = [(0, NCX * 128, g.e_ctx, False), (NCX * 128, T, g.e_own, True)]
    W = NS + 8
    xin = P.sb([128, W])
    for j in range(8):
        P.memset(xin[:, 0:2], 0.0); P.memset(xin[:, 2 + NCX * 128:4 + NCX * 128], 0.0)
        for (s0, n, e0, is_own) in regions:
            xo = 2 + s0 + (4 if is_own else 0)
            lo, hi = (e0 - 2, e0 + n + 2) if is_own else (e0, e0 + n)
            xo_lo = xo - 2 if is_own else xo
            for (c, m) in col_tiles(lo, hi - lo):
                P.push()
                ht = load_h(g, c, m)
                ps = g.pbank()
                proj(g, ps[:, 0:m], wx, j * 128, ht, m)
                P.copy(xin[:, xo_lo + (c - lo):xo_lo + (c - lo) + m], ps[:, 0:m], eng="act")
                P.pop()
            if is_own:
                P.ts(xin[:, xo - 2:xo], xin[:, xo - 2:xo], g.flagL, None, op0=ALU.mult)
                P.ts(xin[:, xo + n:xo + n + 2], xin[:, xo + n:xo + n + 2], g.flagR, None, op0=ALU.mult)
        dst = s.xsT[:, j, :] if j < 4 else (s.bmT[:, j - 4, :] if j < 6 else s.cmT[:, j - 6, :])
        for (s0, n, e0, is_own) in regions:
            xo = 2 + s0 + (4 if is_own else 0)
            P.push()
            acc = P.sb([128, n])
            P.ts(acc, xin[:, xo - 2:xo - 2 + n], s.cw[:, j, 0:1], None, op0=ALU.mult)
            for k in range(1, 5):
                P.stt(acc, xin[:, xo - 2 + k:xo - 2 + k + n], s.cw[:, j, k:k + 1], acc, ALU.mult, ALU.add)
            P.act(dst[:, s0:s0 + n], acc, AF.Silu, bias=s.cb[:, j:j + 1])
            P.pop()
    for (s0, n, e0, is_own) in regions:
        for (c, m) in col_tiles(e0, n):
            P.push()
            ht = load_h(g, c, m)
            so = s0 + (c - e0)
            for j in range(4):
                ps = g.pbank()
                proj(g, ps[:, 0:m], wz, j * 128, ht, m)
                P.act(s.gz[:, j, so:so + m], ps[:, 0:m], AF.Silu)
            for cc in range(m // 128):
                ps = g.pbank()
                for k in range(8):
                    P.mm(ps[:, 0:16], ht[:, k, cc * 128:(cc + 1) * 128], wdt[:, k, :], start=(k == 0), stop=(k == 7))
                ci = (so + cc * 128) // 128
                P.tt(s.dt[:, ci, :], ps[:, 0:16], s.dtb, ALU.add)
            P.pop()
    P.pop()
    P.act(s.dt, s.dt, AF.Exp)
    P.act(s.dt, s.dt, AF.Ln, bias=1.0)
    P.tt(s.dta, s.dt, V(s.ab, [[0, NCX + NC], [1, 16]]), ALU.mult)


def ssd_chunk(g, c, want_y, hin_f, hin_b, ybuf=None):
    P = g.P
    s = g.ssd
    cs = slice(c * 128, (c + 1) * 128)
    ut = g.c["ut"]; lt = g.c["lt"]
    xs_tok = P.sb([128, 8, 64], BF16); bm_tok = P.sb([128, 2, 128], BF16)
    pt = g.pbank_bf()
    for b in range(4):
        P.transpose(pt[:, b * 128:(b + 1) * 128], s.xsT[:, b, cs], g.ident_bf)
    P.copy(V(xs_tok, [[1, 512]]), pt[:, 0:512], eng="act")
    pt2 = g.pbank_bf()
    for b in range(2):
        P.transpose(pt2[:, b * 128:(b + 1) * 128], s.bmT[:, b, cs], g.ident_bf)
    P.copy(V(bm_tok, [[1, 256]]), pt2[:, 0:256], eng="act")
    pc = g.pbank()
    P.mm(pc[:, 0:8], ut, s.dta[:, c, 0:8])
    P.mm(pc[:, 8:16], lt, s.dta[:, c, 8:16])
    nacum = P.sb([128, 16])
    P.ts(nacum, pc[:, 0:16], -1.0, None, op0=ALU.mult)
    acb = []
    for dr_ in range(2):
        m = ut if dr_ == 0 else lt
        for hq in range(2):
            rb = P.sb([128, 4, 128])
            P.tt(rb, V(m, [[0, 4], [1, 128]]), V(s.dta[:, c, dr_ * 8 + hq * 4:dr_ * 8 + hq * 4 + 4], [[1, 4], [0, 128]]), ALU.mult,
                 eng="pool")
            pa = g.pbank()
            P.mm(V(pa, [[1, 512]]), g.c["ones_f"], V(rb, [[1, 512]]))
            pas = P.sb([128, 512])
            P.copy(pas, pa, eng="act" if hq else "dve")
            acb.append(pas)
    tot = P.sb([128, 16])
    for dr_ in range(2):
        for hq in range(2):
            pa = acb[dr_ * 2 + hq]
            col = 127 if dr_ == 0 else 0
            P.copy(tot[:, dr_ * 8 + hq * 4:dr_ * 8 + hq * 4 + 4], V(pa, [[128, 4]], off=col))
    w = P.sb([128, 16]); cd = P.sb([128, 16])
    P.tt(w, tot, nacum, ALU.add)
    P.act(w, w, AF.Exp)
    P.tt(w, w, s.dt[:, c, :], ALU.mult)
    P.act(cd, tot, AF.Exp)
    S = []
    for dr_ in range(2):
        xsw = P.sb([128, 8, 64], BF16)
        P.tt(xsw, xs_tok, V(w[:, dr_ * 8:dr_ * 8 + 8], [[1, 8], [0, 64]]), ALU.mult)
        pS = g.pbank()
        for h in range(8):
            P.mm(pS[:, h * 64:(h + 1) * 64], bm_tok[:, h // 4, :], xsw[:, h, :])
        Ss = P.sb([128, 512])
        P.copy(Ss, pS, eng="act")
        S.append(Ss)
    if not want_y:
        return S, cd, tot
    cbm = []
    for gq in range(2):
        pcb = g.pbank()
        P.mm(pcb[:, 0:128], s.bmT[:, gq, cs], s.cmT[:, gq, cs])
        cf = P.sb([128, 128]); cbk = P.sb([128, 128])
        P.tt(cf, pcb[:, 0:128], ut, ALU.mult)
        P.tt(cbk, pcb[:, 0:128], lt, ALU.mult)
        cbm.append((cf, cbk))
    for pair in range(4):
        py = g.pbank()
        for hh in range(2):
            h = pair * 2 + hh
            gq = h // 4
            hq, hi4 = h // 4, h % 4
            mms = []
            for dr_ in range(2):
                pa = acb[dr_ * 2 + hq]
                e1 = P.sb([128, 128])
                P.ts(e1, pa[:, hi4 * 128:(hi4 + 1) * 128], nacum[:, dr_ * 8 + h:dr_ * 8 + h + 1], g.zero_col, op0=ALU.add, op1=ALU.min)
                P.act(e1, e1, AF.Exp)
                wt = P.sb([128, 128], BF16)
                P.stt(wt, e1, s.dt[:, c, dr_ * 8 + h:dr_ * 8 + h + 1], cbm[gq][dr_], ALU.mult, ALU.mult)
                mms.append((xs_tok[:, h, :], wt))
                hin = hin_f if dr_ == 0 else hin_b
                if hin is not None:
                    dec = P.sb([128, 128])
                    P.act(dec, pa[:, hi4 * 128:(hi4 + 1) * 128], AF.Exp)
                    csd = P.sb([128, 128], BF16)
                    P.tt(csd, s.cmT[:, gq, cs], dec, ALU.mult, eng="pool")
                    mms.append((hin[:, h, :], csd))
            for i_, (l_, r_) in enumerate(mms):
                P.mm(py[hh * 64:(hh + 1) * 64, 0:128], l_, r_, start=(i_ == 0), stop=(i_ == len(mms) - 1))
        P.stt(ybuf[:, pair, :], s.xsT[:, pair, cs], s.dcol[:, pair:pair + 1], py[:, 0:128], ALU.mult, ALU.add)
    return S, cd, tot


def ssd_run(g, NCX, NC, multi=False, yb_cb=None):
    P = g.P
    s = g.ssd
    NCH = NCX + NC
    hf = P.sb([128, 512]); hb = P.sb([128, 512])
    hbf = P.sb([128, 8, 64], BF16)
    for (c0, n) in ((0, NCX), (NCX, NC)):
        if c0 == 0:
            P.memset(hb, 0.0)
        elif multi:
            P.push(); tmpc = P.sb([128, 512]); ssd_carry(g, 1, hb, tmpc); P.copy(hb, tmpc); P.pop()
        for c in range(c0 + n - 1, c0 - 1, -1):
            P.copy(V(hbf, [[1, 512]]), hb)
            P.dma(DV(g.ssd_hb_d, c * 128 * 512, [[512, 128], [1, 512]]), V(hbf, [[1, 512]]))
            P.push()
            S, cd, _t = ssd_chunk(g, c, False, None, None)
            P.tt(V(hb, [[64, 8], [1, 64]]), V(hb, [[64, 8], [1, 64]]), V(cd[:, 8:16], [[1, 8], [0, 64]]), ALU.mult)
            P.tt(hb, hb, S[1], ALU.add)
            P.pop()
    hfb = P.sb([128, 8, 64], BF16); hbb = P.sb([128, 8, 64], BF16)
    ybuf = P.sb([128, 4, 128])
    for (c0, n) in ((0, NCX), (NCX, NC)):
        if c0 == 0:
            P.memset(hf, 0.0)
        elif multi:
            P.push(); tmpc = P.sb([128, 512]); ssd_carry(g, 0, hf, tmpc); P.copy(hf, tmpc); P.pop()
        for c in range(c0, c0 + n):
            P.copy(V(hfb, [[1, 512]]), hf)
            P.dma(V(hbb, [[1, 512]]), DV(g.ssd_hb_d, c * 128 * 512, [[512, 128], [1, 512]]))
            P.push()
            S, cd, _t = ssd_chunk(g, c, True, hfb, hbb, ybuf)
            P.tt(V(hf, [[64, 8], [1, 64]]), V(hf, [[64, 8], [1, 64]]), V(cd[:, 0:8], [[1, 8], [0, 64]]), ALU.mult)
            P.tt(hf, hf, S[0], ALU.add)
            cs = slice(c * 128, (c + 1) * 128)
            yg = P.sb([128, 4, 128]); sq = P.sb([128, 4, 128], BF16)
            P.tt(yg, ybuf, s.gz[:, :, cs], ALU.mult)
            P.tt(sq, yg, yg, ALU.mult, eng="pool")
            pn = g.pbank()
            for b in range(4):
                P.mm(pn[:, 0:128], g.ones_bf, sq[:, b, :], start=(b == 0), stop=(b == 3))
            rstd = P.sb([128, 128])
            P.act(rstd, pn[:, 0:128], AF.Sqrt, scale=1.0 / 512, bias=g.eps_col)
            P.recip(rstd, rstd)
            yo = P.sb([128, 4, 128], BF16)
            for b in range(4):
                P.stt(yo[:, b, :], yg[:, b, :], s.ng[:, b:b + 1], rstd, ALU.mult, ALU.mult)
            yb_cb(c, yo)
            P.pop()


def attn_run(g, NCX, NC, yc_cb):
    P = g.P
    dr = g.dram
    T = NC * 128
    NL = T + 256
    NLT = NL // 128
    P.push()
    wq = P.sb([128, 8, 512], BF16)
    d = dr["w_in"]
    for r in range(4):
        for hh, head in enumerate((r, 4 + r)):
            P.dma(wq[:, :, r * 128 + hh * 64:r * 128 + hh * 64 + 64],
                  DV(d, C_Q + head * 64, [[IN_W, 128], [128 * IN_W, 8], [1, 64]]))
    wk = load_w(g, "w_in", 0, 1024, C_K, 128)
    wv = load_w(g, "w_in", 0, 1024, C_V, 128)
    pswap = P.sb([128, 128], BF16)
    P.copy(pswap, g.c["pswap"])
    esink = P.sb([128, 4])
    for hh in range(2):
        P.dma(esink[hh * 64:(hh + 1) * 64, :], DV(dr["attn_sink"], hh * 4, [[0, 64], [1, 4]]))
    P.act(esink, esink, AF.Exp)
    mprev = P.sb([128, 128], BF16); mnext = P.sb([128, 128], BF16); mprevL = P.sb([128, 128], BF16); mnextR = P.sb([128, 128], BF16)
    P.copy(mprev, g.c["lt"]); P.copy(mnext, g.c["ut"])
    P.ts(mprevL, g.c["lt"], g.flagL, None, op0=ALU.mult)
    P.ts(mnextR, g.c["ut"], g.flagR, None, op0=ALU.mult)
    import os
    CUT = os.environ.get('ATT_CUT', '')
    if CUT == 'setup':
        P.pop(); return
    NS = (NCX + NC) * 128
    qT = P.sb([128, 4, NS], BF16)
    kT = P.sb([128, NL + 256], BF16)
    vtok = P.sb([128, NLT + 2, 128], BF16)

    def rope(dst, ps, n, ecol):
        if 'norope' in CUT:
            P.copy(dst, ps); return
        P.push()
        cs_ = P.sb([128, n]); sn_ = P.sb([128, n]); xb = P.sb([128, n], BF16); t1 = P.sb([128, n])
        if 'nodma' in CUT:
            P.memset(cs_, 1.0); P.memset(sn_, 0.0)
        else:
            P.dma(cs_, DV(dr["rope_cos"], ecol, [[NL, 128], [1, n]]))
            P.dma(sn_, DV(dr["rope_sin"], ecol, [[NL, 128], [1, n]]))
        if 'dmaonly' in CUT:
            P.tt(dst, ps, cs_, ALU.mult); P.pop(); return
        P.copy(xb, ps)
        p2 = g.pbank()
        P.mm(p2[:, 0:n], pswap, xb)
        P.tt(t1, ps, cs_, ALU.mult)
        t2 = P.sb([128, n])
        P.tt(t2, p2[:, 0:n], sn_, ALU.mult)
        P.tt(dst, t1, t2, ALU.add)
        P.pop()

    for (c, m) in col_tiles(0, NL):
        P.push()
        ht = load_h(g, c, m)
        ps = g.pbank()
        proj(g, ps[:, 0:m], wk, 0, ht, m)
        rope(kT[:, c:c + m], ps[:, 0:m], m, c)
        for cc in range(m // 128):
            if 'nov' in CUT:
                break
            pv = g.pbank()
            for k in range(8):
                P.mm(pv[:, 0:128], ht[:, k, cc * 128:(cc + 1) * 128], wv[:, k, :], start=(k == 0), stop=(k == 7))
            P.copy(vtok[:, (c // 128) + cc, :], pv[:, 0:128], eng="act")
        if 'noq' in CUT:
            P.pop(); continue
        lo = max(c, 128); hi = min(c + m, 128 + T)
        if hi > lo:
            for r in range(4):
                pq = g.pbank()
                proj(g, pq[:, 0:hi - lo], wq, r * 128, ht[:, :, lo - c:hi - c], hi - lo)
                so = NCX * 128 + (lo - 128)
                rope(qT[:, r, so:so + (hi - lo)], pq[:, 0:hi - lo], hi - lo, lo)
        P.pop()
    if 'lat' in CUT:
        P.pop(); return
    for (c, m) in col_tiles(g.e_ctx, NCX * 128):
        P.push()
        ht = load_h(g, c, m)
        so = c - g.e_ctx
        ps = g.pbank()
        proj(g, ps[:, 0:m], wk, 0, ht, m)
        P.copy(kT[:, NL + so:NL + so + m], ps[:, 0:m], eng="act")
        for cc in range(m // 128):
            pv = g.pbank()
            for k in range(8):
                P.mm(pv[:, 0:128], ht[:, k, cc * 128:(cc + 1) * 128], wv[:, k, :], start=(k == 0), stop=(k == 7))
            P.copy(vtok[:, NLT + so // 128 + cc, :], pv[:, 0:128], eng="act")
        for r in range(4):
            pq = g.pbank()
            proj(g, pq[:, 0:m], wq, r * 128, ht, m)
            P.copy(qT[:, r, so:so + m], pq[:, 0:m], eng="act")
        P.pop()
    po = g._pb_o; pd = g._pb_d
    import os
    for qb in range(NCX + NC):
        if os.environ.get('ATT_CUT') == 'proj':
            break
        if qb < NCX:
            tiles = [(NLT + 0, NL + 0, None), (NLT + 1, NL + 128, None)]
        else:
            n = qb - NCX
            tiles = [(n, n * 128, mprevL if n == 0 else mprev), (n + 1, (n + 1) * 128, None),
                     (n + 2, (n + 2) * 128, mnextR if n == NC - 1 else mnext),
                     (NLT + 0, NL + 0, None), (NLT + 1, NL + 128, None)]
        P.push()
        nt = len(tiles)
        for hk in range(2):
            h = slice(hk * 64, (hk + 1) * 64)
            for ti, (kt, kcol, mask) in enumerate(tiles):
                ps = g.pbank()
                P.mm(ps[:, 0:512], kT[h, kcol:kcol + 128], V(qT[h], [[NS, 4], [1, 128]], off=qb * 128))
                ex = P.sb([128, 4, 128], BF16)
                P.act(V(ex, [[1, 512]]), ps, AF.Exp, scale=0.125)
                if mask is not None:
                    P.tt(ex, ex, V(mask, [[0, 4], [1, 128]]), ALU.mult, eng="pool")
                P.mm(po[h, :], vtok[:, kt, h], V(ex, [[1, 512]]), start=(ti == 0), stop=(ti == nt - 1))
                P.mm(pd[h, :], g.ones_bf[:, 0:64], V(ex, [[1, 512]]), start=(ti == 0), stop=(ti == nt - 1))
        rd = P.sb([128, 4, 128])
        for r in range(4):
            P.ts(rd[:, r, :], pd[:, r * 128:(r + 1) * 128], esink[:, r:r + 1], None, op0=ALU.add)
        P.recip(V(rd, [[1, 512]]), V(rd, [[1, 512]]))
        yo = P.sb([128, 4, 128], BF16)
        P.tt(V(yo, [[1, 512]]), po[:, :], V(rd, [[1, 512]]), ALU.mult)
        yc_cb(qb, yo)
        P.pop()
    P.pop()


def rms_rstd(g, xt, n, rstd):
    P = g.P
    P.push()
    sq = P.sb([128, 8, n], BF16)
    P.act(sq, xt, AF.Square)
    ps = g.pbank()
    for k in range(8):
        P.mm(ps[:, 0:n], g.ones_bf, sq[:, k, :], start=(k == 0), stop=(k == 7))
    P.act(rstd, ps[:, 0:n], AF.Sqrt, scale=1.0 / D, bias=g.eps_col)
    P.recip(rstd, rstd)
    P.pop()


def mod_norm(g, xt, n, acol, bcol, out, v):
    P = g.P
    P.push()
    rstd = P.sb([128, n])
    rms_rstd(g, xt, n, rstd)
    tmp = P.sb([128, n])
    for k in range(8):
        P.stt(tmp, xt[:, k, :], acol[:, k, v:v + 1], rstd, ALU.mult, ALU.mult)
        P.act(out[:, k, :], tmp, AF.Identity, bias=bcol[:, k, v:v + 1])
    P.pop()


def load_mod(g):
    P = g.P
    m = P.sb([128, 48, 2])
    P.dma(m, g.dram["modT"])
    g.mod = m
    n1 = P.sb([128, 8]); n2 = P.sb([128, 8])
    lst = []
    if "norm1_g" in g.dram:
        P.dma(n1, DV(g.dram["norm1_g"], 0, [[1, 128], [128, 8]]), allow_slow_non_contiguous=True)
        g.a1 = P.sb([128, 8, 2]); lst.append((g.a1, n1, 1))
    if "norm2_g" in g.dram:
        P.dma(n2, DV(g.dram["norm2_g"], 0, [[1, 128], [128, 8]]), allow_slow_non_contiguous=True)
        g.a2 = P.sb([128, 8, 2]); lst.append((g.a2, n2, 4))
    for (a, nn, j) in lst:
        P.ts(a, m[:, j * 8:(j + 1) * 8, :], 1.0, None, op0=ALU.add)
        P.tt(a, a, V(nn, [[1, 8], [0, 2]]), ALU.mult)
    g.b1 = m[:, 0:8, :]; g.b2 = m[:, 24:32, :]
    g.g1 = m[:, 16:24, :]; g.g2 = m[:, 40:48, :]


def router_aff(g, h2f, n, wr, aff_out):
    P = g.P
    for blk in range(n // 128):
        ps = g.pbank()
        for k in range(8):
            P.mm(ps[:, 0:16], h2f[:, k, blk * 128:(blk + 1) * 128], wr[:, k, :], start=(k == 0), stop=(k == 7))
        P.push()
        mx = P.sb([128, 1]); sm = P.sb([128, 1]); ex = P.sb([128, 16])
        P.reduce(mx, ps[:, 0:16], op=ALU.max)
        P.ts(mx, mx, -1.0, None, op0=ALU.mult)
        P.act(ex, ps[:, 0:16], AF.Exp, bias=mx, accum_out=sm)
        P.recip(sm, sm)
        P.ts(aff_out[:, blk, :], ex, sm, None, op0=ALU.mult)
        P.pop()


def s_tiles(NCX, NC, step=512):
    out = [(c, m, True) for (c, m) in col_tiles(0, NCX * 128, step)]
    out += [(c, m, False) for (c, m) in col_tiles(NCX * 128, NC * 128, step)]
    return out


def s2e(g, NCX, s0, is_ctx):
    return g.e_ctx + s0 if is_ctx else g.e_own + (s0 - NCX * 128)


def merge_run(g, NCX, NC):
    P = g.P
    dr = g.dram
    NS = (NCX + NC) * 128
    P.push()
    macc = P.sb([128, 8, NS], BF16)
    for kbr in range(3):
        P.push()
        wg = load_w(g, "w_in", 0, 1024, C_G + kbr * 1024, 1024)
        wb = P.sb([128, 4, 1024], BF16)
        d = dr["w_branch"]
        if kbr < 2:
            P.dma(wb, DV(d, kbr * 512 * 1024, [[1024, 128], [128 * 1024, 4], [1, 1024]]))
        else:
            for r in range(4):
                for hh, head in enumerate((r, 4 + r)):
                    P.dma(wb[hh * 64:(hh + 1) * 64, r, :], DV(d, (2 * 512 + head * 64) * 1024, [[1024, 64], [1, 1024]]))
        yd = g.y_d[kbr]
        for (s0, n, is_ctx) in s_tiles(NCX, NC):
            P.push()
            ht = load_h(g, s2e(g, NCX, s0, is_ctx), n)
            yt = P.sb([128, 4, n], BF16)
            P.dma(yt, DV(yd, s0, [[NS, 128], [128 * NS, 4], [1, n]]))
            for j in range(8):
                pg = g.pbank()
                proj(g, pg[:, 0:n], wg, j * 128, ht, n)
                gt = P.sb([128, n])
                P.act(gt, pg[:, 0:n], AF.Sigmoid)
                pb = g.pbank()
                for cc in range(4):
                    P.mm(pb[:, 0:n], wb[:, cc, j * 128:(j + 1) * 128], yt[:, cc, :], start=(cc == 0), stop=(cc == 3))
                if kbr == 0:
                    P.tt(macc[:, j, s0:s0 + n], gt, pb[:, 0:n], ALU.mult)
                else:
                    P.tt(gt, gt, pb[:, 0:n], ALU.mult)
                    P.tt(macc[:, j, s0:s0 + n], macc[:, j, s0:s0 + n], gt, ALU.add, eng="pool")
            P.pop()
        P.pop()
    wo = load_w(g, "w_out", 0, 1024, 0, 1024)
    wr = load_w(g, "w_router", 0, 1024, 0, 16, dtype=F32)
    for (s0, n, is_ctx) in s_tiles(NCX, NC):
        v = 1 if is_ctx else 0
        P.push()
        xt = P.sb([128, 8, n])
        xsrc = g.dram["xcT"] if is_ctx else g.dram["xT"]
        xw = NCX * 128 if is_ctx else NC * 128 + 256
        xo = s0 if is_ctx else (s0 - NCX * 128) + 128
        P.dma(xt, DV(xsrc, xo, [[xw, 128], [128 * xw, 8], [1, n]]))
        for j in range(8):
            po = g.pbank()
            for k in range(8):
                P.mm(po[:, 0:n], wo[:, k, j * 128:(j + 1) * 128], macc[:, k, s0:s0 + n], start=(k == 0), stop=(k == 7))
            P.stt(xt[:, j, :], po[:, 0:n], g.g1[:, j, v:v + 1], xt[:, j, :], ALU.mult, ALU.add)
        P.dma(DV(g.dram["x1T"], s0, [[NS, 128], [128 * NS, 8], [1, n]]), xt)
        h2 = P.sb([128, 8, n])
        mod_norm(g, xt, n, g.a2, g.b2, h2, v)
        aff = P.sb([128, n // 128, 16])
        router_aff(g, h2, n, wr, aff)
        P.dma(DV(g.dram["aff"], s0 * 16, [[16, 128], [128 * 16, n // 128], [1, 16]]), aff)
        P.pop()
    P.pop()


B_INPUTS = [("s5_lam_re", [2, 32, 64]), ("s5_lam_im", [2, 32, 64]), ("s5_log_dt", [2, 32]), ("s5_b_re", [2, 32, 64, 16]),
            ("s5_b_im", [2, 32, 64, 16]), ("s5_c_re", [2, 32, 16, 64]), ("s5_c_im", [2, 32, 16, 64]), ("s5_d", [512]),
            ("s5_b_glu", [512]), ("ssd_conv_w", [5, 1024]), ("ssd_conv_b", [1024]), ("ssd_a_log", [2, 8]),
            ("ssd_dt_bias", [2, 8]), ("ssd_d", [8]), ("ssd_norm_g", [512]), ("attn_sink", [8]), ("norm1_g", [1024]),
            ("norm2_g", [1024]), ("w_router", [1024, 16]), ("modT", [128, 48, 2]), ("flags", [128, 2])]
B_INPUTS_BF = [("w_in", [1024, IN_W]), ("s5_w_glu", [512, 512]), ("w_branch", [3 * 512, 1024]), ("w_out", [1024, 1024])]


def declare_inputs(g, lst, dt):
    for name, shp in lst:
        g.dram[name] = g.nc.dram_tensor(name, shp, dt, kind="ExternalInput").ap()


def phase0_h(g, NCX, NC):
    P = g.P
    T = NC * 128
    for (c, m, is_ctx) in [(c, m, False) for (c, m) in col_tiles(0, T + 256)] + [(c, m, True) for (c, m) in col_tiles(0, NCX * 128)]:
        P.push()
        xt = P.sb([128, 8, m])
        src = g.dram["xcT"] if is_ctx else g.dram["xT"]
        xw = NCX * 128 if is_ctx else T + 256
        P.dma(xt, DV(src, c, [[xw, 128], [128 * xw, 8], [1, m]]))
        ht = P.sb([128, 8, m], BF16)
        mod_norm(g, xt, m, g.a1, g.b1, ht, 1 if is_ctx else 0)
        e0 = (g.e_ctx + c) if is_ctx else c
        P.dma(DV(g.hT_d, e0, [[g.E, 128], [128 * g.E, 8], [1, m]]), ht)
        P.pop()


def s5_phase(g, NCX, NC, carry_fn=None, states_cb=None, states_only=False):
    P = g.P
    dr = g.dram
    NS = (NCX + NC) * 128
    P.push()
    uT = P.sb([128, 4, NS], BF16)
    aT = P.sb([128, 4, NS], BF16)
    P.push()
    wu = load_w(g, "w_in", 0, 1024, C_U, 512)
    for (s0, n, is_ctx) in s_tiles(NCX, NC):
        P.push()
        ht = load_h(g, s2e(g, NCX, s0, is_ctx), n)
        for j in range(4):
            ps = g.pbank()
            proj(g, ps[:, 0:n], wu, j * 128, ht, n)
            P.copy(uT[:, j, s0:s0 + n], ps[:, 0:n], eng="act" if j % 2 else "dve")
        P.pop()
    P.pop()
    dcol = P.sb([128, 4]); bglu = P.sb([128, 4])
    P.dma(dcol, DV(dr["s5_d"], 0, [[1, 128], [128, 4]]), allow_slow_non_contiguous=True)
    P.dma(bglu, DV(dr["s5_b_glu"], 0, [[1, 128], [128, 4]]), allow_slow_non_contiguous=True)

    def ya_cb(b, yacc):
        P.stt(yacc, uT[:, b, :], dcol[:, b:b + 1], yacc, ALU.mult, ALU.add)
        P.act(aT[:, b, :], yacc, AF.Gelu)

    if carry_fn is None:
        carry_fn = lambda: (g.s5.fin_re[:, :, 0], g.s5.fin_im[:, :, 0])
    s5_core(g, uT, NCX, NC, carry_fn, ya_cb, states_cb, states_only)
    if states_only:
        P.pop()
        return
    wgl = load_w(g, "s5_w_glu", 0, 512, 0, 512)
    for (s0, n) in col_tiles(0, NS):
        P.push()
        yo = P.sb([128, 4, n], BF16)
        for j in range(4):
            ps = g.pbank()
            for k in range(4):
                P.mm(ps[:, 0:n], wgl[:, k, j * 128:(j + 1) * 128], aT[:, k, s0:s0 + n], start=(k == 0), stop=(k == 3))
            gt = P.sb([128, n])
            P.act(gt, ps[:, 0:n], AF.Sigmoid, bias=bglu[:, j:j + 1])
            P.tt(yo[:, j, :], gt, aT[:, j, s0:s0 + n], ALU.mult)
        P.dma(DV(g.y_d[0], s0, [[NS, 128], [128 * NS, 4], [1, n]]), yo)
        P.pop()
    P.pop()


def build_B(T, debug=False, multi=False, mode="B"):
    nc = bass.Bass("TRN2", target_bir_lowering=False)
    g = G(); g.nc = nc; g.P = Prog(nc); P = g.P
    P.init_arenas(18 * 1024, 62 * 1024)
    NCX = 2; NC = T // 128; NS = (NCX + NC) * 128
    g.E = T + 512; g.e_own = 128; g.e_ctx = T + 256
    g.dram = {}
    consts = dict(CONST_SHAPES); consts["pswap"] = [128, 128]
    declare_inputs(g, list(consts.items()), F32)
    declare_inputs(g, B_INPUTS, F32)
    declare_inputs(g, B_INPUTS_BF, BF16)
    declare_inputs(g, [("xT", [8, 128, T + 256]), ("xcT", [8, 128, 256]), ("rope_cos", [128, T + 256]), ("rope_sin", [128, T + 256])], F32)
    if mode == "B":
        g.dram["x1T"] = nc.dram_tensor("x1T", [8, 128, NS], F32, kind="ExternalOutput").ap()
        g.dram["aff"] = nc.dram_tensor("aff", [NS, 16], F32, kind="ExternalOutput").ap()
        if multi:
            declare_inputs(g, [("s5_fin_all", [NCORES, 128, 64]), ("ssd_fin_all", [NCORES, 2, 128, 512]), ("ssd_tot_all", [NCORES, 128, 16]),
                               ("onehot", [128, NCORES])], F32)
    else:
        g.dram["s5_fin"] = nc.dram_tensor("s5_fin", [128, 64], F32, kind="ExternalOutput").ap()
        g.dram["ssd_fin"] = nc.dram_tensor("ssd_fin", [2, 128, 512], F32, kind="ExternalOutput").ap()
        g.dram["ssd_tot"] = nc.dram_tensor("ssd_tot", [128, 16], F32, kind="ExternalOutput").ap()
    g.hT_d = nc.dram_tensor("hT_scr", [8, 128, g.E], BF16).ap()
    g.ssd_hb_d = nc.dram_tensor("ssd_hb_scr", [NCX + NC, 128, 512], BF16).ap()
    kind = "ExternalOutput" if debug else "Internal"
    g.y_d = [nc.dram_tensor(f"y{k}_scr", [4, 128, NS], BF16, kind=kind).ap() for k in range(3)]
    setup_psum(g)
    CONST_SHAPES2 = consts
    g.c = {}
    for k, shp in CONST_SHAPES2.items():
        t = P.sb(shp, F32)
        P.dma(t, g.dram[k])
        g.c[k] = t
    g.ident_bf = P.sb([128, 128], BF16); P.copy(g.ident_bf, g.c["ident"])
    g.ones_bf = P.sb([128, 128], BF16); P.memset(g.ones_bf, 1.0)
    g.zeros_bf = P.sb([128, 128], BF16); P.memset(g.zeros_bf, 0.0)
    g.c["ones_f"] = P.sb([128, 128], F32); P.memset(g.c["ones_f"], 1.0)
    g.eps_col = P.sb([128, 1]); P.memset(g.eps_col, 1e-6)
    g.zero_col = P.sb([128, 1]); P.memset(g.zero_col, 0.0)
    fl = P.sb([128, 2]); P.dma(fl, g.dram["flags"])
    g.flagL = fl[:, 0:1]; g.flagR = fl[:, 1:2]
    import os
    ph = os.environ.get("PH", "s5,ssd,att,merge").split(",")
    load_mod(g)
    phase0_h(g, NCX, NC)
    if multi and mode == "B":
        g.onehot = P.sb([128, NCORES]); P.dma(g.onehot, g.dram["onehot"])
    if mode == "A":
        def dump_fin():
            t_ = P.sb([128, 32, 2])
            P.copy(t_[:, :, 0], g.s5.fin_re[:, :, 1]); P.copy(t_[:, :, 1], g.s5.fin_im[:, :, 1])
            P.dma(g.dram["s5_fin"], V(t_, [[1, 64]]))
        s5_phase(g, NCX, NC, states_cb=dump_fin, states_only=True)
        P.push()
        ssd_prep(g, NCX, NC)
        ssd_local_finals(g, NCX, NC)
        P.pop()
        P.wait_all(); P.emit(); P.close()
        return nc, g
    if "s5" in ph:
        s5_phase(g, NCX, NC, carry_fn=(lambda: s5_carry_chain(g, NC)) if multi else None)
    if "ssd" in ph:
        P.push()
        ssd_prep(g, NCX, NC)
        ssd_run(g, NCX, NC, multi=multi, yb_cb=lambda c, yo: P.dma(DV(g.y_d[1], c * 128, [[NS, 128], [128 * NS, 4], [1, 128]]), yo))
        P.pop()
    if "att" in ph:
        attn_run(g, NCX, NC, lambda qb, yo: P.dma(DV(g.y_d[2], qb * 128, [[NS, 128], [128 * NS, 4], [1, 128]]), yo))
    if "merge" in ph:
        merge_run(g, NCX, NC)
    P.wait_all(); P.emit(); P.close()
    return nc, g


def topk_threshold(g, aff, nblk, cap, tau, iters=30):
    P = g.P
    P.push()
    lo = P.sb([128, 16]); hi = P.sb([128, 16]); mid = P.sb([128, 16]); cmp_ = P.sb([128, 16, nblk])
    cnt = P.sb([128, 16]); ge = P.sb([128, 16]); d1 = P.sb([128, 16])
    P.memset(lo, 0.0); P.memset(hi, 1.0)
    affv = V(aff, [[1, 16], [16, nblk]])
    for it in range(iters):
        P.tt(mid, lo, hi, ALU.add)
        P.ts(mid, mid, 0.5, None, op0=ALU.mult)
        P.tt(cmp_, affv, V(mid, [[1, 16], [0, nblk]]), ALU.is_ge)
        P.reduce(cnt, cmp_)
        ps = g.pbank()
        P.mm(ps[:, 0:16], g.c["ones_f"], cnt)
        P.ts(ge, ps[:, 0:16], float(cap) - 0.5, None, op0=ALU.is_ge)
        P.tt(d1, mid, lo, ALU.subtract); P.tt(d1, d1, ge, ALU.mult); P.tt(lo, lo, d1, ALU.add)
        P.tt(d1, hi, mid, ALU.subtract); P.tt(d1, d1, ge, ALU.mult); P.tt(hi, mid, d1, ALU.add)
    P.copy(tau, lo)
    P.pop()


C_INPUTS = [("norm2_g", [1024]), ("modT", [128, 48, 2]), ("final_norm_g", [1024]), ("ident", [128, 128])]
C_INPUTS_BF = [("w_e_gate", [16 * 1024, 1024]), ("w_e_up", [16 * 1024, 1024]), ("w_e_down", [16 * 1024, 1024])]


def build_C(T, n_total):
    nc = bass.Bass("TRN2", target_bir_lowering=False)
    g = G(); g.nc = nc; g.P = Prog(nc); P = g.P
    P.init_arenas(22 * 1024, 50 * 1024)
    NCX = 2; NC = T // 128; NS = (NCX + NC) * 128
    g.dram = {}
    declare_inputs(g, C_INPUTS, F32)
    declare_inputs(g, C_INPUTS_BF, BF16)
    declare_inputs(g, [("x1T", [8, 128, NS]), ("aff_all", [n_total, 16]), ("aff_own", [NS, 16])], F32)
    x2_d = nc.dram_tensor("x2T", [8, 128, NS], F32, kind="ExternalOutput").ap()
    fin_d = nc.dram_tensor("finT", [8, 128, NS], F32, kind="ExternalOutput").ap()
    setup_psum(g)
    g.c = {}
    g.c["ident"] = P.sb([128, 128]); P.dma(g.c["ident"], g.dram["ident"])
    g.ones_bf = P.sb([128, 128], BF16); P.memset(g.ones_bf, 1.0)
    g.c["ones_f"] = P.sb([128, 128], F32); P.memset(g.c["ones_f"], 1.0)
    g.eps_col = P.sb([128, 1]); P.memset(g.eps_col, 1e-6)
    load_mod(g)
    gfin = P.sb([128, 8]); P.dma(gfin, DV(g.dram["final_norm_g"], 0, [[1, 128], [128, 8]]), allow_slow_non_contiguous=True)
    tau = P.sb([128, 16]); tauc = P.sb([128, 16])
    nblk = n_total // 128
    P.push()
    affa = P.sb([128, nblk, 16])
    P.dma(affa, DV(g.dram["aff_all"], 0, [[16, 128], [2048, nblk], [1, 16]]))
    topk_threshold(g, affa, nblk, 2 * n_total // 16, tau)
    P.pop()
    tiles = [(c, m, True) for (c, m) in col_tiles(0, NCX * 128, 256)] + [(c, m, False) for (c, m) in col_tiles(NCX * 128, T, 256)]
    GROUP = 5
    groups = [tiles[i:i + GROUP] for i in range(0, len(tiles), GROUP)]
    affc = P.sb([128, NCX, 16])
    P.dma(affc, DV(g.dram["aff_own"], 0, [[16, 128], [2048, NCX], [1, 16]]))
    topk_threshold(g, affc, NCX, 2 * NCX * 128 // 16, tauc)
    for grp in groups:
        P.push()
        ncols = sum(n for (_, n, _) in grp)
        gs0 = grp[0][0]
        yacc = P.sb([128, 8, ncols])
        h2b = P.sb([128, 8, ncols], BF16)
        coef = P.sb([128, ncols // 128, 16])
        xres = P.sb([128, 8, ncols]) if False else None
        for (s0, n, is_ctx) in grp:
            P.push()
            o = s0 - gs0
            xt = P.sb([128, 8, n]); P.dma(xt, DV(g.dram["x1T"], s0, [[NS, 128], [128 * NS, 8], [1, n]]))
            h2 = P.sb([128, 8, n])
            mod_norm(g, xt, n, g.a2, g.b2, h2, 1 if is_ctx else 0)
            P.copy(h2b[:, :, o:o + n], h2, eng="act")
            aff = P.sb([128, n // 128, 16])
            P.dma(aff, DV(g.dram["aff_own"], s0 * 16, [[16, 128], [2048, n // 128], [1, 16]]))
            tb = V(tauc if is_ctx else tau, [[0, n // 128], [1, 16]])
            msk = P.sb([128, n // 128, 16])
            P.tt(msk, aff, tb, ALU.is_ge)
            P.tt(coef[:, o // 128:(o + n) // 128, :], aff, msk, ALU.mult)
            P.pop()
        for e in range(16):
            P.push()
            wg = load_w(g, "w_e_gate", e * 1024, 1024, 0, 1024)
            wu = load_w(g, "w_e_up", e * 1024, 1024, 0, 1024)
            wd = load_w(g, "w_e_down", e * 1024, 1024, 0, 1024)
            for (s0, n, is_ctx) in grp:
                P.push()
                o = s0 - gs0
                pcb = g.pbank()
                for blk in range(n // 128):
                    cb_ = coef[:, o // 128 + blk, e:e + 1]
                    P.mm(pcb[:, blk * 128:(blk + 1) * 128], V(cb_, [[0, 128]]), g.c["ident"])
                cbs = P.sb([128, n])
                P.copy(cbs, pcb[:, 0:n], eng="act")
                hid = P.sb([128, 8, n], BF16)
                for f in range(8):
                    pg = g.pbank(); pu = g.pbank()
                    for k in range(8):
                        P.mm(pg[:, 0:n], wg[:, k, f * 128:(f + 1) * 128], h2b[:, k, o:o + n], start=(k == 0), stop=(k == 7))
                    for k in range(8):
                        P.mm(pu[:, 0:n], wu[:, k, f * 128:(f + 1) * 128], h2b[:, k, o:o + n], start=(k == 0), stop=(k == 7))
                    sg = P.sb([128, n])
                    P.act(sg, pg[:, 0:n], AF.Silu)
                    P.tt(sg, sg, pu[:, 0:n], ALU.mult)
                    P.tt(hid[:, f, :], sg, cbs, ALU.mult, eng="pool")
                for j in range(8):
                    pd_ = g.pbank()
                    for f in range(8):
                        P.mm(pd_[:, 0:n], wd[:, f, j * 128:(j + 1) * 128], hid[:, f, :], start=(f == 0), stop=(f == 7))
                    if e == 0:
                        P.copy(yacc[:, j, o:o + n], pd_[:, 0:n], eng="act")
                    else:
                        P.tt(yacc[:, j, o:o + n], yacc[:, j, o:o + n], pd_[:, 0:n], ALU.add)
                P.pop()
            P.pop()
        for (s0, n, is_ctx) in grp:
            P.push()
            o = s0 - gs0
            v = 1 if is_ctx else 0
            xt = P.sb([128, 8, n]); P.dma(xt, DV(g.dram["x1T"], s0, [[NS, 128], [128 * NS, 8], [1, n]]))
            for j in range(8):
                P.stt(xt[:, j, :], yacc[:, j, o:o + n], g.g2[:, j, v:v + 1], xt[:, j, :], ALU.mult, ALU.add)
            P.dma(DV(x2_d, s0, [[NS, 128], [128 * NS, 8], [1, n]]), xt)
            rstd = P.sb([128, n])
            rms_rstd(g, xt, n, rstd)
            for j in range(8):
                P.stt(xt[:, j, :], xt[:, j, :], gfin[:, j:j + 1], rstd, ALU.mult, ALU.mult)
            P.dma(DV(fin_d, s0, [[NS, 128], [128 * NS, 8], [1, n]]), xt)
            P.pop()
        P.pop()
    P.wait_all(); P.emit(); P.close()
    return nc, g


NCORES = 8


def s5_carry_chain(g, NC):
    P = g.P
    s = g.s5
    NCX = 2
    dre = P.sb([128, 32]); dim_ = P.sb([128, 32]); t1 = P.sb([128, 32]); t2 = P.sb([128, 32])
    for half, last in ((slice(0, 64), NCX + NC - 1), (slice(64, 128), NCX)):
        cmul_acc(P, dre[half], dim_[half], s.aqr[half], s.aqi[half], s.apow_re[half, :, last], s.apow_im[half, :, last],
                 None, None, t1[half], t2[half])
    fin = P.sb([128, NCORES, 32, 2])
    P.dma(fin, DV(g.dram["s5_fin_all"], 0, [[64, 128], [128 * 64, NCORES], [1, 64]]))
    H = P.sb([128, NCORES + 1, 32, 2])
    P.copy(H[:, 0, :, 0], s.fin_re[:, :, 0]); P.copy(H[:, 0, :, 1], s.fin_im[:, :, 0])
    for t in range(NCORES):
        for half, m in ((slice(0, 64), t), (slice(64, 128), NCORES - 1 - t)):
            cmul_acc(P, H[half, t + 1, :, 0], H[half, t + 1, :, 1], dre[half], dim_[half], H[half, t, :, 0], H[half, t, :, 1],
                     fin[half, m, :, 0], fin[half, m, :, 1], t1[half], t2[half])
    cr = P.sb([128, 32]); ci = P.sb([128, 32])
    P.memset(cr, 0.0); P.memset(ci, 0.0)
    for t in range(NCORES):
        for half, oh in ((slice(0, 64), g.onehot[0:64, t:t + 1]), (slice(64, 128), g.onehot[64:128, NCORES - 1 - t:NCORES - t])):
            P.stt(cr[half], H[half, t, :, 0], oh, cr[half], ALU.mult, ALU.add)
            P.stt(ci[half], H[half, t, :, 1], oh, ci[half], ALU.mult, ALU.add)
    return cr, ci


def ssd_carry(g, dr_, hctx, out):
    P = g.P
    P.push()
    tot = P.sb([128, NCORES, 16])
    P.dma(tot, DV(g.dram["ssd_tot_all"], 0, [[16, 128], [128 * 16, NCORES], [1, 16]]))
    P.act(tot, tot, AF.Exp)
    H = P.sb([128, 512]); fin = P.sb([128, 512])
    P.copy(H, hctx)
    P.memset(out, 0.0)
    for t in range(NCORES):
        m = t if dr_ == 0 else NCORES - 1 - t
        P.stt(out, H, g.onehot[:, m:m + 1], out, ALU.mult, ALU.add)
        if t == NCORES - 1:
            break
        P.dma(fin, DV(g.dram["ssd_fin_all"], (m * 2 + dr_) * 128 * 512, [[512, 128], [1, 512]]))
        P.tt(V(H, [[64, 8], [1, 64]]), V(H, [[64, 8], [1, 64]]), V(tot[:, m, dr_ * 8:dr_ * 8 + 8], [[1, 8], [0, 64]]), ALU.mult)
        P.tt(H, H, fin, ALU.add)
    P.pop()


def ssd_local_finals(g, NCX, NC):
    P = g.P
    hf = P.sb([128, 512]); hb = P.sb([128, 512]); ts_ = P.sb([128, 16]); pb = P.sb([128, 8]); tmp = P.sb([128, 512])
    P.memset(hf, 0.0); P.memset(hb, 0.0); P.memset(ts_, 0.0); P.memset(pb, 1.0)
    for i in range(NC):
        c = NCX + i
        P.push()
        S, cd, tot = ssd_chunk(g, c, False, None, None)
        P.tt(V(hf, [[64, 8], [1, 64]]), V(hf, [[64, 8], [1, 64]]), V(cd[:, 0:8], [[1, 8], [0, 64]]), ALU.mult)
        P.tt(hf, hf, S[0], ALU.add)
        P.tt(V(tmp, [[64, 8], [1, 64]]), V(S[1], [[64, 8], [1, 64]]), V(pb, [[1, 8], [0, 64]]), ALU.mult)
        P.tt(hb, hb, tmp, ALU.add)
        P.tt(pb, pb, cd[:, 8:16], ALU.mult)
        P.tt(ts_, ts_, tot, ALU.add)
        P.pop()
    P.dma(g.dram["ssd_fin"][0], hf); P.dma(g.dram["ssd_fin"][1], hb); P.dma(g.dram["ssd_tot"], ts_)


W_LIST = [("w_in", 4 * 1024, IN_W), ("s5_w_glu", 4 * 512, 512), ("w_branch", 4 * 1536, 1024), ("w_out", 4 * 1024, 1024),
          ("w_e_gate", 4 * 16 * 1024, 1024), ("w_e_up", 4 * 16 * 1024, 1024), ("w_e_down", 4 * 16 * 1024, 1024)]


def build_W(wlist=W_LIST):
    nc = bass.Bass("TRN2", target_bir_lowering=False)
    g = G(); g.nc = nc; g.P = Prog(nc); P = g.P
    P.init_arenas(24 * 1024, 24 * 1024)
    setup_psum(g)
    g.dram = {}
    engs = ["dve", "act", "pool"]
    ei = 0
    for (name, rows, cols) in wlist:
        rpc = rows // NCORES
        assert rpc % 128 == 0
        src = nc.dram_tensor(name, [rpc, cols], F32, kind="ExternalInput").ap()
        dst = nc.dram_tensor(name + "_bf", [rpc, cols], BF16, kind="ExternalOutput").ap()
        rt = rpc // 128
        cc = cols
        while cc > 2048:
            cc //= 2
        rstep = max(1, 4096 // cc)
        for c0 in range(0, cols, cc):
            for r0 in range(0, rt, rstep):
                r = min(rstep, rt - r0)
                P.push()
                a = P.sb([128, r, cc]); b = P.sb([128, r, cc], BF16)
                P.dma(a, DV(src, r0 * 128 * cols + c0, [[cols, 128], [128 * cols, r], [1, cc]]))
                P.copy(b, a, eng=engs[ei % 3]); ei += 1
                P.dma(DV(dst, r0 * 128 * cols + c0, [[cols, 128], [128 * cols, r], [1, cc]]), b)
                P.pop()
    wm = nc.dram_tensor("w_mod", [1024, 6144], F32, kind="ExternalInput").ap()
    bm = nc.dram_tensor("b_mod", [6144], F32, kind="ExternalInput").ap()
    cc_ = nc.dram_tensor("c2", [2, 1024], F32, kind="ExternalInput").ap()
    mo = nc.dram_tensor("modT", [128, 48, 2], F32, kind="ExternalOutput").ap()
    sc = P.sb([128, 8, 2])
    for v in range(2):
        P.dma(sc[:, :, v], DV(cc_, v * 1024, [[1, 128], [128, 8]]), allow_slow_non_contiguous=True)
    P.act(sc, sc, AF.Silu)
    bcol = P.sb([128, 48])
    P.dma(bcol, DV(bm, 0, [[1, 128], [128, 48]]), allow_slow_non_contiguous=True)
    ps = g.pbank()
    for grp in range(12):
        P.push()
        wt = P.sb([128, 8, 512])
        P.dma(wt, DV(wm, grp * 512, [[6144, 128], [128 * 6144, 8], [1, 512]]))
        for q in range(4):
            ccix = grp * 4 + q
            for k in range(8):
                P.mm(ps[:, ccix * 2:ccix * 2 + 2], wt[:, k, q * 128:(q + 1) * 128], sc[:, k, :], start=(k == 0), stop=(k == 7))
        P.pop()
    mt = P.sb([128, 48, 2])
    P.tt(mt, V(ps, [[2, 48], [1, 2]]), V(bcol, [[1, 48], [0, 2]]), ALU.add)
    P.dma(mo, mt)
    P.wait_all(); P.emit(); P.close()
    return nc, g


BF=ml_dtypes.bfloat16

def rope_tables(own_start, T, n_total):
    NL=T+256
    pos=own_start+np.arange(NL)-128
    valid=(pos>=0)&(pos<n_total)
    pos=np.where(valid,pos,0)
    d=np.arange(64); which=d//32; i=d%16; first=(d%32)<16
    inv=10000.0**(-(i.astype(np.float64))/16)
    axis=np.where(which[:,None]==0, (pos//64)[None,:], (pos%64)[None,:]).astype(np.float64)
    ang=axis*inv[:,None]
    cos=np.cos(ang); sin=np.where(first[:,None], -np.sin(ang), np.sin(ang))
    return np.tile(cos,(2,1)).astype(np.float32), np.tile(sin,(2,1)).astype(np.float32)

def pswap():
    m=np.zeros((128,128),np.float32)
    for j in range(128):
        p = j+16 if (j%32)<16 else j-16
        m[p,j]=1.0
    return m

def fm(a, ncols):
    return np.ascontiguousarray(a.T.reshape(8,128,ncols))

def consts_all():
    c=host_consts(); c["pswap"]=pswap(); return c

def modT_from(mod2):
    return np.ascontiguousarray(mod2.reshape(2,48,128).transpose(2,1,0)).astype(np.float32)


_CACHE = {}

def _prog(key, fn):
    if key not in _CACHE:
        _CACHE[key] = fn()[0]
    return _CACHE[key]

def run_model(inputs, T, ncores=NCORES, depth=4, hook=None):
    assert ncores == NCORES
    n = ncores * T
    NS = T + 256
    f32 = lambda a: np.ascontiguousarray(np.asarray(a, dtype=np.float32))
    ncW = _prog(("W",), build_W)
    flat = {"w_in": f32(inputs["w_in"]).reshape(-1, IN_W), "s5_w_glu": f32(inputs["s5_w_glu"]).reshape(-1, 512),
            "w_branch": f32(inputs["w_branch"]).reshape(-1, 1024), "w_out": f32(inputs["w_out"]).reshape(-1, 1024),
            "w_e_gate": f32(inputs["w_e_gate"]).reshape(-1, 1024), "w_e_up": f32(inputs["w_e_up"]).reshape(-1, 1024),
            "w_e_down": f32(inputs["w_e_down"]).reshape(-1, 1024)}
    c2 = np.stack([f32(inputs["c"])[0], f32(inputs["c_ctx"])], 0)
    maps = []
    for k in range(ncores):
        m = {}
        for name, rows, cols in W_LIST:
            rpc = rows // ncores
            m[name] = np.ascontiguousarray(flat[name][k * rpc:(k + 1) * rpc])
        l = k % depth
        m["w_mod"] = f32(inputs["w_mod"][l]); m["b_mod"] = f32(inputs["b_mod"][l]); m["c2"] = c2
        maps.append(m)
    res = run_bass_kernel_spmd(ncW, maps, core_ids=list(range(ncores))).results
    wbf = {name: np.concatenate([np.asarray(res[k][name + "_bf"]) for k in range(ncores)], 0) for name, _, _ in W_LIST}
    modT = [np.asarray(res[l]["modT"]) for l in range(depth)]
    if hook: hook("W", dict(wbf=wbf, modT=modT))
    consts = consts_all()
    ropes = [rope_tables(k * T, T, n) for k in range(ncores)]
    flags = []
    onehots = []
    for k in range(ncores):
        fl = np.ones((128, 2), np.float32)
        if k == 0: fl[:, 0] = 0
        if k == ncores - 1: fl[:, 1] = 0
        flags.append(fl)
        oh = np.zeros((128, ncores), np.float32); oh[:, k] = 1
        onehots.append(oh)
    x = f32(inputs["x"])[0]
    xc = f32(inputs["ctx"])[0]
    ncA = _prog(("A", T), lambda: build_B(T, multi=True, mode="A"))
    ncB = _prog(("B", T), lambda: build_B(T, multi=True, mode="B"))
    ncC = _prog(("C", T, n), lambda: build_C(T, n))
    fin = None
    for l in range(depth):
        lw = lambda name: f32(inputs[name][l])
        base = dict(consts)
        for name, _ in B_INPUTS:
            if name in inputs: base[name] = lw(name)
        base["modT"] = modT[l]
        base["w_in"] = wbf["w_in"][l * 1024:(l + 1) * 1024]
        base["s5_w_glu"] = wbf["s5_w_glu"][l * 512:(l + 1) * 512]
        base["w_branch"] = wbf["w_branch"][l * 1536:(l + 1) * 1536]
        base["w_out"] = wbf["w_out"][l * 1024:(l + 1) * 1024]
        xpad = np.concatenate([np.zeros((128, 1024), np.float32), x, np.zeros((128, 1024), np.float32)], 0)
        xcT = fm(xc, 256)
        maps = []
        for k in range(ncores):
            m = dict(base)
            m["flags"] = flags[k]
            m["xT"] = fm(xpad[k * T:k * T + T + 256], T + 256)
            m["xcT"] = xcT
            m["rope_cos"], m["rope_sin"] = ropes[k]
            maps.append(m)
        ra = run_bass_kernel_spmd(ncA, maps, core_ids=list(range(ncores))).results
        s5_fin_all = np.stack([np.asarray(ra[k]["s5_fin"]) for k in range(ncores)], 0)
        ssd_fin_all = np.stack([np.asarray(ra[k]["ssd_fin"]) for k in range(ncores)], 0)
        ssd_tot_all = np.stack([np.asarray(ra[k]["ssd_tot"]) for k in range(ncores)], 0)
        for k in range(ncores):
            maps[k]["s5_fin_all"] = s5_fin_all; maps[k]["ssd_fin_all"] = ssd_fin_all; maps[k]["ssd_tot_all"] = ssd_tot_all
            maps[k]["onehot"] = onehots[k]
        rb = run_bass_kernel_spmd(ncB, maps, core_ids=list(range(ncores))).results
        aff_all = np.concatenate([np.asarray(rb[k]["aff"])[256:] for k in range(ncores)], 0)
        if hook: hook(("B", l), dict(rb=rb))
        cmaps = []
        for k in range(ncores):
            cm = {"ident": consts["ident"], "norm2_g": lw("norm2_g"), "modT": modT[l], "final_norm_g": f32(inputs["final_norm_g"]),
                  "w_e_gate": wbf["w_e_gate"][l * 16384:(l + 1) * 16384], "w_e_up": wbf["w_e_up"][l * 16384:(l + 1) * 16384],
                  "w_e_down": wbf["w_e_down"][l * 16384:(l + 1) * 16384],
                  "x1T": np.asarray(rb[k]["x1T"]), "aff_all": aff_all, "aff_own": np.asarray(rb[k]["aff"])}
            cmaps.append(cm)
        rc = run_bass_kernel_spmd(ncC, cmaps, core_ids=list(range(ncores))).results
        unfm = lambda a: np.asarray(a).reshape(1024, -1).T
        x = np.concatenate([unfm(rc[k]["x2T"])[256:] for k in range(ncores)], 0)
        xc = unfm(rc[0]["x2T"])[:256]
        fin = np.concatenate([unfm(rc[k]["finT"])[256:] for k in range(ncores)], 0)
        if hook: hook(("C", l), dict(x=x, xc=xc))
    return np.ascontiguousarray(fin[None].astype(np.float32))


def kernel(**inputs):
    return run_model(inputs, 2048)
```

```python
import ml_dtypes
import contextlib
import numpy as np
import concourse.bass as bass
import concourse.mybir as mybir
from concourse.bass_utils import run_bass_kernel_spmd

F32 = mybir.dt.float32
BF16 = mybir.dt.bfloat16
I32 = mybir.dt.int32
AF = mybir.ActivationFunctionType
ALU = mybir.AluOpType
AX = mybir.AxisListType

ENGS = ("pe", "dve", "act", "pool", "sp")
EPOCH = 20000
N_DMA_SEMS = 12


def _region(ap):
    t = ap.tensor
    shape = list(t.shape)
    space = str(ap.space) if hasattr(ap, "space") else ""
    dims = list(ap.ap)
    off = int(ap.offset)
    if "DRAM" in space.upper() or "HBM" in space.upper() or type(t).__name__.startswith("DRam"):
        lo = off
        hi = off + sum((c - 1) * abs(s) for s, c in dims) + 1
        return (t.name, 0, 1, lo, hi)
    row = 1
    for s in shape[1:]:
        row *= int(s)
    if type(t).__name__.startswith("PSum"):
        return (t.name, 0, 128, 0, row)
    p_lo = off // row
    f_lo = off % row
    p_hi = p_lo + int(dims[0][1])
    f_hi = f_lo + sum((c - 1) * abs(s) for s, c in dims[1:]) + 1
    return (t.name, p_lo, p_hi, f_lo, f_hi)


def _ovl(a, b):
    return a[1] < b[2] and b[1] < a[2] and a[3] < b[4] and b[3] < a[4]


def _covers(a, b):
    return a[1] <= b[1] and a[2] >= b[2] and a[3] <= b[3] and a[4] >= b[4]


class Prog:
    def __init__(self, nc, same_engine_sync=True):
        self.nc = nc
        self.es = contextlib.ExitStack()
        self.ops = {e: [] for e in ENGS}
        self.nops = {e: 0 for e in ENGS}
        self.writes = {}
        self.reads = {}
        import os
        self.same_engine_sync = same_engine_sync and os.environ.get('SAMESYNC', '1') == '1'
        self.dma_tot = [0] * N_DMA_SEMS
        self.dma_rr = 0
        self.known = {e: {} for e in ENGS}
        self._names = 0

    def init_arenas(self, n_f32, n_bf16):
        self.arena = {F32: self.es.enter_context(self.nc.sbuf_tensor("arena_f32", [128, n_f32], F32)),
                      BF16: self.es.enter_context(self.nc.sbuf_tensor("arena_bf16", [128, n_bf16], BF16))}
        self.asize = {F32: n_f32, BF16: n_bf16}
        self.atop = {F32: 0, BF16: 0}
        self.amax = {F32: 0, BF16: 0}
        self.astack = []

    def push(self):
        self.astack.append(dict(self.atop))

    def pop(self):
        self.atop = self.astack.pop()

    def sb(self, shape, dtype=F32, name=None):
        n = 1
        for s_ in shape[1:]:
            n *= int(s_)
        n = (n + 15) // 16 * 16
        off = self.atop[dtype]
        assert off + n <= self.asize[dtype], f"arena {dtype} overflow: {off}+{n} > {self.asize[dtype]} ({name})"
        self.atop[dtype] = off + n
        self.amax[dtype] = max(self.amax[dtype], off + n)
        nn = 1
        for s_ in shape[1:]:
            nn *= int(s_)
        v = self.arena[dtype][:, off:off + nn]
        if len(shape) > 2:
            names = [f"d{i}" for i in range(len(shape) - 1)]
            pat = "p (" + " ".join(names) + ") -> p " + " ".join(names)
            v = v.rearrange(pat, **{nm: int(sz) for nm, sz in zip(names[1:], shape[2:])})
        if shape[0] < 128:
            v = v[0:shape[0]]
        return v

    def ps(self, shape, dtype=F32, name=None):
        self._names += 1
        name = name or f"ps{self._names}"
        return self.es.enter_context(self.nc.psum_tensor(name, list(shape), dtype))

    def _deps(self, reads, writes):
        deps = set()
        rr = [_region(a) for a in reads]
        wr = [_region(a) for a in writes]
        for r in rr:
            for (reg, ev) in self.writes.get(r[0], ()):
                if _ovl(reg, r):
                    deps.add(ev)
            if r[0].startswith("ps"):
                for (reg, ev) in self.reads.get(r[0], ()):
                    deps.add(ev)
        for w in wr:
            for (reg, ev) in self.writes.get(w[0], ()):
                if _ovl(reg, w):
                    deps.add(ev)
            for (reg, ev) in self.reads.get(w[0], ()):
                if _ovl(reg, w):
                    deps.add(ev)
        return deps, rr, wr

    def _record(self, rr, wr, ev):
        for w in wr:
            lst = self.writes.setdefault(w[0], [])
            lst[:] = [(reg, e) for (reg, e) in lst if not _covers(w, reg)]
            lst.append((w, ev))
            rl = self.reads.get(w[0])
            if rl:
                rl[:] = [(reg, e) for (reg, e) in rl if not _covers(w, reg)]
        for r in rr:
            lst = self.reads.setdefault(r[0], [])
            lst[:] = [(reg, e) for (reg, e) in lst if not (e[0] == ev[0] and _covers(r, reg))]
            lst.append((r, ev))

    def op(self, eng, fn, reads, writes, pe_accum=False):
        deps, rr, wr = self._deps(reads, writes)
        idx = self.nops[eng]
        self.nops[eng] += 1
        ev = ((eng, idx // EPOCH), idx % EPOCH + 1)
        waits = self._filter(eng, deps, pe_accum)
        self.ops[eng].append((fn, waits, ev, False))
        self._record(rr, wr, ev)
        return ev

    def _filter(self, eng, deps, pe_accum=False):
        best = {}
        for (s, v) in deps:
            if s[0] == eng:
                if eng == "pe" or not self.same_engine_sync:
                    continue
            if best.get(s, 0) < v:
                best[s] = v
        out = []
        kn = self.known[eng]
        for s, v in best.items():
            if kn.get(s, 0) >= v:
                continue
            kn[s] = v
            out.append((s, v))
        return out

    def dma(self, out, in_, q="sp", **kw):
        deps, rr, wr = self._deps([in_], [out])
        k = self.dma_rr
        self.dma_rr = (self.dma_rr + 1) % N_DMA_SEMS
        sem = ("dma", k)
        prev = self.dma_tot[k]
        if prev:
            deps.add((sem, prev))
        self.dma_tot[k] += 16
        ev = (sem, self.dma_tot[k])
        waits = self._filter(q, deps)
        self.nops[q] += 0
        self.ops[q].append((lambda e, o=out, i=in_, kw=kw: e.dma_start(out=o, in_=i, **kw), waits, ev, True))
        self._record(rr, wr, ev)
        return ev

    def allgather(self, out, in_, n=8):
        deps, rr, wr = self._deps([in_], [out])
        self.cc_tot = getattr(self, "cc_tot", 0) + 1
        ev = (("cc", 0), self.cc_tot)
        if self.cc_tot > 1:
            deps.add((("cc", 0), self.cc_tot - 1))
        waits = self._filter("pool", deps)
        self.ops["pool"].append((lambda e, o=out, i=in_: e.collective_compute(
            "AllGather", ALU.bypass, replica_groups=[list(range(n))], ins=[i], outs=[o]), waits, ev, "cc"))
        self._record(rr, wr, ev)
        return ev

    def wait_all(self, eng="sp"):
        deps = set()
        for lst in self.writes.values():
            for (_, ev) in lst:
                deps.add(ev)
        waits = self._filter(eng, deps)
        self.ops[eng].append((None, waits, None, False))

    def mm(self, out, lhsT, rhs, start=True, stop=True):
        rd = [lhsT, rhs] + ([] if start else [])
        return self.op("pe", lambda e: e.matmul(out, lhsT, rhs, start=start, stop=stop), rd, [out])

    def transpose(self, out, in_, ident):
        return self.op("pe", lambda e: e.transpose(out, in_, ident), [in_, ident], [out])

    def act(self, out, in_, func, bias=0.0, scale=1.0, accum_out=None):
        rd = [in_] + [a for a in (bias, scale) if not isinstance(a, (int, float))]
        wr = [out] + ([accum_out] if accum_out is not None else [])
        kw = {}
        if accum_out is not None:
            kw["accum_out"] = accum_out
        return self.op("act", lambda e: e.activation(out, in_, func, bias=bias, scale=scale, **kw), rd, wr)

    def tt(self, out, in0, in1, op, eng="dve"):
        return self.op(eng, lambda e: e.tensor_tensor(out, in0, in1, op), [in0, in1], [out])

    def ts(self, out, in0, s1, s2=None, op0=ALU.mult, op1=None, eng="dve", accum_out=None):
        rd = [in0] + [a for a in (s1, s2) if a is not None and not isinstance(a, (int, float))]
        wr = [out] + ([accum_out] if accum_out is not None else [])
        kw = {}
        if op1 is not None:
            kw["op1"] = op1
        if accum_out is not None:
            kw["accum_out"] = accum_out
        return self.op(eng, lambda e: e.tensor_scalar(out, in0, s1, s2, op0, **kw), rd, wr)

    def stt(self, out, in0, scalar, in1, op0, op1, eng="dve"):
        rd = [in0, in1] + ([] if isinstance(scalar, (int, float)) else [scalar])
        return self.op(eng, lambda e: e.scalar_tensor_tensor(out, in0, scalar, in1, op0, op1), rd, [out])

    def copy(self, out, in_, eng="dve"):
        if eng == "act":
            return self.op("act", lambda e: e.copy(out, in_), [in_], [out])
        return self.op(eng, lambda e: e.tensor_copy(out, in_), [in_], [out])

    def memset(self, ap, val, eng="dve"):
        return self.op(eng, lambda e: e.memset(ap, val), [], [ap])

    def reduce(self, out, in_, op=ALU.add, axis=AX.X, eng="dve"):
        return self.op(eng, lambda e: e.tensor_reduce(out, in_, axis, op), [in_], [out])

    def recip(self, out, in_):
        return self.op("dve", lambda e: e.reciprocal(out, in_), [in_], [out])

    def scan(self, out, d0, d1, initial, op0=ALU.mult, op1=ALU.add):
        rd = [d0, d1] + ([] if isinstance(initial, (int, float)) else [initial])
        return self.op("dve", lambda e: e.tensor_tensor_scan(out, d0, d1, initial, op0, op1), rd, [out])

    def emit(self):
        nc = self.nc
        sems = {}
        for e in ("pe", "dve", "act", "pool"):
            n_ep = (self.nops[e] + EPOCH - 1) // EPOCH
            for k in range(max(n_ep, 1)):
                sems[(e, k)] = self.es.enter_context(nc.semaphore(f"s_{e}{k}"))
        sems[("cc", 0)] = self.es.enter_context(nc.semaphore("s_cc"))
        for k in range(N_DMA_SEMS):
            sems[("dma", k)] = self.es.enter_context(nc.semaphore(f"s_dma{k}"))
        block = self.es.enter_context(nc.Block())

        def run(engobj, lst):
            for (fn, waits, ev, is_dma) in lst:
                for (s, v) in waits:
                    engobj.wait_ge(sems[s], v)
                if fn is None:
                    continue
                ins = fn(engobj)
                if is_dma == "cc":
                    ins.then_inc(sems[ev[0]])
                elif is_dma:
                    ins.then_inc(sems[ev[0]], 16)
                else:
                    ins.then_inc(sems[ev[0]], 1)

        ops = self.ops

        @block.tensor
        def _(t):
            run(t, ops["pe"])

        @block.vector
        def _(v):
            run(v, ops["dve"])

        @block.scalar
        def _(s):
            run(s, ops["act"])

        @block.gpsimd
        def _(g):
            run(g, ops["pool"])

        @block.sync
        def _(sy):
            run(sy, ops["sp"])

    def close(self):
        self.es.close()


import math
import numpy as np

PI = math.pi
D = 1024
KD = 8
NCTX = 256
HALO = 128
IN_W = 5904
C_U, C_Z, C_XBC, C_DT, C_Q, C_K, C_V, C_G = 0, 512, 1024, 2048, 2064, 2576, 2704, 2832


def V(ap, free_dims, off=0):
    return bass.AP(ap.tensor, ap.offset + off, [list(ap.ap[0])] + [list(d) for d in free_dims])


def DV(t, off, dims):
    return bass.AP(t.tensor, t.offset + off, [list(d) for d in dims])


class G:
    pass


def host_consts():
    c = {}
    c["ident"] = np.eye(128, dtype=np.float32)
    c["ut"] = np.triu(np.ones((128, 128), np.float32))
    c["lt"] = np.tril(np.ones((128, 128), np.float32))
    d = np.arange(128, dtype=np.float32)
    exl = np.tile(d[None, :], (128, 1))
    exr = np.concatenate([np.tile((d + 1)[None, :], (64, 1)), np.tile((128 - d)[None, :], (64, 1))], 0)
    c["exl"] = exl.astype(np.float32)
    c["exr"] = exr.astype(np.float32)
    exv = np.stack([127 - d, d], 1)
    c["exv"] = exv.astype(np.float32)
    m = np.zeros((128, 8), np.float32)
    for p in range(128):
        m[p, p // 16] = 1.0
    c["maskbd"] = m
    return c


CONST_SHAPES = {"ident": [128, 128], "ut": [128, 128], "lt": [128, 128], "exl": [128, 128], "exr": [128, 128],
                "exv": [128, 2], "maskbd": [128, 8]}


def load_consts(g):
    P = g.P
    g.c = {}
    for k, shp in CONST_SHAPES.items():
        t = P.sb(shp, F32)
        P.dma(t, g.dram[k])
        g.c[k] = t
    g.ident_bf = P.sb([128, 128], BF16)
    P.copy(g.ident_bf, g.c["ident"])
    g.ones_bf = P.sb([128, 128], BF16)
    P.memset(g.ones_bf, 1.0)
    g.c["ones_f"] = P.sb([128, 128], F32)
    P.memset(g.c["ones_f"], 1.0)


def sincos(P, out_cos, out_sin, ang, tmp):
    n = 1
    for d_ in ang.shape[1:]:
        n *= int(d_)
    if not hasattr(P, "_kint"):
        P._kint = P.es.enter_context(P.nc.sbuf_tensor("kint", [128, 2048], I32))
        P._halfpi = P.sb([128, 1], F32)
        P.memset(P._halfpi, PI / 2)
    ki = bass.AP(P._kint[:, 0:n].tensor, P._kint[:, 0:n].offset, [list(ang.ap[0])[:1] + [ang.ap[0][1]]] and [[P._kint[:, 0:n].ap[0][0], ang.ap[0][1]], [1, n]])
    angf = bass.AP(ang.tensor, ang.offset, [list(ang.ap[0]), [1, n]])
    tmpf = bass.AP(tmp.tensor, tmp.offset, [list(tmp.ap[0]), [1, n]])
    cosf = bass.AP(out_cos.tensor, out_cos.offset, [list(out_cos.ap[0]), [1, n]])
    sinf = bass.AP(out_sin.tensor, out_sin.offset, [list(out_sin.ap[0]), [1, n]])
    pp = slice(0, 128)
    P.ts(ki, angf, 1.0 / (2 * PI), 0.25, op0=ALU.mult, op1=ALU.add)
    P.stt(tmpf, ki, -2 * PI, angf, ALU.mult, ALU.add)
    P.act(cosf, tmpf, AF.Sin, bias=P._halfpi[0:ang.ap[0][1]] if ang.ap[0][1] < 128 else P._halfpi, scale=1.0)
    P.ts(ki, angf, 1.0 / (2 * PI), None, op0=ALU.mult)
    P.stt(tmpf, ki, -2 * PI, angf, ALU.mult, ALU.add)
    P.act(sinf, tmpf, AF.Sin)


def s5_params(g):
    P = g.P
    dr = g.dram
    s = G()
    g.s5 = s
    NG = getattr(g, "NG", 32)
    s.lrc = P.sb([128, NG]); s.lic = P.sb([128, NG]); s.dtc = P.sb([128, NG])
    for d in range(2):
        P.dma(s.lrc[d * 64:(d + 1) * 64], DV(dr["s5_lam_re"], d * NG * 64, [[1, 64], [64, NG]]), allow_slow_non_contiguous=True)
        P.dma(s.lic[d * 64:(d + 1) * 64], DV(dr["s5_lam_im"], d * NG * 64, [[1, 64], [64, NG]]), allow_slow_non_contiguous=True)
        P.dma(s.dtc[d * 64:(d + 1) * 64], DV(dr["s5_log_dt"], d * NG, [[0, 64], [1, NG]]))
    P.act(s.dtc, s.dtc, AF.Exp)
    s.thc = P.sb([128, NG]); s.lrdtc = P.sb([128, NG])
    P.tt(s.thc, s.lic, s.dtc, ALU.mult)
    P.tt(s.lrdtc, s.lrc, s.dtc, ALU.mult)
    mag = P.sb([128, NG]); co = P.sb([128, NG]); si = P.sb([128, NG]); tmp = P.sb([128, NG])
    P.act(mag, s.lrdtc, AF.Exp)
    sincos(P, co, si, s.thc, tmp)
    abr = P.sb([128, NG]); abi = P.sb([128, NG])
    P.tt(abr, mag, co, ALU.mult)
    P.tt(abi, mag, si, ALU.mult)
    den = P.sb([128, NG]); t2 = P.sb([128, NG])
    P.tt(den, s.lrc, s.lrc, ALU.mult)
    P.tt(t2, s.lic, s.lic, ALU.mult)
    P.tt(den, den, t2, ALU.add)
    P.recip(den, den)
    am1 = P.sb([128, NG])
    P.ts(am1, abr, -1.0, None, op0=ALU.add)
    fr = P.sb([128, NG]); fi = P.sb([128, NG])
    P.tt(fr, am1, s.lrc, ALU.mult); P.tt(t2, abi, s.lic, ALU.mult); P.tt(fr, fr, t2, ALU.add); P.tt(fr, fr, den, ALU.mult)
    P.tt(fi, abi, s.lrc, ALU.mult); P.tt(t2, am1, s.lic, ALU.mult); P.tt(fi, fi, t2, ALU.subtract); P.tt(fi, fi, den, ALU.mult)
    bre = P.sb([128, NG, 16]); bim = P.sb([128, NG, 16])
    s.cre = P.sb([128, NG, 16]); s.cim = P.sb([128, NG, 16])
    for d in range(2):
        sl = slice(d * 64, (d + 1) * 64)
        P.dma(bre[sl], DV(dr["s5_b_re"], d * NG * 1024, [[16, 64], [1024, NG], [1, 16]]))
        P.dma(bim[sl], DV(dr["s5_b_im"], d * NG * 1024, [[16, 64], [1024, NG], [1, 16]]))
        P.dma(s.cre[sl], DV(dr["s5_c_re"], d * NG * 1024, [[1, 64], [1024, NG], [64, 16]]), allow_slow_non_contiguous=True)
        P.dma(s.cim[sl], DV(dr["s5_c_im"], d * NG * 1024, [[1, 64], [1024, NG], [64, 16]]), allow_slow_non_contiguous=True)
    s.bbr = P.sb([128, NG, 16]); s.bbi = P.sb([128, NG, 16])
    frb = V(fr, [[1, NG], [0, 16]]); fib = V(fi, [[1, NG], [0, 16]])
    t3 = P.sb([128, NG, 16])
    P.tt(s.bbr, bre, frb, ALU.mult); P.tt(t3, bim, fib, ALU.mult); P.tt(s.bbr, s.bbr, t3, ALU.subtract)
    P.tt(s.bbi, bim, frb, ALU.mult); P.tt(t3, bre, fib, ALU.mult); P.tt(s.bbi, s.bbi, t3, ALU.add)
    return s


def s5_gen_E(g, ex, out_re, out_im, neg_im=False):
    P = g.P
    s = g.s5
    P.push()
    GH = 16
    NG = getattr(g, "NG", 32)
    ang = P.sb([128, GH, 128]); tmp = P.sb([128, GH, 128]); mag = P.sb([128, GH, 128]); co = P.sb([128, GH, 128])
    exb = V(ex, [[0, GH], [1, 128]])
    for h in range(NG // GH):
        gs = slice(h * GH, (h + 1) * GH)
        P.tt(ang, V(s.thc[:, gs], [[1, GH], [0, 128]]), exb, ALU.mult)
        P.tt(mag, V(s.lrdtc[:, gs], [[1, GH], [0, 128]]), exb, ALU.mult, eng="pool")
        P.act(mag, mag, AF.Exp)
        sincos(P, co, ang, ang, tmp)
        P.tt(out_re[:, gs, :], mag, co, ALU.mult)
        if neg_im:
            P.stt(out_im[:, gs, :], mag, -1.0, ang, ALU.mult, ALU.mult)
        else:
            P.tt(out_im[:, gs, :], mag, ang, ALU.mult)
    P.pop()


def s5_gen_V(g, vr, vi):
    P = g.P
    dr = g.dram
    P.push()
    GH = 16
    lib = P.sb([128, GH, 2, 64]); lrb = P.sb([128, GH, 2, 64]); dtb = P.sb([128, GH, 2])
    tmp = P.sb([128, GH, 2, 64]); co = P.sb([128, GH, 2, 64])
    exvb = V(g.c["exv"], [[0, GH], [1, 2], [0, 64]])
    NG = getattr(g, "NG", 32)
    for h in range(NG // GH):
        g0 = h * GH
        for d_ in range(2):
            P.dma(lib[:, :, d_, :], DV(dr["s5_lam_im"], g0 * 64 + d_ * NG * 64, [[0, 128], [64, GH], [1, 64]]))
            P.dma(lrb[:, :, d_, :], DV(dr["s5_lam_re"], g0 * 64 + d_ * NG * 64, [[0, 128], [64, GH], [1, 64]]))
            P.dma(dtb[:, :, d_], DV(dr["s5_log_dt"], g0 + d_ * NG, [[0, 128], [1, GH]]), allow_slow_non_contiguous=True)
        P.act(dtb, dtb, AF.Exp)
        dtbb = V(dtb, [[2, GH], [1, 2], [0, 64]])
        P.tt(lib, lib, dtbb, ALU.mult)
        P.tt(lrb, lrb, dtbb, ALU.mult, eng="pool")
        P.tt(lib, lib, exvb, ALU.mult)
        P.tt(lrb, lrb, exvb, ALU.mult, eng="pool")
        P.act(lrb, lrb, AF.Exp)
        sincos(P, co, lib, lib, tmp)
        gs = slice(g0, g0 + GH)
        P.tt(vr[:, gs, :], lrb, co, ALU.mult)
        P.tt(vi[:, gs, :], lrb, lib, ALU.mult)
    P.pop()


def cmul_acc(P, out_re, out_im, ar, ai, hr, hi, sr, si, t1, t2, eng="dve"):
    P.tt(t1, ar, hr, ALU.mult, eng=eng)
    P.tt(t2, ai, hi, ALU.mult, eng=eng)
    P.tt(t1, t1, t2, ALU.subtract, eng=eng)
    if sr is not None:
        P.tt(out_re, t1, sr, ALU.add, eng=eng)
    else:
        P.copy(out_re, t1, eng=eng)
    P.tt(t1, ar, hi, ALU.mult, eng=eng)
    P.tt(t2, ai, hr, ALU.mult, eng=eng)
    P.tt(t1, t1, t2, ALU.add, eng=eng)
    if si is not None:
        P.tt(out_im, t1, si, ALU.add, eng=eng)
    else:
        P.copy(out_im, t1, eng=eng)


def s5_states(g, uT, NCH):
    P = g.P
    s = g.s5
    s.sre = P.sb([128, 32, NCH]); s.sim = P.sb([128, 32, NCH])
    P.push()
    vr = P.sb([128, 32, 128], BF16); vi = P.sb([128, 32, 128], BF16)
    if "s5_vr" in g.dram:
        P.dma(vr, g.dram["s5_vr"]); P.dma(vi, g.dram["s5_vi"])
    else:
        s5_gen_V(g, V(vr, [[128, 32], [64, 2], [1, 64]]), V(vi, [[128, 32], [64, 2], [1, 64]]))
    utok = P.sb([128, NCH, 512], BF16)
    for c in range(NCH):
        pt = g.pbank_bf()
        for b in range(4):
            P.transpose(pt[:, b * 128:(b + 1) * 128], uT[:, b, c * 128:(c + 1) * 128], g.ident_bf)
        P.copy(utok[:, c, :], pt[:, 0:512], eng="act" if c % 2 else "dve")
    zr = P.sb([128, NCH, 16]); zi = P.sb([128, NCH, 16]); t1 = P.sb([128, NCH, 16]); t2 = P.sb([128, NCH, 16])
    t3 = P.sb([128, NCH, 16]); t4 = P.sb([128, NCH, 16])
    N = NCH * 16
    for gi in range(32):
        p1 = g.pbank(); p2 = g.pbank()
        rhs = V(utok, [[512, NCH], [1, 16]], off=gi * 16)
        P.mm(V(p1, [[16, NCH], [1, 16]]), vr[:, gi, :], rhs)
        P.mm(V(p2, [[16, NCH], [1, 16]]), vi[:, gi, :], rhs)
        P.copy(zr, V(p1, [[16, NCH], [1, 16]]), eng="act")
        P.copy(zi, V(p2, [[16, NCH], [1, 16]]), eng="act")
        bb_r = V(s.bbr[:, gi, :], [[0, NCH], [1, 16]]); bb_i = V(s.bbi[:, gi, :], [[0, NCH], [1, 16]])
        P.tt(t1, zr, bb_r, ALU.mult); P.tt(t2, zi, bb_i, ALU.mult); P.tt(t1, t1, t2, ALU.subtract)
        P.reduce(s.sre[:, gi, :], t1)
        P.tt(t3, zi, bb_r, ALU.mult, eng="pool"); P.tt(t4, zr, bb_i, ALU.mult, eng="pool"); P.tt(t3, t3, t4, ALU.add, eng="pool")
        P.reduce(s.sim[:, gi, :], t3)
    P.pop()


def s5_local_scan(g, NCX, NC):
    P = g.P
    s = g.s5
    NCH = NCX + NC
    s.pre_re = P.sb([128, 32, NCH]); s.pre_im = P.sb([128, 32, NCH])
    s.fin_re = P.sb([128, 32, 2]); s.fin_im = P.sb([128, 32, 2])
    s.apow_re = P.sb([128, 32, NCH]); s.apow_im = P.sb([128, 32, NCH])
    t1 = P.sb([128, 32]); t2 = P.sb([128, 32])
    P.memset(s.pre_re, 0.0); P.memset(s.pre_im, 0.0)
    for (c0, n, fi) in ((0, NCX, 0), (NCX, NC, 1)):
        for half, order in ((slice(0, 64), list(range(c0, c0 + n))), (slice(64, 128), list(range(c0 + n - 1, c0 - 1, -1)))):
            eng = "dve" if half.start == 0 else "pool"
            aqr = s.aqr[half]; aqi = s.aqi[half]
            P.memset(s.apow_re[half, :, order[0]], 1.0, eng=eng); P.memset(s.apow_im[half, :, order[0]], 0.0, eng=eng)
            for k in range(n):
                c = order[k]
                if k + 1 < n:
                    cn = order[k + 1]
                    ore, oim = s.pre_re[half, :, cn], s.pre_im[half, :, cn]
                    cmul_acc(P, s.apow_re[half, :, cn], s.apow_im[half, :, cn], aqr, aqi, s.apow_re[half, :, c], s.apow_im[half, :, c],
                             None, None, t1[half], t2[half], eng=eng)
                else:
                    ore, oim = s.fin_re[half, :, fi], s.fin_im[half, :, fi]
                cmul_acc(P, ore, oim, aqr, aqi, s.pre_re[half, :, c], s.pre_im[half, :, c], s.sre[half, :, c], s.sim[half, :, c],
                         t1[half], t2[half], eng=eng)


def s5_tables_ro(g):
    P = g.P
    s = g.s5
    s.w2re = P.sb([128, 32, 128], BF16); s.nw2im = P.sb([128, 32, 128], BF16)
    s.aqr = P.sb([128, 32]); s.aqi = P.sb([128, 32])
    if "s5_w2re" in g.dram:
        P.dma(s.w2re, g.dram["s5_w2re"]); P.dma(s.nw2im, g.dram["s5_nw2im"])
    else:
        s5_gen_E(g, g.c["exr"], s.w2re, s.nw2im, neg_im=True)
    P.push()
    ang = P.sb([128, 32]); mag = P.sb([128, 32]); tmp = P.sb([128, 32]); co = P.sb([128, 32])
    P.ts(ang, s.thc, 128.0, None, op0=ALU.mult)
    P.act(mag, s.lrdtc, AF.Exp, scale=128.0)
    sincos(P, co, ang, ang, tmp)
    P.tt(s.aqr, mag, co, ALU.mult)
    P.tt(s.aqi, mag, ang, ALU.mult)
    P.pop()


def s5_readout(g, NCX, NC, carry_re, carry_im):
    P = g.P
    s = g.s5
    NCH = NCX + NC
    hre = P.sb([128, 32, NCH]); him = P.sb([128, 32, NCH])
    P.copy(hre, s.pre_re); P.copy(him, s.pre_im, eng="pool")
    own = slice(NCX, NCH)
    t1 = P.sb([128, 32, NC]); t2 = P.sb([128, 32, NC])
    crb = V(carry_re, [[carry_re.ap[1][0], 32], [0, NC]]); cib = V(carry_im, [[carry_im.ap[1][0], 32], [0, NC]])
    P.tt(t1, s.apow_re[:, :, own], crb, ALU.mult); P.tt(t2, s.apow_im[:, :, own], cib, ALU.mult)
    P.tt(t1, t1, t2, ALU.subtract); P.tt(hre[:, :, own], hre[:, :, own], t1, ALU.add)
    P.tt(t1, s.apow_re[:, :, own], cib, ALU.mult); P.tt(t2, s.apow_im[:, :, own], crb, ALU.mult)
    P.tt(t1, t1, t2, ALU.add); P.tt(him[:, :, own], him[:, :, own], t1, ALU.add)
    P.push()
    gre = P.sb([128, 32, NCH, 16], BF16); gim = P.sb([128, 32, NCH, 16], BF16)
    GQ = 8
    a1 = P.sb([128, GQ, NCH, 16]); a2 = P.sb([128, GQ, NCH, 16])
    for q in range(32 // GQ):
        gs = slice(q * GQ, (q + 1) * GQ)
        crb_ = V(s.cre[:, gs, :], [[16, GQ], [0, NCH], [1, 16]]); cib_ = V(s.cim[:, gs, :], [[16, GQ], [0, NCH], [1, 16]])
        hrb = V(hre[:, gs, :], [[NCH, GQ], [1, NCH], [0, 16]]); hib = V(him[:, gs, :], [[NCH, GQ], [1, NCH], [0, 16]])
        P.tt(a1, crb_, hrb, ALU.mult); P.tt(a2, cib_, hib, ALU.mult, eng="pool"); P.tt(gre[:, gs], a1, a2, ALU.subtract)
        P.tt(a1, crb_, hib, ALU.mult); P.tt(a2, cib_, hrb, ALU.mult, eng="pool"); P.tt(gim[:, gs], a1, a2, ALU.add)
    N = NCH * 16
    for gi in range(32):
        ps = g.pbank()
        o = V(ps, [[16, NCH], [1, 16]])
        P.mm(o, s.w2re[:, gi, :], V(gre[:, gi], [[16, NCH], [1, 16]]), start=True, stop=False)
        P.mm(o, s.nw2im[:, gi, :], V(gim[:, gi], [[16, NCH], [1, 16]]), start=False, stop=True)
        P.copy(V(s.ystok, [[512, NCH], [1, 16]], off=gi * 16), o, eng="act" if gi % 2 else "dve")
    P.pop()


def s5_kt_build(g, NB, kt_d):
    P = g.P
    s = g.s5
    NG = NB * 8
    P.push()
    elr = P.sb([128, NG, 128], BF16); eli = P.sb([128, NG, 128], BF16)
    s5_gen_E(g, g.c["exl"], elr, eli)
    brpad = P.sb([128, NG, 128], BF16); nbipad = P.sb([128, NG, 128], BF16)
    P.memset(brpad, 0.0); P.memset(nbipad, 0.0, eng="pool")
    padv = lambda t: V(t, [[1024, NB], [144, 8], [1, 16]])
    P.copy(padv(brpad), V(s.bbr, [[128, NB], [16, 8], [1, 16]]))
    P.ts(padv(nbipad), V(s.bbi, [[128, NB], [16, 8], [1, 16]]), -1.0, None, op0=ALU.mult)
    care = P.sb([128, 32, 16], BF16); caim = P.sb([128, 32, 16], BF16)
    a1 = P.sb([128, 32, 16]); a2 = P.sb([128, 32, 16])
    ko = P.sb([128, 2, 512], BF16)
    for b in range(NB):
        for lh in range(2):
            for sl in range(2):
                d0 = lh * 64 + sl * 32
                pk = [g.pbank(), g.pbank()]
                for gl in range(8):
                    gi = 8 * b + gl
                    crb = V(s.cre[:, gi, :], [[0, 32], [1, 16]]); cib = V(s.cim[:, gi, :], [[0, 32], [1, 16]])
                    erb = V(elr[:, gi, d0:d0 + 32], [[1, 32], [0, 16]]); eib = V(eli[:, gi, d0:d0 + 32], [[1, 32], [0, 16]])
                    P.tt(a1, crb, erb, ALU.mult); P.tt(a2, cib, eib, ALU.mult, eng="pool"); P.tt(care, a1, a2, ALU.subtract)
                    P.tt(a1, crb, eib, ALU.mult); P.tt(a2, cib, erb, ALU.mult, eng="pool"); P.tt(caim, a1, a2, ALU.add)
                    for dr_ in range(2):
                        h = slice(dr_ * 64, (dr_ + 1) * 64)
                        P.mm(pk[dr_], brpad[h, gi, :], V(care[h], [[1, 512]]), start=(gl == 0), stop=False)
                        P.mm(pk[dr_], nbipad[h, gi, :], V(caim[h], [[1, 512]]), start=False, stop=(gl == 7))
                for dr_ in range(2):
                    P.copy(ko[:, dr_, :], pk[dr_], eng="act" if dr_ else "dve")
                    off = (((b * 2 + lh) * 2 + dr_) * 128) * 1024 + sl * 512
                    P.dma(DV(kt_d, off, [[1024, 128], [1, 512]]), ko[:, dr_, :])
    P.pop()


def s5_lags(g, uT, NCH, ya_acc_cb):
    P = g.P
    s = g.s5
    P.push()
    kt_d = g.dram["s5_kt"]
    kt = P.sb([128, 2, 1024], BF16)
    bd = P.sb([128, 2, 64, 128], BF16)
    NS = NCH * 128
    yacc = P.sb([128, NS])
    mb = V(g.c["maskbd"], [[0, 64], [1, 8], [0, 16]])
    cgs = [(c0, min(4, NCH - c0)) for c0 in range(0, NCH, 4)]
    for b in range(4):
        for lh in range(2):
            P.dma(kt, DV(kt_d, (b * 2 + lh) * 2 * 128 * 1024, [[1024, 128], [128 * 1024, 2], [1, 1024]]))
            for dr_ in range(2):
                P.tt(V(bd[:, dr_], [[128, 64], [16, 8], [1, 16]]), V(kt[:, dr_, :], [[16, 64], [0, 8], [1, 16]]), mb, ALU.mult,
                     eng="pool" if dr_ else "dve")
            for (c0, ncg) in cgs:
                ps = g.pbank()
                first = True
                if lh == 1:
                    P.mm(ps[:, 0:ncg * 128], g.zeros_bf, V(uT[:, b, :], [[1, ncg * 128]], off=c0 * 128), start=True, stop=False)
                    for c in range(c0, c0 + ncg):
                        P.mm(ps[:, (c - c0) * 128:(c - c0 + 1) * 128], s.ystok[:, c, b * 128:(b + 1) * 128], g.ident_bf,
                             start=False, stop=False)
                    first = False
                for d in range(64):
                    dd = lh * 64 + d
                    w = 128 - dd
                    last = (d == 63)
                    P.mm(V(ps, [[128, ncg], [1, w]], off=dd), bd[:, 0, d, :], V(uT[:, b, :], [[128, ncg], [1, w]], off=c0 * 128),
                         start=first, stop=False)
                    first = False
                    P.mm(V(ps, [[128, ncg], [1, w]]), bd[:, 1, d, :], V(uT[:, b, :], [[128, ncg], [1, w]], off=c0 * 128 + dd),
                         start=False, stop=last)
                dst = yacc[:, c0 * 128:(c0 + ncg) * 128]
                if lh == 0:
                    P.copy(dst, ps[:, 0:ncg * 128], eng="act")
                else:
                    P.tt(dst, dst, ps[:, 0:ncg * 128], ALU.add)
        ya_acc_cb(b, yacc)
    P.pop()


def s5_core(g, uT, NCX, NC, carry_fn, ya_cb, states_cb=None, states_only=False):
    P = g.P
    NCH = NCX + NC
    s5_params(g)
    s = g.s5
    s.ystok = P.sb([128, NCH, 512], BF16)
    import os
    S5CUT = os.environ.get("S5CUT", "")
    P.push()
    s5_tables_ro(g)
    if S5CUT == "ro":
        P.pop(); return
    s5_states(g, uT, NCH)
    if S5CUT == "states":
        P.pop(); return
    s5_local_scan(g, NCX, NC)
    if states_cb is not None:
        states_cb()
    if states_only:
        P.pop()
        return
    cr, ci = carry_fn()
    s5_readout(g, NCX, NC, cr, ci)
    P.pop()
    if S5CUT == "readout":
        return
    s5_lags(g, uT, NCH, ya_cb)


def load_w(g, name, r0, nrows, c0, ncols, dtype=BF16, q="sp"):
    P = g.P
    kk = nrows // 128
    t = P.sb([128, kk, ncols], dtype)
    d = g.dram[name]
    ncol_total = d.shape[-1]
    P.dma(t, DV(d, r0 * ncol_total + c0, [[ncol_total, 128], [128 * ncol_total, kk], [1, ncols]]), q=q)
    return t


def load_h(g, c0, n):
    P = g.P
    t = P.sb([128, 8, n], BF16)
    P.dma(t, DV(g.hT_d, c0, [[g.E, 128], [128 * g.E, 8], [1, n]]))
    return t


def proj(g, ps_out, w, j0, hT, n, wcols=128):
    P = g.P
    for k in range(8):
        P.mm(ps_out, w[:, k, j0:j0 + wcols], hT[:, k, 0:n], start=(k == 0), stop=(k == 7))


def col_tiles(c0, n, step=512):
    out = []
    c = c0
    while c < c0 + n:
        m = min(step, c0 + n - c)
        out.append((c, m))
        c += m
    return out


def ssd_prep(g, NCX, NC):
    P = g.P
    dr = g.dram
    s = G(); g.ssd = s
    T = NC * 128; NS = (NCX + NC) * 128
    s.xsT = P.sb([128, 4, NS], BF16); s.bmT = P.sb([128, 2, NS], BF16); s.cmT = P.sb([128, 2, NS], BF16)
    s.gz = P.sb([128, 4, NS], BF16)
    s.dt = P.sb([128, NCX + NC, 16]); s.dta = P.sb([128, NCX + NC, 16])
    s.cw = P.sb([128, 8, 5]); s.cb = P.sb([128, 8])
    for k_ in range(5):
        P.dma(s.cw[:, :, k_], DV(dr["ssd_conv_w"], k_ * 1024, [[1, 128], [128, 8]]), allow_slow_non_contiguous=True)
    P.dma(s.cb, DV(dr["ssd_conv_b"], 0, [[1, 128], [128, 8]]), allow_slow_non_contiguous=True)
    s.dtb = P.sb([128, 16]); s.ab = P.sb([128, 16])
    P.dma(s.dtb, DV(dr["ssd_dt_bias"], 0, [[0, 128], [1, 16]]))
    P.dma(s.ab, DV(dr["ssd_a_log"], 0, [[0, 128], [1, 16]]))
    P.act(s.ab, s.ab, AF.Exp)
    P.ts(s.ab, s.ab, -1.0, None, op0=ALU.mult)
    s.dcol = P.sb([128, 4])
    for hh in range(2):
        P.dma(s.dcol[hh * 64:(hh + 1) * 64, :], DV(dr["ssd_d"], hh, [[0, 64], [2, 4]]), allow_slow_non_contiguous=True)
    s.ng = P.sb([128, 4])
    P.dma(s.ng, DV(dr["ssd_norm_g"], 0, [[1, 128], [128, 4]]), allow_slow_non_contiguous=True)
    P.push()
    wx = load_w(g, "w_in", 0, 1024, C_XBC, 1024)
    wz = load_w(g, "w_in", 0, 1024, C_Z, 512)
    wdt = load_w(g, "w_in", 0, 1024, C_DT, 16)
    regions = [(0, NCX * 128, g.e_ctx, False), (NCX * 128, T, g.e_own, True)]
    W = NS + 8
    xin = P.sb([128, W])
    for j in range(8):
        P.memset(xin[:, 0:2], 0.0); P.memset(xin[:, 2 + NCX * 128:4 + NCX * 128], 0.0)
        for (s0, n, e0, is_own) in regions:
            xo = 2 + s0 + (4 if is_own else 0)
            lo, hi = (e0 - 2, e0 + n + 2) if is_own else (e0, e0 + n)
            xo_lo = xo - 2 if is_own else xo
            for (c, m) in col_tiles(lo, hi - lo):
                P.push()
                ht = load_h(g, c, m)
                ps = g.pbank()
                proj(g, ps[:, 0:m], wx, j * 128, ht, m)
                P.copy(xin[:, xo_lo + (c - lo):xo_lo + (c - lo) + m], ps[:, 0:m], eng="act")
                P.pop()
            if is_own:
                P.ts(xin[:, xo - 2:xo], xin[:, xo - 2:xo], g.flagL, None, op0=ALU.mult)
                P.ts(xin[:, xo + n:xo + n + 2], xin[:, xo + n:xo + n + 2], g.flagR, None, op0=ALU.mult)
        dst = s.xsT[:, j, :] if j < 4 else (s.bmT[:, j - 4, :] if j < 6 else s.cmT[:, j - 6, :])
        for (s0, n, e0, is_own) in regions:
            xo = 2 + s0 + (4 if is_own else 0)
            P.push()
            acc = P.sb([128, n])
            P.ts(acc, xin[:, xo - 2:xo - 2 + n], s.cw[:, j, 0:1], None, op0=ALU.mult)
            for k in range(1, 5):
                P.stt(acc, xin[:, xo - 2 + k:xo - 2 + k + n], s.cw[:, j, k:k + 1], acc, ALU.mult, ALU.add)
            P.act(dst[:, s0:s0 + n], acc, AF.Silu, bias=s.cb[:, j:j + 1])
            P.pop()
    for (s0, n, e0, is_own) in regions:
        for (c, m) in col_tiles(e0, n):
            P.push()
            ht = load_h(g, c, m)
            so = s0 + (c - e0)
            for j in range(4):
                ps = g.pbank()
                proj(g, ps[:, 0:m], wz, j * 128, ht, m)
                P.act(s.gz[:, j, so:so + m], ps[:, 0:m], AF.Silu)
            for cc in range(m // 128):
                ps = g.pbank()
                for k in range(8):
                    P.mm(ps[:, 0:16], ht[:, k, cc * 128:(cc + 1) * 128], wdt[:, k, :], start=(k == 0), stop=(k == 7))
                ci = (so + cc * 128) // 128
                P.tt(s.dt[:, ci, :], ps[:, 0:16], s.dtb, ALU.add)
            P.pop()
    P.pop()
    P.act(s.dt, s.dt, AF.Exp)
    P.act(s.dt, s.dt, AF.Ln, bias=1.0)
    P.tt(s.dta, s.dt, V(s.ab, [[0, NCX + NC], [1, 16]]), ALU.mult)


def ssd_chunk(g, c, want_y, hin_f, hin_b, ybuf=None, sdirs=(0, 1)):
    P = g.P
    s = g.ssd
    cs = slice(c * 128, (c + 1) * 128)
    ut = g.c["ut"]; lt = g.c["lt"]
    xs_tok = P.sb([128, 8, 64], BF16); bm_tok = P.sb([128, 2, 128], BF16)
    pt = g.pbank_bf()
    for b in range(4):
        P.transpose(pt[:, b * 128:(b + 1) * 128], s.xsT[:, b, cs], g.ident_bf)
    P.copy(V(xs_tok, [[1, 512]]), pt[:, 0:512], eng="act")
    pt2 = g.pbank_bf()
    for b in range(2):
        P.transpose(pt2[:, b * 128:(b + 1) * 128], s.bmT[:, b, cs], g.ident_bf)
    P.copy(V(bm_tok, [[1, 256]]), pt2[:, 0:256], eng="act")
    pc = g.pbank()
    P.mm(pc[:, 0:8], ut, s.dta[:, c, 0:8])
    P.mm(pc[:, 8:16], lt, s.dta[:, c, 8:16])
    nacum = P.sb([128, 16])
    P.ts(nacum, pc[:, 0:16], -1.0, None, op0=ALU.mult)
    acb = [None] * 4
    adirs = (0, 1) if want_y else sdirs
    for dr_ in adirs:
        m = ut if dr_ == 0 else lt
        for hq in range(2):
            rb = P.sb([128, 4, 128])
            P.tt(rb, V(m, [[0, 4], [1, 128]]), V(s.dta[:, c, dr_ * 8 + hq * 4:dr_ * 8 + hq * 4 + 4], [[1, 4], [0, 128]]), ALU.mult,
                 eng="pool")
            pa = g.pbank()
            P.mm(V(pa, [[1, 512]]), g.c["ones_f"], V(rb, [[1, 512]]))
            pas = P.sb([128, 512])
            P.copy(pas, pa, eng="act" if hq else "dve")
            acb[dr_ * 2 + hq] = pas
    tot = P.sb([128, 16])
    if len(adirs) < 2:
        P.memset(tot, 0.0)
    for dr_ in adirs:
        for hq in range(2):
            pa = acb[dr_ * 2 + hq]
            col = 127 if dr_ == 0 else 0
            P.copy(tot[:, dr_ * 8 + hq * 4:dr_ * 8 + hq * 4 + 4], V(pa, [[128, 4]], off=col))
    w = P.sb([128, 16]); cd = P.sb([128, 16])
    P.tt(w, tot, nacum, ALU.add)
    P.act(w, w, AF.Exp)
    P.tt(w, w, s.dt[:, c, :], ALU.mult)
    P.act(cd, tot, AF.Exp)
    S = [None, None]
    for dr_ in sdirs:
        xsw = P.sb([128, 8, 64], BF16)
        P.tt(xsw, xs_tok, V(w[:, dr_ * 8:dr_ * 8 + 8], [[1, 8], [0, 64]]), ALU.mult)
        pS = g.pbank()
        for h in range(8):
            P.mm(pS[:, h * 64:(h + 1) * 64], bm_tok[:, h // 4, :], xsw[:, h, :])
        Ss = P.sb([128, 512])
        P.copy(Ss, pS, eng="act")
        S[dr_] = Ss
    if not want_y:
        return S, cd, tot
    cbm = []
    for gq in range(2):
        pcb = g.pbank()
        P.mm(pcb[:, 0:128], s.bmT[:, gq, cs], s.cmT[:, gq, cs])
        cf = P.sb([128, 128]); cbk = P.sb([128, 128])
        P.tt(cf, pcb[:, 0:128], ut, ALU.mult)
        P.tt(cbk, pcb[:, 0:128], lt, ALU.mult)
        cbm.append((cf, cbk))
    for pair in range(4):
        py = g.pbank()
        for hh in range(2):
            h = pair * 2 + hh
            gq = h // 4
            hq, hi4 = h // 4, h % 4
            mms = []
            for dr_ in range(2):
                pa = acb[dr_ * 2 + hq]
                e1 = P.sb([128, 128])
                P.ts(e1, pa[:, hi4 * 128:(hi4 + 1) * 128], nacum[:, dr_ * 8 + h:dr_ * 8 + h + 1], g.zero_col, op0=ALU.add, op1=ALU.min)
                P.act(e1, e1, AF.Exp)
                wt = P.sb([128, 128], BF16)
                P.stt(wt, e1, s.dt[:, c, dr_ * 8 + h:dr_ * 8 + h + 1], cbm[gq][dr_], ALU.mult, ALU.mult)
                mms.append((xs_tok[:, h, :], wt))
                hin = hin_f if dr_ == 0 else hin_b
                if hin is not None:
                    dec = P.sb([128, 128])
                    P.act(dec, pa[:, hi4 * 128:(hi4 + 1) * 128], AF.Exp)
                    csd = P.sb([128, 128], BF16)
                    P.tt(csd, s.cmT[:, gq, cs], dec, ALU.mult, eng="pool")
                    mms.append((hin[:, h, :], csd))
            for i_, (l_, r_) in enumerate(mms):
                P.mm(py[hh * 64:(hh + 1) * 64, 0:128], l_, r_, start=(i_ == 0), stop=(i_ == len(mms) - 1))
        P.stt(ybuf[:, pair, :], s.xsT[:, pair, cs], s.dcol[:, pair:pair + 1], py[:, 0:128], ALU.mult, ALU.add)
    return S, cd, tot


def ssd_run(g, NCX, NC, multi=False, yb_cb=None):
    P = g.P
    s = g.ssd
    NCH = NCX + NC
    hf = P.sb([128, 512]); hb = P.sb([128, 512])
    hbf = P.sb([128, 8, 64], BF16)
    for (c0, n) in ((0, NCX), (NCX, NC)):
        if c0 == 0:
            P.memset(hb, 0.0)
        elif multi:
            P.push(); tmpc = P.sb([128, 512]); ssd_carry(g, 1, hb, tmpc); P.copy(hb, tmpc); P.pop()
        for c in range(c0 + n - 1, c0 - 1, -1):
            P.copy(V(hbf, [[1, 512]]), hb)
            P.dma(DV(g.ssd_hb_d, c * 128 * 512, [[512, 128], [1, 512]]), V(hbf, [[1, 512]]))
            P.push()
            S, cd, _t = ssd_chunk(g, c, False, None, None, sdirs=(1,))
            P.tt(V(hb, [[64, 8], [1, 64]]), V(hb, [[64, 8], [1, 64]]), V(cd[:, 8:16], [[1, 8], [0, 64]]), ALU.mult)
            P.tt(hb, hb, S[1], ALU.add)
            P.pop()
    hfb = P.sb([128, 8, 64], BF16); hbb = P.sb([128, 8, 64], BF16)
    ybuf = P.sb([128, 4, 128])
    for (c0, n) in ((0, NCX), (NCX, NC)):
        if c0 == 0:
            P.memset(hf, 0.0)
        elif multi:
            P.push(); tmpc = P.sb([128, 512]); ssd_carry(g, 0, hf, tmpc); P.copy(hf, tmpc); P.pop()
        for c in range(c0, c0 + n):
            P.copy(V(hfb, [[1, 512]]), hf)
            P.dma(V(hbb, [[1, 512]]), DV(g.ssd_hb_d, c * 128 * 512, [[512, 128], [1, 512]]))
            P.push()
            S, cd, _t = ssd_chunk(g, c, True, hfb, hbb, ybuf, sdirs=(0,))
            P.tt(V(hf, [[64, 8], [1, 64]]), V(hf, [[64, 8], [1, 64]]), V(cd[:, 0:8], [[1, 8], [0, 64]]), ALU.mult)
            P.tt(hf, hf, S[0], ALU.add)
            cs = slice(c * 128, (c + 1) * 128)
            yg = P.sb([128, 4, 128]); sq = P.sb([128, 4, 128], BF16)
            P.tt(yg, ybuf, s.gz[:, :, cs], ALU.mult)
            P.tt(sq, yg, yg, ALU.mult, eng="pool")
            pn = g.pbank()
            for b in range(4):
                P.mm(pn[:, 0:128], g.ones_bf, sq[:, b, :], start=(b == 0), stop=(b == 3))
            rstd = P.sb([128, 128])
            P.act(rstd, pn[:, 0:128], AF.Sqrt, scale=1.0 / 512, bias=g.eps_col)
            P.recip(rstd, rstd)
            yo = P.sb([128, 4, 128], BF16)
            for b in range(4):
                P.stt(yo[:, b, :], yg[:, b, :], s.ng[:, b:b + 1], rstd, ALU.mult, ALU.mult)
            yb_cb(c, yo)
            P.pop()


def attn_run(g, NCX, NC, yc_cb):
    P = g.P
    dr = g.dram
    T = NC * 128
    NL = T + 256
    NLT = NL // 128
    P.push()
    wq = P.sb([128, 8, 512], BF16)
    d = dr["w_in"]
    for r in range(4):
        for hh, head in enumerate((r, 4 + r)):
            P.dma(wq[:, :, r * 128 + hh * 64:r * 128 + hh * 64 + 64],
                  DV(d, C_Q + head * 64, [[IN_W, 128], [128 * IN_W, 8], [1, 64]]))
    wk = load_w(g, "w_in", 0, 1024, C_K, 128)
    wv = load_w(g, "w_in", 0, 1024, C_V, 128)
    pswap = P.sb([128, 128], BF16)
    P.copy(pswap, g.c["pswap"])
    esink = P.sb([128, 4])
    for hh in range(2):
        P.dma(esink[hh * 64:(hh + 1) * 64, :], DV(dr["attn_sink"], hh * 4, [[0, 64], [1, 4]]))
    P.act(esink, esink, AF.Exp)
    mprev = P.sb([128, 128], BF16); mnext = P.sb([128, 128], BF16); mprevL = P.sb([128, 128], BF16); mnextR = P.sb([128, 128], BF16)
    P.copy(mprev, g.c["lt"]); P.copy(mnext, g.c["ut"])
    P.ts(mprevL, g.c["lt"], g.flagL, None, op0=ALU.mult)
    P.ts(mnextR, g.c["ut"], g.flagR, None, op0=ALU.mult)
    import os
    CUT = os.environ.get('ATT_CUT', '')
    if CUT == 'setup':
        P.pop(); return
    NS = (NCX + NC) * 128
    qT = P.sb([128, 4, NS], BF16)
    kT = P.sb([128, NL + 256], BF16)
    vtok = P.sb([128, NLT + 2, 128], BF16)

    def rope(dst, ps, n, ecol):
        if 'norope' in CUT:
            P.copy(dst, ps); return
        P.push()
        cs_ = P.sb([128, n]); sn_ = P.sb([128, n]); xb = P.sb([128, n], BF16); t1 = P.sb([128, n])
        if 'nodma' in CUT:
            P.memset(cs_, 1.0); P.memset(sn_, 0.0)
        else:
            P.dma(cs_, DV(dr["rope_cos"], ecol, [[NL, 128], [1, n]]))
            P.dma(sn_, DV(dr["rope_sin"], ecol, [[NL, 128], [1, n]]))
        if 'dmaonly' in CUT:
            P.tt(dst, ps, cs_, ALU.mult); P.pop(); return
        P.copy(xb, ps)
        p2 = g.pbank()
        P.mm(p2[:, 0:n], pswap, xb)
        P.tt(t1, ps, cs_, ALU.mult)
        t2 = P.sb([128, n])
        P.tt(t2, p2[:, 0:n], sn_, ALU.mult)
        P.tt(dst, t1, t2, ALU.add)
        P.pop()

    for (c, m) in col_tiles(0, NL):
        P.push()
        ht = load_h(g, c, m)
        ps = g.pbank()
        proj(g, ps[:, 0:m], wk, 0, ht, m)
        rope(kT[:, c:c + m], ps[:, 0:m], m, c)
        for cc in range(m // 128):
            if 'nov' in CUT:
                break
            pv = g.pbank()
            for k in range(8):
                P.mm(pv[:, 0:128], ht[:, k, cc * 128:(cc + 1) * 128], wv[:, k, :], start=(k == 0), stop=(k == 7))
            P.copy(vtok[:, (c // 128) + cc, :], pv[:, 0:128], eng="act")
        if 'noq' in CUT:
            P.pop(); continue
        lo = max(c, 128); hi = min(c + m, 128 + T)
        if hi > lo:
            for r in range(4):
                pq = g.pbank()
                proj(g, pq[:, 0:hi - lo], wq, r * 128, ht[:, :, lo - c:hi - c], hi - lo)
                so = NCX * 128 + (lo - 128)
                rope(qT[:, r, so:so + (hi - lo)], pq[:, 0:hi - lo], hi - lo, lo)
        P.pop()
    if 'lat' in CUT:
        P.pop(); return
    for (c, m) in col_tiles(g.e_ctx, NCX * 128):
        P.push()
        ht = load_h(g, c, m)
        so = c - g.e_ctx
        ps = g.pbank()
        proj(g, ps[:, 0:m], wk, 0, ht, m)
        P.copy(kT[:, NL + so:NL + so + m], ps[:, 0:m], eng="act")
        for cc in range(m // 128):
            pv = g.pbank()
            for k in range(8):
                P.mm(pv[:, 0:128], ht[:, k, cc * 128:(cc + 1) * 128], wv[:, k, :], start=(k == 0), stop=(k == 7))
            P.copy(vtok[:, NLT + so // 128 + cc, :], pv[:, 0:128], eng="act")
        for r in range(4):
            pq = g.pbank()
            proj(g, pq[:, 0:m], wq, r * 128, ht, m)
            P.copy(qT[:, r, so:so + m], pq[:, 0:m], eng="act")
        P.pop()
    po = g._pb_o; pd = g._pb_d
    import os
    for qb in range(NCX + NC):
        if os.environ.get('ATT_CUT') == 'proj':
            break
        if qb < NCX:
            tiles = [(NLT + 0, NL + 0, None), (NLT + 1, NL + 128, None)]
        else:
            n = qb - NCX
            tiles = [(n, n * 128, mprevL if n == 0 else mprev), (n + 1, (n + 1) * 128, None),
                     (n + 2, (n + 2) * 128, mnextR if n == NC - 1 else mnext),
                     (NLT + 0, NL + 0, None), (NLT + 1, NL + 128, None)]
        P.push()
        nt = len(tiles)
        for hk in range(2):
            h = slice(hk * 64, (hk + 1) * 64)
            for ti, (kt, kcol, mask) in enumerate(tiles):
                ps = g.pbank()
                P.mm(ps[:, 0:512], kT[h, kcol:kcol + 128], V(qT[h], [[NS, 4], [1, 128]], off=qb * 128))
                ex = P.sb([128, 4, 128], BF16)
                P.act(V(ex, [[1, 512]]), ps, AF.Exp, scale=0.125)
                if mask is not None:
                    P.tt(ex, ex, V(mask, [[0, 4], [1, 128]]), ALU.mult, eng="pool")
                P.mm(po[h, :], vtok[:, kt, h], V(ex, [[1, 512]]), start=(ti == 0), stop=(ti == nt - 1))
                P.mm(pd[h, :], g.ones_bf[:, 0:64], V(ex, [[1, 512]]), start=(ti == 0), stop=(ti == nt - 1))
        rd = P.sb([128, 4, 128])
        for r in range(4):
            P.ts(rd[:, r, :], pd[:, r * 128:(r + 1) * 128], esink[:, r:r + 1], None, op0=ALU.add)
        P.recip(V(rd, [[1, 512]]), V(rd, [[1, 512]]))
        yo = P.sb([128, 4, 128], BF16)
        P.tt(V(yo, [[1, 512]]), po[:, :], V(rd, [[1, 512]]), ALU.mult)
        yc_cb(qb, yo)
        P.pop()
    P.pop()


def rms_rstd(g, xt, n, rstd):
    P = g.P
    P.push()
    sq = P.sb([128, 8, n], BF16)
    P.act(sq, xt, AF.Square)
    ps = g.pbank()
    for k in range(8):
        P.mm(ps[:, 0:n], g.ones_bf, sq[:, k, :], start=(k == 0), stop=(k == 7))
    P.act(rstd, ps[:, 0:n], AF.Sqrt, scale=1.0 / D, bias=g.eps_col)
    P.recip(rstd, rstd)
    P.pop()


def mod_norm(g, xt, n, acol, bcol, out, v):
    P = g.P
    P.push()
    rstd = P.sb([128, n])
    rms_rstd(g, xt, n, rstd)
    tmp = P.sb([128, n])
    for k in range(8):
        P.stt(tmp, xt[:, k, :], acol[:, k, v:v + 1], rstd, ALU.mult, ALU.mult)
        P.act(out[:, k, :], tmp, AF.Identity, bias=bcol[:, k, v:v + 1])
    P.pop()


def load_mod(g):
    P = g.P
    m = P.sb([128, 48, 2])
    P.dma(m, g.dram["modT"])
    g.mod = m
    n1 = P.sb([128, 8]); n2 = P.sb([128, 8])
    lst = []
    if "norm1_g" in g.dram:
        P.dma(n1, DV(g.dram["norm1_g"], 0, [[1, 128], [128, 8]]), allow_slow_non_contiguous=True)
        g.a1 = P.sb([128, 8, 2]); lst.append((g.a1, n1, 1))
    if "norm2_g" in g.dram:
        P.dma(n2, DV(g.dram["norm2_g"], 0, [[1, 128], [128, 8]]), allow_slow_non_contiguous=True)
        g.a2 = P.sb([128, 8, 2]); lst.append((g.a2, n2, 4))
    for (a, nn, j) in lst:
        P.ts(a, m[:, j * 8:(j + 1) * 8, :], 1.0, None, op0=ALU.add)
        P.tt(a, a, V(nn, [[1, 8], [0, 2]]), ALU.mult)
    g.b1 = m[:, 0:8, :]; g.b2 = m[:, 24:32, :]
    g.g1 = m[:, 16:24, :]; g.g2 = m[:, 40:48, :]


def router_aff(g, h2f, n, wr, aff_out):
    P = g.P
    for blk in range(n // 128):
        ps = g.pbank()
        for k in range(8):
            P.mm(ps[:, 0:16], h2f[:, k, blk * 128:(blk + 1) * 128], wr[:, k, :], start=(k == 0), stop=(k == 7))
        P.push()
        mx = P.sb([128, 1]); sm = P.sb([128, 1]); ex = P.sb([128, 16])
        P.reduce(mx, ps[:, 0:16], op=ALU.max)
        P.ts(mx, mx, -1.0, None, op0=ALU.mult)
        P.act(ex, ps[:, 0:16], AF.Exp, bias=mx, accum_out=sm)
        P.recip(sm, sm)
        P.ts(aff_out[:, blk, :], ex, sm, None, op0=ALU.mult)
        P.pop()


def s_tiles(NCX, NC, step=512):
    out = [(c, m, True) for (c, m) in col_tiles(0, NCX * 128, step)]
    out += [(c, m, False) for (c, m) in col_tiles(NCX * 128, NC * 128, step)]
    return out


def s2e(g, NCX, s0, is_ctx):
    return g.e_ctx + s0 if is_ctx else g.e_own + (s0 - NCX * 128)


def merge_run(g, NCX, NC):
    P = g.P
    dr = g.dram
    NS = (NCX + NC) * 128
    P.push()
    macc = P.sb([128, 8, NS], BF16)
    for kbr in range(3):
        P.push()
        wg = load_w(g, "w_in", 0, 1024, C_G + kbr * 1024, 1024)
        wb = P.sb([128, 4, 1024], BF16)
        d = dr["w_branch"]
        if kbr < 2:
            P.dma(wb, DV(d, kbr * 512 * 1024, [[1024, 128], [128 * 1024, 4], [1, 1024]]))
        else:
            for r in range(4):
                for hh, head in enumerate((r, 4 + r)):
                    P.dma(wb[hh * 64:(hh + 1) * 64, r, :], DV(d, (2 * 512 + head * 64) * 1024, [[1024, 64], [1, 1024]]))
        yd = g.y_d[kbr]
        for (s0, n, is_ctx) in s_tiles(NCX, NC):
            P.push()
            ht = load_h(g, s2e(g, NCX, s0, is_ctx), n)
            yt = P.sb([128, 4, n], BF16)
            P.dma(yt, DV(yd, s0, [[NS, 128], [128 * NS, 4], [1, n]]))
            for j in range(8):
                pg = g.pbank()
                proj(g, pg[:, 0:n], wg, j * 128, ht, n)
                gt = P.sb([128, n])
                P.act(gt, pg[:, 0:n], AF.Sigmoid)
                pb = g.pbank()
                for cc in range(4):
                    P.mm(pb[:, 0:n], wb[:, cc, j * 128:(j + 1) * 128], yt[:, cc, :], start=(cc == 0), stop=(cc == 3))
                if kbr == 0:
                    P.tt(macc[:, j, s0:s0 + n], gt, pb[:, 0:n], ALU.mult)
                else:
                    P.tt(gt, gt, pb[:, 0:n], ALU.mult)
                    P.tt(macc[:, j, s0:s0 + n], macc[:, j, s0:s0 + n], gt, ALU.add, eng="pool")
            P.pop()
        P.pop()
    wo = load_w(g, "w_out", 0, 1024, 0, 1024)
    wr = load_w(g, "w_router", 0, 1024, 0, 16, dtype=F32)
    for (s0, n, is_ctx) in s_tiles(NCX, NC):
        v = 1 if is_ctx else 0
        P.push()
        xt = P.sb([128, 8, n])
        xsrc = g.dram["xcT"] if is_ctx else g.dram["xT"]
        xw = NCX * 128 if is_ctx else NC * 128 + 256
        xo = s0 if is_ctx else (s0 - NCX * 128) + 128
        P.dma(xt, DV(xsrc, xo, [[xw, 128], [128 * xw, 8], [1, n]]))
        for j in range(8):
            po = g.pbank()
            for k in range(8):
                P.mm(po[:, 0:n], wo[:, k, j * 128:(j + 1) * 128], macc[:, k, s0:s0 + n], start=(k == 0), stop=(k == 7))
            P.stt(xt[:, j, :], po[:, 0:n], g.g1[:, j, v:v + 1], xt[:, j, :], ALU.mult, ALU.add)
        P.dma(DV(g.dram["x1T"], s0, [[NS, 128], [128 * NS, 8], [1, n]]), xt)
        h2 = P.sb([128, 8, n])
        mod_norm(g, xt, n, g.a2, g.b2, h2, v)
        aff = P.sb([128, n // 128, 16])
        router_aff(g, h2, n, wr, aff)
        P.dma(DV(g.dram["aff"], s0 * 16, [[16, 128], [128 * 16, n // 128], [1, 16]]), aff)
        P.pop()
    P.pop()


B_INPUTS = [("s5_lam_re", [2, 32, 64]), ("s5_lam_im", [2, 32, 64]), ("s5_log_dt", [2, 32]), ("s5_b_re", [2, 32, 64, 16]),
            ("s5_b_im", [2, 32, 64, 16]), ("s5_c_re", [2, 32, 16, 64]), ("s5_c_im", [2, 32, 16, 64]), ("s5_d", [512]),
            ("s5_b_glu", [512]), ("ssd_conv_w", [5, 1024]), ("ssd_conv_b", [1024]), ("ssd_a_log", [2, 8]),
            ("ssd_dt_bias", [2, 8]), ("ssd_d", [8]), ("ssd_norm_g", [512]), ("attn_sink", [8]), ("norm1_g", [1024]),
            ("norm2_g", [1024]), ("w_router", [1024, 16]), ("modT", [128, 48, 2]), ("flags", [128, 2])]
B_INPUTS_BF = [("w_in", [1024, IN_W]), ("s5_w_glu", [512, 512]), ("w_branch", [3 * 512, 1024]), ("w_out", [1024, 1024])]


def declare_inputs(g, lst, dt):
    for name, shp in lst:
        g.dram[name] = g.nc.dram_tensor(name, shp, dt, kind="ExternalInput").ap()


def phase0_h(g, NCX, NC):
    P = g.P
    T = NC * 128
    for (c, m, is_ctx) in [(c, m, False) for (c, m) in col_tiles(0, T + 256)] + [(c, m, True) for (c, m) in col_tiles(0, NCX * 128)]:
        P.push()
        xt = P.sb([128, 8, m])
        src = g.dram["xcT"] if is_ctx else g.dram["xT"]
        xw = NCX * 128 if is_ctx else T + 256
        P.dma(xt, DV(src, c, [[xw, 128], [128 * xw, 8], [1, m]]))
        ht = P.sb([128, 8, m], BF16)
        mod_norm(g, xt, m, g.a1, g.b1, ht, 1 if is_ctx else 0)
        e0 = (g.e_ctx + c) if is_ctx else c
        P.dma(DV(g.hT_d, e0, [[g.E, 128], [128 * g.E, 8], [1, m]]), ht)
        P.pop()


def s5_phase(g, NCX, NC, carry_fn=None, states_cb=None, states_only=False):
    P = g.P
    dr = g.dram
    NS = (NCX + NC) * 128
    P.push()
    uT = P.sb([128, 4, NS], BF16)
    aT = P.sb([128, 4, NS], BF16)
    P.push()
    wu = load_w(g, "w_in", 0, 1024, C_U, 512)
    for (s0, n, is_ctx) in s_tiles(NCX, NC):
        P.push()
        ht = load_h(g, s2e(g, NCX, s0, is_ctx), n)
        for j in range(4):
            ps = g.pbank()
            proj(g, ps[:, 0:n], wu, j * 128, ht, n)
            P.copy(uT[:, j, s0:s0 + n], ps[:, 0:n], eng="act" if j % 2 else "dve")
        P.pop()
    P.pop()
    dcol = P.sb([128, 4]); bglu = P.sb([128, 4])
    P.dma(dcol, DV(dr["s5_d"], 0, [[1, 128], [128, 4]]), allow_slow_non_contiguous=True)
    P.dma(bglu, DV(dr["s5_b_glu"], 0, [[1, 128], [128, 4]]), allow_slow_non_contiguous=True)

    def ya_cb(b, yacc):
        P.stt(yacc, uT[:, b, :], dcol[:, b:b + 1], yacc, ALU.mult, ALU.add)
        P.act(aT[:, b, :], yacc, AF.Gelu)

    if carry_fn is None:
        carry_fn = lambda: (g.s5.fin_re[:, :, 0], g.s5.fin_im[:, :, 0])
    s5_core(g, uT, NCX, NC, carry_fn, ya_cb, states_cb, states_only)
    if states_only:
        P.pop()
        return
    wgl = load_w(g, "s5_w_glu", 0, 512, 0, 512)
    for (s0, n) in col_tiles(0, NS):
        P.push()
        yo = P.sb([128, 4, n], BF16)
        for j in range(4):
            ps = g.pbank()
            for k in range(4):
                P.mm(ps[:, 0:n], wgl[:, k, j * 128:(j + 1) * 128], aT[:, k, s0:s0 + n], start=(k == 0), stop=(k == 3))
            gt = P.sb([128, n])
            P.act(gt, ps[:, 0:n], AF.Sigmoid, bias=bglu[:, j:j + 1])
            P.tt(yo[:, j, :], gt, aT[:, j, s0:s0 + n], ALU.mult)
        P.dma(DV(g.y_d[0], s0, [[NS, 128], [128 * NS, 4], [1, n]]), yo)
        P.pop()
    P.pop()


def build_B(T, debug=False, multi=False, mode="B"):
    nc = bass.Bass("TRN2", target_bir_lowering=False)
    g = G(); g.nc = nc; g.P = Prog(nc); P = g.P
    P.init_arenas(18 * 1024, 62 * 1024)
    NCX = 2; NC = T // 128; NS = (NCX + NC) * 128
    g.E = T + 512; g.e_own = 128; g.e_ctx = T + 256
    g.dram = {}
    consts = dict(CONST_SHAPES); consts["pswap"] = [128, 128]
    declare_inputs(g, list(consts.items()), F32)
    declare_inputs(g, B_INPUTS, F32)
    declare_inputs(g, B_INPUTS_BF, BF16)
    declare_inputs(g, [("s5_vr", [128, 32, 128]), ("s5_vi", [128, 32, 128]), ("s5_w2re", [128, 32, 128]), ("s5_nw2im", [128, 32, 128]),
                       ("s5_kt", [4, 2, 2, 128, 1024])], BF16)
    declare_inputs(g, [("xT", [8, 128, T + 256]), ("xcT", [8, 128, 256]), ("rope_cos", [128, T + 256]), ("rope_sin", [128, T + 256])], F32)
    if mode == "B":
        g.dram["x1T"] = nc.dram_tensor("x1T", [8, 128, NS], F32, kind="ExternalOutput").ap()
        g.dram["aff"] = nc.dram_tensor("aff", [NS, 16], F32, kind="ExternalOutput").ap()
        if multi:
            declare_inputs(g, [("s5_fin_all", [NCORES, 128, 64]), ("ssd_fin_all", [NCORES, 2, 128, 512]), ("ssd_tot_all", [NCORES, 128, 16]),
                               ("onehot", [128, NCORES])], F32)
    else:
        g.dram["s5_fin"] = nc.dram_tensor("s5_fin", [128, 64], F32, kind="ExternalOutput").ap()
        g.dram["ssd_fin"] = nc.dram_tensor("ssd_fin", [2, 128, 512], F32, kind="ExternalOutput").ap()
        g.dram["ssd_tot"] = nc.dram_tensor("ssd_tot", [128, 16], F32, kind="ExternalOutput").ap()
    g.hT_d = nc.dram_tensor("hT_scr", [8, 128, g.E], BF16).ap()
    g.ssd_hb_d = nc.dram_tensor("ssd_hb_scr", [NCX + NC, 128, 512], BF16).ap()
    kind = "ExternalOutput" if debug else "Internal"
    g.y_d = [nc.dram_tensor(f"y{k}_scr", [4, 128, NS], BF16, kind=kind).ap() for k in range(3)]
    setup_psum(g)
    CONST_SHAPES2 = consts
    g.c = {}
    for k, shp in CONST_SHAPES2.items():
        t = P.sb(shp, F32)
        P.dma(t, g.dram[k])
        g.c[k] = t
    g.ident_bf = P.sb([128, 128], BF16); P.copy(g.ident_bf, g.c["ident"])
    g.ones_bf = P.sb([128, 128], BF16); P.memset(g.ones_bf, 1.0)
    g.zeros_bf = P.sb([128, 128], BF16); P.memset(g.zeros_bf, 0.0)
    g.c["ones_f"] = P.sb([128, 128], F32); P.memset(g.c["ones_f"], 1.0)
    g.eps_col = P.sb([128, 1]); P.memset(g.eps_col, 1e-6)
    g.zero_col = P.sb([128, 1]); P.memset(g.zero_col, 0.0)
    fl = P.sb([128, 2]); P.dma(fl, g.dram["flags"])
    g.flagL = fl[:, 0:1]; g.flagR = fl[:, 1:2]
    import os
    ph = os.environ.get("PH", "s5,ssd,att,merge").split(",")
    load_mod(g)
    phase0_h(g, NCX, NC)
    if multi and mode == "B":
        g.onehot = P.sb([128, NCORES]); P.dma(g.onehot, g.dram["onehot"])
    if mode == "A":
        def dump_fin():
            t_ = P.sb([128, 32, 2])
            P.copy(t_[:, :, 0], g.s5.fin_re[:, :, 1]); P.copy(t_[:, :, 1], g.s5.fin_im[:, :, 1])
            P.dma(g.dram["s5_fin"], V(t_, [[1, 64]]))
        s5_phase(g, NCX, NC, states_cb=dump_fin, states_only=True)
        P.push()
        ssd_prep(g, NCX, NC)
        ssd_local_finals(g, NCX, NC)
        P.pop()
        P.wait_all(); P.emit(); P.close()
        return nc, g
    if "s5" in ph:
        s5_phase(g, NCX, NC, carry_fn=(lambda: s5_carry_chain(g, NC)) if multi else None)
    if "ssd" in ph:
        P.push()
        ssd_prep(g, NCX, NC)
        ssd_run(g, NCX, NC, multi=multi, yb_cb=lambda c, yo: P.dma(DV(g.y_d[1], c * 128, [[NS, 128], [128 * NS, 4], [1, 128]]), yo))
        P.pop()
    if "att" in ph:
        attn_run(g, NCX, NC, lambda qb, yo: P.dma(DV(g.y_d[2], qb * 128, [[NS, 128], [128 * NS, 4], [1, 128]]), yo))
    if "merge" in ph:
        merge_run(g, NCX, NC)
    P.wait_all(); P.emit(); P.close()
    return nc, g


def topk_threshold(g, aff, nblk, cap, tau, iters=30):
    P = g.P
    P.push()
    lo = P.sb([128, 16]); hi = P.sb([128, 16]); mid = P.sb([128, 16]); cmp_ = P.sb([128, 16, nblk])
    cnt = P.sb([128, 16]); ge = P.sb([128, 16]); d1 = P.sb([128, 16])
    P.memset(lo, 0.0); P.memset(hi, 1.0)
    affv = V(aff, [[1, 16], [16, nblk]])
    for it in range(iters):
        P.tt(mid, lo, hi, ALU.add)
        P.ts(mid, mid, 0.5, None, op0=ALU.mult)
        P.tt(cmp_, affv, V(mid, [[1, 16], [0, nblk]]), ALU.is_ge)
        P.reduce(cnt, cmp_)
        ps = g.pbank()
        P.mm(ps[:, 0:16], g.c["ones_f"], cnt)
        P.ts(ge, ps[:, 0:16], float(cap) - 0.5, None, op0=ALU.is_ge)
        P.tt(d1, mid, lo, ALU.subtract); P.tt(d1, d1, ge, ALU.mult); P.tt(lo, lo, d1, ALU.add)
        P.tt(d1, hi, mid, ALU.subtract); P.tt(d1, d1, ge, ALU.mult); P.tt(hi, mid, d1, ALU.add)
    P.copy(tau, lo)
    P.pop()


C_INPUTS = [("norm2_g", [1024]), ("modT", [128, 48, 2]), ("final_norm_g", [1024]), ("ident", [128, 128])]
C_INPUTS_BF = [("w_e_gate", [16 * 1024, 1024]), ("w_e_up", [16 * 1024, 1024]), ("w_e_down", [16 * 1024, 1024])]


def build_C(T, n_total):
    nc = bass.Bass("TRN2", target_bir_lowering=False)
    g = G(); g.nc = nc; g.P = Prog(nc); P = g.P
    P.init_arenas(22 * 1024, 50 * 1024)
    NCX = 2; NC = T // 128; NS = (NCX + NC) * 128
    g.dram = {}
    declare_inputs(g, C_INPUTS, F32)
    declare_inputs(g, C_INPUTS_BF, BF16)
    declare_inputs(g, [("x1T", [8, 128, NS]), ("aff_all", [n_total, 16]), ("aff_own", [NS, 16])], F32)
    x2_d = nc.dram_tensor("x2T", [8, 128, NS], F32, kind="ExternalOutput").ap()
    fin_d = nc.dram_tensor("finT", [8, 128, NS], F32, kind="ExternalOutput").ap()
    setup_psum(g)
    g.c = {}
    g.c["ident"] = P.sb([128, 128]); P.dma(g.c["ident"], g.dram["ident"])
    g.ones_bf = P.sb([128, 128], BF16); P.memset(g.ones_bf, 1.0)
    g.c["ones_f"] = P.sb([128, 128], F32); P.memset(g.c["ones_f"], 1.0)
    g.eps_col = P.sb([128, 1]); P.memset(g.eps_col, 1e-6)
    load_mod(g)
    gfin = P.sb([128, 8]); P.dma(gfin, DV(g.dram["final_norm_g"], 0, [[1, 128], [128, 8]]), allow_slow_non_contiguous=True)
    tau = P.sb([128, 16]); tauc = P.sb([128, 16])
    nblk = n_total // 128
    P.push()
    affa = P.sb([128, nblk, 16])
    P.dma(affa, DV(g.dram["aff_all"], 0, [[16, 128], [2048, nblk], [1, 16]]))
    topk_threshold(g, affa, nblk, 2 * n_total // 16, tau)
    P.pop()
    tiles = [(c, m, True) for (c, m) in col_tiles(0, NCX * 128, 256)] + [(c, m, False) for (c, m) in col_tiles(NCX * 128, T, 256)]
    GROUP = 5
    groups = [tiles[i:i + GROUP] for i in range(0, len(tiles), GROUP)]
    affc = P.sb([128, NCX, 16])
    P.dma(affc, DV(g.dram["aff_own"], 0, [[16, 128], [2048, NCX], [1, 16]]))
    topk_threshold(g, affc, NCX, 2 * NCX * 128 // 16, tauc)
    for grp in groups:
        P.push()
        ncols = sum(n for (_, n, _) in grp)
        gs0 = grp[0][0]
        yacc = P.sb([128, 8, ncols])
        h2b = P.sb([128, 8, ncols], BF16)
        coef = P.sb([128, ncols // 128, 16])
        xres = P.sb([128, 8, ncols]) if False else None
        for (s0, n, is_ctx) in grp:
            P.push()
            o = s0 - gs0
            xt = P.sb([128, 8, n]); P.dma(xt, DV(g.dram["x1T"], s0, [[NS, 128], [128 * NS, 8], [1, n]]))
            h2 = P.sb([128, 8, n])
            mod_norm(g, xt, n, g.a2, g.b2, h2, 1 if is_ctx else 0)
            P.copy(h2b[:, :, o:o + n], h2, eng="act")
            aff = P.sb([128, n // 128, 16])
            P.dma(aff, DV(g.dram["aff_own"], s0 * 16, [[16, 128], [2048, n // 128], [1, 16]]))
            tb = V(tauc if is_ctx else tau, [[0, n // 128], [1, 16]])
            msk = P.sb([128, n // 128, 16])
            P.tt(msk, aff, tb, ALU.is_ge)
            P.tt(coef[:, o // 128:(o + n) // 128, :], aff, msk, ALU.mult)
            P.pop()
        for e in range(16):
            P.push()
            wg = load_w(g, "w_e_gate", e * 1024, 1024, 0, 1024)
            wu = load_w(g, "w_e_up", e * 1024, 1024, 0, 1024)
            wd = load_w(g, "w_e_down", e * 1024, 1024, 0, 1024)
            for (s0, n, is_ctx) in grp:
                P.push()
                o = s0 - gs0
                pcb = g.pbank()
                for blk in range(n // 128):
                    cb_ = coef[:, o // 128 + blk, e:e + 1]
                    P.mm(pcb[:, blk * 128:(blk + 1) * 128], V(cb_, [[0, 128]]), g.c["ident"])
                cbs = P.sb([128, n])
                P.copy(cbs, pcb[:, 0:n], eng="act")
                hid = P.sb([128, 8, n], BF16)
                for f in range(8):
                    pg = g.pbank(); pu = g.pbank()
                    for k in range(8):
                        P.mm(pg[:, 0:n], wg[:, k, f * 128:(f + 1) * 128], h2b[:, k, o:o + n], start=(k == 0), stop=(k == 7))
                    for k in range(8):
                        P.mm(pu[:, 0:n], wu[:, k, f * 128:(f + 1) * 128], h2b[:, k, o:o + n], start=(k == 0), stop=(k == 7))
                    sg = P.sb([128, n])
                    P.act(sg, pg[:, 0:n], AF.Silu)
                    P.tt(sg, sg, pu[:, 0:n], ALU.mult)
                    P.tt(hid[:, f, :], sg, cbs, ALU.mult, eng="pool")
                for j in range(8):
                    pd_ = g.pbank()
                    for f in range(8):
                        P.mm(pd_[:, 0:n], wd[:, f, j * 128:(j + 1) * 128], hid[:, f, :], start=(f == 0), stop=(f == 7))
                    if e == 0:
                        P.copy(yacc[:, j, o:o + n], pd_[:, 0:n], eng="act")
                    else:
                        P.tt(yacc[:, j, o:o + n], yacc[:, j, o:o + n], pd_[:, 0:n], ALU.add)
                P.pop()
            P.pop()
        for (s0, n, is_ctx) in grp:
            P.push()
            o = s0 - gs0
            v = 1 if is_ctx else 0
            xt = P.sb([128, 8, n]); P.dma(xt, DV(g.dram["x1T"], s0, [[NS, 128], [128 * NS, 8], [1, n]]))
            for j in range(8):
                P.stt(xt[:, j, :], yacc[:, j, o:o + n], g.g2[:, j, v:v + 1], xt[:, j, :], ALU.mult, ALU.add)
            P.dma(DV(x2_d, s0, [[NS, 128], [128 * NS, 8], [1, n]]), xt)
            rstd = P.sb([128, n])
            rms_rstd(g, xt, n, rstd)
            for j in range(8):
                P.stt(xt[:, j, :], xt[:, j, :], gfin[:, j:j + 1], rstd, ALU.mult, ALU.mult)
            P.dma(DV(fin_d, s0, [[NS, 128], [128 * NS, 8], [1, n]]), xt)
            P.pop()
        P.pop()
    P.wait_all(); P.emit(); P.close()
    return nc, g


NCORES = 8


def s5_carry_chain(g, NC):
    P = g.P
    s = g.s5
    NCX = 2
    dre = P.sb([128, 32]); dim_ = P.sb([128, 32]); t1 = P.sb([128, 32]); t2 = P.sb([128, 32])
    for half, last in ((slice(0, 64), NCX + NC - 1), (slice(64, 128), NCX)):
        cmul_acc(P, dre[half], dim_[half], s.aqr[half], s.aqi[half], s.apow_re[half, :, last], s.apow_im[half, :, last],
                 None, None, t1[half], t2[half])
    fin = P.sb([128, NCORES, 32, 2])
    P.dma(fin, DV(g.dram["s5_fin_all"], 0, [[64, 128], [128 * 64, NCORES], [1, 64]]))
    H = P.sb([128, NCORES + 1, 32, 2])
    P.copy(H[:, 0, :, 0], s.fin_re[:, :, 0]); P.copy(H[:, 0, :, 1], s.fin_im[:, :, 0])
    for t in range(NCORES):
        for half, m in ((slice(0, 64), t), (slice(64, 128), NCORES - 1 - t)):
            cmul_acc(P, H[half, t + 1, :, 0], H[half, t + 1, :, 1], dre[half], dim_[half], H[half, t, :, 0], H[half, t, :, 1],
                     fin[half, m, :, 0], fin[half, m, :, 1], t1[half], t2[half])
    cr = P.sb([128, 32]); ci = P.sb([128, 32])
    P.memset(cr, 0.0); P.memset(ci, 0.0)
    for t in range(NCORES):
        for half, oh in ((slice(0, 64), g.onehot[0:64, t:t + 1]), (slice(64, 128), g.onehot[64:128, NCORES - 1 - t:NCORES - t])):
            P.stt(cr[half], H[half, t, :, 0], oh, cr[half], ALU.mult, ALU.add)
            P.stt(ci[half], H[half, t, :, 1], oh, ci[half], ALU.mult, ALU.add)
    return cr, ci


def ssd_carry(g, dr_, hctx, out):
    P = g.P
    P.push()
    tot = P.sb([128, NCORES, 16])
    P.dma(tot, DV(g.dram["ssd_tot_all"], 0, [[16, 128], [128 * 16, NCORES], [1, 16]]))
    P.act(tot, tot, AF.Exp)
    H = P.sb([128, 512]); fin = P.sb([128, 512])
    P.copy(H, hctx)
    P.memset(out, 0.0)
    for t in range(NCORES):
        m = t if dr_ == 0 else NCORES - 1 - t
        P.stt(out, H, g.onehot[:, m:m + 1], out, ALU.mult, ALU.add)
        if t == NCORES - 1:
            break
        P.dma(fin, DV(g.dram["ssd_fin_all"], (m * 2 + dr_) * 128 * 512, [[512, 128], [1, 512]]))
        P.tt(V(H, [[64, 8], [1, 64]]), V(H, [[64, 8], [1, 64]]), V(tot[:, m, dr_ * 8:dr_ * 8 + 8], [[1, 8], [0, 64]]), ALU.mult)
        P.tt(H, H, fin, ALU.add)
    P.pop()


def ssd_local_finals(g, NCX, NC):
    P = g.P
    hf = P.sb([128, 512]); hb = P.sb([128, 512]); ts_ = P.sb([128, 16]); pb = P.sb([128, 8]); tmp = P.sb([128, 512])
    P.memset(hf, 0.0); P.memset(hb, 0.0); P.memset(ts_, 0.0); P.memset(pb, 1.0)
    for i in range(NC):
        c = NCX + i
        P.push()
        S, cd, tot = ssd_chunk(g, c, False, None, None)
        P.tt(V(hf, [[64, 8], [1, 64]]), V(hf, [[64, 8], [1, 64]]), V(cd[:, 0:8], [[1, 8], [0, 64]]), ALU.mult)
        P.tt(hf, hf, S[0], ALU.add)
        P.tt(V(tmp, [[64, 8], [1, 64]]), V(S[1], [[64, 8], [1, 64]]), V(pb, [[1, 8], [0, 64]]), ALU.mult)
        P.tt(hb, hb, tmp, ALU.add)
        P.tt(pb, pb, cd[:, 8:16], ALU.mult)
        P.tt(ts_, ts_, tot, ALU.add)
        P.pop()
    P.dma(g.dram["ssd_fin"][0], hf); P.dma(g.dram["ssd_fin"][1], hb); P.dma(g.dram["ssd_tot"], ts_)


W_LIST = [("w_in", 4 * 1024, IN_W), ("s5_w_glu", 4 * 512, 512), ("w_branch", 4 * 1536, 1024), ("w_out", 4 * 1024, 1024),
          ("w_e_gate", 4 * 16 * 1024, 1024), ("w_e_up", 4 * 16 * 1024, 1024), ("w_e_down", 4 * 16 * 1024, 1024)]


def build_W(wlist=W_LIST):
    nc = bass.Bass("TRN2", target_bir_lowering=False)
    g = G(); g.nc = nc; g.P = Prog(nc); P = g.P
    P.init_arenas(24 * 1024, 24 * 1024)
    setup_psum(g)
    g.dram = {}
    engs = ["dve", "act", "pool"]
    ei = 0
    for (name, rows, cols) in wlist:
        rpc = rows // NCORES
        assert rpc % 128 == 0
        src = nc.dram_tensor(name, [rpc, cols], F32, kind="ExternalInput").ap()
        dst = nc.dram_tensor(name + "_bf", [rpc, cols], BF16, kind="ExternalOutput").ap()
        rt = rpc // 128
        cc = cols
        while cc > 2048:
            cc //= 2
        rstep = max(1, 4096 // cc)
        for c0 in range(0, cols, cc):
            for r0 in range(0, rt, rstep):
                r = min(rstep, rt - r0)
                P.push()
                a = P.sb([128, r, cc]); b = P.sb([128, r, cc], BF16)
                P.dma(a, DV(src, r0 * 128 * cols + c0, [[cols, 128], [128 * cols, r], [1, cc]]))
                P.copy(b, a, eng=engs[ei % 3]); ei += 1
                P.dma(DV(dst, r0 * 128 * cols + c0, [[cols, 128], [128 * cols, r], [1, cc]]), b)
                P.pop()
    g.NG = 16
    declare_inputs(g, [("s5_lam_re", [2, 16, 64]), ("s5_lam_im", [2, 16, 64]), ("s5_log_dt", [2, 16]), ("s5_b_re", [2, 16, 64, 16]),
                       ("s5_b_im", [2, 16, 64, 16]), ("s5_c_re", [2, 16, 16, 64]), ("s5_c_im", [2, 16, 16, 64]),
                       ("exl", [128, 128]), ("exr", [128, 128]), ("exv", [128, 2])], F32)
    g.c = {}
    for k_ in ("exl", "exr", "exv"):
        t_ = P.sb(CONST_SHAPES[k_], F32); P.dma(t_, g.dram[k_]); g.c[k_] = t_
    outs = {nm: nc.dram_tensor(nm, [128, 16, 128], BF16, kind="ExternalOutput").ap() for nm in ("s5_vr", "s5_vi", "s5_w2re", "s5_nw2im")}
    kt_o = nc.dram_tensor("s5_kt", [2, 2, 2, 128, 1024], BF16, kind="ExternalOutput").ap()
    P.push()
    s5_params(g)
    P.push()
    vr = P.sb([128, 16, 128], BF16); vi = P.sb([128, 16, 128], BF16)
    s5_gen_V(g, V(vr, [[128, 16], [64, 2], [1, 64]]), V(vi, [[128, 16], [64, 2], [1, 64]]))
    P.dma(outs["s5_vr"], vr); P.dma(outs["s5_vi"], vi)
    w2 = P.sb([128, 16, 128], BF16); nw2 = P.sb([128, 16, 128], BF16)
    s5_gen_E(g, g.c["exr"], w2, nw2, neg_im=True)
    P.dma(outs["s5_w2re"], w2); P.dma(outs["s5_nw2im"], nw2)
    P.pop()
    s5_kt_build(g, 2, kt_o)
    P.pop()
    wm = nc.dram_tensor("w_mod", [1024, 6144], F32, kind="ExternalInput").ap()
    bm = nc.dram_tensor("b_mod", [6144], F32, kind="ExternalInput").ap()
    cc_ = nc.dram_tensor("c2", [2, 1024], F32, kind="ExternalInput").ap()
    mo = nc.dram_tensor("modT", [128, 48, 2], F32, kind="ExternalOutput").ap()
    sc = P.sb([128, 8, 2])
    for v in range(2):
        P.dma(sc[:, :, v], DV(cc_, v * 1024, [[1, 128], [128, 8]]), allow_slow_non_contiguous=True)
    P.act(sc, sc, AF.Silu)
    bcol = P.sb([128, 48])
    P.dma(bcol, DV(bm, 0, [[1, 128], [128, 48]]), allow_slow_non_contiguous=True)
    ps = g.pbank()
    for grp in range(12):
        P.push()
        wt = P.sb([128, 8, 512])
        P.dma(wt, DV(wm, grp * 512, [[6144, 128], [128 * 6144, 8], [1, 512]]))
        for q in range(4):
            ccix = grp * 4 + q
            for k in range(8):
                P.mm(ps[:, ccix * 2:ccix * 2 + 2], wt[:, k, q * 128:(q + 1) * 128], sc[:, k, :], start=(k == 0), stop=(k == 7))
        P.pop()
    mt = P.sb([128, 48, 2])
    P.tt(mt, V(ps, [[2, 48], [1, 2]]), V(bcol, [[1, 48], [0, 2]]), ALU.add)
    P.dma(mo, mt)
    P.wait_all(); P.emit(); P.close()
    return nc, g


def setup_psum(g):
    P = g.P
    g._pb = [P.ps([128, 512], F32) for _ in range(4)]
    g._pb_o = P.ps([128, 512], F32)[:, :]
    g._pb_d = P.ps([128, 512], F32)[:, :]
    g._pbf = [P.ps([128, 1024], BF16) for _ in range(2)]
    g._pi = 0
    g._pbi = 0

    def pbank():
        t = g._pb[g._pi % 4]
        g._pi += 1
        return t[:, :]

    def pbank_bf():
        h = g._pbi % 2
        g._pbi += 1
        return g._pbf[h][:, 0:512]
    g.pbank = pbank
    g.pbank_bf = pbank_bf


BF=ml_dtypes.bfloat16

def rope_tables(own_start, T, n_total):
    NL=T+256
    pos=own_start+np.arange(NL)-128
    valid=(pos>=0)&(pos<n_total)
    pos=np.where(valid,pos,0)
    d=np.arange(64); which=d//32; i=d%16; first=(d%32)<16
    inv=10000.0**(-(i.astype(np.float64))/16)
    axis=np.where(which[:,None]==0, (pos//64)[None,:], (pos%64)[None,:]).astype(np.float64)
    ang=axis*inv[:,None]
    cos=np.cos(ang); sin=np.where(first[:,None], -np.sin(ang), np.sin(ang))
    return np.tile(cos,(2,1)).astype(np.float32), np.tile(sin,(2,1)).astype(np.float32)

def pswap():
    m=np.zeros((128,128),np.float32)
    for j in range(128):
        p = j+16 if (j%32)<16 else j-16
        m[p,j]=1.0
    return m

def fm(a, ncols):
    return np.ascontiguousarray(a.T.reshape(8,128,ncols))

def consts_all():
    c=host_consts(); c["pswap"]=pswap(); return c

def modT_from(mod2):
    return np.ascontiguousarray(mod2.reshape(2,48,128).transpose(2,1,0)).astype(np.float32)


_CACHE = {}

def _prog(key, fn):
    if key not in _CACHE:
        _CACHE[key] = fn()[0]
    return _CACHE[key]

def run_model(inputs, T, ncores=NCORES, depth=4, hook=None):
    assert ncores == NCORES
    n = ncores * T
    NS = T + 256
    f32 = lambda a: np.ascontiguousarray(np.asarray(a, dtype=np.float32))
    ncW = _prog(("W",), build_W)
    flat = {"w_in": f32(inputs["w_in"]).reshape(-1, IN_W), "s5_w_glu": f32(inputs["s5_w_glu"]).reshape(-1, 512),
            "w_branch": f32(inputs["w_branch"]).reshape(-1, 1024), "w_out": f32(inputs["w_out"]).reshape(-1, 1024),
            "w_e_gate": f32(inputs["w_e_gate"]).reshape(-1, 1024), "w_e_up": f32(inputs["w_e_up"]).reshape(-1, 1024),
            "w_e_down": f32(inputs["w_e_down"]).reshape(-1, 1024)}
    c2 = np.stack([f32(inputs["c"])[0], f32(inputs["c_ctx"])], 0)
    maps = []
    for k in range(ncores):
        m = {}
        for name, rows, cols in W_LIST:
            rpc = rows // ncores
            m[name] = np.ascontiguousarray(flat[name][k * rpc:(k + 1) * rpc])
        nl = 4
        l = k % nl
        m["w_mod"] = f32(inputs["w_mod"][l]); m["b_mod"] = f32(inputs["b_mod"][l]); m["c2"] = c2
        hf_ = k // nl
        gsl = slice(hf_ * 16, hf_ * 16 + 16)
        for nm in ("s5_lam_re", "s5_lam_im", "s5_log_dt", "s5_b_re", "s5_b_im", "s5_c_re", "s5_c_im"):
            m[nm] = np.ascontiguousarray(f32(inputs[nm][l])[:, gsl])
        hc_ = host_consts()
        for nm in ("exl", "exr", "exv"):
            m[nm] = hc_[nm]
        maps.append(m)
    res = run_bass_kernel_spmd(ncW, maps, core_ids=list(range(ncores))).results
    wbf = {name: np.concatenate([np.asarray(res[k][name + "_bf"]) for k in range(ncores)], 0) for name, _, _ in W_LIST}
    nl = 4
    modT = [np.asarray(res[l]["modT"]) for l in range(nl)]
    s5tab = []
    for l in range(nl):
        d_ = {nm: np.concatenate([np.asarray(res[l][nm]), np.asarray(res[l + nl][nm])], 1) for nm in ("s5_vr", "s5_vi", "s5_w2re", "s5_nw2im")}
        d_["s5_kt"] = np.concatenate([np.asarray(res[l]["s5_kt"]), np.asarray(res[l + nl]["s5_kt"])], 0)
        s5tab.append(d_)
    if hook: hook("W", dict(wbf=wbf, modT=modT))
    consts = consts_all()
    ropes = [rope_tables(k * T, T, n) for k in range(ncores)]
    flags = []
    onehots = []
    for k in range(ncores):
        fl = np.ones((128, 2), np.float32)
        if k == 0: fl[:, 0] = 0
        if k == ncores - 1: fl[:, 1] = 0
        flags.append(fl)
        oh = np.zeros((128, ncores), np.float32); oh[:, k] = 1
        onehots.append(oh)
    x = f32(inputs["x"])[0]
    xc = f32(inputs["ctx"])[0]
    ncA = _prog(("A", T), lambda: build_B(T, multi=True, mode="A"))
    ncB = _prog(("B", T), lambda: build_B(T, multi=True, mode="B"))
    ncC = _prog(("C", T, n), lambda: build_C(T, n))
    fin = None
    for l in range(depth):
        lw = lambda name: f32(inputs[name][l])
        base = dict(consts)
        for name, _ in B_INPUTS:
            if name in inputs: base[name] = lw(name)
        base["modT"] = modT[l]
        base.update(s5tab[l])
        base["w_in"] = wbf["w_in"][l * 1024:(l + 1) * 1024]
        base["s5_w_glu"] = wbf["s5_w_glu"][l * 512:(l + 1) * 512]
        base["w_branch"] = wbf["w_branch"][l * 1536:(l + 1) * 1536]
        base["w_out"] = wbf["w_out"][l * 1024:(l + 1) * 1024]
        xpad = np.concatenate([np.zeros((128, 1024), np.float32), x, np.zeros((128, 1024), np.float32)], 0)
        xcT = fm(xc, 256)
        maps = []
        for k in range(ncores):
            m = dict(base)
            m["flags"] = flags[k]
            m["xT"] = fm(xpad[k * T:k * T + T + 256], T + 256)
            m["xcT"] = xcT
            m["rope_cos"], m["rope_sin"] = ropes[k]
            maps.append(m)
        ra = run_bass_kernel_spmd(ncA, maps, core_ids=list(range(ncores))).results
        s5_fin_all = np.stack([np.asarray(ra[k]["s5_fin"]) for k in range(ncores)], 0)
        ssd_fin_all = np.stack([np.asarray(ra[k]["ssd_fin"]) for k in range(ncores)], 0)
        ssd_tot_all = np.stack([np.asarray(ra[k]["ssd_tot"]) for k in range(ncores)], 0)
        for k in range(ncores):
            maps[k]["s5_fin_all"] = s5_fin_all; maps[k]["ssd_fin_all"] = ssd_fin_all; maps[k]["ssd_tot_all"] = ssd_tot_all
            maps[k]["onehot"] = onehots[k]
        rb = run_bass_kernel_spmd(ncB, maps, core_ids=list(range(ncores))).results
        aff_all = np.concatenate([np.asarray(rb[k]["aff"])[256:] for k in range(ncores)], 0)
        if hook: hook(("B", l), dict(rb=rb))
        cmaps = []
        for k in range(ncores):
            cm = {"ident": consts["ident"], "norm2_g": lw("norm2_g"), "modT": modT[l], "final_norm_g": f32(inputs["final_norm_g"]),
                  "w_e_gate": wbf["w_e_gate"][l * 16384:(l + 1) * 16384], "w_e_up": wbf["w_e_up"][l * 16384:(l + 1) * 16384],
                  "w_e_down": wbf["w_e_down"][l * 16384:(l + 1) * 16384],
                  "x1T": np.asarray(rb[k]["x1T"]), "aff_all": aff_all, "aff_own": np.asarray(rb[k]["aff"])}
            cmaps.append(cm)
        rc = run_bass_kernel_spmd(ncC, cmaps, core_ids=list(range(ncores))).results
        unfm = lambda a: np.asarray(a).reshape(1024, -1).T
        x = np.concatenate([unfm(rc[k]["x2T"])[256:] for k in range(ncores)], 0)
        xc = unfm(rc[0]["x2T"])[:256]
        fin = np.concatenate([unfm(rc[k]["finT"])[256:] for k in range(ncores)], 0)
        if hook: hook(("C", l), dict(x=x, xc=xc))
    return np.ascontiguousarray(fin[None].astype(np.float32))


def kernel(**inputs):
    return run_model(inputs, 2048)
```

```python
import ml_dtypes
import contextlib
import numpy as np
import concourse.bass as bass
import concourse.mybir as mybir
from concourse.bass_utils import run_bass_kernel_spmd

F32 = mybir.dt.float32
BF16 = mybir.dt.bfloat16
I32 = mybir.dt.int32
AF = mybir.ActivationFunctionType
ALU = mybir.AluOpType
AX = mybir.AxisListType

ENGS = ("pe", "dve", "act", "pool", "sp")
EPOCH = 20000
N_DMA_SEMS = 12


def _region(ap):
    t = ap.tensor
    shape = list(t.shape)
    space = str(ap.space) if hasattr(ap, "space") else ""
    dims = list(ap.ap)
    off = int(ap.offset)
    if "DRAM" in space.upper() or "HBM" in space.upper() or type(t).__name__.startswith("DRam"):
        lo = off
        hi = off + sum((c - 1) * abs(s) for s, c in dims) + 1
        return (t.name, 0, 1, lo, hi)
    row = 1
    for s in shape[1:]:
        row *= int(s)
    if type(t).__name__.startswith("PSum"):
        return (t.name, 0, 128, 0, row)
    p_lo = off // row
    f_lo = off % row
    p_hi = p_lo + int(dims[0][1])
    f_hi = f_lo + sum((c - 1) * abs(s) for s, c in dims[1:]) + 1
    return (t.name, p_lo, p_hi, f_lo, f_hi)


def _ovl(a, b):
    return a[1] < b[2] and b[1] < a[2] and a[3] < b[4] and b[3] < a[4]


def _covers(a, b):
    return a[1] <= b[1] and a[2] >= b[2] and a[3] <= b[3] and a[4] >= b[4]


class Prog:
    def __init__(self, nc, same_engine_sync=True):
        self.nc = nc
        self.es = contextlib.ExitStack()
        self.ops = {e: [] for e in ENGS}
        self.nops = {e: 0 for e in ENGS}
        self.writes = {}
        self.reads = {}
        import os
        self.same_engine_sync = same_engine_sync and os.environ.get('SAMESYNC', '1') == '1'
        self.dma_tot = [0] * N_DMA_SEMS
        self.dma_rr = 0
        self.known = {e: {} for e in ENGS}
        self._names = 0

    def init_arenas(self, n_f32, n_bf16):
        self.arena = {F32: self.es.enter_context(self.nc.sbuf_tensor("arena_f32", [128, n_f32], F32)),
                      BF16: self.es.enter_context(self.nc.sbuf_tensor("arena_bf16", [128, n_bf16], BF16))}
        self.asize = {F32: n_f32, BF16: n_bf16}
        self.atop = {F32: 0, BF16: 0}
        self.amax = {F32: 0, BF16: 0}
        self.astack = []

    def push(self):
        self.astack.append(dict(self.atop))
        self.pstack = getattr(self, "pstack", [])
        self.pstack.append(dict(self.atop))

    def pop(self):
        pk = self.pstack.pop()
        if self.pstack:
            for k in pk:
                self.pstack[-1][k] = max(self.pstack[-1][k], pk[k])
        self.last_peak = pk
        self.atop = self.astack.pop()

    def iter_push(self, i, tag):
        self.push()
        self._pp = getattr(self, "_pp", {})
        if i % 2 == 1 and tag in self._pp:
            need = self._pp[tag]
            ok = all(self.atop[dt] + 2 * need[dt] + 64 <= self.asize[dt] for dt in need)
            if ok:
                for dt in need:
                    if need[dt] > 0:
                        self.sb([128, need[dt]], dt)
        self._pp_base = getattr(self, "_pp_base", {})
        self._pp_base[(tag, i)] = dict(self.atop)

    def iter_pop(self, i, tag):
        base = self._pp_base.pop((tag, i))
        pk = self.pstack[-1]
        if i == 0:
            self._pp[tag] = {dt: pk[dt] - base[dt] for dt in pk}
        self.pop()

    def sb(self, shape, dtype=F32, name=None):
        n = 1
        for s_ in shape[1:]:
            n *= int(s_)
        n = (n + 15) // 16 * 16
        off = self.atop[dtype]
        assert off + n <= self.asize[dtype], f"arena {dtype} overflow: {off}+{n} > {self.asize[dtype]} ({name})"
        self.atop[dtype] = off + n
        self.amax[dtype] = max(self.amax[dtype], off + n)
        if getattr(self, "pstack", None):
            self.pstack[-1][dtype] = max(self.pstack[-1][dtype], off + n)
        nn = 1
        for s_ in shape[1:]:
            nn *= int(s_)
        v = self.arena[dtype][:, off:off + nn]
        if len(shape) > 2:
            names = [f"d{i}" for i in range(len(shape) - 1)]
            pat = "p (" + " ".join(names) + ") -> p " + " ".join(names)
            v = v.rearrange(pat, **{nm: int(sz) for nm, sz in zip(names[1:], shape[2:])})
        if shape[0] < 128:
            v = v[0:shape[0]]
        return v

    def ps(self, shape, dtype=F32, name=None):
        self._names += 1
        name = name or f"ps{self._names}"
        return self.es.enter_context(self.nc.psum_tensor(name, list(shape), dtype))

    def _deps(self, reads, writes):
        deps = set()
        rr = [_region(a) for a in reads]
        wr = [_region(a) for a in writes]
        for r in rr:
            for (reg, ev) in self.writes.get(r[0], ()):
                if _ovl(reg, r):
                    deps.add(ev)
            if r[0].startswith("ps"):
                for (reg, ev) in self.reads.get(r[0], ()):
                    deps.add(ev)
        for w in wr:
            for (reg, ev) in self.writes.get(w[0], ()):
                if _ovl(reg, w):
                    deps.add(ev)
            for (reg, ev) in self.reads.get(w[0], ()):
                if _ovl(reg, w):
                    deps.add(ev)
        return deps, rr, wr

    def _record(self, rr, wr, ev):
        for w in wr:
            lst = self.writes.setdefault(w[0], [])
            lst[:] = [(reg, e) for (reg, e) in lst if not _covers(w, reg)]
            lst.append((w, ev))
            rl = self.reads.get(w[0])
            if rl:
                rl[:] = [(reg, e) for (reg, e) in rl if not _covers(w, reg)]
        for r in rr:
            lst = self.reads.setdefault(r[0], [])
            lst[:] = [(reg, e) for (reg, e) in lst if not (e[0] == ev[0] and _covers(r, reg))]
            lst.append((r, ev))

    def op(self, eng, fn, reads, writes, pe_accum=False):
        deps, rr, wr = self._deps(reads, writes)
        idx = self.nops[eng]
        self.nops[eng] += 1
        ev = ((eng, idx // EPOCH), idx % EPOCH + 1)
        waits = self._filter(eng, deps, pe_accum)
        self.ops[eng].append((fn, waits, ev, False))
        self._record(rr, wr, ev)
        return ev

    def _filter(self, eng, deps, pe_accum=False):
        best = {}
        for (s, v) in deps:
            if s[0] == eng:
                if eng == "pe" or not self.same_engine_sync:
                    continue
            if best.get(s, 0) < v:
                best[s] = v
        out = []
        kn = self.known[eng]
        for s, v in best.items():
            if kn.get(s, 0) >= v:
                continue
            kn[s] = v
            out.append((s, v))
        return out

    def dma(self, out, in_, q="sp", **kw):
        deps, rr, wr = self._deps([in_], [out])
        k = self.dma_rr
        self.dma_rr = (self.dma_rr + 1) % N_DMA_SEMS
        sem = ("dma", k)
        prev = self.dma_tot[k]
        if prev:
            deps.add((sem, prev))
        self.dma_tot[k] += 16
        ev = (sem, self.dma_tot[k])
        waits = self._filter(q, deps)
        self.nops[q] += 0
        self.ops[q].append((lambda e, o=out, i=in_, kw=kw: e.dma_start(out=o, in_=i, **kw), waits, ev, True))
        self._record(rr, wr, ev)
        return ev

    def allgather(self, out, in_, n=8):
        deps, rr, wr = self._deps([in_], [out])
        self.cc_tot = getattr(self, "cc_tot", 0) + 1
        ev = (("cc", 0), self.cc_tot)
        if self.cc_tot > 1:
            deps.add((("cc", 0), self.cc_tot - 1))
        waits = self._filter("pool", deps)
        self.ops["pool"].append((lambda e, o=out, i=in_: e.collective_compute(
            "AllGather", ALU.bypass, replica_groups=[list(range(n))], ins=[i], outs=[o]), waits, ev, "cc"))
        self._record(rr, wr, ev)
        return ev

    def wait_all(self, eng="sp"):
        deps = set()
        for lst in self.writes.values():
            for (_, ev) in lst:
                deps.add(ev)
        waits = self._filter(eng, deps)
        self.ops[eng].append((None, waits, None, False))

    def mm(self, out, lhsT, rhs, start=True, stop=True):
        rd = [lhsT, rhs] + ([] if start else [])
        return self.op("pe", lambda e: e.matmul(out, lhsT, rhs, start=start, stop=stop), rd, [out])

    def transpose(self, out, in_, ident):
        return self.op("pe", lambda e: e.transpose(out, in_, ident), [in_, ident], [out])

    def act(self, out, in_, func, bias=0.0, scale=1.0, accum_out=None):
        rd = [in_] + [a for a in (bias, scale) if not isinstance(a, (int, float))]
        wr = [out] + ([accum_out] if accum_out is not None else [])
        kw = {}
        if accum_out is not None:
            kw["accum_out"] = accum_out
        return self.op("act", lambda e: e.activation(out, in_, func, bias=bias, scale=scale, **kw), rd, wr)

    def tt(self, out, in0, in1, op, eng="dve"):
        return self.op(eng, lambda e: e.tensor_tensor(out, in0, in1, op), [in0, in1], [out])

    def ts(self, out, in0, s1, s2=None, op0=ALU.mult, op1=None, eng="dve", accum_out=None):
        rd = [in0] + [a for a in (s1, s2) if a is not None and not isinstance(a, (int, float))]
        wr = [out] + ([accum_out] if accum_out is not None else [])
        kw = {}
        if op1 is not None:
            kw["op1"] = op1
        if accum_out is not None:
            kw["accum_out"] = accum_out
        return self.op(eng, lambda e: e.tensor_scalar(out, in0, s1, s2, op0, **kw), rd, wr)

    def stt(self, out, in0, scalar, in1, op0, op1, eng="dve"):
        rd = [in0, in1] + ([] if isinstance(scalar, (int, float)) else [scalar])
        return self.op(eng, lambda e: e.scalar_tensor_tensor(out, in0, scalar, in1, op0, op1), rd, [out])

    def copy(self, out, in_, eng="dve"):
        if eng == "act":
            return self.op("act", lambda e: e.copy(out, in_), [in_], [out])
        return self.op(eng, lambda e: e.tensor_copy(out, in_), [in_], [out])

    def memset(self, ap, val, eng="dve"):
        return self.op(eng, lambda e: e.memset(ap, val), [], [ap])

    def reduce(self, out, in_, op=ALU.add, axis=AX.X, eng="dve"):
        return self.op(eng, lambda e: e.tensor_reduce(out, in_, axis, op), [in_], [out])

    def recip(self, out, in_):
        return self.op("dve", lambda e: e.reciprocal(out, in_), [in_], [out])

    def scan(self, out, d0, d1, initial, op0=ALU.mult, op1=ALU.add):
        rd = [d0, d1] + ([] if isinstance(initial, (int, float)) else [initial])
        return self.op("dve", lambda e: e.tensor_tensor_scan(out, d0, d1, initial, op0, op1), rd, [out])

    def emit(self):
        nc = self.nc
        sems = {}
        for e in ("pe", "dve", "act", "pool"):
            n_ep = (self.nops[e] + EPOCH - 1) // EPOCH
            for k in range(max(n_ep, 1)):
                sems[(e, k)] = self.es.enter_context(nc.semaphore(f"s_{e}{k}"))
        sems[("cc", 0)] = self.es.enter_context(nc.semaphore("s_cc"))
        for k in range(N_DMA_SEMS):
            sems[("dma", k)] = self.es.enter_context(nc.semaphore(f"s_dma{k}"))
        block = self.es.enter_context(nc.Block())

        def run(engobj, lst):
            for (fn, waits, ev, is_dma) in lst:
                for (s, v) in waits:
                    engobj.wait_ge(sems[s], v)
                if fn is None:
                    continue
                ins = fn(engobj)
                if is_dma == "cc":
                    ins.then_inc(sems[ev[0]])
                elif is_dma:
                    ins.then_inc(sems[ev[0]], 16)
                else:
                    ins.then_inc(sems[ev[0]], 1)

        ops = self.ops

        @block.tensor
        def _(t):
            run(t, ops["pe"])

        @block.vector
        def _(v):
            run(v, ops["dve"])

        @block.scalar
        def _(s):
            run(s, ops["act"])

        @block.gpsimd
        def _(g):
            run(g, ops["pool"])

        @block.sync
        def _(sy):
            run(sy, ops["sp"])

    def close(self):
        self.es.close()


import math
import numpy as np

PI = math.pi
D = 1024
KD = 8
NCTX = 256
HALO = 128
IN_W = 5904
C_U, C_Z, C_XBC, C_DT, C_Q, C_K, C_V, C_G = 0, 512, 1024, 2048, 2064, 2576, 2704, 2832


def V(ap, free_dims, off=0):
    return bass.AP(ap.tensor, ap.offset + off, [list(ap.ap[0])] + [list(d) for d in free_dims])


def DV(t, off, dims):
    return bass.AP(t.tensor, t.offset + off, [list(d) for d in dims])


class G:
    pass


def host_consts():
    c = {}
    c["ident"] = np.eye(128, dtype=np.float32)
    c["ut"] = np.triu(np.ones((128, 128), np.float32))
    c["lt"] = np.tril(np.ones((128, 128), np.float32))
    d = np.arange(128, dtype=np.float32)
    exl = np.tile(d[None, :], (128, 1))
    exr = np.concatenate([np.tile((d + 1)[None, :], (64, 1)), np.tile((128 - d)[None, :], (64, 1))], 0)
    c["exl"] = exl.astype(np.float32)
    c["exr"] = exr.astype(np.float32)
    exv = np.stack([127 - d, d], 1)
    c["exv"] = exv.astype(np.float32)
    m = np.zeros((128, 8), np.float32)
    for p in range(128):
        m[p, p // 16] = 1.0
    c["maskbd"] = m
    return c


CONST_SHAPES = {"ident": [128, 128], "ut": [128, 128], "lt": [128, 128], "exl": [128, 128], "exr": [128, 128],
                "exv": [128, 2], "maskbd": [128, 8]}


def load_consts(g):
    P = g.P
    g.c = {}
    for k, shp in CONST_SHAPES.items():
        t = P.sb(shp, F32)
        P.dma(t, g.dram[k])
        g.c[k] = t
    g.ident_bf = P.sb([128, 128], BF16)
    P.copy(g.ident_bf, g.c["ident"])
    g.ones_bf = P.sb([128, 128], BF16)
    P.memset(g.ones_bf, 1.0)
    g.c["ones_f"] = P.sb([128, 128], F32)
    P.memset(g.c["ones_f"], 1.0)


def sincos(P, out_cos, out_sin, ang, tmp):
    n = 1
    for d_ in ang.shape[1:]:
        n *= int(d_)
    if not hasattr(P, "_kint"):
        P._kint = P.es.enter_context(P.nc.sbuf_tensor("kint", [128, 2048], I32))
        P._halfpi = P.sb([128, 1], F32)
        P.memset(P._halfpi, PI / 2)
    ki = bass.AP(P._kint[:, 0:n].tensor, P._kint[:, 0:n].offset, [list(ang.ap[0])[:1] + [ang.ap[0][1]]] and [[P._kint[:, 0:n].ap[0][0], ang.ap[0][1]], [1, n]])
    angf = bass.AP(ang.tensor, ang.offset, [list(ang.ap[0]), [1, n]])
    tmpf = bass.AP(tmp.tensor, tmp.offset, [list(tmp.ap[0]), [1, n]])
    cosf = bass.AP(out_cos.tensor, out_cos.offset, [list(out_cos.ap[0]), [1, n]])
    sinf = bass.AP(out_sin.tensor, out_sin.offset, [list(out_sin.ap[0]), [1, n]])
    pp = slice(0, 128)
    P.ts(ki, angf, 1.0 / (2 * PI), 0.25, op0=ALU.mult, op1=ALU.add)
    P.stt(tmpf, ki, -2 * PI, angf, ALU.mult, ALU.add)
    P.act(cosf, tmpf, AF.Sin, bias=P._halfpi[0:ang.ap[0][1]] if ang.ap[0][1] < 128 else P._halfpi, scale=1.0)
    P.ts(ki, angf, 1.0 / (2 * PI), None, op0=ALU.mult)
    P.stt(tmpf, ki, -2 * PI, angf, ALU.mult, ALU.add)
    P.act(sinf, tmpf, AF.Sin)


def s5_params(g):
    P = g.P
    dr = g.dram
    s = G()
    g.s5 = s
    NG = getattr(g, "NG", 32)
    s.lrc = P.sb([128, NG]); s.lic = P.sb([128, NG]); s.dtc = P.sb([128, NG])
    for d in range(2):
        P.dma(s.lrc[d * 64:(d + 1) * 64], DV(dr["s5_lam_re"], d * NG * 64, [[1, 64], [64, NG]]), allow_slow_non_contiguous=True)
        P.dma(s.lic[d * 64:(d + 1) * 64], DV(dr["s5_lam_im"], d * NG * 64, [[1, 64], [64, NG]]), allow_slow_non_contiguous=True)
        P.dma(s.dtc[d * 64:(d + 1) * 64], DV(dr["s5_log_dt"], d * NG, [[0, 64], [1, NG]]))
    P.act(s.dtc, s.dtc, AF.Exp)
    s.thc = P.sb([128, NG]); s.lrdtc = P.sb([128, NG])
    P.tt(s.thc, s.lic, s.dtc, ALU.mult)
    P.tt(s.lrdtc, s.lrc, s.dtc, ALU.mult)
    mag = P.sb([128, NG]); co = P.sb([128, NG]); si = P.sb([128, NG]); tmp = P.sb([128, NG])
    P.act(mag, s.lrdtc, AF.Exp)
    sincos(P, co, si, s.thc, tmp)
    abr = P.sb([128, NG]); abi = P.sb([128, NG])
    P.tt(abr, mag, co, ALU.mult)
    P.tt(abi, mag, si, ALU.mult)
    den = P.sb([128, NG]); t2 = P.sb([128, NG])
    P.tt(den, s.lrc, s.lrc, ALU.mult)
    P.tt(t2, s.lic, s.lic, ALU.mult)
    P.tt(den, den, t2, ALU.add)
    P.recip(den, den)
    am1 = P.sb([128, NG])
    P.ts(am1, abr, -1.0, None, op0=ALU.add)
    fr = P.sb([128, NG]); fi = P.sb([128, NG])
    P.tt(fr, am1, s.lrc, ALU.mult); P.tt(t2, abi, s.lic, ALU.mult); P.tt(fr, fr, t2, ALU.add); P.tt(fr, fr, den, ALU.mult)
    P.tt(fi, abi, s.lrc, ALU.mult); P.tt(t2, am1, s.lic, ALU.mult); P.tt(fi, fi, t2, ALU.subtract); P.tt(fi, fi, den, ALU.mult)
    bre = P.sb([128, NG, 16]); bim = P.sb([128, NG, 16])
    s.cre = P.sb([128, NG, 16]); s.cim = P.sb([128, NG, 16])
    for d in range(2):
        sl = slice(d * 64, (d + 1) * 64)
        P.dma(bre[sl], DV(dr["s5_b_re"], d * NG * 1024, [[16, 64], [1024, NG], [1, 16]]))
        P.dma(bim[sl], DV(dr["s5_b_im"], d * NG * 1024, [[16, 64], [1024, NG], [1, 16]]))
        P.dma(s.cre[sl], DV(dr["s5_c_re"], d * NG * 1024, [[1, 64], [1024, NG], [64, 16]]), allow_slow_non_contiguous=True)
        P.dma(s.cim[sl], DV(dr["s5_c_im"], d * NG * 1024, [[1, 64], [1024, NG], [64, 16]]), allow_slow_non_contiguous=True)
    s.bbr = P.sb([128, NG, 16]); s.bbi = P.sb([128, NG, 16])
    frb = V(fr, [[1, NG], [0, 16]]); fib = V(fi, [[1, NG], [0, 16]])
    t3 = P.sb([128, NG, 16])
    P.tt(s.bbr, bre, frb, ALU.mult); P.tt(t3, bim, fib, ALU.mult); P.tt(s.bbr, s.bbr, t3, ALU.subtract)
    P.tt(s.bbi, bim, frb, ALU.mult); P.tt(t3, bre, fib, ALU.mult); P.tt(s.bbi, s.bbi, t3, ALU.add)
    return s


def s5_gen_E(g, ex, out_re, out_im, neg_im=False):
    P = g.P
    s = g.s5
    P.push()
    GH = 16
    NG = getattr(g, "NG", 32)
    ang = P.sb([128, GH, 128]); tmp = P.sb([128, GH, 128]); mag = P.sb([128, GH, 128]); co = P.sb([128, GH, 128])
    exb = V(ex, [[0, GH], [1, 128]])
    for h in range(NG // GH):
        gs = slice(h * GH, (h + 1) * GH)
        P.tt(ang, V(s.thc[:, gs], [[1, GH], [0, 128]]), exb, ALU.mult)
        P.tt(mag, V(s.lrdtc[:, gs], [[1, GH], [0, 128]]), exb, ALU.mult, eng="pool")
        P.act(mag, mag, AF.Exp)
        sincos(P, co, ang, ang, tmp)
        P.tt(out_re[:, gs, :], mag, co, ALU.mult)
        if neg_im:
            P.stt(out_im[:, gs, :], mag, -1.0, ang, ALU.mult, ALU.mult)
        else:
            P.tt(out_im[:, gs, :], mag, ang, ALU.mult)
    P.pop()


def s5_gen_V(g, vr, vi):
    P = g.P
    dr = g.dram
    P.push()
    GH = 16
    lib = P.sb([128, GH, 2, 64]); lrb = P.sb([128, GH, 2, 64]); dtb = P.sb([128, GH, 2])
    tmp = P.sb([128, GH, 2, 64]); co = P.sb([128, GH, 2, 64])
    exvb = V(g.c["exv"], [[0, GH], [1, 2], [0, 64]])
    NG = getattr(g, "NG", 32)
    for h in range(NG // GH):
        g0 = h * GH
        for d_ in range(2):
            P.dma(lib[:, :, d_, :], DV(dr["s5_lam_im"], g0 * 64 + d_ * NG * 64, [[0, 128], [64, GH], [1, 64]]))
            P.dma(lrb[:, :, d_, :], DV(dr["s5_lam_re"], g0 * 64 + d_ * NG * 64, [[0, 128], [64, GH], [1, 64]]))
            P.dma(dtb[:, :, d_], DV(dr["s5_log_dt"], g0 + d_ * NG, [[0, 128], [1, GH]]), allow_slow_non_contiguous=True)
        P.act(dtb, dtb, AF.Exp)
        dtbb = V(dtb, [[2, GH], [1, 2], [0, 64]])
        P.tt(lib, lib, dtbb, ALU.mult)
        P.tt(lrb, lrb, dtbb, ALU.mult, eng="pool")
        P.tt(lib, lib, exvb, ALU.mult)
        P.tt(lrb, lrb, exvb, ALU.mult, eng="pool")
        P.act(lrb, lrb, AF.Exp)
        sincos(P, co, lib, lib, tmp)
        gs = slice(g0, g0 + GH)
        P.tt(vr[:, gs, :], lrb, co, ALU.mult)
        P.tt(vi[:, gs, :], lrb, lib, ALU.mult)
    P.pop()


def cmul_acc(P, out_re, out_im, ar, ai, hr, hi, sr, si, t1, t2, eng="dve"):
    P.tt(t1, ar, hr, ALU.mult, eng=eng)
    P.tt(t2, ai, hi, ALU.mult, eng=eng)
    P.tt(t1, t1, t2, ALU.subtract, eng=eng)
    if sr is not None:
        P.tt(out_re, t1, sr, ALU.add, eng=eng)
    else:
        P.copy(out_re, t1, eng=eng)
    P.tt(t1, ar, hi, ALU.mult, eng=eng)
    P.tt(t2, ai, hr, ALU.mult, eng=eng)
    P.tt(t1, t1, t2, ALU.add, eng=eng)
    if si is not None:
        P.tt(out_im, t1, si, ALU.add, eng=eng)
    else:
        P.copy(out_im, t1, eng=eng)


def s5_states(g, uT, NCH):
    P = g.P
    s = g.s5
    s.sre = P.sb([128, 32, NCH]); s.sim = P.sb([128, 32, NCH])
    P.push()
    vr = P.sb([128, 32, 128], BF16); vi = P.sb([128, 32, 128], BF16)
    if "s5_vr" in g.dram:
        P.dma(vr, g.dram["s5_vr"]); P.dma(vi, g.dram["s5_vi"])
    else:
        s5_gen_V(g, V(vr, [[128, 32], [64, 2], [1, 64]]), V(vi, [[128, 32], [64, 2], [1, 64]]))
    utok = P.sb([128, NCH, 512], BF16)
    for c in range(NCH):
        pt = g.pbank_bf()
        for b in range(4):
            P.transpose(pt[:, b * 128:(b + 1) * 128], uT[:, b, c * 128:(c + 1) * 128], g.ident_bf)
        P.copy(utok[:, c, :], pt[:, 0:512], eng="act" if c % 2 else "dve")
    zr = P.sb([128, NCH, 16]); zi = P.sb([128, NCH, 16]); t1 = P.sb([128, NCH, 16]); t2 = P.sb([128, NCH, 16])
    t3 = P.sb([128, NCH, 16]); t4 = P.sb([128, NCH, 16])
    N = NCH * 16
    for gi in range(32):
        p1 = g.pbank(); p2 = g.pbank()
        rhs = V(utok, [[512, NCH], [1, 16]], off=gi * 16)
        P.mm(V(p1, [[16, NCH], [1, 16]]), vr[:, gi, :], rhs)
        P.mm(V(p2, [[16, NCH], [1, 16]]), vi[:, gi, :], rhs)
        P.copy(zr, V(p1, [[16, NCH], [1, 16]]), eng="act")
        P.copy(zi, V(p2, [[16, NCH], [1, 16]]), eng="act")
        bb_r = V(s.bbr[:, gi, :], [[0, NCH], [1, 16]]); bb_i = V(s.bbi[:, gi, :], [[0, NCH], [1, 16]])
        P.tt(t1, zr, bb_r, ALU.mult); P.tt(t2, zi, bb_i, ALU.mult); P.tt(t1, t1, t2, ALU.subtract)
        P.reduce(s.sre[:, gi, :], t1)
        P.tt(t3, zi, bb_r, ALU.mult, eng="pool"); P.tt(t4, zr, bb_i, ALU.mult, eng="pool"); P.tt(t3, t3, t4, ALU.add, eng="pool")
        P.reduce(s.sim[:, gi, :], t3)
    P.pop()


def s5_local_scan(g, NCX, NC):
    P = g.P
    s = g.s5
    NCH = NCX + NC
    s.pre_re = P.sb([128, 32, NCH]); s.pre_im = P.sb([128, 32, NCH])
    s.fin_re = P.sb([128, 32, 2]); s.fin_im = P.sb([128, 32, 2])
    s.apow_re = P.sb([128, 32, NCH]); s.apow_im = P.sb([128, 32, NCH])
    t1 = P.sb([128, 32]); t2 = P.sb([128, 32])
    P.memset(s.pre_re, 0.0); P.memset(s.pre_im, 0.0)
    for (c0, n, fi) in ((0, NCX, 0), (NCX, NC, 1)):
        for half, order in ((slice(0, 64), list(range(c0, c0 + n))), (slice(64, 128), list(range(c0 + n - 1, c0 - 1, -1)))):
            eng = "dve" if half.start == 0 else "pool"
            aqr = s.aqr[half]; aqi = s.aqi[half]
            P.memset(s.apow_re[half, :, order[0]], 1.0, eng=eng); P.memset(s.apow_im[half, :, order[0]], 0.0, eng=eng)
            for k in range(n):
                c = order[k]
                if k + 1 < n:
                    cn = order[k + 1]
                    ore, oim = s.pre_re[half, :, cn], s.pre_im[half, :, cn]
                    cmul_acc(P, s.apow_re[half, :, cn], s.apow_im[half, :, cn], aqr, aqi, s.apow_re[half, :, c], s.apow_im[half, :, c],
                             None, None, t1[half], t2[half], eng=eng)
                else:
                    ore, oim = s.fin_re[half, :, fi], s.fin_im[half, :, fi]
                cmul_acc(P, ore, oim, aqr, aqi, s.pre_re[half, :, c], s.pre_im[half, :, c], s.sre[half, :, c], s.sim[half, :, c],
                         t1[half], t2[half], eng=eng)


def s5_tables_ro(g):
    P = g.P
    s = g.s5
    s.w2re = P.sb([128, 32, 128], BF16); s.nw2im = P.sb([128, 32, 128], BF16)
    s.aqr = P.sb([128, 32]); s.aqi = P.sb([128, 32])
    if "s5_w2re" in g.dram:
        P.dma(s.w2re, g.dram["s5_w2re"]); P.dma(s.nw2im, g.dram["s5_nw2im"])
    else:
        s5_gen_E(g, g.c["exr"], s.w2re, s.nw2im, neg_im=True)
    P.push()
    ang = P.sb([128, 32]); mag = P.sb([128, 32]); tmp = P.sb([128, 32]); co = P.sb([128, 32])
    P.ts(ang, s.thc, 128.0, None, op0=ALU.mult)
    P.act(mag, s.lrdtc, AF.Exp, scale=128.0)
    sincos(P, co, ang, ang, tmp)
    P.tt(s.aqr, mag, co, ALU.mult)
    P.tt(s.aqi, mag, ang, ALU.mult)
    P.pop()


def s5_readout(g, NCX, NC, carry_re, carry_im):
    P = g.P
    s = g.s5
    NCH = NCX + NC
    hre = P.sb([128, 32, NCH]); him = P.sb([128, 32, NCH])
    P.copy(hre, s.pre_re); P.copy(him, s.pre_im, eng="pool")
    own = slice(NCX, NCH)
    t1 = P.sb([128, 32, NC]); t2 = P.sb([128, 32, NC])
    crb = V(carry_re, [[carry_re.ap[1][0], 32], [0, NC]]); cib = V(carry_im, [[carry_im.ap[1][0], 32], [0, NC]])
    P.tt(t1, s.apow_re[:, :, own], crb, ALU.mult); P.tt(t2, s.apow_im[:, :, own], cib, ALU.mult)
    P.tt(t1, t1, t2, ALU.subtract); P.tt(hre[:, :, own], hre[:, :, own], t1, ALU.add)
    P.tt(t1, s.apow_re[:, :, own], cib, ALU.mult); P.tt(t2, s.apow_im[:, :, own], crb, ALU.mult)
    P.tt(t1, t1, t2, ALU.add); P.tt(him[:, :, own], him[:, :, own], t1, ALU.add)
    P.push()
    gre = P.sb([128, 32, NCH, 16], BF16); gim = P.sb([128, 32, NCH, 16], BF16)
    GQ = 8
    a1 = P.sb([128, GQ, NCH, 16]); a2 = P.sb([128, GQ, NCH, 16])
    for q in range(32 // GQ):
        gs = slice(q * GQ, (q + 1) * GQ)
        crb_ = V(s.cre[:, gs, :], [[16, GQ], [0, NCH], [1, 16]]); cib_ = V(s.cim[:, gs, :], [[16, GQ], [0, NCH], [1, 16]])
        hrb = V(hre[:, gs, :], [[NCH, GQ], [1, NCH], [0, 16]]); hib = V(him[:, gs, :], [[NCH, GQ], [1, NCH], [0, 16]])
        P.tt(a1, crb_, hrb, ALU.mult); P.tt(a2, cib_, hib, ALU.mult, eng="pool"); P.tt(gre[:, gs], a1, a2, ALU.subtract)
        P.tt(a1, crb_, hib, ALU.mult); P.tt(a2, cib_, hrb, ALU.mult, eng="pool"); P.tt(gim[:, gs], a1, a2, ALU.add)
    N = NCH * 16
    for gi in range(32):
        ps = g.pbank()
        o = V(ps, [[16, NCH], [1, 16]])
        P.mm(o, s.w2re[:, gi, :], V(gre[:, gi], [[16, NCH], [1, 16]]), start=True, stop=False)
        P.mm(o, s.nw2im[:, gi, :], V(gim[:, gi], [[16, NCH], [1, 16]]), start=False, stop=True)
        P.copy(V(s.ystok, [[512, NCH], [1, 16]], off=gi * 16), o, eng="act" if gi % 2 else "dve")
    P.pop()


def s5_kt_build(g, NB, kt_d):
    P = g.P
    s = g.s5
    NG = NB * 8
    P.push()
    elr = P.sb([128, NG, 128], BF16); eli = P.sb([128, NG, 128], BF16)
    s5_gen_E(g, g.c["exl"], elr, eli)
    brpad = P.sb([128, NG, 128], BF16); nbipad = P.sb([128, NG, 128], BF16)
    P.memset(brpad, 0.0); P.memset(nbipad, 0.0, eng="pool")
    padv = lambda t: V(t, [[1024, NB], [144, 8], [1, 16]])
    P.copy(padv(brpad), V(s.bbr, [[128, NB], [16, 8], [1, 16]]))
    P.ts(padv(nbipad), V(s.bbi, [[128, NB], [16, 8], [1, 16]]), -1.0, None, op0=ALU.mult)
    care = P.sb([128, 32, 16], BF16); caim = P.sb([128, 32, 16], BF16)
    a1 = P.sb([128, 32, 16]); a2 = P.sb([128, 32, 16])
    ko = P.sb([128, 2, 512], BF16)
    for b in range(NB):
        for lh in range(2):
            for sl in range(2):
                d0 = lh * 64 + sl * 32
                pk = [g.pbank(), g.pbank()]
                for gl in range(8):
                    gi = 8 * b + gl
                    crb = V(s.cre[:, gi, :], [[0, 32], [1, 16]]); cib = V(s.cim[:, gi, :], [[0, 32], [1, 16]])
                    erb = V(elr[:, gi, d0:d0 + 32], [[1, 32], [0, 16]]); eib = V(eli[:, gi, d0:d0 + 32], [[1, 32], [0, 16]])
                    P.tt(a1, crb, erb, ALU.mult); P.tt(a2, cib, eib, ALU.mult, eng="pool"); P.tt(care, a1, a2, ALU.subtract)
                    P.tt(a1, crb, eib, ALU.mult); P.tt(a2, cib, erb, ALU.mult, eng="pool"); P.tt(caim, a1, a2, ALU.add)
                    for dr_ in range(2):
                        h = slice(dr_ * 64, (dr_ + 1) * 64)
                        P.mm(pk[dr_], brpad[h, gi, :], V(care[h], [[1, 512]]), start=(gl == 0), stop=False)
                        P.mm(pk[dr_], nbipad[h, gi, :], V(caim[h], [[1, 512]]), start=False, stop=(gl == 7))
                for dr_ in range(2):
                    P.copy(ko[:, dr_, :], pk[dr_], eng="act" if dr_ else "dve")
                    off = (((b * 2 + lh) * 2 + dr_) * 128) * 1024 + sl * 512
                    P.dma(DV(kt_d, off, [[1024, 128], [1, 512]]), ko[:, dr_, :])
    P.pop()


def s5_lags(g, uT, NCH, ya_acc_cb):
    P = g.P
    s = g.s5
    P.push()
    kt_d = g.dram["s5_kt"]
    kt = P.sb([128, 2, 1024], BF16)
    bd = P.sb([128, 2, 64, 128], BF16)
    NS = NCH * 128
    yacc = P.sb([128, NS])
    mb = V(g.c["maskbd"], [[0, 64], [1, 8], [0, 16]])
    cgs = [(c0, min(4, NCH - c0)) for c0 in range(0, NCH, 4)]
    for b in range(4):
        for lh in range(2):
            P.dma(kt, DV(kt_d, (b * 2 + lh) * 2 * 128 * 1024, [[1024, 128], [128 * 1024, 2], [1, 1024]]))
            for dr_ in range(2):
                P.tt(V(bd[:, dr_], [[128, 64], [16, 8], [1, 16]]), V(kt[:, dr_, :], [[16, 64], [0, 8], [1, 16]]), mb, ALU.mult,
                     eng="pool" if dr_ else "dve")
            for (c0, ncg) in cgs:
                ps = g.pbank()
                first = True
                if lh == 1:
                    P.mm(ps[:, 0:ncg * 128], g.zeros_bf, V(uT[:, b, :], [[1, ncg * 128]], off=c0 * 128), start=True, stop=False)
                    for c in range(c0, c0 + ncg):
                        P.mm(ps[:, (c - c0) * 128:(c - c0 + 1) * 128], s.ystok[:, c, b * 128:(b + 1) * 128], g.ident_bf,
                             start=False, stop=False)
                    first = False
                for d in range(64):
                    dd = lh * 64 + d
                    w = 128 - dd
                    last = (d == 63)
                    P.mm(V(ps, [[128, ncg], [1, w]], off=dd), bd[:, 0, d, :], V(uT[:, b, :], [[128, ncg], [1, w]], off=c0 * 128),
                         start=first, stop=False)
                    first = False
                    P.mm(V(ps, [[128, ncg], [1, w]]), bd[:, 1, d, :], V(uT[:, b, :], [[128, ncg], [1, w]], off=c0 * 128 + dd),
                         start=False, stop=last)
                dst = yacc[:, c0 * 128:(c0 + ncg) * 128]
                if lh == 0:
                    P.copy(dst, ps[:, 0:ncg * 128], eng="act")
                else:
                    P.tt(dst, dst, ps[:, 0:ncg * 128], ALU.add)
        ya_acc_cb(b, yacc)
    P.pop()


def s5_core(g, uT, NCX, NC, carry_fn, ya_cb, states_cb=None, states_only=False):
    P = g.P
    NCH = NCX + NC
    s5_params(g)
    s = g.s5
    s.ystok = P.sb([128, NCH, 512], BF16)
    import os
    S5CUT = os.environ.get("S5CUT", "")
    P.push()
    s5_tables_ro(g)
    if S5CUT == "ro":
        P.pop(); return
    s5_states(g, uT, NCH)
    if S5CUT == "states":
        P.pop(); return
    s5_local_scan(g, NCX, NC)
    if states_cb is not None:
        states_cb()
    if states_only:
        P.pop()
        return
    cr, ci = carry_fn()
    s5_readout(g, NCX, NC, cr, ci)
    P.pop()
    if S5CUT == "readout":
        return
    s5_lags(g, uT, NCH, ya_cb)


def load_w(g, name, r0, nrows, c0, ncols, dtype=BF16, q="sp"):
    P = g.P
    kk = nrows // 128
    t = P.sb([128, kk, ncols], dtype)
    d = g.dram[name]
    ncol_total = d.shape[-1]
    P.dma(t, DV(d, r0 * ncol_total + c0, [[ncol_total, 128], [128 * ncol_total, kk], [1, ncols]]), q=q)
    return t


def load_h(g, c0, n):
    P = g.P
    t = P.sb([128, 8, n], BF16)
    P.dma(t, DV(g.hT_d, c0, [[g.E, 128], [128 * g.E, 8], [1, n]]))
    return t


def proj(g, ps_out, w, j0, hT, n, wcols=128):
    P = g.P
    for k in range(8):
        P.mm(ps_out, w[:, k, j0:j0 + wcols], hT[:, k, 0:n], start=(k == 0), stop=(k == 7))


def col_tiles(c0, n, step=512):
    out = []
    c = c0
    while c < c0 + n:
        m = min(step, c0 + n - c)
        out.append((c, m))
        c += m
    return out


def ssd_prep(g, NCX, NC):
    P = g.P
    dr = g.dram
    s = G(); g.ssd = s
    T = NC * 128; NS = (NCX + NC) * 128
    s.xsT = P.sb([128, 4, NS], BF16); s.bmT = P.sb([128, 2, NS], BF16); s.cmT = P.sb([128, 2, NS], BF16)
    s.gz = P.sb([128, 4, NS], BF16)
    s.dt = P.sb([128, NCX + NC, 16]); s.dta = P.sb([128, NCX + NC, 16])
    s.cw = P.sb([128, 8, 5]); s.cb = P.sb([128, 8])
    for k_ in range(5):
        P.dma(s.cw[:, :, k_], DV(dr["ssd_conv_w"], k_ * 1024, [[1, 128], [128, 8]]), allow_slow_non_contiguous=True)
    P.dma(s.cb, DV(dr["ssd_conv_b"], 0, [[1, 128], [128, 8]]), allow_slow_non_contiguous=True)
    s.dtb = P.sb([128, 16]); s.ab = P.sb([128, 16])
    P.dma(s.dtb, DV(dr["ssd_dt_bias"], 0, [[0, 128], [1, 16]]))
    P.dma(s.ab, DV(dr["ssd_a_log"], 0, [[0, 128], [1, 16]]))
    P.act(s.ab, s.ab, AF.Exp)
    P.ts(s.ab, s.ab, -1.0, None, op0=ALU.mult)
    s.dcol = P.sb([128, 4])
    for hh in range(2):
        P.dma(s.dcol[hh * 64:(hh + 1) * 64, :], DV(dr["ssd_d"], hh, [[0, 64], [2, 4]]), allow_slow_non_contiguous=True)
    s.ng = P.sb([128, 4])
    P.dma(s.ng, DV(dr["ssd_norm_g"], 0, [[1, 128], [128, 4]]), allow_slow_non_contiguous=True)
    P.push()
    wx = load_w(g, "w_in", 0, 1024, C_XBC, 1024)
    wz = load_w(g, "w_in", 0, 1024, C_Z, 512)
    wdt = load_w(g, "w_in", 0, 1024, C_DT, 16)
    regions = [(0, NCX * 128, g.e_ctx, False), (NCX * 128, T, g.e_own, True)]
    W = NS + 8
    xin = P.sb([128, W])
    for j in range(8):
        P.memset(xin[:, 0:2], 0.0); P.memset(xin[:, 2 + NCX * 128:4 + NCX * 128], 0.0)
        for (s0, n, e0, is_own) in regions:
            xo = 2 + s0 + (4 if is_own else 0)
            lo, hi = (e0 - 2, e0 + n + 2) if is_own else (e0, e0 + n)
            xo_lo = xo - 2 if is_own else xo
            for (c, m) in col_tiles(lo, hi - lo):
                P.push()
                ht = load_h(g, c, m)
                ps = g.pbank()
                proj(g, ps[:, 0:m], wx, j * 128, ht, m)
                P.copy(xin[:, xo_lo + (c - lo):xo_lo + (c - lo) + m], ps[:, 0:m], eng="act")
                P.pop()
            if is_own:
                P.ts(xin[:, xo - 2:xo], xin[:, xo - 2:xo], g.flagL, None, op0=ALU.mult)
                P.ts(xin[:, xo + n:xo + n + 2], xin[:, xo + n:xo + n + 2], g.flagR, None, op0=ALU.mult)
        dst = s.xsT[:, j, :] if j < 4 else (s.bmT[:, j - 4, :] if j < 6 else s.cmT[:, j - 6, :])
        for (s0, n, e0, is_own) in regions:
            xo = 2 + s0 + (4 if is_own else 0)
            P.push()
            acc = P.sb([128, n])
            P.ts(acc, xin[:, xo - 2:xo - 2 + n], s.cw[:, j, 0:1], None, op0=ALU.mult)
            for k in range(1, 5):
                P.stt(acc, xin[:, xo - 2 + k:xo - 2 + k + n], s.cw[:, j, k:k + 1], acc, ALU.mult, ALU.add)
            P.act(dst[:, s0:s0 + n], acc, AF.Silu, bias=s.cb[:, j:j + 1])
            P.pop()
    for (s0, n, e0, is_own) in regions:
        for (c, m) in col_tiles(e0, n):
            P.push()
            ht = load_h(g, c, m)
            so = s0 + (c - e0)
            for j in range(4):
                ps = g.pbank()
                proj(g, ps[:, 0:m], wz, j * 128, ht, m)
                P.act(s.gz[:, j, so:so + m], ps[:, 0:m], AF.Silu)
            for cc in range(m // 128):
                ps = g.pbank()
                for k in range(8):
                    P.mm(ps[:, 0:16], ht[:, k, cc * 128:(cc + 1) * 128], wdt[:, k, :], start=(k == 0), stop=(k == 7))
                ci = (so + cc * 128) // 128
                P.tt(s.dt[:, ci, :], ps[:, 0:16], s.dtb, ALU.add)
            P.pop()
    P.pop()
    P.act(s.dt, s.dt, AF.Exp)
    P.act(s.dt, s.dt, AF.Ln, bias=1.0)
    P.tt(s.dta, s.dt, V(s.ab, [[0, NCX + NC], [1, 16]]), ALU.mult)


def ssd_chunk(g, c, want_y, hin_f, hin_b, ybuf=None, sdirs=(0, 1)):
    P = g.P
    s = g.ssd
    cs = slice(c * 128, (c + 1) * 128)
    ut = g.c["ut"]; lt = g.c["lt"]
    xs_tok = P.sb([128, 8, 64], BF16); bm_tok = P.sb([128, 2, 128], BF16)
    pt = g.pbank_bf()
    for b in range(4):
        P.transpose(pt[:, b * 128:(b + 1) * 128], s.xsT[:, b, cs], g.ident_bf)
    P.copy(V(xs_tok, [[1, 512]]), pt[:, 0:512], eng="act")
    pt2 = g.pbank_bf()
    for b in range(2):
        P.transpose(pt2[:, b * 128:(b + 1) * 128], s.bmT[:, b, cs], g.ident_bf)
    P.copy(V(bm_tok, [[1, 256]]), pt2[:, 0:256], eng="act")
    pc = g.pbank()
    P.mm(pc[:, 0:8], ut, s.dta[:, c, 0:8])
    P.mm(pc[:, 8:16], lt, s.dta[:, c, 8:16])
    nacum = P.sb([128, 16])
    P.ts(nacum, pc[:, 0:16], -1.0, None, op0=ALU.mult)
    acb = [None] * 4
    adirs = (0, 1) if want_y else sdirs
    for dr_ in adirs:
        m = ut if dr_ == 0 else lt
        for hq in range(2):
            rb = P.sb([128, 4, 128])
            P.tt(rb, V(m, [[0, 4], [1, 128]]), V(s.dta[:, c, dr_ * 8 + hq * 4:dr_ * 8 + hq * 4 + 4], [[1, 4], [0, 128]]), ALU.mult,
                 eng="pool")
            pa = g.pbank()
            P.mm(V(pa, [[1, 512]]), g.c["ones_f"], V(rb, [[1, 512]]))
            pas = P.sb([128, 512])
            P.copy(pas, pa, eng="act" if hq else "dve")
            acb[dr_ * 2 + hq] = pas
    tot = P.sb([128, 16])
    if len(adirs) < 2:
        P.memset(tot, 0.0)
    for dr_ in adirs:
        for hq in range(2):
            pa = acb[dr_ * 2 + hq]
            col = 127 if dr_ == 0 else 0
            P.copy(tot[:, dr_ * 8 + hq * 4:dr_ * 8 + hq * 4 + 4], V(pa, [[128, 4]], off=col))
    w = P.sb([128, 16]); cd = P.sb([128, 16])
    P.tt(w, tot, nacum, ALU.add)
    P.act(w, w, AF.Exp)
    P.tt(w, w, s.dt[:, c, :], ALU.mult)
    P.act(cd, tot, AF.Exp)
    S = [None, None]
    for dr_ in sdirs:
        xsw = P.sb([128, 8, 64], BF16)
        P.tt(xsw, xs_tok, V(w[:, dr_ * 8:dr_ * 8 + 8], [[1, 8], [0, 64]]), ALU.mult)
        pS = g.pbank()
        for h in range(8):
            P.mm(pS[:, h * 64:(h + 1) * 64], bm_tok[:, h // 4, :], xsw[:, h, :])
        Ss = P.sb([128, 512])
        P.copy(Ss, pS, eng="act")
        S[dr_] = Ss
    if not want_y:
        return S, cd, tot
    cbm = []
    for gq in range(2):
        pcb = g.pbank()
        P.mm(pcb[:, 0:128], s.bmT[:, gq, cs], s.cmT[:, gq, cs])
        cf = P.sb([128, 128]); cbk = P.sb([128, 128])
        P.tt(cf, pcb[:, 0:128], ut, ALU.mult)
        P.tt(cbk, pcb[:, 0:128], lt, ALU.mult)
        cbm.append((cf, cbk))
    ring_e = [P.sb([128, 128]) for _ in range(4)]; ring_d = [P.sb([128, 128]) for _ in range(4)]
    ring_w = [P.sb([128, 128], BF16) for _ in range(4)]; ring_c = [P.sb([128, 128], BF16) for _ in range(4)]
    ri = 0
    for pair in range(4):
        py = g.pbank()
        for hh in range(2):
            h = pair * 2 + hh
            gq = h // 4
            hq, hi4 = h // 4, h % 4
            mms = []
            for dr_ in range(2):
                pa = acb[dr_ * 2 + hq]
                ri += 1
                e1 = ring_e[ri % 4]
                P.ts(e1, pa[:, hi4 * 128:(hi4 + 1) * 128], nacum[:, dr_ * 8 + h:dr_ * 8 + h + 1], g.zero_col, op0=ALU.add, op1=ALU.min)
                P.act(e1, e1, AF.Exp)
                wt = ring_w[ri % 4]
                P.stt(wt, e1, s.dt[:, c, dr_ * 8 + h:dr_ * 8 + h + 1], cbm[gq][dr_], ALU.mult, ALU.mult)
                mms.append((xs_tok[:, h, :], wt))
                hin = hin_f if dr_ == 0 else hin_b
                if hin is not None:
                    dec = ring_d[ri % 4]
                    P.act(dec, pa[:, hi4 * 128:(hi4 + 1) * 128], AF.Exp)
                    csd = ring_c[ri % 4]
                    P.tt(csd, s.cmT[:, gq, cs], dec, ALU.mult, eng="pool")
                    mms.append((hin[:, h, :], csd))
            for i_, (l_, r_) in enumerate(mms):
                P.mm(py[hh * 64:(hh + 1) * 64, 0:128], l_, r_, start=(i_ == 0), stop=(i_ == len(mms) - 1))
        P.stt(ybuf[:, pair, :], s.xsT[:, pair, cs], s.dcol[:, pair:pair + 1], py[:, 0:128], ALU.mult, ALU.add)
    return S, cd, tot


def ssd_run(g, NCX, NC, multi=False, yb_cb=None):
    P = g.P
    s = g.ssd
    NCH = NCX + NC
    hf = P.sb([128, 512]); hb = P.sb([128, 512])
    hbf = P.sb([128, 8, 64], BF16)
    it1 = 0; it2 = 0
    for (c0, n) in ((0, NCX), (NCX, NC)):
        if c0 == 0:
            P.memset(hb, 0.0)
        elif multi:
            P.push(); tmpc = P.sb([128, 512]); ssd_carry(g, 1, hb, tmpc); P.copy(hb, tmpc); P.pop()
        for c in range(c0 + n - 1, c0 - 1, -1):
            P.copy(V(hbf, [[1, 512]]), hb)
            P.dma(DV(g.ssd_hb_d, c * 128 * 512, [[512, 128], [1, 512]]), V(hbf, [[1, 512]]))
            P.iter_push(it1, "ssd1")
            S, cd, _t = ssd_chunk(g, c, False, None, None, sdirs=(1,))
            P.tt(V(hb, [[64, 8], [1, 64]]), V(hb, [[64, 8], [1, 64]]), V(cd[:, 8:16], [[1, 8], [0, 64]]), ALU.mult)
            P.tt(hb, hb, S[1], ALU.add)
            P.iter_pop(it1, "ssd1"); it1 += 1
    hfb = P.sb([128, 8, 64], BF16); hbb = P.sb([128, 8, 64], BF16)
    ybuf = P.sb([128, 4, 128])
    for (c0, n) in ((0, NCX), (NCX, NC)):
        if c0 == 0:
            P.memset(hf, 0.0)
        elif multi:
            P.push(); tmpc = P.sb([128, 512]); ssd_carry(g, 0, hf, tmpc); P.copy(hf, tmpc); P.pop()
        for c in range(c0, c0 + n):
            P.copy(V(hfb, [[1, 512]]), hf)
            P.dma(V(hbb, [[1, 512]]), DV(g.ssd_hb_d, c * 128 * 512, [[512, 128], [1, 512]]))
            P.iter_push(it2, "ssd2")
            S, cd, _t = ssd_chunk(g, c, True, hfb, hbb, ybuf, sdirs=(0,))
            P.tt(V(hf, [[64, 8], [1, 64]]), V(hf, [[64, 8], [1, 64]]), V(cd[:, 0:8], [[1, 8], [0, 64]]), ALU.mult)
            P.tt(hf, hf, S[0], ALU.add)
            cs = slice(c * 128, (c + 1) * 128)
            yg = P.sb([128, 4, 128]); sq = P.sb([128, 4, 128], BF16)
            P.tt(yg, ybuf, s.gz[:, :, cs], ALU.mult)
            P.tt(sq, yg, yg, ALU.mult, eng="pool")
            pn = g.pbank()
            for b in range(4):
                P.mm(pn[:, 0:128], g.ones_bf, sq[:, b, :], start=(b == 0), stop=(b == 3))
            rstd = P.sb([128, 128])
            P.act(rstd, pn[:, 0:128], AF.Sqrt, scale=1.0 / 512, bias=g.eps_col)
            P.recip(rstd, rstd)
            yo = P.sb([128, 4, 128], BF16)
            for b in range(4):
                P.stt(yo[:, b, :], yg[:, b, :], s.ng[:, b:b + 1], rstd, ALU.mult, ALU.mult)
            yb_cb(c, yo)
            P.iter_pop(it2, "ssd2"); it2 += 1


def attn_run(g, NCX, NC, yc_cb):
    P = g.P
    dr = g.dram
    T = NC * 128
    NL = T + 256
    NLT = NL // 128
    P.push()
    wq = P.sb([128, 8, 512], BF16)
    d = dr["w_in"]
    for r in range(4):
        for hh, head in enumerate((r, 4 + r)):
            P.dma(wq[:, :, r * 128 + hh * 64:r * 128 + hh * 64 + 64],
                  DV(d, C_Q + head * 64, [[IN_W, 128], [128 * IN_W, 8], [1, 64]]))
    wk = load_w(g, "w_in", 0, 1024, C_K, 128)
    wv = load_w(g, "w_in", 0, 1024, C_V, 128)
    pswap = P.sb([128, 128], BF16)
    P.copy(pswap, g.c["pswap"])
    esink = P.sb([128, 4])
    for hh in range(2):
        P.dma(esink[hh * 64:(hh + 1) * 64, :], DV(dr["attn_sink"], hh * 4, [[0, 64], [1, 4]]))
    P.act(esink, esink, AF.Exp)
    mprev = P.sb([128, 128], BF16); mnext = P.sb([128, 128], BF16); mprevL = P.sb([128, 128], BF16); mnextR = P.sb([128, 128], BF16)
    P.copy(mprev, g.c["lt"]); P.copy(mnext, g.c["ut"])
    P.ts(mprevL, g.c["lt"], g.flagL, None, op0=ALU.mult)
    P.ts(mnextR, g.c["ut"], g.flagR, None, op0=ALU.mult)
    import os
    CUT = os.environ.get('ATT_CUT', '')
    if CUT == 'setup':
        P.pop(); return
    NS = (NCX + NC) * 128
    qT = P.sb([128, 4, NS], BF16)
    kT = P.sb([128, NL + 256], BF16)
    vtok = P.sb([128, NLT + 2, 128], BF16)

    def rope(dst, ps, n, ecol):
        if 'norope' in CUT:
            P.copy(dst, ps); return
        P.push()
        cs_ = P.sb([128, n]); sn_ = P.sb([128, n]); xb = P.sb([128, n], BF16); t1 = P.sb([128, n])
        if 'nodma' in CUT:
            P.memset(cs_, 1.0); P.memset(sn_, 0.0)
        else:
            P.dma(cs_, DV(dr["rope_cos"], ecol, [[NL, 128], [1, n]]))
            P.dma(sn_, DV(dr["rope_sin"], ecol, [[NL, 128], [1, n]]))
        if 'dmaonly' in CUT:
            P.tt(dst, ps, cs_, ALU.mult); P.pop(); return
        P.copy(xb, ps)
        p2 = g.pbank()
        P.mm(p2[:, 0:n], pswap, xb)
        P.tt(t1, ps, cs_, ALU.mult)
        t2 = P.sb([128, n])
        P.tt(t2, p2[:, 0:n], sn_, ALU.mult)
        P.tt(dst, t1, t2, ALU.add)
        P.pop()

    for (c, m) in col_tiles(0, NL):
        P.push()
        ht = load_h(g, c, m)
        ps = g.pbank()
        proj(g, ps[:, 0:m], wk, 0, ht, m)
        rope(kT[:, c:c + m], ps[:, 0:m], m, c)
        for cc in range(m // 128):
            if 'nov' in CUT:
                break
            pv = g.pbank()
            for k in range(8):
                P.mm(pv[:, 0:128], ht[:, k, cc * 128:(cc + 1) * 128], wv[:, k, :], start=(k == 0), stop=(k == 7))
            P.copy(vtok[:, (c // 128) + cc, :], pv[:, 0:128], eng="act")
        if 'noq' in CUT:
            P.pop(); continue
        lo = max(c, 128); hi = min(c + m, 128 + T)
        if hi > lo:
            for r in range(4):
                pq = g.pbank()
                proj(g, pq[:, 0:hi - lo], wq, r * 128, ht[:, :, lo - c:hi - c], hi - lo)
                so = NCX * 128 + (lo - 128)
                rope(qT[:, r, so:so + (hi - lo)], pq[:, 0:hi - lo], hi - lo, lo)
        P.pop()
    if 'lat' in CUT:
        P.pop(); return
    for (c, m) in col_tiles(g.e_ctx, NCX * 128):
        P.push()
        ht = load_h(g, c, m)
        so = c - g.e_ctx
        ps = g.pbank()
        proj(g, ps[:, 0:m], wk, 0, ht, m)
        P.copy(kT[:, NL + so:NL + so + m], ps[:, 0:m], eng="act")
        for cc in range(m // 128):
            pv = g.pbank()
            for k in range(8):
                P.mm(pv[:, 0:128], ht[:, k, cc * 128:(cc + 1) * 128], wv[:, k, :], start=(k == 0), stop=(k == 7))
            P.copy(vtok[:, NLT + so // 128 + cc, :], pv[:, 0:128], eng="act")
        for r in range(4):
            pq = g.pbank()
            proj(g, pq[:, 0:m], wq, r * 128, ht, m)
            P.copy(qT[:, r, so:so + m], pq[:, 0:m], eng="act")
        P.pop()
    po = g._pb_o; pd = g._pb_d
    import os
    for qb in range(NCX + NC):
        if os.environ.get('ATT_CUT') == 'proj':
            break
        if qb < NCX:
            tiles = [(NLT + 0, NL + 0, None), (NLT + 1, NL + 128, None)]
        else:
            n = qb - NCX
            tiles = [(n, n * 128, mprevL if n == 0 else mprev), (n + 1, (n + 1) * 128, None),
                     (n + 2, (n + 2) * 128, mnextR if n == NC - 1 else mnext),
                     (NLT + 0, NL + 0, None), (NLT + 1, NL + 128, None)]
        P.iter_push(qb, "attq")
        nt = len(tiles)
        for hk in range(2):
            h = slice(hk * 64, (hk + 1) * 64)
            for ti, (kt, kcol, mask) in enumerate(tiles):
                ps = g.pbank()
                P.mm(ps[:, 0:512], kT[h, kcol:kcol + 128], V(qT[h], [[NS, 4], [1, 128]], off=qb * 128))
                ex = P.sb([128, 4, 128], BF16)
                P.act(V(ex, [[1, 512]]), ps, AF.Exp, scale=0.125)
                if mask is not None:
                    P.tt(ex, ex, V(mask, [[0, 4], [1, 128]]), ALU.mult, eng="pool")
                P.mm(po[h, :], vtok[:, kt, h], V(ex, [[1, 512]]), start=(ti == 0), stop=(ti == nt - 1))
                P.mm(pd[h, :], g.ones_bf[:, 0:64], V(ex, [[1, 512]]), start=(ti == 0), stop=(ti == nt - 1))
        rd = P.sb([128, 4, 128])
        for r in range(4):
            P.ts(rd[:, r, :], pd[:, r * 128:(r + 1) * 128], esink[:, r:r + 1], None, op0=ALU.add)
        P.recip(V(rd, [[1, 512]]), V(rd, [[1, 512]]))
        yo = P.sb([128, 4, 128], BF16)
        P.tt(V(yo, [[1, 512]]), po[:, :], V(rd, [[1, 512]]), ALU.mult)
        yc_cb(qb, yo)
        P.iter_pop(qb, "attq")
    P.pop()


def rms_rstd(g, xt, n, rstd):
    P = g.P
    P.push()
    sq = P.sb([128, 8, n], BF16)
    P.act(sq, xt, AF.Square)
    ps = g.pbank()
    for k in range(8):
        P.mm(ps[:, 0:n], g.ones_bf, sq[:, k, :], start=(k == 0), stop=(k == 7))
    P.act(rstd, ps[:, 0:n], AF.Sqrt, scale=1.0 / D, bias=g.eps_col)
    P.recip(rstd, rstd)
    P.pop()


def mod_norm(g, xt, n, acol, bcol, out, v):
    P = g.P
    P.push()
    rstd = P.sb([128, n])
    rms_rstd(g, xt, n, rstd)
    tmp = P.sb([128, n])
    for k in range(8):
        P.stt(tmp, xt[:, k, :], acol[:, k, v:v + 1], rstd, ALU.mult, ALU.mult)
        P.act(out[:, k, :], tmp, AF.Identity, bias=bcol[:, k, v:v + 1])
    P.pop()


def load_mod(g):
    P = g.P
    m = P.sb([128, 48, 2])
    P.dma(m, g.dram["modT"])
    g.mod = m
    n1 = P.sb([128, 8]); n2 = P.sb([128, 8])
    lst = []
    if "norm1_g" in g.dram:
        P.dma(n1, DV(g.dram["norm1_g"], 0, [[1, 128], [128, 8]]), allow_slow_non_contiguous=True)
        g.a1 = P.sb([128, 8, 2]); lst.append((g.a1, n1, 1))
    if "norm2_g" in g.dram:
        P.dma(n2, DV(g.dram["norm2_g"], 0, [[1, 128], [128, 8]]), allow_slow_non_contiguous=True)
        g.a2 = P.sb([128, 8, 2]); lst.append((g.a2, n2, 4))
    for (a, nn, j) in lst:
        P.ts(a, m[:, j * 8:(j + 1) * 8, :], 1.0, None, op0=ALU.add)
        P.tt(a, a, V(nn, [[1, 8], [0, 2]]), ALU.mult)
    g.b1 = m[:, 0:8, :]; g.b2 = m[:, 24:32, :]
    g.g1 = m[:, 16:24, :]; g.g2 = m[:, 40:48, :]


def router_aff(g, h2f, n, wr, aff_out):
    P = g.P
    for blk in range(n // 128):
        ps = g.pbank()
        for k in range(8):
            P.mm(ps[:, 0:16], h2f[:, k, blk * 128:(blk + 1) * 128], wr[:, k, :], start=(k == 0), stop=(k == 7))
        P.push()
        mx = P.sb([128, 1]); sm = P.sb([128, 1]); ex = P.sb([128, 16])
        P.reduce(mx, ps[:, 0:16], op=ALU.max)
        P.ts(mx, mx, -1.0, None, op0=ALU.mult)
        P.act(ex, ps[:, 0:16], AF.Exp, bias=mx, accum_out=sm)
        P.recip(sm, sm)
        P.ts(aff_out[:, blk, :], ex, sm, None, op0=ALU.mult)
        P.pop()


def s_tiles(NCX, NC, step=512):
    out = [(c, m, True) for (c, m) in col_tiles(0, NCX * 128, step)]
    out += [(c, m, False) for (c, m) in col_tiles(NCX * 128, NC * 128, step)]
    return out


def s2e(g, NCX, s0, is_ctx):
    return g.e_ctx + s0 if is_ctx else g.e_own + (s0 - NCX * 128)


def merge_run(g, NCX, NC):
    P = g.P
    dr = g.dram
    NS = (NCX + NC) * 128
    P.push()
    macc = P.sb([128, 8, NS], BF16)
    for kbr in range(3):
        P.push()
        wg = load_w(g, "w_in", 0, 1024, C_G + kbr * 1024, 1024)
        wb = P.sb([128, 4, 1024], BF16)
        d = dr["w_branch"]
        if kbr < 2:
            P.dma(wb, DV(d, kbr * 512 * 1024, [[1024, 128], [128 * 1024, 4], [1, 1024]]))
        else:
            for r in range(4):
                for hh, head in enumerate((r, 4 + r)):
                    P.dma(wb[hh * 64:(hh + 1) * 64, r, :], DV(d, (2 * 512 + head * 64) * 1024, [[1024, 64], [1, 1024]]))
        yd = g.y_d[kbr]
        for (s0, n, is_ctx) in s_tiles(NCX, NC):
            P.push()
            ht = load_h(g, s2e(g, NCX, s0, is_ctx), n)
            yt = P.sb([128, 4, n], BF16)
            P.dma(yt, DV(yd, s0, [[NS, 128], [128 * NS, 4], [1, n]]))
            for j in range(8):
                pg = g.pbank()
                proj(g, pg[:, 0:n], wg, j * 128, ht, n)
                gt = P.sb([128, n])
                P.act(gt, pg[:, 0:n], AF.Sigmoid)
                pb = g.pbank()
                for cc in range(4):
                    P.mm(pb[:, 0:n], wb[:, cc, j * 128:(j + 1) * 128], yt[:, cc, :], start=(cc == 0), stop=(cc == 3))
                if kbr == 0:
                    P.tt(macc[:, j, s0:s0 + n], gt, pb[:, 0:n], ALU.mult)
                else:
                    P.tt(gt, gt, pb[:, 0:n], ALU.mult)
                    P.tt(macc[:, j, s0:s0 + n], macc[:, j, s0:s0 + n], gt, ALU.add, eng="pool")
            P.pop()
        P.pop()
    wo = load_w(g, "w_out", 0, 1024, 0, 1024)
    wr = load_w(g, "w_router", 0, 1024, 0, 16, dtype=F32)
    for (s0, n, is_ctx) in s_tiles(NCX, NC):
        v = 1 if is_ctx else 0
        P.push()
        xt = P.sb([128, 8, n])
        xsrc = g.dram["xcT"] if is_ctx else g.dram["xT"]
        xw = NCX * 128 if is_ctx else NC * 128 + 256
        xo = s0 if is_ctx else (s0 - NCX * 128) + 128
        P.dma(xt, DV(xsrc, xo, [[xw, 128], [128 * xw, 8], [1, n]]))
        for j in range(8):
            po = g.pbank()
            for k in range(8):
                P.mm(po[:, 0:n], wo[:, k, j * 128:(j + 1) * 128], macc[:, k, s0:s0 + n], start=(k == 0), stop=(k == 7))
            P.stt(xt[:, j, :], po[:, 0:n], g.g1[:, j, v:v + 1], xt[:, j, :], ALU.mult, ALU.add)
        P.dma(DV(g.dram["x1T"], s0, [[NS, 128], [128 * NS, 8], [1, n]]), xt)
        h2 = P.sb([128, 8, n])
        mod_norm(g, xt, n, g.a2, g.b2, h2, v)
        aff = P.sb([128, n // 128, 16])
        router_aff(g, h2, n, wr, aff)
        P.dma(DV(g.dram["aff"], s0 * 16, [[16, 128], [128 * 16, n // 128], [1, 16]]), aff)
        P.pop()
    P.pop()


B_INPUTS = [("s5_lam_re", [2, 32, 64]), ("s5_lam_im", [2, 32, 64]), ("s5_log_dt", [2, 32]), ("s5_b_re", [2, 32, 64, 16]),
            ("s5_b_im", [2, 32, 64, 16]), ("s5_c_re", [2, 32, 16, 64]), ("s5_c_im", [2, 32, 16, 64]), ("s5_d", [512]),
            ("s5_b_glu", [512]), ("ssd_conv_w", [5, 1024]), ("ssd_conv_b", [1024]), ("ssd_a_log", [2, 8]),
            ("ssd_dt_bias", [2, 8]), ("ssd_d", [8]), ("ssd_norm_g", [512]), ("attn_sink", [8]), ("norm1_g", [1024]),
            ("norm2_g", [1024]), ("w_router", [1024, 16]), ("modT", [128, 48, 2]), ("flags", [128, 2])]
B_INPUTS_BF = [("w_in", [1024, IN_W]), ("s5_w_glu", [512, 512]), ("w_branch", [3 * 512, 1024]), ("w_out", [1024, 1024])]


def declare_inputs(g, lst, dt):
    for name, shp in lst:
        g.dram[name] = g.nc.dram_tensor(name, shp, dt, kind="ExternalInput").ap()


def phase0_h(g, NCX, NC):
    P = g.P
    T = NC * 128
    for (c, m, is_ctx) in [(c, m, False) for (c, m) in col_tiles(0, T + 256)] + [(c, m, True) for (c, m) in col_tiles(0, NCX * 128)]:
        P.push()
        xt = P.sb([128, 8, m])
        src = g.dram["xcT"] if is_ctx else g.dram["xT"]
        xw = NCX * 128 if is_ctx else T + 256
        P.dma(xt, DV(src, c, [[xw, 128], [128 * xw, 8], [1, m]]))
        ht = P.sb([128, 8, m], BF16)
        mod_norm(g, xt, m, g.a1, g.b1, ht, 1 if is_ctx else 0)
        e0 = (g.e_ctx + c) if is_ctx else c
        P.dma(DV(g.hT_d, e0, [[g.E, 128], [128 * g.E, 8], [1, m]]), ht)
        P.pop()


def s5_phase(g, NCX, NC, carry_fn=None, states_cb=None, states_only=False):
    P = g.P
    dr = g.dram
    NS = (NCX + NC) * 128
    P.push()
    uT = P.sb([128, 4, NS], BF16)
    aT = P.sb([128, 4, NS], BF16)
    P.push()
    wu = load_w(g, "w_in", 0, 1024, C_U, 512)
    for (s0, n, is_ctx) in s_tiles(NCX, NC):
        P.push()
        ht = load_h(g, s2e(g, NCX, s0, is_ctx), n)
        for j in range(4):
            ps = g.pbank()
            proj(g, ps[:, 0:n], wu, j * 128, ht, n)
            P.copy(uT[:, j, s0:s0 + n], ps[:, 0:n], eng="act" if j % 2 else "dve")
        P.pop()
    P.pop()
    dcol = P.sb([128, 4]); bglu = P.sb([128, 4])
    P.dma(dcol, DV(dr["s5_d"], 0, [[1, 128], [128, 4]]), allow_slow_non_contiguous=True)
    P.dma(bglu, DV(dr["s5_b_glu"], 0, [[1, 128], [128, 4]]), allow_slow_non_contiguous=True)

    def ya_cb(b, yacc):
        P.stt(yacc, uT[:, b, :], dcol[:, b:b + 1], yacc, ALU.mult, ALU.add)
        P.act(aT[:, b, :], yacc, AF.Gelu)

    if carry_fn is None:
        carry_fn = lambda: (g.s5.fin_re[:, :, 0], g.s5.fin_im[:, :, 0])
    s5_core(g, uT, NCX, NC, carry_fn, ya_cb, states_cb, states_only)
    if states_only:
        P.pop()
        return
    wgl = load_w(g, "s5_w_glu", 0, 512, 0, 512)
    for (s0, n) in col_tiles(0, NS):
        P.push()
        yo = P.sb([128, 4, n], BF16)
        for j in range(4):
            ps = g.pbank()
            for k in range(4):
                P.mm(ps[:, 0:n], wgl[:, k, j * 128:(j + 1) * 128], aT[:, k, s0:s0 + n], start=(k == 0), stop=(k == 3))
            gt = P.sb([128, n])
            P.act(gt, ps[:, 0:n], AF.Sigmoid, bias=bglu[:, j:j + 1])
            P.tt(yo[:, j, :], gt, aT[:, j, s0:s0 + n], ALU.mult)
        P.dma(DV(g.y_d[0], s0, [[NS, 128], [128 * NS, 4], [1, n]]), yo)
        P.pop()
    P.pop()


def build_B(T, debug=False, multi=False, mode="B"):
    nc = bass.Bass("TRN2", target_bir_lowering=False)
    g = G(); g.nc = nc; g.P = Prog(nc); P = g.P
    P.init_arenas(18 * 1024, 62 * 1024)
    NCX = 2; NC = T // 128; NS = (NCX + NC) * 128
    g.E = T + 512; g.e_own = 128; g.e_ctx = T + 256
    g.dram = {}
    consts = dict(CONST_SHAPES); consts["pswap"] = [128, 128]
    declare_inputs(g, list(consts.items()), F32)
    declare_inputs(g, B_INPUTS, F32)
    declare_inputs(g, B_INPUTS_BF, BF16)
    declare_inputs(g, [("s5_vr", [128, 32, 128]), ("s5_vi", [128, 32, 128]), ("s5_w2re", [128, 32, 128]), ("s5_nw2im", [128, 32, 128]),
                       ("s5_kt", [4, 2, 2, 128, 1024])], BF16)
    declare_inputs(g, [("xT", [8, 128, T + 256]), ("xcT", [8, 128, 256]), ("rope_cos", [128, T + 256]), ("rope_sin", [128, T + 256])], F32)
    if mode == "B":
        g.dram["x1T"] = nc.dram_tensor("x1T", [8, 128, NS], F32, kind="ExternalOutput").ap()
        g.dram["aff"] = nc.dram_tensor("aff", [NS, 16], F32, kind="ExternalOutput").ap()
        if multi:
            declare_inputs(g, [("s5_fin_all", [NCORES, 128, 64]), ("ssd_fin_all", [NCORES, 2, 128, 512]), ("ssd_tot_all", [NCORES, 128, 16]),
                               ("onehot", [128, NCORES])], F32)
    else:
        g.dram["s5_fin"] = nc.dram_tensor("s5_fin", [128, 64], F32, kind="ExternalOutput").ap()
        g.dram["ssd_fin"] = nc.dram_tensor("ssd_fin", [2, 128, 512], F32, kind="ExternalOutput").ap()
        g.dram["ssd_tot"] = nc.dram_tensor("ssd_tot", [128, 16], F32, kind="ExternalOutput").ap()
    g.hT_d = nc.dram_tensor("hT_scr", [8, 128, g.E], BF16).ap()
    g.ssd_hb_d = nc.dram_tensor("ssd_hb_scr", [NCX + NC, 128, 512], BF16).ap()
    kind = "ExternalOutput" if debug else "Internal"
    g.y_d = [nc.dram_tensor(f"y{k}_scr", [4, 128, NS], BF16, kind=kind).ap() for k in range(3)]
    setup_psum(g)
    CONST_SHAPES2 = consts
    g.c = {}
    for k, shp in CONST_SHAPES2.items():
        t = P.sb(shp, F32)
        P.dma(t, g.dram[k])
        g.c[k] = t
    g.ident_bf = P.sb([128, 128], BF16); P.copy(g.ident_bf, g.c["ident"])
    g.ones_bf = P.sb([128, 128], BF16); P.memset(g.ones_bf, 1.0)
    g.zeros_bf = P.sb([128, 128], BF16); P.memset(g.zeros_bf, 0.0)
    g.c["ones_f"] = P.sb([128, 128], F32); P.memset(g.c["ones_f"], 1.0)
    g.eps_col = P.sb([128, 1]); P.memset(g.eps_col, 1e-6)
    g.zero_col = P.sb([128, 1]); P.memset(g.zero_col, 0.0)
    fl = P.sb([128, 2]); P.dma(fl, g.dram["flags"])
    g.flagL = fl[:, 0:1]; g.flagR = fl[:, 1:2]
    import os
    ph = os.environ.get("PH", "s5,ssd,att,merge").split(",")
    load_mod(g)
    phase0_h(g, NCX, NC)
    if multi and mode == "B":
        g.onehot = P.sb([128, NCORES]); P.dma(g.onehot, g.dram["onehot"])
    if mode == "A":
        def dump_fin():
            t_ = P.sb([128, 32, 2])
            P.copy(t_[:, :, 0], g.s5.fin_re[:, :, 1]); P.copy(t_[:, :, 1], g.s5.fin_im[:, :, 1])
            P.dma(g.dram["s5_fin"], V(t_, [[1, 64]]))
        s5_phase(g, NCX, NC, states_cb=dump_fin, states_only=True)
        P.push()
        ssd_prep(g, NCX, NC)
        ssd_local_finals(g, NCX, NC)
        P.pop()
        P.wait_all(); P.emit(); P.close()
        return nc, g
    if "s5" in ph:
        s5_phase(g, NCX, NC, carry_fn=(lambda: s5_carry_chain(g, NC)) if multi else None)
    if "ssd" in ph:
        P.push()
        ssd_prep(g, NCX, NC)
        ssd_run(g, NCX, NC, multi=multi, yb_cb=lambda c, yo: P.dma(DV(g.y_d[1], c * 128, [[NS, 128], [128 * NS, 4], [1, 128]]), yo))
        P.pop()
    if "att" in ph:
        attn_run(g, NCX, NC, lambda qb, yo: P.dma(DV(g.y_d[2], qb * 128, [[NS, 128], [128 * NS, 4], [1, 128]]), yo))
    if "merge" in ph:
        merge_run(g, NCX, NC)
    P.wait_all(); P.emit(); P.close()
    return nc, g


def topk_threshold(g, aff, nblk, cap, tau, iters=30):
    P = g.P
    P.push()
    lo = P.sb([128, 16]); mid = P.sb([128, 16]); cmp_ = P.sb([128, 16, nblk])
    cnt = P.sb([128, 16]); ge = P.sb([128, 16])
    P.memset(lo, 0.0)
    affv = V(aff, [[1, 16], [16, nblk]])
    for it in range(iters):
        cst = 2.0 ** (-(it + 1))
        P.ts(mid, lo, cst, None, op0=ALU.add)
        P.tt(cmp_, affv, V(mid, [[1, 16], [0, nblk]]), ALU.is_ge)
        P.reduce(cnt, cmp_)
        ps = g.pbank()
        P.mm(ps[:, 0:16], g.c["ones_f"], cnt)
        P.ts(ge, ps[:, 0:16], float(cap) - 0.5, cst, op0=ALU.is_ge, op1=ALU.mult)
        P.tt(lo, lo, ge, ALU.add)
    P.copy(tau, lo)
    P.pop()


C_INPUTS = [("norm2_g", [1024]), ("modT", [128, 48, 2]), ("final_norm_g", [1024]), ("ident", [128, 128])]
C_INPUTS_BF = [("w_e_gate", [16 * 1024, 1024]), ("w_e_up", [16 * 1024, 1024]), ("w_e_down", [16 * 1024, 1024])]


def build_C(T, n_total):
    nc = bass.Bass("TRN2", target_bir_lowering=False)
    g = G(); g.nc = nc; g.P = Prog(nc); P = g.P
    P.init_arenas(22 * 1024, 50 * 1024)
    NCX = 2; NC = T // 128; NS = (NCX + NC) * 128
    g.dram = {}
    declare_inputs(g, C_INPUTS, F32)
    declare_inputs(g, C_INPUTS_BF, BF16)
    declare_inputs(g, [("x1T", [8, 128, NS]), ("aff_all", [n_total, 16]), ("aff_own", [NS, 16])], F32)
    x2_d = nc.dram_tensor("x2T", [8, 128, NS], F32, kind="ExternalOutput").ap()
    fin_d = nc.dram_tensor("finT", [8, 128, NS], F32, kind="ExternalOutput").ap()
    setup_psum(g)
    g.c = {}
    g.c["ident"] = P.sb([128, 128]); P.dma(g.c["ident"], g.dram["ident"])
    g.ones_bf = P.sb([128, 128], BF16); P.memset(g.ones_bf, 1.0)
    g.c["ones_f"] = P.sb([128, 128], F32); P.memset(g.c["ones_f"], 1.0)
    g.eps_col = P.sb([128, 1]); P.memset(g.eps_col, 1e-6)
    load_mod(g)
    gfin = P.sb([128, 8]); P.dma(gfin, DV(g.dram["final_norm_g"], 0, [[1, 128], [128, 8]]), allow_slow_non_contiguous=True)
    tau = P.sb([128, 16]); tauc = P.sb([128, 16])
    nblk = n_total // 128
    P.push()
    affa = P.sb([128, nblk, 16])
    P.dma(affa, DV(g.dram["aff_all"], 0, [[16, 128], [2048, nblk], [1, 16]]))
    topk_threshold(g, affa, nblk, 2 * n_total // 16, tau)
    P.pop()
    tiles = [(c, m, True) for (c, m) in col_tiles(0, NCX * 128, 256)] + [(c, m, False) for (c, m) in col_tiles(NCX * 128, T, 512)]
    GROUP = 3
    groups = [tiles[i:i + GROUP] for i in range(0, len(tiles), GROUP)]
    affc = P.sb([128, NCX, 16])
    P.dma(affc, DV(g.dram["aff_own"], 0, [[16, 128], [2048, NCX], [1, 16]]))
    topk_threshold(g, affc, NCX, 2 * NCX * 128 // 16, tauc)
    for grp in groups:
        P.push()
        ncols = sum(n for (_, n, _) in grp)
        gs0 = grp[0][0]
        yacc = P.sb([128, 8, ncols])
        h2b = P.sb([128, 8, ncols], BF16)
        coef = P.sb([128, ncols // 128, 16])
        xres = P.sb([128, 8, ncols]) if False else None
        for (s0, n, is_ctx) in grp:
            P.push()
            o = s0 - gs0
            xt = P.sb([128, 8, n]); P.dma(xt, DV(g.dram["x1T"], s0, [[NS, 128], [128 * NS, 8], [1, n]]))
            mod_norm(g, xt, n, g.a2, g.b2, h2b[:, :, o:o + n], 1 if is_ctx else 0)
            aff = P.sb([128, n // 128, 16])
            P.dma(aff, DV(g.dram["aff_own"], s0 * 16, [[16, 128], [2048, n // 128], [1, 16]]))
            tb = V(tauc if is_ctx else tau, [[0, n // 128], [1, 16]])
            msk = P.sb([128, n // 128, 16])
            P.tt(msk, aff, tb, ALU.is_ge)
            P.tt(coef[:, o // 128:(o + n) // 128, :], aff, msk, ALU.mult)
            P.pop()
        for e in range(16):
            P.push()
            wg = load_w(g, "w_e_gate", e * 1024, 1024, 0, 1024)
            wu = load_w(g, "w_e_up", e * 1024, 1024, 0, 1024)
            wd = load_w(g, "w_e_down", e * 1024, 1024, 0, 1024)
            for (s0, n, is_ctx) in grp:
                P.push()
                o = s0 - gs0
                pcb = g.pbank()
                for blk in range(n // 128):
                    cb_ = coef[:, o // 128 + blk, e:e + 1]
                    P.mm(pcb[:, blk * 128:(blk + 1) * 128], V(cb_, [[0, 128]]), g.c["ident"])
                cbs = P.sb([128, n])
                P.copy(cbs, pcb[:, 0:n], eng="act")
                hid = P.sb([128, 8, n], BF16)
                for f in range(8):
                    pg = g.pbank(); pu = g.pbank()
                    for k in range(8):
                        P.mm(pg[:, 0:n], wg[:, k, f * 128:(f + 1) * 128], h2b[:, k, o:o + n], start=(k == 0), stop=(k == 7))
                    for k in range(8):
                        P.mm(pu[:, 0:n], wu[:, k, f * 128:(f + 1) * 128], h2b[:, k, o:o + n], start=(k == 0), stop=(k == 7))
                    sg = P.sb([128, n])
                    P.act(sg, pg[:, 0:n], AF.Silu)
                    P.tt(sg, sg, pu[:, 0:n], ALU.mult)
                    P.tt(hid[:, f, :], sg, cbs, ALU.mult, eng="pool")
                for j in range(8):
                    pd_ = g.pbank()
                    for f in range(8):
                        P.mm(pd_[:, 0:n], wd[:, f, j * 128:(j + 1) * 128], hid[:, f, :], start=(f == 0), stop=(f == 7))
                    if e == 0:
                        P.copy(yacc[:, j, o:o + n], pd_[:, 0:n], eng="act")
                    else:
                        P.tt(yacc[:, j, o:o + n], yacc[:, j, o:o + n], pd_[:, 0:n], ALU.add)
                P.pop()
            P.pop()
        for (s0, n, is_ctx) in grp:
            P.push()
            o = s0 - gs0
            v = 1 if is_ctx else 0
            xt = P.sb([128, 8, n]); P.dma(xt, DV(g.dram["x1T"], s0, [[NS, 128], [128 * NS, 8], [1, n]]))
            for j in range(8):
                P.stt(xt[:, j, :], yacc[:, j, o:o + n], g.g2[:, j, v:v + 1], xt[:, j, :], ALU.mult, ALU.add)
            P.dma(DV(x2_d, s0, [[NS, 128], [128 * NS, 8], [1, n]]), xt)
            rstd = P.sb([128, n])
            rms_rstd(g, xt, n, rstd)
            for j in range(8):
                P.stt(xt[:, j, :], xt[:, j, :], gfin[:, j:j + 1], rstd, ALU.mult, ALU.mult)
            P.dma(DV(fin_d, s0, [[NS, 128], [128 * NS, 8], [1, n]]), xt)
            P.pop()
        P.pop()
    P.wait_all(); P.emit(); P.close()
    return nc, g


NCORES = 8


def s5_carry_chain(g, NC):
    P = g.P
    s = g.s5
    NCX = 2
    dre = P.sb([128, 32]); dim_ = P.sb([128, 32]); t1 = P.sb([128, 32]); t2 = P.sb([128, 32])
    for half, last in ((slice(0, 64), NCX + NC - 1), (slice(64, 128), NCX)):
        cmul_acc(P, dre[half], dim_[half], s.aqr[half], s.aqi[half], s.apow_re[half, :, last], s.apow_im[half, :, last],
                 None, None, t1[half], t2[half])
    fin = P.sb([128, NCORES, 32, 2])
    P.dma(fin, DV(g.dram["s5_fin_all"], 0, [[64, 128], [128 * 64, NCORES], [1, 64]]))
    H = P.sb([128, NCORES + 1, 32, 2])
    P.copy(H[:, 0, :, 0], s.fin_re[:, :, 0]); P.copy(H[:, 0, :, 1], s.fin_im[:, :, 0])
    for t in range(NCORES):
        for half, m in ((slice(0, 64), t), (slice(64, 128), NCORES - 1 - t)):
            cmul_acc(P, H[half, t + 1, :, 0], H[half, t + 1, :, 1], dre[half], dim_[half], H[half, t, :, 0], H[half, t, :, 1],
                     fin[half, m, :, 0], fin[half, m, :, 1], t1[half], t2[half])
    cr = P.sb([128, 32]); ci = P.sb([128, 32])
    P.memset(cr, 0.0); P.memset(ci, 0.0)
    for t in range(NCORES):
        for half, oh in ((slice(0, 64), g.onehot[0:64, t:t + 1]), (slice(64, 128), g.onehot[64:128, NCORES - 1 - t:NCORES - t])):
            P.stt(cr[half], H[half, t, :, 0], oh, cr[half], ALU.mult, ALU.add)
            P.stt(ci[half], H[half, t, :, 1], oh, ci[half], ALU.mult, ALU.add)
    return cr, ci


def ssd_carry(g, dr_, hctx, out):
    P = g.P
    P.push()
    tot = P.sb([128, NCORES, 16])
    P.dma(tot, DV(g.dram["ssd_tot_all"], 0, [[16, 128], [128 * 16, NCORES], [1, 16]]))
    P.act(tot, tot, AF.Exp)
    H = P.sb([128, 512]); fin = P.sb([128, 512])
    P.copy(H, hctx)
    P.memset(out, 0.0)
    for t in range(NCORES):
        m = t if dr_ == 0 else NCORES - 1 - t
        P.stt(out, H, g.onehot[:, m:m + 1], out, ALU.mult, ALU.add)
        if t == NCORES - 1:
            break
        P.dma(fin, DV(g.dram["ssd_fin_all"], (m * 2 + dr_) * 128 * 512, [[512, 128], [1, 512]]))
        P.tt(V(H, [[64, 8], [1, 64]]), V(H, [[64, 8], [1, 64]]), V(tot[:, m, dr_ * 8:dr_ * 8 + 8], [[1, 8], [0, 64]]), ALU.mult)
        P.tt(H, H, fin, ALU.add)
    P.pop()


def ssd_local_finals(g, NCX, NC):
    P = g.P
    hf = P.sb([128, 512]); hb = P.sb([128, 512]); ts_ = P.sb([128, 16]); pb = P.sb([128, 8]); tmp = P.sb([128, 512])
    P.memset(hf, 0.0); P.memset(hb, 0.0); P.memset(ts_, 0.0); P.memset(pb, 1.0)
    for i in range(NC):
        c = NCX + i
        P.push()
        S, cd, tot = ssd_chunk(g, c, False, None, None)
        P.tt(V(hf, [[64, 8], [1, 64]]), V(hf, [[64, 8], [1, 64]]), V(cd[:, 0:8], [[1, 8], [0, 64]]), ALU.mult)
        P.tt(hf, hf, S[0], ALU.add)
        P.tt(V(tmp, [[64, 8], [1, 64]]), V(S[1], [[64, 8], [1, 64]]), V(pb, [[1, 8], [0, 64]]), ALU.mult)
        P.tt(hb, hb, tmp, ALU.add)
        P.tt(pb, pb, cd[:, 8:16], ALU.mult)
        P.tt(ts_, ts_, tot, ALU.add)
        P.pop()
    P.dma(g.dram["ssd_fin"][0], hf); P.dma(g.dram["ssd_fin"][1], hb); P.dma(g.dram["ssd_tot"], ts_)


W_LIST = [("w_in", 4 * 1024, IN_W), ("s5_w_glu", 4 * 512, 512), ("w_branch", 4 * 1536, 1024), ("w_out", 4 * 1024, 1024),
          ("w_e_gate", 4 * 16 * 1024, 1024), ("w_e_up", 4 * 16 * 1024, 1024), ("w_e_down", 4 * 16 * 1024, 1024)]


def build_W(wlist=W_LIST):
    nc = bass.Bass("TRN2", target_bir_lowering=False)
    g = G(); g.nc = nc; g.P = Prog(nc); P = g.P
    P.init_arenas(24 * 1024, 24 * 1024)
    setup_psum(g)
    g.dram = {}
    engs = ["dve", "act", "pool"]
    ei = 0
    for (name, rows, cols) in wlist:
        rpc = rows // NCORES
        assert rpc % 128 == 0
        src = nc.dram_tensor(name, [rpc, cols], F32, kind="ExternalInput").ap()
        dst = nc.dram_tensor(name + "_bf", [rpc, cols], BF16, kind="ExternalOutput").ap()
        rt = rpc // 128
        cc = cols
        while cc > 2048:
            cc //= 2
        rstep = max(1, 4096 // cc)
        for c0 in range(0, cols, cc):
            for r0 in range(0, rt, rstep):
                r = min(rstep, rt - r0)
                P.push()
                a = P.sb([128, r, cc]); b = P.sb([128, r, cc], BF16)
                P.dma(a, DV(src, r0 * 128 * cols + c0, [[cols, 128], [128 * cols, r], [1, cc]]))
                P.copy(b, a, eng=engs[ei % 3]); ei += 1
                P.dma(DV(dst, r0 * 128 * cols + c0, [[cols, 128], [128 * cols, r], [1, cc]]), b)
                P.pop()
    g.NG = 16
    declare_inputs(g, [("s5_lam_re", [2, 16, 64]), ("s5_lam_im", [2, 16, 64]), ("s5_log_dt", [2, 16]), ("s5_b_re", [2, 16, 64, 16]),
                       ("s5_b_im", [2, 16, 64, 16]), ("s5_c_re", [2, 16, 16, 64]), ("s5_c_im", [2, 16, 16, 64]),
                       ("exl", [128, 128]), ("exr", [128, 128]), ("exv", [128, 2])], F32)
    g.c = {}
    for k_ in ("exl", "exr", "exv"):
        t_ = P.sb(CONST_SHAPES[k_], F32); P.dma(t_, g.dram[k_]); g.c[k_] = t_
    outs = {nm: nc.dram_tensor(nm, [128, 16, 128], BF16, kind="ExternalOutput").ap() for nm in ("s5_vr", "s5_vi", "s5_w2re", "s5_nw2im")}
    kt_o = nc.dram_tensor("s5_kt", [2, 2, 2, 128, 1024], BF16, kind="ExternalOutput").ap()
    P.push()
    s5_params(g)
    P.push()
    vr = P.sb([128, 16, 128], BF16); vi = P.sb([128, 16, 128], BF16)
    s5_gen_V(g, V(vr, [[128, 16], [64, 2], [1, 64]]), V(vi, [[128, 16], [64, 2], [1, 64]]))
    P.dma(outs["s5_vr"], vr); P.dma(outs["s5_vi"], vi)
    w2 = P.sb([128, 16, 128], BF16); nw2 = P.sb([128, 16, 128], BF16)
    s5_gen_E(g, g.c["exr"], w2, nw2, neg_im=True)
    P.dma(outs["s5_w2re"], w2); P.dma(outs["s5_nw2im"], nw2)
    P.pop()
    s5_kt_build(g, 2, kt_o)
    P.pop()
    wm = nc.dram_tensor("w_mod", [1024, 6144], F32, kind="ExternalInput").ap()
    bm = nc.dram_tensor("b_mod", [6144], F32, kind="ExternalInput").ap()
    cc_ = nc.dram_tensor("c2", [2, 1024], F32, kind="ExternalInput").ap()
    mo = nc.dram_tensor("modT", [128, 48, 2], F32, kind="ExternalOutput").ap()
    sc = P.sb([128, 8, 2])
    for v in range(2):
        P.dma(sc[:, :, v], DV(cc_, v * 1024, [[1, 128], [128, 8]]), allow_slow_non_contiguous=True)
    P.act(sc, sc, AF.Silu)
    bcol = P.sb([128, 48])
    P.dma(bcol, DV(bm, 0, [[1, 128], [128, 48]]), allow_slow_non_contiguous=True)
    ps = g.pbank()
    for grp in range(12):
        P.push()
        wt = P.sb([128, 8, 512])
        P.dma(wt, DV(wm, grp * 512, [[6144, 128], [128 * 6144, 8], [1, 512]]))
        for q in range(4):
            ccix = grp * 4 + q
            for k in range(8):
                P.mm(ps[:, ccix * 2:ccix * 2 + 2], wt[:, k, q * 128:(q + 1) * 128], sc[:, k, :], start=(k == 0), stop=(k == 7))
        P.pop()
    mt = P.sb([128, 48, 2])
    P.tt(mt, V(ps, [[2, 48], [1, 2]]), V(bcol, [[1, 48], [0, 2]]), ALU.add)
    P.dma(mo, mt)
    P.wait_all(); P.emit(); P.close()
    return nc, g


def setup_psum(g):
    P = g.P
    g._pb = [P.ps([128, 512], F32) for _ in range(4)]
    g._pb_o = P.ps([128, 512], F32)[:, :]
    g._pb_d = P.ps([128, 512], F32)[:, :]
    g._pbf = [P.ps([128, 1024], BF16) for _ in range(2)]
    g._pi = 0
    g._pbi = 0

    def pbank():
        t = g._pb[g._pi % 4]
        g._pi += 1
        return t[:, :]

    def pbank_bf():
        h = g._pbi % 2
        g._pbi += 1
        return g._pbf[h][:, 0:512]
    g.pbank = pbank
    g.pbank_bf = pbank_bf


BF=ml_dtypes.bfloat16

def rope_tables(own_start, T, n_total):
    NL=T+256
    pos=own_start+np.arange(NL)-128
    valid=(pos>=0)&(pos<n_total)
    pos=np.where(valid,pos,0)
    d=np.arange(64); which=d//32; i=d%16; first=(d%32)<16
    inv=10000.0**(-(i.astype(np.float64))/16)
    axis=np.where(which[:,None]==0, (pos//64)[None,:], (pos%64)[None,:]).astype(np.float64)
    ang=axis*inv[:,None]
    cos=np.cos(ang); sin=np.where(first[:,None], -np.sin(ang), np.sin(ang))
    return np.tile(cos,(2,1)).astype(np.float32), np.tile(sin,(2,1)).astype(np.float32)

def pswap():
    m=np.zeros((128,128),np.float32)
    for j in range(128):
        p = j+16 if (j%32)<16 else j-16
        m[p,j]=1.0
    return m

def fm(a, ncols):
    return np.ascontiguousarray(a.T.reshape(8,128,ncols))

def consts_all():
    c=host_consts(); c["pswap"]=pswap(); return c

def modT_from(mod2):
    return np.ascontiguousarray(mod2.reshape(2,48,128).transpose(2,1,0)).astype(np.float32)


_CACHE = {}

def _prog(key, fn):
    if key not in _CACHE:
        _CACHE[key] = fn()[0]
    return _CACHE[key]

def run_model(inputs, T, ncores=NCORES, depth=4, hook=None):
    assert ncores == NCORES
    n = ncores * T
    NS = T + 256
    f32 = lambda a: np.ascontiguousarray(np.asarray(a, dtype=np.float32))
    ncW = _prog(("W",), build_W)
    flat = {"w_in": f32(inputs["w_in"]).reshape(-1, IN_W), "s5_w_glu": f32(inputs["s5_w_glu"]).reshape(-1, 512),
            "w_branch": f32(inputs["w_branch"]).reshape(-1, 1024), "w_out": f32(inputs["w_out"]).reshape(-1, 1024),
            "w_e_gate": f32(inputs["w_e_gate"]).reshape(-1, 1024), "w_e_up": f32(inputs["w_e_up"]).reshape(-1, 1024),
            "w_e_down": f32(inputs["w_e_down"]).reshape(-1, 1024)}
    c2 = np.stack([f32(inputs["c"])[0], f32(inputs["c_ctx"])], 0)
    maps = []
    for k in range(ncores):
        m = {}
        for name, rows, cols in W_LIST:
            rpc = rows // ncores
            m[name] = np.ascontiguousarray(flat[name][k * rpc:(k + 1) * rpc])
        nl = 4
        l = k % nl
        m["w_mod"] = f32(inputs["w_mod"][l]); m["b_mod"] = f32(inputs["b_mod"][l]); m["c2"] = c2
        hf_ = k // nl
        gsl = slice(hf_ * 16, hf_ * 16 + 16)
        for nm in ("s5_lam_re", "s5_lam_im", "s5_log_dt", "s5_b_re", "s5_b_im", "s5_c_re", "s5_c_im"):
            m[nm] = np.ascontiguousarray(f32(inputs[nm][l])[:, gsl])
        hc_ = host_consts()
        for nm in ("exl", "exr", "exv"):
            m[nm] = hc_[nm]
        maps.append(m)
    res = run_bass_kernel_spmd(ncW, maps, core_ids=list(range(ncores))).results
    wbf = {name: np.concatenate([np.asarray(res[k][name + "_bf"]) for k in range(ncores)], 0) for name, _, _ in W_LIST}
    nl = 4
    modT = [np.asarray(res[l]["modT"]) for l in range(nl)]
    s5tab = []
    for l in range(nl):
        d_ = {nm: np.concatenate([np.asarray(res[l][nm]), np.asarray(res[l + nl][nm])], 1) for nm in ("s5_vr", "s5_vi", "s5_w2re", "s5_nw2im")}
        d_["s5_kt"] = np.concatenate([np.asarray(res[l]["s5_kt"]), np.asarray(res[l + nl]["s5_kt"])], 0)
        s5tab.append(d_)
    if hook: hook("W", dict(wbf=wbf, modT=modT))
    consts = consts_all()
    ropes = [rope_tables(k * T, T, n) for k in range(ncores)]
    flags = []
    onehots = []
    for k in range(ncores):
        fl = np.ones((128, 2), np.float32)
        if k == 0: fl[:, 0] = 0
        if k == ncores - 1: fl[:, 1] = 0
        flags.append(fl)
        oh = np.zeros((128, ncores), np.float32); oh[:, k] = 1
        onehots.append(oh)
    x = f32(inputs["x"])[0]
    xc = f32(inputs["ctx"])[0]
    ncA = _prog(("A", T), lambda: build_B(T, multi=True, mode="A"))
    ncB = _prog(("B", T), lambda: build_B(T, multi=True, mode="B"))
    ncC = _prog(("C", T, n), lambda: build_C(T, n))
    fin = None
    for l in range(depth):
        lw = lambda name: f32(inputs[name][l])
        base = dict(consts)
        for name, _ in B_INPUTS:
            if name in inputs: base[name] = lw(name)
        base["modT"] = modT[l]
        base.update(s5tab[l])
        base["w_in"] = wbf["w_in"][l * 1024:(l + 1) * 1024]
        base["s5_w_glu"] = wbf["s5_w_glu"][l * 512:(l + 1) * 512]
        base["w_branch"] = wbf["w_branch"][l * 1536:(l + 1) * 1536]
        base["w_out"] = wbf["w_out"][l * 1024:(l + 1) * 1024]
        xpad = np.concatenate([np.zeros((128, 1024), np.float32), x, np.zeros((128, 1024), np.float32)], 0)
        xcT = fm(xc, 256)
        maps = []
        for k in range(ncores):
            m = dict(base)
            m["flags"] = flags[k]
            m["xT"] = fm(xpad[k * T:k * T + T + 256], T + 256)
            m["xcT"] = xcT
            m["rope_cos"], m["rope_sin"] = ropes[k]
            maps.append(m)
        ra = run_bass_kernel_spmd(ncA, maps, core_ids=list(range(ncores))).results
        s5_fin_all = np.stack([np.asarray(ra[k]["s5_fin"]) for k in range(ncores)], 0)
        ssd_fin_all = np.stack([np.asarray(ra[k]["ssd_fin"]) for k in range(ncores)], 0)
        ssd_tot_all = np.stack([np.asarray(ra[k]["ssd_tot"]) for k in range(ncores)], 0)
        for k in range(ncores):
            maps[k]["s5_fin_all"] = s5_fin_all; maps[k]["ssd_fin_all"] = ssd_fin_all; maps[k]["ssd_tot_all"] = ssd_tot_all
            maps[k]["onehot"] = onehots[k]
        rb = run_bass_kernel_spmd(ncB, maps, core_ids=list(range(ncores))).results
        aff_all = np.concatenate([np.asarray(rb[k]["aff"])[256:] for k in range(ncores)], 0)
        if hook: hook(("B", l), dict(rb=rb))
        cmaps = []
        for k in range(ncores):
            cm = {"ident": consts["ident"], "norm2_g": lw("norm2_g"), "modT": modT[l], "final_norm_g": f32(inputs["final_norm_g"]),
                  "w_e_gate": wbf["w_e_gate"][l * 16384:(l + 1) * 16384], "w_e_up": wbf["w_e_up"][l * 16384:(l + 1) * 16384],
                  "w_e_down": wbf["w_e_down"][l * 16384:(l + 1) * 16384],
                  "x1T": np.asarray(rb[k]["x1T"]), "aff_all": aff_all, "aff_own": np.asarray(rb[k]["aff"])}
            cmaps.append(cm)
        rc = run_bass_kernel_spmd(ncC, cmaps, core_ids=list(range(ncores))).results
        unfm = lambda a: np.asarray(a).reshape(1024, -1).T
        x = np.concatenate([unfm(rc[k]["x2T"])[256:] for k in range(ncores)], 0)
        xc = unfm(rc[0]["x2T"])[:256]
        fin = np.concatenate([unfm(rc[k]["finT"])[256:] for k in range(ncores)], 0)
        if hook: hook(("C", l), dict(x=x, xc=xc))
    return np.ascontiguousarray(fin[None].astype(np.float32))


def kernel(**inputs):
    return run_model(inputs, 2048)
```

```python
import ml_dtypes
import contextlib
import os
import numpy as np
import concourse.bass as bass
import concourse.mybir as mybir
from concourse.bass_utils import run_bass_kernel_spmd

F32 = mybir.dt.float32
BF16 = mybir.dt.bfloat16
I32 = mybir.dt.int32
AF = mybir.ActivationFunctionType
ALU = mybir.AluOpType
AX = mybir.AxisListType

ENGS = ("pe", "dve", "act", "pool", "sp")
EPOCH = 20000
N_DMA_SEMS = 12


def _region(ap):
    t = ap.tensor
    shape = list(t.shape)
    space = str(ap.space) if hasattr(ap, "space") else ""
    dims = list(ap.ap)
    off = int(ap.offset)
    if "DRAM" in space.upper() or "HBM" in space.upper() or type(t).__name__.startswith("DRam"):
        lo = off
        hi = off + sum((c - 1) * abs(s) for s, c in dims) + 1
        return (t.name, 0, 1, lo, hi)
    row = 1
    for s in shape[1:]:
        row *= int(s)
    if type(t).__name__.startswith("PSum"):
        return (t.name, 0, 128, 0, row)
    p_lo = off // row
    f_lo = off % row
    p_hi = p_lo + int(dims[0][1])
    f_hi = f_lo + sum((c - 1) * abs(s) for s, c in dims[1:]) + 1
    return (t.name, p_lo, p_hi, f_lo, f_hi)


def _ovl(a, b):
    return a[1] < b[2] and b[1] < a[2] and a[3] < b[4] and b[3] < a[4]


def _covers(a, b):
    return a[1] <= b[1] and a[2] >= b[2] and a[3] <= b[3] and a[4] >= b[4]


class Prog:
    def __init__(self, nc, same_engine_sync=True):
        self.nc = nc
        self.es = contextlib.ExitStack()
        self.ops = {e: [] for e in ENGS}
        self.nops = {e: 0 for e in ENGS}
        self.writes = {}
        self.reads = {}
        import os
        self.same_engine_sync = same_engine_sync and os.environ.get('SAMESYNC', '1') == '1'
        self.dma_tot = [0] * N_DMA_SEMS
        self.dma_rr = 0
        self.known = {e: {} for e in ENGS}
        self._names = 0

    def init_arenas(self, n_f32, n_bf16):
        self.arena = {F32: self.es.enter_context(self.nc.sbuf_tensor("arena_f32", [128, n_f32], F32)),
                      BF16: self.es.enter_context(self.nc.sbuf_tensor("arena_bf16", [128, n_bf16], BF16))}
        self.asize = {F32: n_f32, BF16: n_bf16}
        self.atop = {F32: 0, BF16: 0}
        self.amax = {F32: 0, BF16: 0}
        self.astack = []

    def push(self):
        self.astack.append(dict(self.atop))
        self.pstack = getattr(self, "pstack", [])
        self.pstack.append(dict(self.atop))

    def pop(self):
        pk = self.pstack.pop()
        if self.pstack:
            for k in pk:
                self.pstack[-1][k] = max(self.pstack[-1][k], pk[k])
        self.last_peak = pk
        self.atop = self.astack.pop()

    def iter_push(self, i, tag):
        self.push()
        self._pp = getattr(self, "_pp", {})
        if i % 2 == 1 and tag in self._pp:
            need = self._pp[tag]
            ok = all(self.atop[dt] + 2 * need[dt] + 64 <= self.asize[dt] for dt in need)
            if ok:
                for dt in need:
                    if need[dt] > 0:
                        self.sb([128, need[dt]], dt)
        self._pp_base = getattr(self, "_pp_base", {})
        self._pp_base[(tag, i)] = dict(self.atop)

    def iter_pop(self, i, tag):
        base = self._pp_base.pop((tag, i))
        pk = self.pstack[-1]
        if i == 0:
            self._pp[tag] = {dt: pk[dt] - base[dt] for dt in pk}
        self.pop()

    def sb(self, shape, dtype=F32, name=None):
        n = 1
        for s_ in shape[1:]:
            n *= int(s_)
        n = (n + 15) // 16 * 16
        off = self.atop[dtype]
        assert off + n <= self.asize[dtype], f"arena {dtype} overflow: {off}+{n} > {self.asize[dtype]} ({name})"
        self.atop[dtype] = off + n
        self.amax[dtype] = max(self.amax[dtype], off + n)
        if getattr(self, "pstack", None):
            self.pstack[-1][dtype] = max(self.pstack[-1][dtype], off + n)
        nn = 1
        for s_ in shape[1:]:
            nn *= int(s_)
        v = self.arena[dtype][:, off:off + nn]
        if len(shape) > 2:
            names = [f"d{i}" for i in range(len(shape) - 1)]
            pat = "p (" + " ".join(names) + ") -> p " + " ".join(names)
            v = v.rearrange(pat, **{nm: int(sz) for nm, sz in zip(names[1:], shape[2:])})
        if shape[0] < 128:
            v = v[0:shape[0]]
        return v

    def ps(self, shape, dtype=F32, name=None):
        self._names += 1
        name = name or f"ps{self._names}"
        return self.es.enter_context(self.nc.psum_tensor(name, list(shape), dtype))

    def _deps(self, reads, writes):
        deps = set()
        rr = [_region(a) for a in reads]
        wr = [_region(a) for a in writes]
        self._raw = set()
        for r in rr:
            for (reg, ev) in self.writes.get(r[0], ()):
                if _ovl(reg, r):
                    deps.add(ev)
                    self._raw.add(ev)
            if r[0].startswith("ps"):
                for (reg, ev) in self.reads.get(r[0], ()):
                    deps.add(ev)
        for w in wr:
            for (reg, ev) in self.writes.get(w[0], ()):
                if _ovl(reg, w):
                    deps.add(ev)
            for (reg, ev) in self.reads.get(w[0], ()):
                if _ovl(reg, w):
                    deps.add(ev)
        return deps, rr, wr

    def _record(self, rr, wr, ev):
        for w in wr:
            lst = self.writes.setdefault(w[0], [])
            lst[:] = [(reg, e) for (reg, e) in lst if not _covers(w, reg)]
            lst.append((w, ev))
            rl = self.reads.get(w[0])
            if rl:
                rl[:] = [(reg, e) for (reg, e) in rl if not _covers(w, reg)]
        for r in rr:
            lst = self.reads.setdefault(r[0], [])
            lst[:] = [(reg, e) for (reg, e) in lst if not (e[0] == ev[0] and _covers(r, reg))]
            lst.append((r, ev))

    def op(self, eng, fn, reads, writes, pe_accum=False):
        deps, rr, wr = self._deps(reads, writes)
        idx = self.nops[eng]
        self.nops[eng] += 1
        ev = ((eng, idx // EPOCH), idx % EPOCH + 1)
        waits = self._filter(eng, deps, pe_accum)
        self.ops[eng].append((fn, waits, ev, False))
        self._record(rr, wr, ev)
        return ev

    def _filter(self, eng, deps, pe_accum=False):
        best = {}
        relax = os.environ.get("RELAX_WAR", "0") == "1"
        for (s, v) in deps:
            if s[0] == eng:
                if eng == "pe" or not self.same_engine_sync:
                    continue
                if relax and (s, v) not in getattr(self, "_raw", ()):
                    continue
            if best.get(s, 0) < v:
                best[s] = v
        out = []
        kn = self.known[eng]
        for s, v in best.items():
            if kn.get(s, 0) >= v:
                continue
            kn[s] = v
            out.append((s, v))
        return out

    def dma(self, out, in_, q="sp", **kw):
        deps, rr, wr = self._deps([in_], [out])
        k = self.dma_rr
        self.dma_rr = (self.dma_rr + 1) % N_DMA_SEMS
        sem = ("dma", k)
        prev = self.dma_tot[k]
        if prev:
            deps.add((sem, prev))
        self.dma_tot[k] += 16
        ev = (sem, self.dma_tot[k])
        waits = self._filter(q, deps)
        self.nops[q] += 0
        self.ops[q].append((lambda e, o=out, i=in_, kw=kw: e.dma_start(out=o, in_=i, **kw), waits, ev, True))
        self._record(rr, wr, ev)
        return ev

    def allgather(self, out, in_, n=8):
        deps, rr, wr = self._deps([in_], [out])
        self.cc_tot = getattr(self, "cc_tot", 0) + 1
        ev = (("cc", 0), self.cc_tot)
        if self.cc_tot > 1:
            deps.add((("cc", 0), self.cc_tot - 1))
        waits = self._filter("pool", deps)
        self.ops["pool"].append((lambda e, o=out, i=in_: e.collective_compute(
            "AllGather", ALU.bypass, replica_groups=[list(range(n))], ins=[i], outs=[o]), waits, ev, "cc"))
        self._record(rr, wr, ev)
        return ev

    def wait_all(self, eng="sp"):
        deps = set()
        for lst in self.writes.values():
            for (_, ev) in lst:
                deps.add(ev)
        waits = self._filter(eng, deps)
        self.ops[eng].append((None, waits, None, False))

    def mm(self, out, lhsT, rhs, start=True, stop=True):
        rd = [lhsT, rhs] + ([] if start else [])
        return self.op("pe", lambda e: e.matmul(out, lhsT, rhs, start=start, stop=stop), rd, [out])

    def transpose(self, out, in_, ident):
        return self.op("pe", lambda e: e.transpose(out, in_, ident), [in_, ident], [out])

    def act(self, out, in_, func, bias=0.0, scale=1.0, accum_out=None):
        rd = [in_] + [a for a in (bias, scale) if not isinstance(a, (int, float))]
        wr = [out] + ([accum_out] if accum_out is not None else [])
        kw = {}
        if accum_out is not None:
            kw["accum_out"] = accum_out
        return self.op("act", lambda e: e.activation(out, in_, func, bias=bias, scale=scale, **kw), rd, wr)

    def tt(self, out, in0, in1, op, eng="dve"):
        return self.op(eng, lambda e: e.tensor_tensor(out, in0, in1, op), [in0, in1], [out])

    def ts(self, out, in0, s1, s2=None, op0=ALU.mult, op1=None, eng="dve", accum_out=None):
        rd = [in0] + [a for a in (s1, s2) if a is not None and not isinstance(a, (int, float))]
        wr = [out] + ([accum_out] if accum_out is not None else [])
        kw = {}
        if op1 is not None:
            kw["op1"] = op1
        if accum_out is not None:
            kw["accum_out"] = accum_out
        return self.op(eng, lambda e: e.tensor_scalar(out, in0, s1, s2, op0, **kw), rd, wr)

    def stt(self, out, in0, scalar, in1, op0, op1, eng="dve"):
        rd = [in0, in1] + ([] if isinstance(scalar, (int, float)) else [scalar])
        return self.op(eng, lambda e: e.scalar_tensor_tensor(out, in0, scalar, in1, op0, op1), rd, [out])

    def copy(self, out, in_, eng="dve"):
        if eng == "act":
            return self.op("act", lambda e: e.copy(out, in_), [in_], [out])
        return self.op(eng, lambda e: e.tensor_copy(out, in_), [in_], [out])

    def memset(self, ap, val, eng="dve"):
        return self.op(eng, lambda e: e.memset(ap, val), [], [ap])

    def reduce(self, out, in_, op=ALU.add, axis=AX.X, eng="dve"):
        return self.op(eng, lambda e: e.tensor_reduce(out, in_, axis, op), [in_], [out])

    def recip(self, out, in_):
        return self.op("dve", lambda e: e.reciprocal(out, in_), [in_], [out])

    def scan(self, out, d0, d1, initial, op0=ALU.mult, op1=ALU.add):
        rd = [d0, d1] + ([] if isinstance(initial, (int, float)) else [initial])
        return self.op("dve", lambda e: e.tensor_tensor_scan(out, d0, d1, initial, op0, op1), rd, [out])

    def emit(self):
        nc = self.nc
        sems = {}
        for e in ("pe", "dve", "act", "pool"):
            n_ep = (self.nops[e] + EPOCH - 1) // EPOCH
            for k in range(max(n_ep, 1)):
                sems[(e, k)] = self.es.enter_context(nc.semaphore(f"s_{e}{k}"))
        sems[("cc", 0)] = self.es.enter_context(nc.semaphore("s_cc"))
        for k in range(N_DMA_SEMS):
            sems[("dma", k)] = self.es.enter_context(nc.semaphore(f"s_dma{k}"))
        block = self.es.enter_context(nc.Block())

        def run(engobj, lst):
            for (fn, waits, ev, is_dma) in lst:
                for (s, v) in waits:
                    engobj.wait_ge(sems[s], v)
                if fn is None:
                    continue
                ins = fn(engobj)
                if is_dma == "cc":
                    ins.then_inc(sems[ev[0]])
                elif is_dma:
                    ins.then_inc(sems[ev[0]], 16)
                else:
                    ins.then_inc(sems[ev[0]], 1)

        ops = self.ops

        @block.tensor
        def _(t):
            run(t, ops["pe"])

        @block.vector
        def _(v):
            run(v, ops["dve"])

        @block.scalar
        def _(s):
            run(s, ops["act"])

        @block.gpsimd
        def _(g):
            run(g, ops["pool"])

        @block.sync
        def _(sy):
            run(sy, ops["sp"])

    def close(self):
        self.es.close()


import math
import numpy as np

PI = math.pi
D = 1024
KD = 8
NCTX = 256
HALO = 128
IN_W = 5904
C_U, C_Z, C_XBC, C_DT, C_Q, C_K, C_V, C_G = 0, 512, 1024, 2048, 2064, 2576, 2704, 2832


def V(ap, free_dims, off=0):
    return bass.AP(ap.tensor, ap.offset + off, [list(ap.ap[0])] + [list(d) for d in free_dims])


def DV(t, off, dims):
    return bass.AP(t.tensor, t.offset + off, [list(d) for d in dims])


class G:
    pass


def host_consts():
    c = {}
    c["ident"] = np.eye(128, dtype=np.float32)
    c["ut"] = np.triu(np.ones((128, 128), np.float32))
    c["lt"] = np.tril(np.ones((128, 128), np.float32))
    d = np.arange(128, dtype=np.float32)
    exl = np.tile(d[None, :], (128, 1))
    exr = np.concatenate([np.tile((d + 1)[None, :], (64, 1)), np.tile((128 - d)[None, :], (64, 1))], 0)
    c["exl"] = exl.astype(np.float32)
    c["exr"] = exr.astype(np.float32)
    exv = np.stack([127 - d, d], 1)
    c["exv"] = exv.astype(np.float32)
    m = np.zeros((128, 8), np.float32)
    for p in range(128):
        m[p, p // 16] = 1.0
    c["maskbd"] = m
    return c


CONST_SHAPES = {"ident": [128, 128], "ut": [128, 128], "lt": [128, 128], "exl": [128, 128], "exr": [128, 128],
                "exv": [128, 2], "maskbd": [128, 8]}


def load_consts(g):
    P = g.P
    g.c = {}
    for k, shp in CONST_SHAPES.items():
        t = P.sb(shp, F32)
        P.dma(t, g.dram[k])
        g.c[k] = t
    g.ident_bf = P.sb([128, 128], BF16)
    P.copy(g.ident_bf, g.c["ident"])
    g.ones_bf = P.sb([128, 128], BF16)
    P.memset(g.ones_bf, 1.0)
    g.c["ones_f"] = P.sb([128, 128], F32)
    P.memset(g.c["ones_f"], 1.0)


def sincos(P, out_cos, out_sin, ang, tmp):
    n = 1
    for d_ in ang.shape[1:]:
        n *= int(d_)
    if not hasattr(P, "_kint"):
        P._kint = P.es.enter_context(P.nc.sbuf_tensor("kint", [128, 2048], I32))
        P._halfpi = P.sb([128, 1], F32)
        P.memset(P._halfpi, PI / 2)
    ki = bass.AP(P._kint[:, 0:n].tensor, P._kint[:, 0:n].offset, [list(ang.ap[0])[:1] + [ang.ap[0][1]]] and [[P._kint[:, 0:n].ap[0][0], ang.ap[0][1]], [1, n]])
    angf = bass.AP(ang.tensor, ang.offset, [list(ang.ap[0]), [1, n]])
    tmpf = bass.AP(tmp.tensor, tmp.offset, [list(tmp.ap[0]), [1, n]])
    cosf = bass.AP(out_cos.tensor, out_cos.offset, [list(out_cos.ap[0]), [1, n]])
    sinf = bass.AP(out_sin.tensor, out_sin.offset, [list(out_sin.ap[0]), [1, n]])
    pp = slice(0, 128)
    P.ts(ki, angf, 1.0 / (2 * PI), 0.25, op0=ALU.mult, op1=ALU.add)
    P.stt(tmpf, ki, -2 * PI, angf, ALU.mult, ALU.add)
    P.act(cosf, tmpf, AF.Sin, bias=P._halfpi[0:ang.ap[0][1]] if ang.ap[0][1] < 128 else P._halfpi, scale=1.0)
    P.ts(ki, angf, 1.0 / (2 * PI), None, op0=ALU.mult)
    P.stt(tmpf, ki, -2 * PI, angf, ALU.mult, ALU.add)
    P.act(sinf, tmpf, AF.Sin)


def s5_params(g):
    P = g.P
    dr = g.dram
    s = G()
    g.s5 = s
    NG = getattr(g, "NG", 32)
    s.lrc = P.sb([128, NG]); s.lic = P.sb([128, NG]); s.dtc = P.sb([128, NG])
    for d in range(2):
        P.dma(s.lrc[d * 64:(d + 1) * 64], DV(dr["s5_lam_re"], d * NG * 64, [[1, 64], [64, NG]]), allow_slow_non_contiguous=True)
        P.dma(s.lic[d * 64:(d + 1) * 64], DV(dr["s5_lam_im"], d * NG * 64, [[1, 64], [64, NG]]), allow_slow_non_contiguous=True)
        P.dma(s.dtc[d * 64:(d + 1) * 64], DV(dr["s5_log_dt"], d * NG, [[0, 64], [1, NG]]))
    P.act(s.dtc, s.dtc, AF.Exp)
    s.thc = P.sb([128, NG]); s.lrdtc = P.sb([128, NG])
    P.tt(s.thc, s.lic, s.dtc, ALU.mult)
    P.tt(s.lrdtc, s.lrc, s.dtc, ALU.mult)
    mag = P.sb([128, NG]); co = P.sb([128, NG]); si = P.sb([128, NG]); tmp = P.sb([128, NG])
    P.act(mag, s.lrdtc, AF.Exp)
    sincos(P, co, si, s.thc, tmp)
    abr = P.sb([128, NG]); abi = P.sb([128, NG])
    P.tt(abr, mag, co, ALU.mult)
    P.tt(abi, mag, si, ALU.mult)
    den = P.sb([128, NG]); t2 = P.sb([128, NG])
    P.tt(den, s.lrc, s.lrc, ALU.mult)
    P.tt(t2, s.lic, s.lic, ALU.mult)
    P.tt(den, den, t2, ALU.add)
    P.recip(den, den)
    am1 = P.sb([128, NG])
    P.ts(am1, abr, -1.0, None, op0=ALU.add)
    fr = P.sb([128, NG]); fi = P.sb([128, NG])
    P.tt(fr, am1, s.lrc, ALU.mult); P.tt(t2, abi, s.lic, ALU.mult); P.tt(fr, fr, t2, ALU.add); P.tt(fr, fr, den, ALU.mult)
    P.tt(fi, abi, s.lrc, ALU.mult); P.tt(t2, am1, s.lic, ALU.mult); P.tt(fi, fi, t2, ALU.subtract); P.tt(fi, fi, den, ALU.mult)
    bre = P.sb([128, NG, 16]); bim = P.sb([128, NG, 16])
    s.cre = P.sb([128, NG, 16]); s.cim = P.sb([128, NG, 16])
    for d in range(2):
        sl = slice(d * 64, (d + 1) * 64)
        P.dma(bre[sl], DV(dr["s5_b_re"], d * NG * 1024, [[16, 64], [1024, NG], [1, 16]]))
        P.dma(bim[sl], DV(dr["s5_b_im"], d * NG * 1024, [[16, 64], [1024, NG], [1, 16]]))
        P.dma(s.cre[sl], DV(dr["s5_c_re"], d * NG * 1024, [[1, 64], [1024, NG], [64, 16]]), allow_slow_non_contiguous=True)
        P.dma(s.cim[sl], DV(dr["s5_c_im"], d * NG * 1024, [[1, 64], [1024, NG], [64, 16]]), allow_slow_non_contiguous=True)
    s.bbr = P.sb([128, NG, 16]); s.bbi = P.sb([128, NG, 16])
    frb = V(fr, [[1, NG], [0, 16]]); fib = V(fi, [[1, NG], [0, 16]])
    t3 = P.sb([128, NG, 16])
    P.tt(s.bbr, bre, frb, ALU.mult); P.tt(t3, bim, fib, ALU.mult); P.tt(s.bbr, s.bbr, t3, ALU.subtract)
    P.tt(s.bbi, bim, frb, ALU.mult); P.tt(t3, bre, fib, ALU.mult); P.tt(s.bbi, s.bbi, t3, ALU.add)
    return s


def s5_gen_E(g, ex, out_re, out_im, neg_im=False):
    P = g.P
    s = g.s5
    P.push()
    GH = 16
    NG = getattr(g, "NG", 32)
    ang = P.sb([128, GH, 128]); tmp = P.sb([128, GH, 128]); mag = P.sb([128, GH, 128]); co = P.sb([128, GH, 128])
    exb = V(ex, [[0, GH], [1, 128]])
    for h in range(NG // GH):
        gs = slice(h * GH, (h + 1) * GH)
        P.tt(ang, V(s.thc[:, gs], [[1, GH], [0, 128]]), exb, ALU.mult)
        P.tt(mag, V(s.lrdtc[:, gs], [[1, GH], [0, 128]]), exb, ALU.mult, eng="pool")
        P.act(mag, mag, AF.Exp)
        sincos(P, co, ang, ang, tmp)
        P.tt(out_re[:, gs, :], mag, co, ALU.mult)
        if neg_im:
            P.stt(out_im[:, gs, :], mag, -1.0, ang, ALU.mult, ALU.mult)
        else:
            P.tt(out_im[:, gs, :], mag, ang, ALU.mult)
    P.pop()


def s5_gen_V(g, vr, vi):
    P = g.P
    dr = g.dram
    P.push()
    GH = 16
    lib = P.sb([128, GH, 2, 64]); lrb = P.sb([128, GH, 2, 64]); dtb = P.sb([128, GH, 2])
    tmp = P.sb([128, GH, 2, 64]); co = P.sb([128, GH, 2, 64])
    exvb = V(g.c["exv"], [[0, GH], [1, 2], [0, 64]])
    NG = getattr(g, "NG", 32)
    for h in range(NG // GH):
        g0 = h * GH
        for d_ in range(2):
            P.dma(lib[:, :, d_, :], DV(dr["s5_lam_im"], g0 * 64 + d_ * NG * 64, [[0, 128], [64, GH], [1, 64]]))
            P.dma(lrb[:, :, d_, :], DV(dr["s5_lam_re"], g0 * 64 + d_ * NG * 64, [[0, 128], [64, GH], [1, 64]]))
            P.dma(dtb[:, :, d_], DV(dr["s5_log_dt"], g0 + d_ * NG, [[0, 128], [1, GH]]), allow_slow_non_contiguous=True)
        P.act(dtb, dtb, AF.Exp)
        dtbb = V(dtb, [[2, GH], [1, 2], [0, 64]])
        P.tt(lib, lib, dtbb, ALU.mult)
        P.tt(lrb, lrb, dtbb, ALU.mult, eng="pool")
        P.tt(lib, lib, exvb, ALU.mult)
        P.tt(lrb, lrb, exvb, ALU.mult, eng="pool")
        P.act(lrb, lrb, AF.Exp)
        sincos(P, co, lib, lib, tmp)
        gs = slice(g0, g0 + GH)
        P.tt(vr[:, gs, :], lrb, co, ALU.mult)
        P.tt(vi[:, gs, :], lrb, lib, ALU.mult)
    P.pop()


def cmul_acc(P, out_re, out_im, ar, ai, hr, hi, sr, si, t1, t2, eng="dve"):
    P.tt(t1, ar, hr, ALU.mult, eng=eng)
    P.tt(t2, ai, hi, ALU.mult, eng=eng)
    P.tt(t1, t1, t2, ALU.subtract, eng=eng)
    if sr is not None:
        P.tt(out_re, t1, sr, ALU.add, eng=eng)
    else:
        P.copy(out_re, t1, eng=eng)
    P.tt(t1, ar, hi, ALU.mult, eng=eng)
    P.tt(t2, ai, hr, ALU.mult, eng=eng)
    P.tt(t1, t1, t2, ALU.add, eng=eng)
    if si is not None:
        P.tt(out_im, t1, si, ALU.add, eng=eng)
    else:
        P.copy(out_im, t1, eng=eng)


def s5_states(g, uT, NCH):
    P = g.P
    s = g.s5
    s.sre = P.sb([128, 32, NCH]); s.sim = P.sb([128, 32, NCH])
    P.push()
    vr = P.sb([128, 32, 128], BF16); vi = P.sb([128, 32, 128], BF16)
    if "s5_vr" in g.dram:
        P.dma(vr, g.dram["s5_vr"]); P.dma(vi, g.dram["s5_vi"])
    else:
        s5_gen_V(g, V(vr, [[128, 32], [64, 2], [1, 64]]), V(vi, [[128, 32], [64, 2], [1, 64]]))
    utok = P.sb([128, NCH, 512], BF16)
    for c in range(NCH):
        pt = g.pbank_bf()
        for b in range(4):
            P.transpose(pt[:, b * 128:(b + 1) * 128], uT[:, b, c * 128:(c + 1) * 128], g.ident_bf)
        P.copy(utok[:, c, :], pt[:, 0:512], eng="act" if c % 2 else "dve")
    GB = 4
    zr = P.sb([128, GB, NCH, 16]); zi = P.sb([128, GB, NCH, 16])
    t1 = P.sb([128, GB, NCH, 16]); t2 = P.sb([128, GB, NCH, 16]); t3 = P.sb([128, GB, NCH, 16]); t4 = P.sb([128, GB, NCH, 16])
    for g0 in range(0, 32, GB):
        for q in range(GB):
            gi = g0 + q
            p1 = g.pbank(); p2 = g.pbank()
            rhs = V(utok, [[512, NCH], [1, 16]], off=gi * 16)
            P.mm(V(p1, [[16, NCH], [1, 16]]), vr[:, gi, :], rhs)
            P.mm(V(p2, [[16, NCH], [1, 16]]), vi[:, gi, :], rhs)
            P.copy(zr[:, q], V(p1, [[16, NCH], [1, 16]]), eng="act")
            P.copy(zi[:, q], V(p2, [[16, NCH], [1, 16]]), eng="act")
        bb_r = V(s.bbr[:, g0:g0 + GB, :], [[16, GB], [0, NCH], [1, 16]]); bb_i = V(s.bbi[:, g0:g0 + GB, :], [[16, GB], [0, NCH], [1, 16]])
        P.tt(t1, zr, bb_r, ALU.mult); P.tt(t2, zi, bb_i, ALU.mult); P.tt(t1, t1, t2, ALU.subtract)
        P.reduce(s.sre[:, g0:g0 + GB, :], t1)
        P.tt(t3, zi, bb_r, ALU.mult, eng="pool"); P.tt(t4, zr, bb_i, ALU.mult, eng="pool"); P.tt(t3, t3, t4, ALU.add, eng="pool")
        P.reduce(s.sim[:, g0:g0 + GB, :], t3)
    P.pop()


def s5_local_scan(g, NCX, NC):
    P = g.P
    s = g.s5
    NCH = NCX + NC
    s.pre_re = P.sb([128, 32, NCH]); s.pre_im = P.sb([128, 32, NCH])
    s.fin_re = P.sb([128, 32, 2]); s.fin_im = P.sb([128, 32, 2])
    s.apow_re = P.sb([128, 32, NCH]); s.apow_im = P.sb([128, 32, NCH])
    t1 = P.sb([128, 32]); t2 = P.sb([128, 32])
    P.memset(s.pre_re, 0.0); P.memset(s.pre_im, 0.0)
    for (c0, n, fi) in ((0, NCX, 0), (NCX, NC, 1)):
        for half, order in ((slice(0, 64), list(range(c0, c0 + n))), (slice(64, 128), list(range(c0 + n - 1, c0 - 1, -1)))):
            eng = "dve" if half.start == 0 else "pool"
            aqr = s.aqr[half]; aqi = s.aqi[half]
            P.memset(s.apow_re[half, :, order[0]], 1.0, eng=eng); P.memset(s.apow_im[half, :, order[0]], 0.0, eng=eng)
            for k in range(n):
                c = order[k]
                if k + 1 < n:
                    cn = order[k + 1]
                    ore, oim = s.pre_re[half, :, cn], s.pre_im[half, :, cn]
                    cmul_acc(P, s.apow_re[half, :, cn], s.apow_im[half, :, cn], aqr, aqi, s.apow_re[half, :, c], s.apow_im[half, :, c],
                             None, None, t1[half], t2[half], eng=eng)
                else:
                    ore, oim = s.fin_re[half, :, fi], s.fin_im[half, :, fi]
                cmul_acc(P, ore, oim, aqr, aqi, s.pre_re[half, :, c], s.pre_im[half, :, c], s.sre[half, :, c], s.sim[half, :, c],
                         t1[half], t2[half], eng=eng)


def s5_tables_ro(g):
    P = g.P
    s = g.s5
    s.w2re = P.sb([128, 32, 128], BF16); s.nw2im = P.sb([128, 32, 128], BF16)
    s.aqr = P.sb([128, 32]); s.aqi = P.sb([128, 32])
    if "s5_w2re" in g.dram:
        P.dma(s.w2re, g.dram["s5_w2re"]); P.dma(s.nw2im, g.dram["s5_nw2im"])
    else:
        s5_gen_E(g, g.c["exr"], s.w2re, s.nw2im, neg_im=True)
    P.push()
    ang = P.sb([128, 32]); mag = P.sb([128, 32]); tmp = P.sb([128, 32]); co = P.sb([128, 32])
    P.ts(ang, s.thc, 128.0, None, op0=ALU.mult)
    P.act(mag, s.lrdtc, AF.Exp, scale=128.0)
    sincos(P, co, ang, ang, tmp)
    P.tt(s.aqr, mag, co, ALU.mult)
    P.tt(s.aqi, mag, ang, ALU.mult)
    P.pop()


def s5_readout(g, NCX, NC, carry_re, carry_im):
    P = g.P
    s = g.s5
    NCH = NCX + NC
    hre = P.sb([128, 32, NCH]); him = P.sb([128, 32, NCH])
    P.copy(hre, s.pre_re); P.copy(him, s.pre_im, eng="pool")
    own = slice(NCX, NCH)
    t1 = P.sb([128, 32, NC]); t2 = P.sb([128, 32, NC])
    crb = V(carry_re, [[carry_re.ap[1][0], 32], [0, NC]]); cib = V(carry_im, [[carry_im.ap[1][0], 32], [0, NC]])
    P.tt(t1, s.apow_re[:, :, own], crb, ALU.mult); P.tt(t2, s.apow_im[:, :, own], cib, ALU.mult)
    P.tt(t1, t1, t2, ALU.subtract); P.tt(hre[:, :, own], hre[:, :, own], t1, ALU.add)
    P.tt(t1, s.apow_re[:, :, own], cib, ALU.mult); P.tt(t2, s.apow_im[:, :, own], crb, ALU.mult)
    P.tt(t1, t1, t2, ALU.add); P.tt(him[:, :, own], him[:, :, own], t1, ALU.add)
    P.push()
    gre = P.sb([128, 32, NCH, 16], BF16); gim = P.sb([128, 32, NCH, 16], BF16)
    GQ = 8
    a1 = P.sb([128, GQ, NCH, 16]); a2 = P.sb([128, GQ, NCH, 16])
    for q in range(32 // GQ):
        gs = slice(q * GQ, (q + 1) * GQ)
        crb_ = V(s.cre[:, gs, :], [[16, GQ], [0, NCH], [1, 16]]); cib_ = V(s.cim[:, gs, :], [[16, GQ], [0, NCH], [1, 16]])
        hrb = V(hre[:, gs, :], [[NCH, GQ], [1, NCH], [0, 16]]); hib = V(him[:, gs, :], [[NCH, GQ], [1, NCH], [0, 16]])
        P.tt(a1, crb_, hrb, ALU.mult); P.tt(a2, cib_, hib, ALU.mult, eng="pool"); P.tt(gre[:, gs], a1, a2, ALU.subtract)
        P.tt(a1, crb_, hib, ALU.mult); P.tt(a2, cib_, hrb, ALU.mult, eng="pool"); P.tt(gim[:, gs], a1, a2, ALU.add)
    N = NCH * 16
    for gi in range(32):
        ps = g.pbank()
        o = V(ps, [[16, NCH], [1, 16]])
        P.mm(o, s.w2re[:, gi, :], V(gre[:, gi], [[16, NCH], [1, 16]]), start=True, stop=False)
        P.mm(o, s.nw2im[:, gi, :], V(gim[:, gi], [[16, NCH], [1, 16]]), start=False, stop=True)
        P.copy(V(s.ystok, [[512, NCH], [1, 16]], off=gi * 16), o, eng="act" if gi % 2 else "dve")
    P.pop()


def s5_kt_build(g, NB, kt_d):
    P = g.P
    s = g.s5
    NG = NB * 8
    P.push()
    elr = P.sb([128, NG, 128], BF16); eli = P.sb([128, NG, 128], BF16)
    s5_gen_E(g, g.c["exl"], elr, eli)
    brpad = P.sb([128, NG, 128], BF16); nbipad = P.sb([128, NG, 128], BF16)
    P.memset(brpad, 0.0); P.memset(nbipad, 0.0, eng="pool")
    padv = lambda t: V(t, [[1024, NB], [144, 8], [1, 16]])
    P.copy(padv(brpad), V(s.bbr, [[128, NB], [16, 8], [1, 16]]))
    P.ts(padv(nbipad), V(s.bbi, [[128, NB], [16, 8], [1, 16]]), -1.0, None, op0=ALU.mult)
    care = P.sb([128, 32, 16], BF16); caim = P.sb([128, 32, 16], BF16)
    a1 = P.sb([128, 32, 16]); a2 = P.sb([128, 32, 16])
    ko = P.sb([128, 2, 512], BF16)
    for b in range(NB):
        for lh in range(2):
            for sl in range(2):
                d0 = lh * 64 + sl * 32
                pk = [g.pbank(), g.pbank()]
                for gl in range(8):
                    gi = 8 * b + gl
                    crb = V(s.cre[:, gi, :], [[0, 32], [1, 16]]); cib = V(s.cim[:, gi, :], [[0, 32], [1, 16]])
                    erb = V(elr[:, gi, d0:d0 + 32], [[1, 32], [0, 16]]); eib = V(eli[:, gi, d0:d0 + 32], [[1, 32], [0, 16]])
                    P.tt(a1, crb, erb, ALU.mult); P.tt(a2, cib, eib, ALU.mult, eng="pool"); P.tt(care, a1, a2, ALU.subtract)
                    P.tt(a1, crb, eib, ALU.mult); P.tt(a2, cib, erb, ALU.mult, eng="pool"); P.tt(caim, a1, a2, ALU.add)
                    for dr_ in range(2):
                        h = slice(dr_ * 64, (dr_ + 1) * 64)
                        P.mm(pk[dr_], brpad[h, gi, :], V(care[h], [[1, 512]]), start=(gl == 0), stop=False)
                        P.mm(pk[dr_], nbipad[h, gi, :], V(caim[h], [[1, 512]]), start=False, stop=(gl == 7))
                for dr_ in range(2):
                    P.copy(ko[:, dr_, :], pk[dr_], eng="act" if dr_ else "dve")
                    off = (((b * 2 + lh) * 2 + dr_) * 128) * 1024 + sl * 512
                    P.dma(DV(kt_d, off, [[1024, 128], [1, 512]]), ko[:, dr_, :])
    P.pop()


def s5_lags(g, uT, NCH, ya_acc_cb):
    P = g.P
    s = g.s5
    P.push()
    kt_d = g.dram["s5_kt"]
    kt = P.sb([128, 2, 1024], BF16)
    bd = P.sb([128, 2, 64, 128], BF16)
    NS = NCH * 128
    yacc = P.sb([128, NS])
    mb = V(g.c["maskbd"], [[0, 64], [1, 8], [0, 16]])
    cgs = [(c0, min(4, NCH - c0)) for c0 in range(0, NCH, 4)]
    for b in range(4):
        for lh in range(2):
            P.dma(kt, DV(kt_d, (b * 2 + lh) * 2 * 128 * 1024, [[1024, 128], [128 * 1024, 2], [1, 1024]]))
            for dr_ in range(2):
                P.tt(V(bd[:, dr_], [[128, 64], [16, 8], [1, 16]]), V(kt[:, dr_, :], [[16, 64], [0, 8], [1, 16]]), mb, ALU.mult,
                     eng="pool" if dr_ else "dve")
            for (c0, ncg) in cgs:
                ps = g.pbank()
                first = True
                if lh == 1:
                    P.mm(ps[:, 0:ncg * 128], g.zeros_bf, V(uT[:, b, :], [[1, ncg * 128]], off=c0 * 128), start=True, stop=False)
                    for c in range(c0, c0 + ncg):
                        P.mm(ps[:, (c - c0) * 128:(c - c0 + 1) * 128], s.ystok[:, c, b * 128:(b + 1) * 128], g.ident_bf,
                             start=False, stop=False)
                    first = False
                for d in range(64):
                    dd = lh * 64 + d
                    w = 128 - dd
                    last = (d == 63)
                    P.mm(V(ps, [[128, ncg], [1, w]], off=dd), bd[:, 0, d, :], V(uT[:, b, :], [[128, ncg], [1, w]], off=c0 * 128),
                         start=first, stop=False)
                    first = False
                    P.mm(V(ps, [[128, ncg], [1, w]]), bd[:, 1, d, :], V(uT[:, b, :], [[128, ncg], [1, w]], off=c0 * 128 + dd),
                         start=False, stop=last)
                dst = yacc[:, c0 * 128:(c0 + ncg) * 128]
                if lh == 0:
                    P.copy(dst, ps[:, 0:ncg * 128], eng="act")
                else:
                    P.tt(dst, dst, ps[:, 0:ncg * 128], ALU.add)
        ya_acc_cb(b, yacc)
    P.pop()


def s5_core(g, uT, NCX, NC, carry_fn, ya_cb, states_cb=None, states_only=False):
    P = g.P
    NCH = NCX + NC
    s5_params(g)
    s = g.s5
    s.ystok = P.sb([128, NCH, 512], BF16)
    import os
    S5CUT = os.environ.get("S5CUT", "")
    P.push()
    s5_tables_ro(g)
    if S5CUT == "ro":
        P.pop(); return
    s5_states(g, uT, NCH)
    if S5CUT == "states":
        P.pop(); return
    s5_local_scan(g, NCX, NC)
    if states_cb is not None:
        states_cb()
    if states_only:
        P.pop()
        return
    cr, ci = carry_fn()
    s5_readout(g, NCX, NC, cr, ci)
    P.pop()
    if S5CUT == "readout":
        return
    s5_lags(g, uT, NCH, ya_cb)


def load_w(g, name, r0, nrows, c0, ncols, dtype=BF16, q="sp"):
    P = g.P
    kk = nrows // 128
    t = P.sb([128, kk, ncols], dtype)
    d = g.dram[name]
    ncol_total = d.shape[-1]
    P.dma(t, DV(d, r0 * ncol_total + c0, [[ncol_total, 128], [128 * ncol_total, kk], [1, ncols]]), q=q)
    return t


def load_h(g, c0, n):
    P = g.P
    t = P.sb([128, 8, n], BF16)
    P.dma(t, DV(g.hT_d, c0, [[g.E, 128], [128 * g.E, 8], [1, n]]))
    return t


def proj(g, ps_out, w, j0, hT, n, wcols=128):
    P = g.P
    for k in range(8):
        P.mm(ps_out, w[:, k, j0:j0 + wcols], hT[:, k, 0:n], start=(k == 0), stop=(k == 7))


def col_tiles(c0, n, step=512):
    out = []
    c = c0
    while c < c0 + n:
        m = min(step, c0 + n - c)
        out.append((c, m))
        c += m
    return out


def ssd_prep(g, NCX, NC, need_z=True):
    P = g.P
    dr = g.dram
    s = G(); g.ssd = s
    T = NC * 128; NS = (NCX + NC) * 128
    s.xsT = P.sb([128, 4, NS], BF16); s.bmT = P.sb([128, 2, NS], BF16); s.cmT = P.sb([128, 2, NS], BF16)
    s.gz = P.sb([128, 4, NS], BF16)
    s.dt = P.sb([128, NCX + NC, 16]); s.dta = P.sb([128, NCX + NC, 16])
    s.cw = P.sb([128, 8, 5]); s.cb = P.sb([128, 8])
    for k_ in range(5):
        P.dma(s.cw[:, :, k_], DV(dr["ssd_conv_w"], k_ * 1024, [[1, 128], [128, 8]]), allow_slow_non_contiguous=True)
    P.dma(s.cb, DV(dr["ssd_conv_b"], 0, [[1, 128], [128, 8]]), allow_slow_non_contiguous=True)
    s.dtb = P.sb([128, 16]); s.ab = P.sb([128, 16])
    P.dma(s.dtb, DV(dr["ssd_dt_bias"], 0, [[0, 128], [1, 16]]))
    P.dma(s.ab, DV(dr["ssd_a_log"], 0, [[0, 128], [1, 16]]))
    P.act(s.ab, s.ab, AF.Exp)
    P.ts(s.ab, s.ab, -1.0, None, op0=ALU.mult)
    s.dcol = P.sb([128, 4])
    for hh in range(2):
        P.dma(s.dcol[hh * 64:(hh + 1) * 64, :], DV(dr["ssd_d"], hh, [[0, 64], [2, 4]]), allow_slow_non_contiguous=True)
    s.ng = P.sb([128, 4])
    P.dma(s.ng, DV(dr["ssd_norm_g"], 0, [[1, 128], [128, 4]]), allow_slow_non_contiguous=True)
    P.push()
    wx = load_w(g, "w_in", 0, 1024, C_XBC, 1024)
    wz = load_w(g, "w_in", 0, 1024, C_Z, 512)
    wdt = load_w(g, "w_in", 0, 1024, C_DT, 16)
    regions = [(0, NCX * 128, g.e_ctx, False), (NCX * 128, T, g.e_own, True)]
    W = NS + 8
    xin = P.sb([128, W])
    for j in range(8):
        P.memset(xin[:, 0:2], 0.0); P.memset(xin[:, 2 + NCX * 128:4 + NCX * 128], 0.0)
        for (s0, n, e0, is_own) in regions:
            xo = 2 + s0 + (4 if is_own else 0)
            lo, hi = (e0 - 2, e0 + n + 2) if is_own else (e0, e0 + n)
            xo_lo = xo - 2 if is_own else xo
            for (c, m) in col_tiles(lo, hi - lo):
                P.push()
                ht = load_h(g, c, m)
                ps = g.pbank()
                proj(g, ps[:, 0:m], wx, j * 128, ht, m)
                P.copy(xin[:, xo_lo + (c - lo):xo_lo + (c - lo) + m], ps[:, 0:m], eng="act")
                P.pop()
            if is_own:
                P.ts(xin[:, xo - 2:xo], xin[:, xo - 2:xo], g.flagL, None, op0=ALU.mult)
                P.ts(xin[:, xo + n:xo + n + 2], xin[:, xo + n:xo + n + 2], g.flagR, None, op0=ALU.mult)
        dst = s.xsT[:, j, :] if j < 4 else (s.bmT[:, j - 4, :] if j < 6 else s.cmT[:, j - 6, :])
        for (s0, n, e0, is_own) in regions:
            xo = 2 + s0 + (4 if is_own else 0)
            P.push()
            acc = P.sb([128, n])
            P.ts(acc, xin[:, xo - 2:xo - 2 + n], s.cw[:, j, 0:1], None, op0=ALU.mult)
            for k in range(1, 5):
                P.stt(acc, xin[:, xo - 2 + k:xo - 2 + k + n], s.cw[:, j, k:k + 1], acc, ALU.mult, ALU.add)
            P.act(dst[:, s0:s0 + n], acc, AF.Silu, bias=s.cb[:, j:j + 1])
            P.pop()
    for (s0, n, e0, is_own) in regions:
        for (c, m) in col_tiles(e0, n):
            P.push()
            ht = load_h(g, c, m)
            so = s0 + (c - e0)
            for j in range(4 if need_z else 0):
                ps = g.pbank()
                proj(g, ps[:, 0:m], wz, j * 128, ht, m)
                P.act(s.gz[:, j, so:so + m], ps[:, 0:m], AF.Silu)
            for cc in range(m // 128):
                ps = g.pbank()
                for k in range(8):
                    P.mm(ps[:, 0:16], ht[:, k, cc * 128:(cc + 1) * 128], wdt[:, k, :], start=(k == 0), stop=(k == 7))
                ci = (so + cc * 128) // 128
                P.tt(s.dt[:, ci, :], ps[:, 0:16], s.dtb, ALU.add)
            P.pop()
    P.pop()
    P.act(s.dt, s.dt, AF.Exp)
    P.act(s.dt, s.dt, AF.Ln, bias=1.0)
    P.tt(s.dta, s.dt, V(s.ab, [[0, NCX + NC], [1, 16]]), ALU.mult)


def ssd_chunk(g, c, want_y, hin_f, hin_b, ybuf=None, sdirs=(0, 1)):
    P = g.P
    s = g.ssd
    cs = slice(c * 128, (c + 1) * 128)
    ut = g.c["ut"]; lt = g.c["lt"]
    xs_tok = P.sb([128, 8, 64], BF16); bm_tok = P.sb([128, 2, 128], BF16)
    pt = g.pbank_bf()
    for b in range(4):
        P.transpose(pt[:, b * 128:(b + 1) * 128], s.xsT[:, b, cs], g.ident_bf)
    P.copy(V(xs_tok, [[1, 512]]), pt[:, 0:512], eng="act")
    pt2 = g.pbank_bf()
    for b in range(2):
        P.transpose(pt2[:, b * 128:(b + 1) * 128], s.bmT[:, b, cs], g.ident_bf)
    P.copy(V(bm_tok, [[1, 256]]), pt2[:, 0:256], eng="act")
    pc = g.pbank()
    P.mm(pc[:, 0:8], ut, s.dta[:, c, 0:8])
    P.mm(pc[:, 8:16], lt, s.dta[:, c, 8:16])
    nacum = P.sb([128, 16])
    P.ts(nacum, pc[:, 0:16], -1.0, None, op0=ALU.mult)
    acb = [None] * 4
    adirs = (0, 1) if want_y else sdirs
    for dr_ in adirs:
        m = ut if dr_ == 0 else lt
        for hq in range(2):
            rb = P.sb([128, 4, 128])
            P.tt(rb, V(m, [[0, 4], [1, 128]]), V(s.dta[:, c, dr_ * 8 + hq * 4:dr_ * 8 + hq * 4 + 4], [[1, 4], [0, 128]]), ALU.mult,
                 eng="pool")
            pa = g.pbank()
            P.mm(V(pa, [[1, 512]]), g.c["ones_f"], V(rb, [[1, 512]]))
            pas = P.sb([128, 512])
            P.copy(pas, pa, eng="act" if hq else "dve")
            acb[dr_ * 2 + hq] = pas
    tot = P.sb([128, 16])
    if len(adirs) < 2:
        P.memset(tot, 0.0)
    for dr_ in adirs:
        for hq in range(2):
            pa = acb[dr_ * 2 + hq]
            col = 127 if dr_ == 0 else 0
            P.copy(tot[:, dr_ * 8 + hq * 4:dr_ * 8 + hq * 4 + 4], V(pa, [[128, 4]], off=col))
    w = P.sb([128, 16]); cd = P.sb([128, 16])
    P.tt(w, tot, nacum, ALU.add)
    P.act(w, w, AF.Exp)
    P.tt(w, w, s.dt[:, c, :], ALU.mult)
    P.act(cd, tot, AF.Exp)
    S = [None, None]
    for dr_ in sdirs:
        xsw = P.sb([128, 8, 64], BF16)
        P.tt(xsw, xs_tok, V(w[:, dr_ * 8:dr_ * 8 + 8], [[1, 8], [0, 64]]), ALU.mult)
        pS = g.pbank()
        for h in range(8):
            P.mm(pS[:, h * 64:(h + 1) * 64], bm_tok[:, h // 4, :], xsw[:, h, :])
        Ss = P.sb([128, 512])
        P.copy(Ss, pS, eng="act")
        S[dr_] = Ss
    if not want_y:
        return S, cd, tot
    cbm = []
    for gq in range(2):
        pcb = g.pbank()
        P.mm(pcb[:, 0:128], s.bmT[:, gq, cs], s.cmT[:, gq, cs])
        cf = P.sb([128, 128]); cbk = P.sb([128, 128])
        P.tt(cf, pcb[:, 0:128], ut, ALU.mult)
        P.tt(cbk, pcb[:, 0:128], lt, ALU.mult)
        cbm.append((cf, cbk))
    ring_e = [P.sb([128, 128]) for _ in range(4)]; ring_d = [P.sb([128, 128]) for _ in range(4)]
    ring_w = [P.sb([128, 128], BF16) for _ in range(4)]; ring_c = [P.sb([128, 128], BF16) for _ in range(4)]
    ri = 0
    for pair in range(4):
        py = g.pbank()
        for hh in range(2):
            h = pair * 2 + hh
            gq = h // 4
            hq, hi4 = h // 4, h % 4
            mms = []
            for dr_ in range(2):
                pa = acb[dr_ * 2 + hq]
                ri += 1
                e1 = ring_e[ri % 4]
                P.ts(e1, pa[:, hi4 * 128:(hi4 + 1) * 128], nacum[:, dr_ * 8 + h:dr_ * 8 + h + 1], g.zero_col, op0=ALU.add, op1=ALU.min)
                P.act(e1, e1, AF.Exp)
                wt = ring_w[ri % 4]
                P.stt(wt, e1, s.dt[:, c, dr_ * 8 + h:dr_ * 8 + h + 1], cbm[gq][dr_], ALU.mult, ALU.mult)
                mms.append((xs_tok[:, h, :], wt))
                hin = hin_f if dr_ == 0 else hin_b
                if hin is not None:
                    dec = ring_d[ri % 4]
                    P.act(dec, pa[:, hi4 * 128:(hi4 + 1) * 128], AF.Exp)
                    csd = ring_c[ri % 4]
                    P.tt(csd, s.cmT[:, gq, cs], dec, ALU.mult, eng="pool")
                    mms.append((hin[:, h, :], csd))
            for i_, (l_, r_) in enumerate(mms):
                P.mm(py[hh * 64:(hh + 1) * 64, 0:128], l_, r_, start=(i_ == 0), stop=(i_ == len(mms) - 1))
        P.stt(ybuf[:, pair, :], s.xsT[:, pair, cs], s.dcol[:, pair:pair + 1], py[:, 0:128], ALU.mult, ALU.add)
    return S, cd, tot


def ssd_run(g, NCX, NC, multi=False, yb_cb=None):
    P = g.P
    s = g.ssd
    NCH = NCX + NC
    hf = P.sb([128, 512]); hb = P.sb([128, 512])
    hbf = P.sb([128, 8, 64], BF16)
    it1 = 0; it2 = 0
    for (c0, n) in ((0, NCX), (NCX, NC)):
        if c0 == 0:
            P.memset(hb, 0.0)
        elif multi:
            P.push(); tmpc = P.sb([128, 512]); ssd_carry(g, 1, hb, tmpc); P.copy(hb, tmpc); P.pop()
        for c in range(c0 + n - 1, c0 - 1, -1):
            P.copy(V(hbf, [[1, 512]]), hb)
            P.dma(DV(g.ssd_hb_d, c * 128 * 512, [[512, 128], [1, 512]]), V(hbf, [[1, 512]]))
            P.iter_push(it1, "ssd1")
            S, cd, _t = ssd_chunk(g, c, False, None, None, sdirs=(1,))
            P.tt(V(hb, [[64, 8], [1, 64]]), V(hb, [[64, 8], [1, 64]]), V(cd[:, 8:16], [[1, 8], [0, 64]]), ALU.mult)
            P.tt(hb, hb, S[1], ALU.add)
            P.iter_pop(it1, "ssd1"); it1 += 1
    hfb = P.sb([128, 8, 64], BF16); hbb = P.sb([128, 8, 64], BF16)
    ybuf = P.sb([128, 4, 128])
    for (c0, n) in ((0, NCX), (NCX, NC)):
        if c0 == 0:
            P.memset(hf, 0.0)
        elif multi:
            P.push(); tmpc = P.sb([128, 512]); ssd_carry(g, 0, hf, tmpc); P.copy(hf, tmpc); P.pop()
        for c in range(c0, c0 + n):
            P.copy(V(hfb, [[1, 512]]), hf)
            P.dma(V(hbb, [[1, 512]]), DV(g.ssd_hb_d, c * 128 * 512, [[512, 128], [1, 512]]))
            P.iter_push(it2, "ssd2")
            S, cd, _t = ssd_chunk(g, c, True, hfb, hbb, ybuf, sdirs=(0,))
            P.tt(V(hf, [[64, 8], [1, 64]]), V(hf, [[64, 8], [1, 64]]), V(cd[:, 0:8], [[1, 8], [0, 64]]), ALU.mult)
            P.tt(hf, hf, S[0], ALU.add)
            cs = slice(c * 128, (c + 1) * 128)
            yg = P.sb([128, 4, 128]); sq = P.sb([128, 4, 128], BF16)
            P.tt(yg, ybuf, s.gz[:, :, cs], ALU.mult)
            P.tt(sq, yg, yg, ALU.mult, eng="pool")
            pn = g.pbank()
            for b in range(4):
                P.mm(pn[:, 0:128], g.ones_bf, sq[:, b, :], start=(b == 0), stop=(b == 3))
            rstd = P.sb([128, 128])
            P.act(rstd, pn[:, 0:128], AF.Sqrt, scale=1.0 / 512, bias=g.eps_col)
            P.recip(rstd, rstd)
            yo = P.sb([128, 4, 128], BF16)
            for b in range(4):
                P.stt(yo[:, b, :], yg[:, b, :], s.ng[:, b:b + 1], rstd, ALU.mult, ALU.mult)
            yb_cb(c, yo)
            P.iter_pop(it2, "ssd2"); it2 += 1


def attn_run(g, NCX, NC, yc_cb):
    P = g.P
    dr = g.dram
    T = NC * 128
    NL = T + 256
    NLT = NL // 128
    P.push()
    wq = P.sb([128, 8, 512], BF16)
    d = dr["w_in"]
    for r in range(4):
        for hh, head in enumerate((r, 4 + r)):
            P.dma(wq[:, :, r * 128 + hh * 64:r * 128 + hh * 64 + 64],
                  DV(d, C_Q + head * 64, [[IN_W, 128], [128 * IN_W, 8], [1, 64]]))
    wk = load_w(g, "w_in", 0, 1024, C_K, 128)
    wv = load_w(g, "w_in", 0, 1024, C_V, 128)
    pswap = P.sb([128, 128], BF16)
    P.copy(pswap, g.c["pswap"])
    esink = P.sb([128, 4])
    for hh in range(2):
        P.dma(esink[hh * 64:(hh + 1) * 64, :], DV(dr["attn_sink"], hh * 4, [[0, 64], [1, 4]]))
    P.act(esink, esink, AF.Exp)
    mprev = P.sb([128, 128], BF16); mnext = P.sb([128, 128], BF16); mprevL = P.sb([128, 128], BF16); mnextR = P.sb([128, 128], BF16)
    P.copy(mprev, g.c["lt"]); P.copy(mnext, g.c["ut"])
    P.ts(mprevL, g.c["lt"], g.flagL, None, op0=ALU.mult)
    P.ts(mnextR, g.c["ut"], g.flagR, None, op0=ALU.mult)
    import os
    CUT = os.environ.get('ATT_CUT', '')
    if CUT == 'setup':
        P.pop(); return
    NS = (NCX + NC) * 128
    qT = P.sb([128, 4, NS], BF16)
    kT = P.sb([128, NL + 256], BF16)
    vtok = P.sb([128, NLT + 2, 128], BF16)

    def rope(dst, ps, n, ecol):
        if 'norope' in CUT:
            P.copy(dst, ps); return
        P.push()
        cs_ = P.sb([128, n]); sn_ = P.sb([128, n]); xb = P.sb([128, n], BF16); t1 = P.sb([128, n])
        if 'nodma' in CUT:
            P.memset(cs_, 1.0); P.memset(sn_, 0.0)
        else:
            P.dma(cs_, DV(dr["rope_cos"], ecol, [[NL, 128], [1, n]]))
            P.dma(sn_, DV(dr["rope_sin"], ecol, [[NL, 128], [1, n]]))
        if 'dmaonly' in CUT:
            P.tt(dst, ps, cs_, ALU.mult); P.pop(); return
        P.copy(xb, ps)
        p2 = g.pbank()
        P.mm(p2[:, 0:n], pswap, xb)
        P.tt(t1, ps, cs_, ALU.mult)
        t2 = P.sb([128, n])
        P.tt(t2, p2[:, 0:n], sn_, ALU.mult)
        P.tt(dst, t1, t2, ALU.add)
        P.pop()

    for (c, m) in col_tiles(0, NL):
        P.push()
        ht = load_h(g, c, m)
        ps = g.pbank()
        proj(g, ps[:, 0:m], wk, 0, ht, m)
        rope(kT[:, c:c + m], ps[:, 0:m], m, c)
        for cc in range(m // 128):
            if 'nov' in CUT:
                break
            pv = g.pbank()
            for k in range(8):
                P.mm(pv[:, 0:128], ht[:, k, cc * 128:(cc + 1) * 128], wv[:, k, :], start=(k == 0), stop=(k == 7))
            P.copy(vtok[:, (c // 128) + cc, :], pv[:, 0:128], eng="act")
        if 'noq' in CUT:
            P.pop(); continue
        lo = max(c, 128); hi = min(c + m, 128 + T)
        if hi > lo:
            for r in range(4):
                pq = g.pbank()
                proj(g, pq[:, 0:hi - lo], wq, r * 128, ht[:, :, lo - c:hi - c], hi - lo)
                so = NCX * 128 + (lo - 128)
                rope(qT[:, r, so:so + (hi - lo)], pq[:, 0:hi - lo], hi - lo, lo)
        P.pop()
    if 'lat' in CUT:
        P.pop(); return
    for (c, m) in col_tiles(g.e_ctx, NCX * 128):
        P.push()
        ht = load_h(g, c, m)
        so = c - g.e_ctx
        ps = g.pbank()
        proj(g, ps[:, 0:m], wk, 0, ht, m)
        P.copy(kT[:, NL + so:NL + so + m], ps[:, 0:m], eng="act")
        for cc in range(m // 128):
            pv = g.pbank()
            for k in range(8):
                P.mm(pv[:, 0:128], ht[:, k, cc * 128:(cc + 1) * 128], wv[:, k, :], start=(k == 0), stop=(k == 7))
            P.copy(vtok[:, NLT + so // 128 + cc, :], pv[:, 0:128], eng="act")
        for r in range(4):
            pq = g.pbank()
            proj(g, pq[:, 0:m], wq, r * 128, ht, m)
            P.copy(qT[:, r, so:so + m], pq[:, 0:m], eng="act")
        P.pop()
    g._nrot = 4
    po = g._pb_o; pd = g._pb_d
    import os
    for qb in range(NCX + NC):
        if os.environ.get('ATT_CUT') == 'proj':
            break
        if qb < NCX:
            tiles = [(NLT + 0, NL + 0, None), (NLT + 1, NL + 128, None)]
        else:
            n = qb - NCX
            tiles = [(n, n * 128, mprevL if n == 0 else mprev), (n + 1, (n + 1) * 128, None),
                     (n + 2, (n + 2) * 128, mnextR if n == NC - 1 else mnext),
                     (NLT + 0, NL + 0, None), (NLT + 1, NL + 128, None)]
        P.iter_push(qb, "attq")
        nt = len(tiles)
        for hk in range(2):
            h = slice(hk * 64, (hk + 1) * 64)
            for ti, (kt, kcol, mask) in enumerate(tiles):
                ps = g.pbank()
                P.mm(ps[:, 0:512], kT[h, kcol:kcol + 128], V(qT[h], [[NS, 4], [1, 128]], off=qb * 128))
                ex = P.sb([128, 4, 128], BF16)
                P.act(V(ex, [[1, 512]]), ps, AF.Exp, scale=0.125)
                if mask is not None:
                    P.tt(ex, ex, V(mask, [[0, 4], [1, 128]]), ALU.mult, eng="pool")
                P.mm(po[h, :], vtok[:, kt, h], V(ex, [[1, 512]]), start=(ti == 0), stop=(ti == nt - 1))
                P.mm(pd[h, :], g.ones_bf[:, 0:64], V(ex, [[1, 512]]), start=(ti == 0), stop=(ti == nt - 1))
        rd = P.sb([128, 4, 128])
        for r in range(4):
            P.ts(rd[:, r, :], pd[:, r * 128:(r + 1) * 128], esink[:, r:r + 1], None, op0=ALU.add)
        P.recip(V(rd, [[1, 512]]), V(rd, [[1, 512]]))
        yo = P.sb([128, 4, 128], BF16)
        P.tt(V(yo, [[1, 512]]), po[:, :], V(rd, [[1, 512]]), ALU.mult)
        yc_cb(qb, yo)
        P.iter_pop(qb, "attq")
    g._nrot = 6
    P.pop()


def rms_rstd(g, xt, n, rstd):
    P = g.P
    P.push()
    sq = P.sb([128, 8, n], BF16)
    P.act(sq, xt, AF.Square)
    ps = g.pbank()
    for k in range(8):
        P.mm(ps[:, 0:n], g.ones_bf, sq[:, k, :], start=(k == 0), stop=(k == 7))
    P.act(rstd, ps[:, 0:n], AF.Sqrt, scale=1.0 / D, bias=g.eps_col)
    P.recip(rstd, rstd)
    P.pop()


def mod_norm(g, xt, n, acol, bcol, out, v):
    P = g.P
    P.push()
    rstd = P.sb([128, n])
    rms_rstd(g, xt, n, rstd)
    tmp = P.sb([128, n])
    for k in range(8):
        P.stt(tmp, xt[:, k, :], acol[:, k, v:v + 1], rstd, ALU.mult, ALU.mult)
        P.act(out[:, k, :], tmp, AF.Identity, bias=bcol[:, k, v:v + 1])
    P.pop()


def load_mod(g):
    P = g.P
    m = P.sb([128, 48, 2])
    P.dma(m, g.dram["modT"])
    g.mod = m
    n1 = P.sb([128, 8]); n2 = P.sb([128, 8])
    lst = []
    if "norm1_g" in g.dram:
        P.dma(n1, DV(g.dram["norm1_g"], 0, [[1, 128], [128, 8]]), allow_slow_non_contiguous=True)
        g.a1 = P.sb([128, 8, 2]); lst.append((g.a1, n1, 1))
    if "norm2_g" in g.dram:
        P.dma(n2, DV(g.dram["norm2_g"], 0, [[1, 128], [128, 8]]), allow_slow_non_contiguous=True)
        g.a2 = P.sb([128, 8, 2]); lst.append((g.a2, n2, 4))
    for (a, nn, j) in lst:
        P.ts(a, m[:, j * 8:(j + 1) * 8, :], 1.0, None, op0=ALU.add)
        P.tt(a, a, V(nn, [[1, 8], [0, 2]]), ALU.mult)
    g.b1 = m[:, 0:8, :]; g.b2 = m[:, 24:32, :]
    g.g1 = m[:, 16:24, :]; g.g2 = m[:, 40:48, :]


def router_aff(g, h2f, n, wr, aff_out):
    P = g.P
    for blk in range(n // 128):
        ps = g.pbank()
        for k in range(8):
            P.mm(ps[:, 0:16], h2f[:, k, blk * 128:(blk + 1) * 128], wr[:, k, :], start=(k == 0), stop=(k == 7))
        P.push()
        mx = P.sb([128, 1]); sm = P.sb([128, 1]); ex = P.sb([128, 16])
        P.reduce(mx, ps[:, 0:16], op=ALU.max)
        P.ts(mx, mx, -1.0, None, op0=ALU.mult)
        P.act(ex, ps[:, 0:16], AF.Exp, bias=mx, accum_out=sm)
        P.recip(sm, sm)
        P.ts(aff_out[:, blk, :], ex, sm, None, op0=ALU.mult)
        P.pop()


def s_tiles(NCX, NC, step=512):
    out = [(c, m, True) for (c, m) in col_tiles(0, NCX * 128, step)]
    out += [(c, m, False) for (c, m) in col_tiles(NCX * 128, NC * 128, step)]
    return out


def s2e(g, NCX, s0, is_ctx):
    return g.e_ctx + s0 if is_ctx else g.e_own + (s0 - NCX * 128)


def merge_run(g, NCX, NC):
    P = g.P
    dr = g.dram
    NS = (NCX + NC) * 128
    P.push()
    macc = P.sb([128, 8, NS], BF16)
    for kbr in range(3):
        P.push()
        wg = load_w(g, "w_in", 0, 1024, C_G + kbr * 1024, 1024)
        wb = P.sb([128, 4, 1024], BF16)
        d = dr["w_branch"]
        if kbr < 2:
            P.dma(wb, DV(d, kbr * 512 * 1024, [[1024, 128], [128 * 1024, 4], [1, 1024]]))
        else:
            for r in range(4):
                for hh, head in enumerate((r, 4 + r)):
                    P.dma(wb[hh * 64:(hh + 1) * 64, r, :], DV(d, (2 * 512 + head * 64) * 1024, [[1024, 64], [1, 1024]]))
        yd = g.y_d[kbr]
        for (s0, n, is_ctx) in s_tiles(NCX, NC):
            P.push()
            ht = load_h(g, s2e(g, NCX, s0, is_ctx), n)
            yt = P.sb([128, 4, n], BF16)
            P.dma(yt, DV(yd, s0, [[NS, 128], [128 * NS, 4], [1, n]]))
            for j in range(8):
                pg = g.pbank()
                proj(g, pg[:, 0:n], wg, j * 128, ht, n)
                gt = P.sb([128, n])
                P.act(gt, pg[:, 0:n], AF.Sigmoid)
                pb = g.pbank()
                for cc in range(4):
                    P.mm(pb[:, 0:n], wb[:, cc, j * 128:(j + 1) * 128], yt[:, cc, :], start=(cc == 0), stop=(cc == 3))
                if kbr == 0:
                    P.tt(macc[:, j, s0:s0 + n], gt, pb[:, 0:n], ALU.mult)
                else:
                    P.tt(gt, gt, pb[:, 0:n], ALU.mult)
                    P.tt(macc[:, j, s0:s0 + n], macc[:, j, s0:s0 + n], gt, ALU.add, eng="pool")
            P.pop()
        P.pop()
    wo = load_w(g, "w_out", 0, 1024, 0, 1024)
    wr = load_w(g, "w_router", 0, 1024, 0, 16, dtype=F32)
    for (s0, n, is_ctx) in s_tiles(NCX, NC):
        v = 1 if is_ctx else 0
        P.push()
        xt = P.sb([128, 8, n])
        xsrc = g.dram["xcT"] if is_ctx else g.dram["xT"]
        xw = NCX * 128 if is_ctx else NC * 128 + 256
        xo = s0 if is_ctx else (s0 - NCX * 128) + 128
        P.dma(xt, DV(xsrc, xo, [[xw, 128], [128 * xw, 8], [1, n]]))
        for j in range(8):
            po = g.pbank()
            for k in range(8):
                P.mm(po[:, 0:n], wo[:, k, j * 128:(j + 1) * 128], macc[:, k, s0:s0 + n], start=(k == 0), stop=(k == 7))
            P.stt(xt[:, j, :], po[:, 0:n], g.g1[:, j, v:v + 1], xt[:, j, :], ALU.mult, ALU.add)
        P.dma(DV(g.dram["x1T"], s0, [[NS, 128], [128 * NS, 8], [1, n]]), xt)
        h2 = P.sb([128, 8, n])
        mod_norm(g, xt, n, g.a2, g.b2, h2, v)
        aff = P.sb([128, n // 128, 16])
        router_aff(g, h2, n, wr, aff)
        P.dma(DV(g.dram["aff"], s0 * 16, [[16, 128], [128 * 16, n // 128], [1, 16]]), aff)
        P.pop()
    P.pop()


B_INPUTS = [("s5_lam_re", [2, 32, 64]), ("s5_lam_im", [2, 32, 64]), ("s5_log_dt", [2, 32]), ("s5_b_re", [2, 32, 64, 16]),
            ("s5_b_im", [2, 32, 64, 16]), ("s5_c_re", [2, 32, 16, 64]), ("s5_c_im", [2, 32, 16, 64]), ("s5_d", [512]),
            ("s5_b_glu", [512]), ("ssd_conv_w", [5, 1024]), ("ssd_conv_b", [1024]), ("ssd_a_log", [2, 8]),
            ("ssd_dt_bias", [2, 8]), ("ssd_d", [8]), ("ssd_norm_g", [512]), ("attn_sink", [8]), ("norm1_g", [1024]),
            ("norm2_g", [1024]), ("w_router", [1024, 16]), ("modT", [128, 48, 2]), ("flags", [128, 2])]
B_INPUTS_BF = [("w_in", [1024, IN_W]), ("s5_w_glu", [512, 512]), ("w_branch", [3 * 512, 1024]), ("w_out", [1024, 1024])]


def declare_inputs(g, lst, dt):
    for name, shp in lst:
        g.dram[name] = g.nc.dram_tensor(name, shp, dt, kind="ExternalInput").ap()


def phase0_h(g, NCX, NC):
    P = g.P
    T = NC * 128
    for (c, m, is_ctx) in [(c, m, False) for (c, m) in col_tiles(0, T + 256)] + [(c, m, True) for (c, m) in col_tiles(0, NCX * 128)]:
        P.push()
        xt = P.sb([128, 8, m])
        src = g.dram["xcT"] if is_ctx else g.dram["xT"]
        xw = NCX * 128 if is_ctx else T + 256
        P.dma(xt, DV(src, c, [[xw, 128], [128 * xw, 8], [1, m]]))
        ht = P.sb([128, 8, m], BF16)
        mod_norm(g, xt, m, g.a1, g.b1, ht, 1 if is_ctx else 0)
        e0 = (g.e_ctx + c) if is_ctx else c
        P.dma(DV(g.hT_d, e0, [[g.E, 128], [128 * g.E, 8], [1, m]]), ht)
        P.pop()


def s5_phase(g, NCX, NC, carry_fn=None, states_cb=None, states_only=False):
    P = g.P
    dr = g.dram
    NS = (NCX + NC) * 128
    P.push()
    uT = P.sb([128, 4, NS], BF16)
    aT = P.sb([128, 4, NS], BF16)
    P.push()
    wu = load_w(g, "w_in", 0, 1024, C_U, 512)
    for (s0, n, is_ctx) in s_tiles(NCX, NC):
        P.push()
        ht = load_h(g, s2e(g, NCX, s0, is_ctx), n)
        for j in range(4):
            ps = g.pbank()
            proj(g, ps[:, 0:n], wu, j * 128, ht, n)
            P.copy(uT[:, j, s0:s0 + n], ps[:, 0:n], eng="act" if j % 2 else "dve")
        P.pop()
    P.pop()
    dcol = P.sb([128, 4]); bglu = P.sb([128, 4])
    P.dma(dcol, DV(dr["s5_d"], 0, [[1, 128], [128, 4]]), allow_slow_non_contiguous=True)
    P.dma(bglu, DV(dr["s5_b_glu"], 0, [[1, 128], [128, 4]]), allow_slow_non_contiguous=True)

    def ya_cb(b, yacc):
        P.stt(yacc, uT[:, b, :], dcol[:, b:b + 1], yacc, ALU.mult, ALU.add)
        P.act(aT[:, b, :], yacc, AF.Gelu)

    if carry_fn is None:
        carry_fn = lambda: (g.s5.fin_re[:, :, 0], g.s5.fin_im[:, :, 0])
    s5_core(g, uT, NCX, NC, carry_fn, ya_cb, states_cb, states_only)
    if states_only:
        P.pop()
        return
    wgl = load_w(g, "s5_w_glu", 0, 512, 0, 512)
    for (s0, n) in col_tiles(0, NS):
        P.push()
        yo = P.sb([128, 4, n], BF16)
        for j in range(4):
            ps = g.pbank()
            for k in range(4):
                P.mm(ps[:, 0:n], wgl[:, k, j * 128:(j + 1) * 128], aT[:, k, s0:s0 + n], start=(k == 0), stop=(k == 3))
            gt = P.sb([128, n])
            P.act(gt, ps[:, 0:n], AF.Sigmoid, bias=bglu[:, j:j + 1])
            P.tt(yo[:, j, :], gt, aT[:, j, s0:s0 + n], ALU.mult)
        P.dma(DV(g.y_d[0], s0, [[NS, 128], [128 * NS, 4], [1, n]]), yo)
        P.pop()
    P.pop()


def build_B(T, debug=False, multi=False, mode="B"):
    nc = bass.Bass("TRN2", target_bir_lowering=False)
    g = G(); g.nc = nc; g.P = Prog(nc); P = g.P
    P.init_arenas(18 * 1024, 62 * 1024)
    NCX = 2; NC = T // 128; NS = (NCX + NC) * 128
    g.E = T + 512; g.e_own = 128; g.e_ctx = T + 256
    g.dram = {}
    consts = dict(CONST_SHAPES); consts["pswap"] = [128, 128]
    declare_inputs(g, list(consts.items()), F32)
    declare_inputs(g, B_INPUTS, F32)
    declare_inputs(g, B_INPUTS_BF, BF16)
    declare_inputs(g, [("s5_vr", [128, 32, 128]), ("s5_vi", [128, 32, 128]), ("s5_w2re", [128, 32, 128]), ("s5_nw2im", [128, 32, 128]),
                       ("s5_kt", [4, 2, 2, 128, 1024])], BF16)
    declare_inputs(g, [("xT", [8, 128, T + 256]), ("xcT", [8, 128, 256]), ("rope_cos", [128, T + 256]), ("rope_sin", [128, T + 256])], F32)
    if mode == "B":
        g.dram["x1T"] = nc.dram_tensor("x1T", [8, 128, NS], F32, kind="ExternalOutput").ap()
        g.dram["aff"] = nc.dram_tensor("aff", [NS, 16], F32, kind="ExternalOutput").ap()
        if multi:
            declare_inputs(g, [("s5_fin_all", [NCORES, 128, 64]), ("ssd_fin_all", [NCORES, 2, 128, 512]), ("ssd_tot_all", [NCORES, 128, 16]),
                               ("onehot", [128, NCORES])], F32)
    else:
        g.dram["s5_fin"] = nc.dram_tensor("s5_fin", [128, 64], F32, kind="ExternalOutput").ap()
        g.dram["ssd_fin"] = nc.dram_tensor("ssd_fin", [2, 128, 512], F32, kind="ExternalOutput").ap()
        g.dram["ssd_tot"] = nc.dram_tensor("ssd_tot", [128, 16], F32, kind="ExternalOutput").ap()
    g.hT_d = nc.dram_tensor("hT_scr", [8, 128, g.E], BF16).ap()
    g.ssd_hb_d = nc.dram_tensor("ssd_hb_scr", [NCX + NC, 128, 512], BF16).ap()
    kind = "ExternalOutput" if debug else "Internal"
    g.y_d = [nc.dram_tensor(f"y{k}_scr", [4, 128, NS], BF16, kind=kind).ap() for k in range(3)]
    setup_psum(g)
    CONST_SHAPES2 = consts
    g.c = {}
    for k, shp in CONST_SHAPES2.items():
        t = P.sb(shp, F32)
        P.dma(t, g.dram[k])
        g.c[k] = t
    g.ident_bf = P.sb([128, 128], BF16); P.copy(g.ident_bf, g.c["ident"])
    g.ones_bf = P.sb([128, 128], BF16); P.memset(g.ones_bf, 1.0)
    g.zeros_bf = P.sb([128, 128], BF16); P.memset(g.zeros_bf, 0.0)
    g.c["ones_f"] = P.sb([128, 128], F32); P.memset(g.c["ones_f"], 1.0)
    g.eps_col = P.sb([128, 1]); P.memset(g.eps_col, 1e-6)
    g.zero_col = P.sb([128, 1]); P.memset(g.zero_col, 0.0)
    fl = P.sb([128, 2]); P.dma(fl, g.dram["flags"])
    g.flagL = fl[:, 0:1]; g.flagR = fl[:, 1:2]
    import os
    ph = os.environ.get("PH", "s5,ssd,att,merge").split(",")
    load_mod(g)
    phase0_h(g, NCX, NC)
    if multi and mode == "B":
        g.onehot = P.sb([128, NCORES]); P.dma(g.onehot, g.dram["onehot"])
    if mode == "A":
        def dump_fin():
            t_ = P.sb([128, 32, 2])
            P.copy(t_[:, :, 0], g.s5.fin_re[:, :, 1]); P.copy(t_[:, :, 1], g.s5.fin_im[:, :, 1])
            P.dma(g.dram["s5_fin"], V(t_, [[1, 64]]))
        s5_phase(g, NCX, NC, states_cb=dump_fin, states_only=True)
        P.push()
        ssd_prep(g, NCX, NC, need_z=False)
        ssd_local_finals(g, NCX, NC)
        P.pop()
        P.wait_all(); P.emit(); P.close()
        return nc, g
    if "s5" in ph:
        s5_phase(g, NCX, NC, carry_fn=(lambda: s5_carry_chain(g, NC)) if multi else None)
    if "ssd" in ph:
        P.push()
        ssd_prep(g, NCX, NC)
        ssd_run(g, NCX, NC, multi=multi, yb_cb=lambda c, yo: P.dma(DV(g.y_d[1], c * 128, [[NS, 128], [128 * NS, 4], [1, 128]]), yo))
        P.pop()
    if "att" in ph:
        attn_run(g, NCX, NC, lambda qb, yo: P.dma(DV(g.y_d[2], qb * 128, [[NS, 128], [128 * NS, 4], [1, 128]]), yo))
    if "merge" in ph:
        merge_run(g, NCX, NC)
    P.wait_all(); P.emit(); P.close()
    return nc, g


def topk_threshold(g, aff, nblk, cap, tau, iters=30):
    P = g.P
    P.push()
    lo = P.sb([128, 16]); mid = P.sb([128, 16]); cmp_ = P.sb([128, 16, nblk])
    cnt = P.sb([128, 16]); ge = P.sb([128, 16])
    P.memset(lo, 0.0)
    affv = V(aff, [[1, 16], [16, nblk]])
    for it in range(iters):
        cst = 2.0 ** (-(it + 1))
        P.ts(mid, lo, cst, None, op0=ALU.add)
        P.tt(cmp_, affv, V(mid, [[1, 16], [0, nblk]]), ALU.is_ge)
        P.reduce(cnt, cmp_)
        ps = g.pbank()
        P.mm(ps[:, 0:16], g.c["ones_f"], cnt)
        P.ts(ge, ps[:, 0:16], float(cap) - 0.5, cst, op0=ALU.is_ge, op1=ALU.mult)
        P.tt(lo, lo, ge, ALU.add)
    P.copy(tau, lo)
    P.pop()


C_INPUTS = [("norm2_g", [1024]), ("modT", [128, 48, 2]), ("final_norm_g", [1024]), ("ident", [128, 128])]
C_INPUTS_BF = [("w_e_gate", [16 * 1024, 1024]), ("w_e_up", [16 * 1024, 1024]), ("w_e_down", [16 * 1024, 1024])]


def build_C(T, n_total):
    nc = bass.Bass("TRN2", target_bir_lowering=False)
    g = G(); g.nc = nc; g.P = Prog(nc); P = g.P
    P.init_arenas(22 * 1024, 50 * 1024)
    NCX = 2; NC = T // 128; NS = (NCX + NC) * 128
    g.dram = {}
    declare_inputs(g, C_INPUTS, F32)
    declare_inputs(g, C_INPUTS_BF, BF16)
    declare_inputs(g, [("x1T", [8, 128, NS]), ("aff_all", [n_total, 16]), ("aff_own", [NS, 16])], F32)
    x2_d = nc.dram_tensor("x2T", [8, 128, NS], F32, kind="ExternalOutput").ap()
    fin_d = nc.dram_tensor("finT", [8, 128, NS], F32, kind="ExternalOutput").ap()
    setup_psum(g)
    g.c = {}
    g.c["ident"] = P.sb([128, 128]); P.dma(g.c["ident"], g.dram["ident"])
    g.ones_bf = P.sb([128, 128], BF16); P.memset(g.ones_bf, 1.0)
    g.c["ones_f"] = P.sb([128, 128], F32); P.memset(g.c["ones_f"], 1.0)
    g.eps_col = P.sb([128, 1]); P.memset(g.eps_col, 1e-6)
    load_mod(g)
    gfin = P.sb([128, 8]); P.dma(gfin, DV(g.dram["final_norm_g"], 0, [[1, 128], [128, 8]]), allow_slow_non_contiguous=True)
    tau = P.sb([128, 16]); tauc = P.sb([128, 16])
    nblk = n_total // 128
    P.push()
    affa = P.sb([128, nblk, 16])
    P.dma(affa, DV(g.dram["aff_all"], 0, [[16, 128], [2048, nblk], [1, 16]]))
    topk_threshold(g, affa, nblk, 2 * n_total // 16, tau)
    P.pop()
    tiles = [(c, m, True) for (c, m) in col_tiles(0, NCX * 128, 256)] + [(c, m, False) for (c, m) in col_tiles(NCX * 128, T, 512)]
    GROUP = 3
    groups = [tiles[i:i + GROUP] for i in range(0, len(tiles), GROUP)]
    affc = P.sb([128, NCX, 16])
    P.dma(affc, DV(g.dram["aff_own"], 0, [[16, 128], [2048, NCX], [1, 16]]))
    topk_threshold(g, affc, NCX, 2 * NCX * 128 // 16, tauc)
    for grp in groups:
        P.push()
        ncols = sum(n for (_, n, _) in grp)
        gs0 = grp[0][0]
        yacc = P.sb([128, 8, ncols])
        h2b = P.sb([128, 8, ncols], BF16)
        coef = P.sb([128, ncols // 128, 16])
        xres = P.sb([128, 8, ncols]) if False else None
        for (s0, n, is_ctx) in grp:
            P.push()
            o = s0 - gs0
            xt = P.sb([128, 8, n]); P.dma(xt, DV(g.dram["x1T"], s0, [[NS, 128], [128 * NS, 8], [1, n]]))
            mod_norm(g, xt, n, g.a2, g.b2, h2b[:, :, o:o + n], 1 if is_ctx else 0)
            aff = P.sb([128, n // 128, 16])
            P.dma(aff, DV(g.dram["aff_own"], s0 * 16, [[16, 128], [2048, n // 128], [1, 16]]))
            tb = V(tauc if is_ctx else tau, [[0, n // 128], [1, 16]])
            msk = P.sb([128, n // 128, 16])
            P.tt(msk, aff, tb, ALU.is_ge)
            P.tt(coef[:, o // 128:(o + n) // 128, :], aff, msk, ALU.mult)
            P.pop()
        for e in range(16):
            P.push()
            wg = load_w(g, "w_e_gate", e * 1024, 1024, 0, 1024)
            wu = load_w(g, "w_e_up", e * 1024, 1024, 0, 1024)
            wd = load_w(g, "w_e_down", e * 1024, 1024, 0, 1024)
            for (s0, n, is_ctx) in grp:
                P.push()
                o = s0 - gs0
                pcb = g.pbank()
                for blk in range(n // 128):
                    cb_ = coef[:, o // 128 + blk, e:e + 1]
                    P.mm(pcb[:, blk * 128:(blk + 1) * 128], V(cb_, [[0, 128]]), g.c["ident"])
                cbs = P.sb([128, n])
                P.copy(cbs, pcb[:, 0:n], eng="act")
                hid = P.sb([128, 8, n], BF16)
                for f in range(8):
                    pg = g.pbank(); pu = g.pbank()
                    for k in range(8):
                        P.mm(pg[:, 0:n], wg[:, k, f * 128:(f + 1) * 128], h2b[:, k, o:o + n], start=(k == 0), stop=(k == 7))
                    for k in range(8):
                        P.mm(pu[:, 0:n], wu[:, k, f * 128:(f + 1) * 128], h2b[:, k, o:o + n], start=(k == 0), stop=(k == 7))
                    sg = P.sb([128, n])
                    P.act(sg, pg[:, 0:n], AF.Silu)
                    P.tt(sg, sg, pu[:, 0:n], ALU.mult)
                    P.tt(hid[:, f, :], sg, cbs, ALU.mult, eng="pool")
                for j in range(8):
                    pd_ = g.pbank()
                    for f in range(8):
                        P.mm(pd_[:, 0:n], wd[:, f, j * 128:(j + 1) * 128], hid[:, f, :], start=(f == 0), stop=(f == 7))
                    if e == 0:
                        P.copy(yacc[:, j, o:o + n], pd_[:, 0:n], eng="act")
                    else:
                        P.tt(yacc[:, j, o:o + n], yacc[:, j, o:o + n], pd_[:, 0:n], ALU.add)
                P.pop()
            P.pop()
        for (s0, n, is_ctx) in grp:
            P.push()
            o = s0 - gs0
            v = 1 if is_ctx else 0
            xt = P.sb([128, 8, n]); P.dma(xt, DV(g.dram["x1T"], s0, [[NS, 128], [128 * NS, 8], [1, n]]))
            for j in range(8):
                P.stt(xt[:, j, :], yacc[:, j, o:o + n], g.g2[:, j, v:v + 1], xt[:, j, :], ALU.mult, ALU.add)
            P.dma(DV(x2_d, s0, [[NS, 128], [128 * NS, 8], [1, n]]), xt)
            rstd = P.sb([128, n])
            rms_rstd(g, xt, n, rstd)
            for j in range(8):
                P.stt(xt[:, j, :], xt[:, j, :], gfin[:, j:j + 1], rstd, ALU.mult, ALU.mult)
            P.dma(DV(fin_d, s0, [[NS, 128], [128 * NS, 8], [1, n]]), xt)
            P.pop()
        P.pop()
    P.wait_all(); P.emit(); P.close()
    return nc, g


NCORES = 8


def s5_carry_chain(g, NC):
    P = g.P
    s = g.s5
    NCX = 2
    dre = P.sb([128, 32]); dim_ = P.sb([128, 32]); t1 = P.sb([128, 32]); t2 = P.sb([128, 32])
    for half, last in ((slice(0, 64), NCX + NC - 1), (slice(64, 128), NCX)):
        cmul_acc(P, dre[half], dim_[half], s.aqr[half], s.aqi[half], s.apow_re[half, :, last], s.apow_im[half, :, last],
                 None, None, t1[half], t2[half])
    fin = P.sb([128, NCORES, 32, 2])
    P.dma(fin, DV(g.dram["s5_fin_all"], 0, [[64, 128], [128 * 64, NCORES], [1, 64]]))
    H = P.sb([128, NCORES + 1, 32, 2])
    P.copy(H[:, 0, :, 0], s.fin_re[:, :, 0]); P.copy(H[:, 0, :, 1], s.fin_im[:, :, 0])
    for t in range(NCORES):
        for half, m in ((slice(0, 64), t), (slice(64, 128), NCORES - 1 - t)):
            cmul_acc(P, H[half, t + 1, :, 0], H[half, t + 1, :, 1], dre[half], dim_[half], H[half, t, :, 0], H[half, t, :, 1],
                     fin[half, m, :, 0], fin[half, m, :, 1], t1[half], t2[half])
    cr = P.sb([128, 32]); ci = P.sb([128, 32])
    P.memset(cr, 0.0); P.memset(ci, 0.0)
    for t in range(NCORES):
        for half, oh in ((slice(0, 64), g.onehot[0:64, t:t + 1]), (slice(64, 128), g.onehot[64:128, NCORES - 1 - t:NCORES - t])):
            P.stt(cr[half], H[half, t, :, 0], oh, cr[half], ALU.mult, ALU.add)
            P.stt(ci[half], H[half, t, :, 1], oh, ci[half], ALU.mult, ALU.add)
    return cr, ci


def ssd_carry(g, dr_, hctx, out):
    P = g.P
    P.push()
    tot = P.sb([128, NCORES, 16])
    P.dma(tot, DV(g.dram["ssd_tot_all"], 0, [[16, 128], [128 * 16, NCORES], [1, 16]]))
    P.act(tot, tot, AF.Exp)
    H = P.sb([128, 512]); fin = P.sb([128, 512])
    P.copy(H, hctx)
    P.memset(out, 0.0)
    for t in range(NCORES):
        m = t if dr_ == 0 else NCORES - 1 - t
        P.stt(out, H, g.onehot[:, m:m + 1], out, ALU.mult, ALU.add)
        if t == NCORES - 1:
            break
        P.dma(fin, DV(g.dram["ssd_fin_all"], (m * 2 + dr_) * 128 * 512, [[512, 128], [1, 512]]))
        P.tt(V(H, [[64, 8], [1, 64]]), V(H, [[64, 8], [1, 64]]), V(tot[:, m, dr_ * 8:dr_ * 8 + 8], [[1, 8], [0, 64]]), ALU.mult)
        P.tt(H, H, fin, ALU.add)
    P.pop()


def ssd_local_finals(g, NCX, NC):
    P = g.P
    hf = P.sb([128, 512]); hb = P.sb([128, 512]); ts_ = P.sb([128, 16]); pb = P.sb([128, 8]); tmp = P.sb([128, 512])
    P.memset(hf, 0.0); P.memset(hb, 0.0); P.memset(ts_, 0.0); P.memset(pb, 1.0)
    for i in range(NC):
        c = NCX + i
        P.push()
        S, cd, tot = ssd_chunk(g, c, False, None, None)
        P.tt(V(hf, [[64, 8], [1, 64]]), V(hf, [[64, 8], [1, 64]]), V(cd[:, 0:8], [[1, 8], [0, 64]]), ALU.mult)
        P.tt(hf, hf, S[0], ALU.add)
        P.tt(V(tmp, [[64, 8], [1, 64]]), V(S[1], [[64, 8], [1, 64]]), V(pb, [[1, 8], [0, 64]]), ALU.mult)
        P.tt(hb, hb, tmp, ALU.add)
        P.tt(pb, pb, cd[:, 8:16], ALU.mult)
        P.tt(ts_, ts_, tot, ALU.add)
        P.pop()
    P.dma(g.dram["ssd_fin"][0], hf); P.dma(g.dram["ssd_fin"][1], hb); P.dma(g.dram["ssd_tot"], ts_)


W_LIST = [("w_in", 4 * 1024, IN_W), ("s5_w_glu", 4 * 512, 512), ("w_branch", 4 * 1536, 1024), ("w_out", 4 * 1024, 1024),
          ("w_e_gate", 4 * 16 * 1024, 1024), ("w_e_up", 4 * 16 * 1024, 1024), ("w_e_down", 4 * 16 * 1024, 1024)]


def build_W(wlist=W_LIST):
    nc = bass.Bass("TRN2", target_bir_lowering=False)
    g = G(); g.nc = nc; g.P = Prog(nc); P = g.P
    P.init_arenas(24 * 1024, 24 * 1024)
    setup_psum(g)
    g.dram = {}
    engs = ["dve", "act", "pool"]
    ei = 0
    for (name, rows, cols) in wlist:
        rpc = rows // NCORES
        assert rpc % 128 == 0
        src = nc.dram_tensor(name, [rpc, cols], F32, kind="ExternalInput").ap()
        dst = nc.dram_tensor(name + "_bf", [rpc, cols], BF16, kind="ExternalOutput").ap()
        rt = rpc // 128
        cc = cols
        while cc > 2048:
            cc //= 2
        rstep = max(1, 4096 // cc)
        for c0 in range(0, cols, cc):
            for r0 in range(0, rt, rstep):
                r = min(rstep, rt - r0)
                P.push()
                a = P.sb([128, r, cc]); b = P.sb([128, r, cc], BF16)
                P.dma(a, DV(src, r0 * 128 * cols + c0, [[cols, 128], [128 * cols, r], [1, cc]]))
                P.copy(b, a, eng=engs[ei % 3]); ei += 1
                P.dma(DV(dst, r0 * 128 * cols + c0, [[cols, 128], [128 * cols, r], [1, cc]]), b)
                P.pop()
    g.NG = 16
    declare_inputs(g, [("s5_lam_re", [2, 16, 64]), ("s5_lam_im", [2, 16, 64]), ("s5_log_dt", [2, 16]), ("s5_b_re", [2, 16, 64, 16]),
                       ("s5_b_im", [2, 16, 64, 16]), ("s5_c_re", [2, 16, 16, 64]), ("s5_c_im", [2, 16, 16, 64]),
                       ("exl", [128, 128]), ("exr", [128, 128]), ("exv", [128, 2])], F32)
    g.c = {}
    for k_ in ("exl", "exr", "exv"):
        t_ = P.sb(CONST_SHAPES[k_], F32); P.dma(t_, g.dram[k_]); g.c[k_] = t_
    outs = {nm: nc.dram_tensor(nm, [128, 16, 128], BF16, kind="ExternalOutput").ap() for nm in ("s5_vr", "s5_vi", "s5_w2re", "s5_nw2im")}
    kt_o = nc.dram_tensor("s5_kt", [2, 2, 2, 128, 1024], BF16, kind="ExternalOutput").ap()
    P.push()
    s5_params(g)
    P.push()
    vr = P.sb([128, 16, 128], BF16); vi = P.sb([128, 16, 128], BF16)
    s5_gen_V(g, V(vr, [[128, 16], [64, 2], [1, 64]]), V(vi, [[128, 16], [64, 2], [1, 64]]))
    P.dma(outs["s5_vr"], vr); P.dma(outs["s5_vi"], vi)
    w2 = P.sb([128, 16, 128], BF16); nw2 = P.sb([128, 16, 128], BF16)
    s5_gen_E(g, g.c["exr"], w2, nw2, neg_im=True)
    P.dma(outs["s5_w2re"], w2); P.dma(outs["s5_nw2im"], nw2)
    P.pop()
    s5_kt_build(g, 2, kt_o)
    P.pop()
    wm = nc.dram_tensor("w_mod", [1024, 6144], F32, kind="ExternalInput").ap()
    bm = nc.dram_tensor("b_mod", [6144], F32, kind="ExternalInput").ap()
    cc_ = nc.dram_tensor("c2", [2, 1024], F32, kind="ExternalInput").ap()
    mo = nc.dram_tensor("modT", [128, 48, 2], F32, kind="ExternalOutput").ap()
    sc = P.sb([128, 8, 2])
    for v in range(2):
        P.dma(sc[:, :, v], DV(cc_, v * 1024, [[1, 128], [128, 8]]), allow_slow_non_contiguous=True)
    P.act(sc, sc, AF.Silu)
    bcol = P.sb([128, 48])
    P.dma(bcol, DV(bm, 0, [[1, 128], [128, 48]]), allow_slow_non_contiguous=True)
    ps = g.pbank()
    for grp in range(12):
        P.push()
        wt = P.sb([128, 8, 512])
        P.dma(wt, DV(wm, grp * 512, [[6144, 128], [128 * 6144, 8], [1, 512]]))
        for q in range(4):
            ccix = grp * 4 + q
            for k in range(8):
                P.mm(ps[:, ccix * 2:ccix * 2 + 2], wt[:, k, q * 128:(q + 1) * 128], sc[:, k, :], start=(k == 0), stop=(k == 7))
        P.pop()
    mt = P.sb([128, 48, 2])
    P.tt(mt, V(ps, [[2, 48], [1, 2]]), V(bcol, [[1, 48], [0, 2]]), ALU.add)
    P.dma(mo, mt)
    P.wait_all(); P.emit(); P.close()
    return nc, g


def setup_psum(g):
    P = g.P
    g._pb = [P.ps([128, 512], F32) for _ in range(6)]
    g._pb_o = g._pb[4][:, :]
    g._pb_d = g._pb[5][:, :]
    g._nrot = 6
    g._pbf = [P.ps([128, 1024], BF16) for _ in range(2)]
    g._pi = 0
    g._pbi = 0

    def pbank():
        t = g._pb[g._pi % g._nrot]
        g._pi += 1
        return t[:, :]

    def pbank_bf():
        h = g._pbi % 2
        g._pbi += 1
        return g._pbf[h][:, 0:512]
    g.pbank = pbank
    g.pbank_bf = pbank_bf


BF=ml_dtypes.bfloat16

def rope_tables(own_start, T, n_total):
    NL=T+256
    pos=own_start+np.arange(NL)-128
    valid=(pos>=0)&(pos<n_total)
    pos=np.where(valid,pos,0)
    d=np.arange(64); which=d//32; i=d%16; first=(d%32)<16
    inv=10000.0**(-(i.astype(np.float64))/16)
    axis=np.where(which[:,None]==0, (pos//64)[None,:], (pos%64)[None,:]).astype(np.float64)
    ang=axis*inv[:,None]
    cos=np.cos(ang); sin=np.where(first[:,None], -np.sin(ang), np.sin(ang))
    return np.tile(cos,(2,1)).astype(np.float32), np.tile(sin,(2,1)).astype(np.float32)

def pswap():
    m=np.zeros((128,128),np.float32)
    for j in range(128):
        p = j+16 if (j%32)<16 else j-16
        m[p,j]=1.0
    return m

def fm(a, ncols):
    return np.ascontiguousarray(a.T.reshape(8,128,ncols))

def consts_all():
    c=host_consts(); c["pswap"]=pswap(); return c

def modT_from(mod2):
    return np.ascontiguousarray(mod2.reshape(2,48,128).transpose(2,1,0)).astype(np.float32)


import numpy as np, ml_dtypes
import os

_CACHE = {}


def _run(nc_, maps, ids):
    if os.environ.get('ORCH_TRACE'):
        r = run_bass_kernel_spmd(nc_, maps, core_ids=ids, trace=True)
        print('exec_time_ns', r.exec_time_ns, flush=True)
        return r
    return run_bass_kernel_spmd(nc_, maps, core_ids=ids)

def _prog(key, fn):
    if key not in _CACHE:
        _CACHE[key] = fn()[0]
    return _CACHE[key]

def run_model(inputs, T, ncores=NCORES, depth=4, hook=None):
    assert ncores == NCORES
    n = ncores * T
    NS = T + 256
    f32 = lambda a: np.ascontiguousarray(np.asarray(a, dtype=np.float32))
    ncW = _prog(("W",), build_W)
    flat = {"w_in": f32(inputs["w_in"]).reshape(-1, IN_W), "s5_w_glu": f32(inputs["s5_w_glu"]).reshape(-1, 512),
            "w_branch": f32(inputs["w_branch"]).reshape(-1, 1024), "w_out": f32(inputs["w_out"]).reshape(-1, 1024),
            "w_e_gate": f32(inputs["w_e_gate"]).reshape(-1, 1024), "w_e_up": f32(inputs["w_e_up"]).reshape(-1, 1024),
            "w_e_down": f32(inputs["w_e_down"]).reshape(-1, 1024)}
    c2 = np.stack([f32(inputs["c"])[0], f32(inputs["c_ctx"])], 0)
    maps = []
    for k in range(ncores):
        m = {}
        for name, rows, cols in W_LIST:
            rpc = rows // ncores
            m[name] = np.ascontiguousarray(flat[name][k * rpc:(k + 1) * rpc])
        nl = 4
        l = k % nl
        m["w_mod"] = f32(inputs["w_mod"][l]); m["b_mod"] = f32(inputs["b_mod"][l]); m["c2"] = c2
        hf_ = k // nl
        gsl = slice(hf_ * 16, hf_ * 16 + 16)
        for nm in ("s5_lam_re", "s5_lam_im", "s5_log_dt", "s5_b_re", "s5_b_im", "s5_c_re", "s5_c_im"):
            m[nm] = np.ascontiguousarray(f32(inputs[nm][l])[:, gsl])
        hc_ = host_consts()
        for nm in ("exl", "exr", "exv"):
            m[nm] = hc_[nm]
        maps.append(m)
    res = _run(ncW, maps, list(range(ncores))).results
    wbf = {name: np.concatenate([np.asarray(res[k][name + "_bf"]) for k in range(ncores)], 0) for name, _, _ in W_LIST}
    nl = 4
    modT = [np.asarray(res[l]["modT"]) for l in range(nl)]
    s5tab = []
    for l in range(nl):
        d_ = {nm: np.concatenate([np.asarray(res[l][nm]), np.asarray(res[l + nl][nm])], 1) for nm in ("s5_vr", "s5_vi", "s5_w2re", "s5_nw2im")}
        d_["s5_kt"] = np.concatenate([np.asarray(res[l]["s5_kt"]), np.asarray(res[l + nl]["s5_kt"])], 0)
        s5tab.append(d_)
    if hook: hook("W", dict(wbf=wbf, modT=modT))
    consts = consts_all()
    ropes = [rope_tables(k * T, T, n) for k in range(ncores)]
    flags = []
    onehots = []
    for k in range(ncores):
        fl = np.ones((128, 2), np.float32)
        if k == 0: fl[:, 0] = 0
        if k == ncores - 1: fl[:, 1] = 0
        flags.append(fl)
        oh = np.zeros((128, ncores), np.float32); oh[:, k] = 1
        onehots.append(oh)
    x = f32(inputs["x"])[0]
    xc = f32(inputs["ctx"])[0]
    ncA = _prog(("A", T), lambda: build_B(T, multi=True, mode="A"))
    ncB = _prog(("B", T), lambda: build_B(T, multi=True, mode="B"))
    ncC = _prog(("C", T, n), lambda: build_C(T, n))
    fin = None
    for l in range(depth):
        lw = lambda name: f32(inputs[name][l])
        base = dict(consts)
        for name, _ in B_INPUTS:
            if name in inputs: base[name] = lw(name)
        base["modT"] = modT[l]
        base.update(s5tab[l])
        base["w_in"] = wbf["w_in"][l * 1024:(l + 1) * 1024]
        base["s5_w_glu"] = wbf["s5_w_glu"][l * 512:(l + 1) * 512]
        base["w_branch"] = wbf["w_branch"][l * 1536:(l + 1) * 1536]
        base["w_out"] = wbf["w_out"][l * 1024:(l + 1) * 1024]
        xpad = np.concatenate([np.zeros((128, 1024), np.float32), x, np.zeros((128, 1024), np.float32)], 0)
        xcT = fm(xc, 256)
        maps = []
        for k in range(ncores):
            m = dict(base)
            m["flags"] = flags[k]
            m["xT"] = fm(xpad[k * T:k * T + T + 256], T + 256)
            m["xcT"] = xcT
            m["rope_cos"], m["rope_sin"] = ropes[k]
            maps.append(m)
        ra = _run(ncA, maps, list(range(ncores))).results
        s5_fin_all = np.stack([np.asarray(ra[k]["s5_fin"]) for k in range(ncores)], 0)
        ssd_fin_all = np.stack([np.asarray(ra[k]["ssd_fin"]) for k in range(ncores)], 0)
        ssd_tot_all = np.stack([np.asarray(ra[k]["ssd_tot"]) for k in range(ncores)], 0)
        for k in range(ncores):
            maps[k]["s5_fin_all"] = s5_fin_all; maps[k]["ssd_fin_all"] = ssd_fin_all; maps[k]["ssd_tot_all"] = ssd_tot_all
            maps[k]["onehot"] = onehots[k]
        rb = _run(ncB, maps, list(range(ncores))).results
        aff_all = np.concatenate([np.asarray(rb[k]["aff"])[256:] for k in range(ncores)], 0)
        if hook: hook(("B", l), dict(rb=rb))
        cmaps = []
        for k in range(ncores):
            cm = {"ident": consts["ident"], "norm2_g": lw("norm2_g"), "modT": modT[l], "final_norm_g": f32(inputs["final_norm_g"]),
                  "w_e_gate": wbf["w_e_gate"][l * 16384:(l + 1) * 16384], "w_e_up": wbf["w_e_up"][l * 16384:(l + 1) * 16384],
                  "w_e_down": wbf["w_e_down"][l * 16384:(l + 1) * 16384],
                  "x1T": np.asarray(rb[k]["x1T"]), "aff_all": aff_all, "aff_own": np.asarray(rb[k]["aff"])}
            cmaps.append(cm)
        rc = _run(ncC, cmaps, list(range(ncores))).results
        unfm = lambda a: np.asarray(a).reshape(1024, -1).T
        x = np.concatenate([unfm(rc[k]["x2T"])[256:] for k in range(ncores)], 0)
        xc = unfm(rc[0]["x2T"])[:256]
        fin = np.concatenate([unfm(rc[k]["finT"])[256:] for k in range(ncores)], 0)
        if hook: hook(("C", l), dict(x=x, xc=xc))
    return np.ascontiguousarray(fin[None].astype(np.float32))


def kernel(**inputs):
    return run_model(inputs, 2048)
```

```python
import ml_dtypes
import contextlib
import os
import numpy as np
import concourse.bass as bass
import concourse.mybir as mybir
from concourse.bass_utils import run_bass_kernel_spmd

F32 = mybir.dt.float32
BF16 = mybir.dt.bfloat16
I32 = mybir.dt.int32
AF = mybir.ActivationFunctionType
ALU = mybir.AluOpType
AX = mybir.AxisListType

ENGS = ("pe", "dve", "act", "pool", "sp")
EPOCH = 20000
N_DMA_SEMS = 12


def _region(ap):
    t = ap.tensor
    shape = list(t.shape)
    space = str(ap.space) if hasattr(ap, "space") else ""
    dims = list(ap.ap)
    off = int(ap.offset)
    if "DRAM" in space.upper() or "HBM" in space.upper() or type(t).__name__.startswith("DRam"):
        lo = off
        hi = off + sum((c - 1) * abs(s) for s, c in dims) + 1
        return (t.name, 0, 1, lo, hi)
    row = 1
    for s in shape[1:]:
        row *= int(s)
    if type(t).__name__.startswith("PSum"):
        return (t.name, 0, 128, 0, row)
    p_lo = off // row
    f_lo = off % row
    p_hi = p_lo + int(dims[0][1])
    f_hi = f_lo + sum((c - 1) * abs(s) for s, c in dims[1:]) + 1
    return (t.name, p_lo, p_hi, f_lo, f_hi)


def _ovl(a, b):
    return a[1] < b[2] and b[1] < a[2] and a[3] < b[4] and b[3] < a[4]


def _covers(a, b):
    return a[1] <= b[1] and a[2] >= b[2] and a[3] <= b[3] and a[4] >= b[4]


class Prog:
    def __init__(self, nc, same_engine_sync=True):
        self.nc = nc
        self.es = contextlib.ExitStack()
        self.ops = {e: [] for e in ENGS}
        self.nops = {e: 0 for e in ENGS}
        self.writes = {}
        self.reads = {}
        import os
        self.same_engine_sync = same_engine_sync and os.environ.get('SAMESYNC', '1') == '1'
        self.dma_tot = [0] * N_DMA_SEMS
        self.dma_rr = 0
        self.known = {e: {} for e in ENGS}
        self._names = 0

    def init_arenas(self, n_f32, n_bf16):
        self.arena = {F32: self.es.enter_context(self.nc.sbuf_tensor("arena_f32", [128, n_f32], F32)),
                      BF16: self.es.enter_context(self.nc.sbuf_tensor("arena_bf16", [128, n_bf16], BF16))}
        self.asize = {F32: n_f32, BF16: n_bf16}
        self.atop = {F32: 0, BF16: 0}
        self.amax = {F32: 0, BF16: 0}
        self.astack = []

    def push(self):
        self.astack.append(dict(self.atop))
        self.pstack = getattr(self, "pstack", [])
        self.pstack.append(dict(self.atop))

    def pop(self):
        pk = self.pstack.pop()
        if self.pstack:
            for k in pk:
                self.pstack[-1][k] = max(self.pstack[-1][k], pk[k])
        self.last_peak = pk
        self.atop = self.astack.pop()

    def iter_push(self, i, tag):
        self.push()
        self._pp = getattr(self, "_pp", {})
        if i % 2 == 1 and tag in self._pp:
            need = self._pp[tag]
            ok = all(self.atop[dt] + 2 * need[dt] + 64 <= self.asize[dt] for dt in need)
            if ok:
                for dt in need:
                    if need[dt] > 0:
                        self.sb([128, need[dt]], dt)
        self._pp_base = getattr(self, "_pp_base", {})
        self._pp_base[(tag, i)] = dict(self.atop)

    def iter_pop(self, i, tag):
        base = self._pp_base.pop((tag, i))
        pk = self.pstack[-1]
        if i == 0:
            self._pp[tag] = {dt: pk[dt] - base[dt] for dt in pk}
        self.pop()

    def sb(self, shape, dtype=F32, name=None):
        n = 1
        for s_ in shape[1:]:
            n *= int(s_)
        n = (n + 15) // 16 * 16
        off = self.atop[dtype]
        assert off + n <= self.asize[dtype], f"arena {dtype} overflow: {off}+{n} > {self.asize[dtype]} ({name})"
        self.atop[dtype] = off + n
        self.amax[dtype] = max(self.amax[dtype], off + n)
        if getattr(self, "pstack", None):
            self.pstack[-1][dtype] = max(self.pstack[-1][dtype], off + n)
        nn = 1
        for s_ in shape[1:]:
            nn *= int(s_)
        v = self.arena[dtype][:, off:off + nn]
        if len(shape) > 2:
            names = [f"d{i}" for i in range(len(shape) - 1)]
            pat = "p (" + " ".join(names) + ") -> p " + " ".join(names)
            v = v.rearrange(pat, **{nm: int(sz) for nm, sz in zip(names[1:], shape[2:])})
        if shape[0] < 128:
            v = v[0:shape[0]]
        return v

    def ps(self, shape, dtype=F32, name=None):
        self._names += 1
        name = name or f"ps{self._names}"
        return self.es.enter_context(self.nc.psum_tensor(name, list(shape), dtype))

    def _deps(self, reads, writes):
        deps = set()
        rr = [_region(a) for a in reads]
        wr = [_region(a) for a in writes]
        self._raw = set()
        for r in rr:
            for (reg, ev) in self.writes.get(r[0], ()):
                if _ovl(reg, r):
                    deps.add(ev)
                    self._raw.add(ev)
            if r[0].startswith("ps"):
                for (reg, ev) in self.reads.get(r[0], ()):
                    deps.add(ev)
        for w in wr:
            for (reg, ev) in self.writes.get(w[0], ()):
                if _ovl(reg, w):
                    deps.add(ev)
            for (reg, ev) in self.reads.get(w[0], ()):
                if _ovl(reg, w):
                    deps.add(ev)
        return deps, rr, wr

    def _record(self, rr, wr, ev):
        for w in wr:
            lst = self.writes.setdefault(w[0], [])
            lst[:] = [(reg, e) for (reg, e) in lst if not _covers(w, reg)]
            lst.append((w, ev))
            rl = self.reads.get(w[0])
            if rl:
                rl[:] = [(reg, e) for (reg, e) in rl if not _covers(w, reg)]
        for r in rr:
            lst = self.reads.setdefault(r[0], [])
            lst[:] = [(reg, e) for (reg, e) in lst if not (e[0] == ev[0] and _covers(r, reg))]
            lst.append((r, ev))

    def op(self, eng, fn, reads, writes, pe_accum=False):
        deps, rr, wr = self._deps(reads, writes)
        idx = self.nops[eng]
        self.nops[eng] += 1
        ev = ((eng, idx // EPOCH), idx % EPOCH + 1)
        waits = self._filter(eng, deps, pe_accum)
        self.ops[eng].append((fn, waits, ev, False))
        self._record(rr, wr, ev)
        return ev

    def _filter(self, eng, deps, pe_accum=False):
        best = {}
        relax = os.environ.get("RELAX_WAR", "0") == "1"
        for (s, v) in deps:
            if s[0] == eng:
                if eng == "pe" or not self.same_engine_sync:
                    continue
                if relax and (s, v) not in getattr(self, "_raw", ()):
                    continue
            if best.get(s, 0) < v:
                best[s] = v
        out = []
        kn = self.known[eng]
        for s, v in best.items():
            if kn.get(s, 0) >= v:
                continue
            kn[s] = v
            out.append((s, v))
        return out

    def dma(self, out, in_, q="sp", **kw):
        deps, rr, wr = self._deps([in_], [out])
        k = self.dma_rr
        self.dma_rr = (self.dma_rr + 1) % N_DMA_SEMS
        sem = ("dma", k)
        prev = self.dma_tot[k]
        if prev:
            deps.add((sem, prev))
        self.dma_tot[k] += 16
        ev = (sem, self.dma_tot[k])
        waits = self._filter(q, deps)
        self.nops[q] += 0
        self.ops[q].append((lambda e, o=out, i=in_, kw=kw: e.dma_start(out=o, in_=i, **kw), waits, ev, True))
        self._record(rr, wr, ev)
        return ev

    def allgather(self, out, in_, n=8):
        deps, rr, wr = self._deps([in_], [out])
        self.cc_tot = getattr(self, "cc_tot", 0) + 1
        ev = (("cc", 0), self.cc_tot)
        if self.cc_tot > 1:
            deps.add((("cc", 0), self.cc_tot - 1))
        waits = self._filter("pool", deps)
        self.ops["pool"].append((lambda e, o=out, i=in_: e.collective_compute(
            "AllGather", ALU.bypass, replica_groups=[list(range(n))], ins=[i], outs=[o]), waits, ev, "cc"))
        self._record(rr, wr, ev)
        return ev

    def wait_all(self, eng="sp"):
        deps = set()
        for lst in self.writes.values():
            for (_, ev) in lst:
                deps.add(ev)
        waits = self._filter(eng, deps)
        self.ops[eng].append((None, waits, None, False))

    def mm(self, out, lhsT, rhs, start=True, stop=True):
        rd = [lhsT, rhs] + ([] if start else [])
        return self.op("pe", lambda e: e.matmul(out, lhsT, rhs, start=start, stop=stop), rd, [out])

    def transpose(self, out, in_, ident):
        return self.op("pe", lambda e: e.transpose(out, in_, ident), [in_, ident], [out])

    def act(self, out, in_, func, bias=0.0, scale=1.0, accum_out=None):
        rd = [in_] + [a for a in (bias, scale) if not isinstance(a, (int, float))]
        wr = [out] + ([accum_out] if accum_out is not None else [])
        kw = {}
        if accum_out is not None:
            kw["accum_out"] = accum_out
        return self.op("act", lambda e: e.activation(out, in_, func, bias=bias, scale=scale, **kw), rd, wr)

    def tt(self, out, in0, in1, op, eng="dve"):
        return self.op(eng, lambda e: e.tensor_tensor(out, in0, in1, op), [in0, in1], [out])

    def ts(self, out, in0, s1, s2=None, op0=ALU.mult, op1=None, eng="dve", accum_out=None):
        rd = [in0] + [a for a in (s1, s2) if a is not None and not isinstance(a, (int, float))]
        wr = [out] + ([accum_out] if accum_out is not None else [])
        kw = {}
        if op1 is not None:
            kw["op1"] = op1
        if accum_out is not None:
            kw["accum_out"] = accum_out
        return self.op(eng, lambda e: e.tensor_scalar(out, in0, s1, s2, op0, **kw), rd, wr)

    def stt(self, out, in0, scalar, in1, op0, op1, eng="dve"):
        rd = [in0, in1] + ([] if isinstance(scalar, (int, float)) else [scalar])
        return self.op(eng, lambda e: e.scalar_tensor_tensor(out, in0, scalar, in1, op0, op1), rd, [out])

    def copy(self, out, in_, eng="dve"):
        if eng == "act":
            return self.op("act", lambda e: e.copy(out, in_), [in_], [out])
        return self.op(eng, lambda e: e.tensor_copy(out, in_), [in_], [out])

    def memset(self, ap, val, eng="dve"):
        return self.op(eng, lambda e: e.memset(ap, val), [], [ap])

    def reduce(self, out, in_, op=ALU.add, axis=AX.X, eng="dve"):
        return self.op(eng, lambda e: e.tensor_reduce(out, in_, axis, op), [in_], [out])

    def recip(self, out, in_):
        return self.op("dve", lambda e: e.reciprocal(out, in_), [in_], [out])

    def scan(self, out, d0, d1, initial, op0=ALU.mult, op1=ALU.add):
        rd = [d0, d1] + ([] if isinstance(initial, (int, float)) else [initial])
        return self.op("dve", lambda e: e.tensor_tensor_scan(out, d0, d1, initial, op0, op1), rd, [out])

    def emit(self):
        nc = self.nc
        sems = {}
        for e in ("pe", "dve", "act", "pool"):
            n_ep = (self.nops[e] + EPOCH - 1) // EPOCH
            for k in range(max(n_ep, 1)):
                sems[(e, k)] = self.es.enter_context(nc.semaphore(f"s_{e}{k}"))
        sems[("cc", 0)] = self.es.enter_context(nc.semaphore("s_cc"))
        for k in range(N_DMA_SEMS):
            sems[("dma", k)] = self.es.enter_context(nc.semaphore(f"s_dma{k}"))
        block = self.es.enter_context(nc.Block())

        def run(engobj, lst):
            for (fn, waits, ev, is_dma) in lst:
                for (s, v) in waits:
                    engobj.wait_ge(sems[s], v)
                if fn is None:
                    continue
                ins = fn(engobj)
                if is_dma == "cc":
                    ins.then_inc(sems[ev[0]])
                elif is_dma:
                    ins.then_inc(sems[ev[0]], 16)
                else:
                    ins.then_inc(sems[ev[0]], 1)

        ops = self.ops

        @block.tensor
        def _(t):
            run(t, ops["pe"])

        @block.vector
        def _(v):
            run(v, ops["dve"])

        @block.scalar
        def _(s):
            run(s, ops["act"])

        @block.gpsimd
        def _(g):
            run(g, ops["pool"])

        @block.sync
        def _(sy):
            run(sy, ops["sp"])

    def close(self):
        self.es.close()


import math
import numpy as np

PI = math.pi
D = 1024
KD = 8
NCTX = 256
HALO = 128
IN_W = 5904
C_U, C_Z, C_XBC, C_DT, C_Q, C_K, C_V, C_G = 0, 512, 1024, 2048, 2064, 2576, 2704, 2832


def V(ap, free_dims, off=0):
    return bass.AP(ap.tensor, ap.offset + off, [list(ap.ap[0])] + [list(d) for d in free_dims])


def DV(t, off, dims):
    return bass.AP(t.tensor, t.offset + off, [list(d) for d in dims])


class G:
    pass


def host_consts():
    c = {}
    c["ident"] = np.eye(128, dtype=np.float32)
    c["ut"] = np.triu(np.ones((128, 128), np.float32))
    c["lt"] = np.tril(np.ones((128, 128), np.float32))
    d = np.arange(128, dtype=np.float32)
    exl = np.tile(d[None, :], (128, 1))
    exr = np.concatenate([np.tile((d + 1)[None, :], (64, 1)), np.tile((128 - d)[None, :], (64, 1))], 0)
    c["exl"] = exl.astype(np.float32)
    c["exr"] = exr.astype(np.float32)
    exv = np.stack([127 - d, d], 1)
    c["exv"] = exv.astype(np.float32)
    m = np.zeros((128, 8), np.float32)
    for p in range(128):
        m[p, p // 16] = 1.0
    c["maskbd"] = m
    return c


CONST_SHAPES = {"ident": [128, 128], "ut": [128, 128], "lt": [128, 128], "exl": [128, 128], "exr": [128, 128],
                "exv": [128, 2], "maskbd": [128, 8]}


def load_consts(g):
    P = g.P
    g.c = {}
    for k, shp in CONST_SHAPES.items():
        t = P.sb(shp, F32)
        P.dma(t, g.dram[k])
        g.c[k] = t
    g.ident_bf = P.sb([128, 128], BF16)
    P.copy(g.ident_bf, g.c["ident"])
    g.ones_bf = P.sb([128, 128], BF16)
    P.memset(g.ones_bf, 1.0)
    g.c["ones_f"] = P.sb([128, 128], F32)
    P.memset(g.c["ones_f"], 1.0)


def sincos(P, out_cos, out_sin, ang, tmp):
    n = 1
    for d_ in ang.shape[1:]:
        n *= int(d_)
    if not hasattr(P, "_kint"):
        P._kint = P.es.enter_context(P.nc.sbuf_tensor("kint", [128, 2048], I32))
        P._halfpi = P.sb([128, 1], F32)
        P.memset(P._halfpi, PI / 2)
    ki = bass.AP(P._kint[:, 0:n].tensor, P._kint[:, 0:n].offset, [list(ang.ap[0])[:1] + [ang.ap[0][1]]] and [[P._kint[:, 0:n].ap[0][0], ang.ap[0][1]], [1, n]])
    angf = bass.AP(ang.tensor, ang.offset, [list(ang.ap[0]), [1, n]])
    tmpf = bass.AP(tmp.tensor, tmp.offset, [list(tmp.ap[0]), [1, n]])
    cosf = bass.AP(out_cos.tensor, out_cos.offset, [list(out_cos.ap[0]), [1, n]])
    sinf = bass.AP(out_sin.tensor, out_sin.offset, [list(out_sin.ap[0]), [1, n]])
    pp = slice(0, 128)
    P.ts(ki, angf, 1.0 / (2 * PI), 0.25, op0=ALU.mult, op1=ALU.add)
    P.stt(tmpf, ki, -2 * PI, angf, ALU.mult, ALU.add)
    P.act(cosf, tmpf, AF.Sin, bias=P._halfpi[0:ang.ap[0][1]] if ang.ap[0][1] < 128 else P._halfpi, scale=1.0)
    P.ts(ki, angf, 1.0 / (2 * PI), None, op0=ALU.mult)
    P.stt(tmpf, ki, -2 * PI, angf, ALU.mult, ALU.add)
    P.act(sinf, tmpf, AF.Sin)


def s5_params(g):
    P = g.P
    dr = g.dram
    s = G()
    g.s5 = s
    NG = getattr(g, "NG", 32)
    s.lrc = P.sb([128, NG]); s.lic = P.sb([128, NG]); s.dtc = P.sb([128, NG])
    for d in range(2):
        P.dma(s.lrc[d * 64:(d + 1) * 64], DV(dr["s5_lam_re"], d * NG * 64, [[1, 64], [64, NG]]), allow_slow_non_contiguous=True)
        P.dma(s.lic[d * 64:(d + 1) * 64], DV(dr["s5_lam_im"], d * NG * 64, [[1, 64], [64, NG]]), allow_slow_non_contiguous=True)
        P.dma(s.dtc[d * 64:(d + 1) * 64], DV(dr["s5_log_dt"], d * NG, [[0, 64], [1, NG]]))
    P.act(s.dtc, s.dtc, AF.Exp)
    s.thc = P.sb([128, NG]); s.lrdtc = P.sb([128, NG])
    P.tt(s.thc, s.lic, s.dtc, ALU.mult)
    P.tt(s.lrdtc, s.lrc, s.dtc, ALU.mult)
    mag = P.sb([128, NG]); co = P.sb([128, NG]); si = P.sb([128, NG]); tmp = P.sb([128, NG])
    P.act(mag, s.lrdtc, AF.Exp)
    sincos(P, co, si, s.thc, tmp)
    abr = P.sb([128, NG]); abi = P.sb([128, NG])
    P.tt(abr, mag, co, ALU.mult)
    P.tt(abi, mag, si, ALU.mult)
    den = P.sb([128, NG]); t2 = P.sb([128, NG])
    P.tt(den, s.lrc, s.lrc, ALU.mult)
    P.tt(t2, s.lic, s.lic, ALU.mult)
    P.tt(den, den, t2, ALU.add)
    P.recip(den, den)
    am1 = P.sb([128, NG])
    P.ts(am1, abr, -1.0, None, op0=ALU.add)
    fr = P.sb([128, NG]); fi = P.sb([128, NG])
    P.tt(fr, am1, s.lrc, ALU.mult); P.tt(t2, abi, s.lic, ALU.mult); P.tt(fr, fr, t2, ALU.add); P.tt(fr, fr, den, ALU.mult)
    P.tt(fi, abi, s.lrc, ALU.mult); P.tt(t2, am1, s.lic, ALU.mult); P.tt(fi, fi, t2, ALU.subtract); P.tt(fi, fi, den, ALU.mult)
    bre = P.sb([128, NG, 16]); bim = P.sb([128, NG, 16])
    s.cre = P.sb([128, NG, 16]); s.cim = P.sb([128, NG, 16])
    for d in range(2):
        sl = slice(d * 64, (d + 1) * 64)
        P.dma(bre[sl], DV(dr["s5_b_re"], d * NG * 1024, [[16, 64], [1024, NG], [1, 16]]))
        P.dma(bim[sl], DV(dr["s5_b_im"], d * NG * 1024, [[16, 64], [1024, NG], [1, 16]]))
        P.dma(s.cre[sl], DV(dr["s5_c_re"], d * NG * 1024, [[1, 64], [1024, NG], [64, 16]]), allow_slow_non_contiguous=True)
        P.dma(s.cim[sl], DV(dr["s5_c_im"], d * NG * 1024, [[1, 64], [1024, NG], [64, 16]]), allow_slow_non_contiguous=True)
    s.bbr = P.sb([128, NG, 16]); s.bbi = P.sb([128, NG, 16])
    frb = V(fr, [[1, NG], [0, 16]]); fib = V(fi, [[1, NG], [0, 16]])
    t3 = P.sb([128, NG, 16])
    P.tt(s.bbr, bre, frb, ALU.mult); P.tt(t3, bim, fib, ALU.mult); P.tt(s.bbr, s.bbr, t3, ALU.subtract)
    P.tt(s.bbi, bim, frb, ALU.mult); P.tt(t3, bre, fib, ALU.mult); P.tt(s.bbi, s.bbi, t3, ALU.add)
    return s


def s5_gen_E(g, ex, out_re, out_im, neg_im=False):
    P = g.P
    s = g.s5
    P.push()
    GH = 16
    NG = getattr(g, "NG", 32)
    ang = P.sb([128, GH, 128]); tmp = P.sb([128, GH, 128]); mag = P.sb([128, GH, 128]); co = P.sb([128, GH, 128])
    exb = V(ex, [[0, GH], [1, 128]])
    for h in range(NG // GH):
        gs = slice(h * GH, (h + 1) * GH)
        P.tt(ang, V(s.thc[:, gs], [[1, GH], [0, 128]]), exb, ALU.mult)
        P.tt(mag, V(s.lrdtc[:, gs], [[1, GH], [0, 128]]), exb, ALU.mult, eng="pool")
        P.act(mag, mag, AF.Exp)
        sincos(P, co, ang, ang, tmp)
        P.tt(out_re[:, gs, :], mag, co, ALU.mult)
        if neg_im:
            P.stt(out_im[:, gs, :], mag, -1.0, ang, ALU.mult, ALU.mult)
        else:
            P.tt(out_im[:, gs, :], mag, ang, ALU.mult)
    P.pop()


def s5_gen_V(g, vr, vi):
    P = g.P
    dr = g.dram
    P.push()
    GH = 16
    lib = P.sb([128, GH, 2, 64]); lrb = P.sb([128, GH, 2, 64]); dtb = P.sb([128, GH, 2])
    tmp = P.sb([128, GH, 2, 64]); co = P.sb([128, GH, 2, 64])
    exvb = V(g.c["exv"], [[0, GH], [1, 2], [0, 64]])
    NG = getattr(g, "NG", 32)
    for h in range(NG // GH):
        g0 = h * GH
        for d_ in range(2):
            P.dma(lib[:, :, d_, :], DV(dr["s5_lam_im"], g0 * 64 + d_ * NG * 64, [[0, 128], [64, GH], [1, 64]]))
            P.dma(lrb[:, :, d_, :], DV(dr["s5_lam_re"], g0 * 64 + d_ * NG * 64, [[0, 128], [64, GH], [1, 64]]))
            P.dma(dtb[:, :, d_], DV(dr["s5_log_dt"], g0 + d_ * NG, [[0, 128], [1, GH]]), allow_slow_non_contiguous=True)
        P.act(dtb, dtb, AF.Exp)
        dtbb = V(dtb, [[2, GH], [1, 2], [0, 64]])
        P.tt(lib, lib, dtbb, ALU.mult)
        P.tt(lrb, lrb, dtbb, ALU.mult, eng="pool")
        P.tt(lib, lib, exvb, ALU.mult)
        P.tt(lrb, lrb, exvb, ALU.mult, eng="pool")
        P.act(lrb, lrb, AF.Exp)
        sincos(P, co, lib, lib, tmp)
        gs = slice(g0, g0 + GH)
        P.tt(vr[:, gs, :], lrb, co, ALU.mult)
        P.tt(vi[:, gs, :], lrb, lib, ALU.mult)
    P.pop()


def cmul_acc(P, out_re, out_im, ar, ai, hr, hi, sr, si, t1, t2, eng="dve"):
    P.tt(t1, ar, hr, ALU.mult, eng=eng)
    P.tt(t2, ai, hi, ALU.mult, eng=eng)
    P.tt(t1, t1, t2, ALU.subtract, eng=eng)
    if sr is not None:
        P.tt(out_re, t1, sr, ALU.add, eng=eng)
    else:
        P.copy(out_re, t1, eng=eng)
    P.tt(t1, ar, hi, ALU.mult, eng=eng)
    P.tt(t2, ai, hr, ALU.mult, eng=eng)
    P.tt(t1, t1, t2, ALU.add, eng=eng)
    if si is not None:
        P.tt(out_im, t1, si, ALU.add, eng=eng)
    else:
        P.copy(out_im, t1, eng=eng)


def s5_states(g, uT, NCH):
    P = g.P
    s = g.s5
    s.sre = P.sb([128, 32, NCH]); s.sim = P.sb([128, 32, NCH])
    P.push()
    vr = P.sb([128, 32, 128], BF16); vi = P.sb([128, 32, 128], BF16)
    if "s5_vr" in g.dram:
        P.dma(vr, g.dram["s5_vr"]); P.dma(vi, g.dram["s5_vi"])
    else:
        s5_gen_V(g, V(vr, [[128, 32], [64, 2], [1, 64]]), V(vi, [[128, 32], [64, 2], [1, 64]]))
    utok = P.sb([128, NCH, 512], BF16)
    for c in range(NCH):
        pt = g.pbank_bf()
        for b in range(4):
            P.transpose(pt[:, b * 128:(b + 1) * 128], uT[:, b, c * 128:(c + 1) * 128], g.ident_bf)
        P.copy(utok[:, c, :], pt[:, 0:512], eng="act" if c % 2 else "dve")
    GB = 4
    zr = P.sb([128, GB, NCH, 16]); zi = P.sb([128, GB, NCH, 16])
    t1 = P.sb([128, GB, NCH, 16]); t2 = P.sb([128, GB, NCH, 16]); t3 = P.sb([128, GB, NCH, 16]); t4 = P.sb([128, GB, NCH, 16])
    for g0 in range(0, 32, GB):
        for q in range(GB):
            gi = g0 + q
            p1 = g.pbank(); p2 = g.pbank()
            rhs = V(utok, [[512, NCH], [1, 16]], off=gi * 16)
            P.mm(V(p1, [[16, NCH], [1, 16]]), vr[:, gi, :], rhs)
            P.mm(V(p2, [[16, NCH], [1, 16]]), vi[:, gi, :], rhs)
            P.copy(zr[:, q], V(p1, [[16, NCH], [1, 16]]), eng="act")
            P.copy(zi[:, q], V(p2, [[16, NCH], [1, 16]]), eng="act")
        bb_r = V(s.bbr[:, g0:g0 + GB, :], [[16, GB], [0, NCH], [1, 16]]); bb_i = V(s.bbi[:, g0:g0 + GB, :], [[16, GB], [0, NCH], [1, 16]])
        P.tt(t1, zr, bb_r, ALU.mult); P.tt(t2, zi, bb_i, ALU.mult); P.tt(t1, t1, t2, ALU.subtract)
        P.reduce(s.sre[:, g0:g0 + GB, :], t1)
        P.tt(t3, zi, bb_r, ALU.mult, eng="pool"); P.tt(t4, zr, bb_i, ALU.mult, eng="pool"); P.tt(t3, t3, t4, ALU.add, eng="pool")
        P.reduce(s.sim[:, g0:g0 + GB, :], t3)
    P.pop()


def s5_local_scan(g, NCX, NC):
    P = g.P
    s = g.s5
    NCH = NCX + NC
    s.pre_re = P.sb([128, 32, NCH]); s.pre_im = P.sb([128, 32, NCH])
    s.fin_re = P.sb([128, 32, 2]); s.fin_im = P.sb([128, 32, 2])
    s.apow_re = P.sb([128, 32, NCH]); s.apow_im = P.sb([128, 32, NCH])
    t1 = P.sb([128, 32]); t2 = P.sb([128, 32])
    P.memset(s.pre_re, 0.0); P.memset(s.pre_im, 0.0)
    for (c0, n, fi) in ((0, NCX, 0), (NCX, NC, 1)):
        for half, order in ((slice(0, 64), list(range(c0, c0 + n))), (slice(64, 128), list(range(c0 + n - 1, c0 - 1, -1)))):
            eng = "dve" if half.start == 0 else "pool"
            aqr = s.aqr[half]; aqi = s.aqi[half]
            P.memset(s.apow_re[half, :, order[0]], 1.0, eng=eng); P.memset(s.apow_im[half, :, order[0]], 0.0, eng=eng)
            for k in range(n):
                c = order[k]
                if k + 1 < n:
                    cn = order[k + 1]
                    ore, oim = s.pre_re[half, :, cn], s.pre_im[half, :, cn]
                    cmul_acc(P, s.apow_re[half, :, cn], s.apow_im[half, :, cn], aqr, aqi, s.apow_re[half, :, c], s.apow_im[half, :, c],
                             None, None, t1[half], t2[half], eng=eng)
                else:
                    ore, oim = s.fin_re[half, :, fi], s.fin_im[half, :, fi]
                cmul_acc(P, ore, oim, aqr, aqi, s.pre_re[half, :, c], s.pre_im[half, :, c], s.sre[half, :, c], s.sim[half, :, c],
                         t1[half], t2[half], eng=eng)


def s5_tables_ro(g):
    P = g.P
    s = g.s5
    s.w2re = P.sb([128, 32, 128], BF16); s.nw2im = P.sb([128, 32, 128], BF16)
    s.aqr = P.sb([128, 32]); s.aqi = P.sb([128, 32])
    if "s5_w2re" in g.dram:
        P.dma(s.w2re, g.dram["s5_w2re"]); P.dma(s.nw2im, g.dram["s5_nw2im"])
    else:
        s5_gen_E(g, g.c["exr"], s.w2re, s.nw2im, neg_im=True)
    P.push()
    ang = P.sb([128, 32]); mag = P.sb([128, 32]); tmp = P.sb([128, 32]); co = P.sb([128, 32])
    P.ts(ang, s.thc, 128.0, None, op0=ALU.mult)
    P.act(mag, s.lrdtc, AF.Exp, scale=128.0)
    sincos(P, co, ang, ang, tmp)
    P.tt(s.aqr, mag, co, ALU.mult)
    P.tt(s.aqi, mag, ang, ALU.mult)
    P.pop()


def s5_readout(g, NCX, NC, carry_re, carry_im):
    P = g.P
    s = g.s5
    NCH = NCX + NC
    hre = P.sb([128, 32, NCH]); him = P.sb([128, 32, NCH])
    P.copy(hre, s.pre_re); P.copy(him, s.pre_im, eng="pool")
    own = slice(NCX, NCH)
    t1 = P.sb([128, 32, NC]); t2 = P.sb([128, 32, NC])
    crb = V(carry_re, [[carry_re.ap[1][0], 32], [0, NC]]); cib = V(carry_im, [[carry_im.ap[1][0], 32], [0, NC]])
    P.tt(t1, s.apow_re[:, :, own], crb, ALU.mult); P.tt(t2, s.apow_im[:, :, own], cib, ALU.mult)
    P.tt(t1, t1, t2, ALU.subtract); P.tt(hre[:, :, own], hre[:, :, own], t1, ALU.add)
    P.tt(t1, s.apow_re[:, :, own], cib, ALU.mult); P.tt(t2, s.apow_im[:, :, own], crb, ALU.mult)
    P.tt(t1, t1, t2, ALU.add); P.tt(him[:, :, own], him[:, :, own], t1, ALU.add)
    P.push()
    gre = P.sb([128, 32, NCH, 16], BF16); gim = P.sb([128, 32, NCH, 16], BF16)
    GQ = 8
    a1 = P.sb([128, GQ, NCH, 16]); a2 = P.sb([128, GQ, NCH, 16])
    for q in range(32 // GQ):
        gs = slice(q * GQ, (q + 1) * GQ)
        crb_ = V(s.cre[:, gs, :], [[16, GQ], [0, NCH], [1, 16]]); cib_ = V(s.cim[:, gs, :], [[16, GQ], [0, NCH], [1, 16]])
        hrb = V(hre[:, gs, :], [[NCH, GQ], [1, NCH], [0, 16]]); hib = V(him[:, gs, :], [[NCH, GQ], [1, NCH], [0, 16]])
        P.tt(a1, crb_, hrb, ALU.mult); P.tt(a2, cib_, hib, ALU.mult, eng="pool"); P.tt(gre[:, gs], a1, a2, ALU.subtract)
        P.tt(a1, crb_, hib, ALU.mult); P.tt(a2, cib_, hrb, ALU.mult, eng="pool"); P.tt(gim[:, gs], a1, a2, ALU.add)
    N = NCH * 16
    for gi in range(32):
        ps = g.pbank()
        o = V(ps, [[16, NCH], [1, 16]])
        P.mm(o, s.w2re[:, gi, :], V(gre[:, gi], [[16, NCH], [1, 16]]), start=True, stop=False)
        P.mm(o, s.nw2im[:, gi, :], V(gim[:, gi], [[16, NCH], [1, 16]]), start=False, stop=True)
        P.copy(V(s.ystok, [[512, NCH], [1, 16]], off=gi * 16), o, eng="act" if gi % 2 else "dve")
    P.pop()


def s5_kt_build(g, NB, kt_d):
    P = g.P
    s = g.s5
    NG = NB * 8
    P.push()
    elr = P.sb([128, NG, 128], BF16); eli = P.sb([128, NG, 128], BF16)
    s5_gen_E(g, g.c["exl"], elr, eli)
    brpad = P.sb([128, NG, 128], BF16); nbipad = P.sb([128, NG, 128], BF16)
    P.memset(brpad, 0.0); P.memset(nbipad, 0.0, eng="pool")
    padv = lambda t: V(t, [[1024, NB], [144, 8], [1, 16]])
    P.copy(padv(brpad), V(s.bbr, [[128, NB], [16, 8], [1, 16]]))
    P.ts(padv(nbipad), V(s.bbi, [[128, NB], [16, 8], [1, 16]]), -1.0, None, op0=ALU.mult)
    care = P.sb([128, 32, 16], BF16); caim = P.sb([128, 32, 16], BF16)
    a1 = P.sb([128, 32, 16]); a2 = P.sb([128, 32, 16])
    ko = P.sb([128, 2, 512], BF16)
    for b in range(NB):
        for lh in range(2):
            for sl in range(2):
                d0 = lh * 64 + sl * 32
                pk = [g.pbank(), g.pbank()]
                for gl in range(8):
                    gi = 8 * b + gl
                    crb = V(s.cre[:, gi, :], [[0, 32], [1, 16]]); cib = V(s.cim[:, gi, :], [[0, 32], [1, 16]])
                    erb = V(elr[:, gi, d0:d0 + 32], [[1, 32], [0, 16]]); eib = V(eli[:, gi, d0:d0 + 32], [[1, 32], [0, 16]])
                    P.tt(a1, crb, erb, ALU.mult); P.tt(a2, cib, eib, ALU.mult, eng="pool"); P.tt(care, a1, a2, ALU.subtract)
                    P.tt(a1, crb, eib, ALU.mult); P.tt(a2, cib, erb, ALU.mult, eng="pool"); P.tt(caim, a1, a2, ALU.add)
                    for dr_ in range(2):
                        h = slice(dr_ * 64, (dr_ + 1) * 64)
                        P.mm(pk[dr_], brpad[h, gi, :], V(care[h], [[1, 512]]), start=(gl == 0), stop=False)
                        P.mm(pk[dr_], nbipad[h, gi, :], V(caim[h], [[1, 512]]), start=False, stop=(gl == 7))
                for dr_ in range(2):
                    P.copy(ko[:, dr_, :], pk[dr_], eng="act" if dr_ else "dve")
                    off = (((b * 2 + lh) * 2 + dr_) * 128) * 1024 + sl * 512
                    P.dma(DV(kt_d, off, [[1024, 128], [1, 512]]), ko[:, dr_, :])
    P.pop()


def s5_lags(g, uT, NCH, ya_acc_cb):
    P = g.P
    s = g.s5
    P.push()
    kt_d = g.dram["s5_kt"]
    kt = P.sb([128, 2, 1024], BF16)
    bd = P.sb([128, 2, 64, 128], BF16)
    NS = NCH * 128
    yacc = P.sb([128, NS])
    mb = V(g.c["maskbd"], [[0, 64], [1, 8], [0, 16]])
    cgs = [(c0, min(4, NCH - c0)) for c0 in range(0, NCH, 4)]
    for b in range(4):
        for lh in range(2):
            P.dma(kt, DV(kt_d, (b * 2 + lh) * 2 * 128 * 1024, [[1024, 128], [128 * 1024, 2], [1, 1024]]))
            for dr_ in range(2):
                P.tt(V(bd[:, dr_], [[128, 64], [16, 8], [1, 16]]), V(kt[:, dr_, :], [[16, 64], [0, 8], [1, 16]]), mb, ALU.mult,
                     eng="pool" if dr_ else "dve")
            for (c0, ncg) in cgs:
                ps = g.pbank()
                first = True
                if lh == 1:
                    P.mm(ps[:, 0:ncg * 128], g.zeros_bf, V(uT[:, b, :], [[1, ncg * 128]], off=c0 * 128), start=True, stop=False)
                    for c in range(c0, c0 + ncg):
                        P.mm(ps[:, (c - c0) * 128:(c - c0 + 1) * 128], s.ystok[:, c, b * 128:(b + 1) * 128], g.ident_bf,
                             start=False, stop=False)
                    first = False
                for d in range(64):
                    dd = lh * 64 + d
                    w = 128 - dd
                    last = (d == 63)
                    P.mm(V(ps, [[128, ncg], [1, w]], off=dd), bd[:, 0, d, :], V(uT[:, b, :], [[128, ncg], [1, w]], off=c0 * 128),
                         start=first, stop=False)
                    first = False
                    P.mm(V(ps, [[128, ncg], [1, w]]), bd[:, 1, d, :], V(uT[:, b, :], [[128, ncg], [1, w]], off=c0 * 128 + dd),
                         start=False, stop=last)
                dst = yacc[:, c0 * 128:(c0 + ncg) * 128]
                if lh == 0:
                    P.copy(dst, ps[:, 0:ncg * 128], eng="act")
                else:
                    P.tt(dst, dst, ps[:, 0:ncg * 128], ALU.add)
        ya_acc_cb(b, yacc)
    P.pop()


def s5_core(g, uT, NCX, NC, carry_fn, ya_cb, states_cb=None, states_only=False):
    P = g.P
    NCH = NCX + NC
    s5_params(g)
    s = g.s5
    s.ystok = P.sb([128, NCH, 512], BF16)
    import os
    S5CUT = os.environ.get("S5CUT", "")
    P.push()
    s5_tables_ro(g)
    if S5CUT == "ro":
        P.pop(); return
    s5_states(g, uT, NCH)
    if S5CUT == "states":
        P.pop(); return
    s5_local_scan(g, NCX, NC)
    if states_cb is not None:
        states_cb()
    if states_only:
        P.pop()
        return
    cr, ci = carry_fn()
    s5_readout(g, NCX, NC, cr, ci)
    P.pop()
    if S5CUT == "readout":
        return
    s5_lags(g, uT, NCH, ya_cb)


def load_w(g, name, r0, nrows, c0, ncols, dtype=BF16, q="sp"):
    P = g.P
    kk = nrows // 128
    t = P.sb([128, kk, ncols], dtype)
    d = g.dram[name]
    ncol_total = d.shape[-1]
    P.dma(t, DV(d, r0 * ncol_total + c0, [[ncol_total, 128], [128 * ncol_total, kk], [1, ncols]]), q=q)
    return t


def load_h(g, c0, n):
    P = g.P
    t = P.sb([128, 8, n], BF16)
    P.dma(t, DV(g.hT_d, c0, [[g.E, 128], [128 * g.E, 8], [1, n]]))
    return t


def proj(g, ps_out, w, j0, hT, n, wcols=128):
    P = g.P
    for k in range(8):
        P.mm(ps_out, w[:, k, j0:j0 + wcols], hT[:, k, 0:n], start=(k == 0), stop=(k == 7))


def col_tiles(c0, n, step=512):
    out = []
    c = c0
    while c < c0 + n:
        m = min(step, c0 + n - c)
        out.append((c, m))
        c += m
    return out


def ssd_prep(g, NCX, NC, need_z=True):
    P = g.P
    dr = g.dram
    s = G(); g.ssd = s
    T = NC * 128; NS = (NCX + NC) * 128
    s.xsT = P.sb([128, 4, NS], BF16); s.bmT = P.sb([128, 2, NS], BF16); s.cmT = P.sb([128, 2, NS], BF16)
    s.gz = P.sb([128, 4, NS], BF16)
    s.dt = P.sb([128, NCX + NC, 16]); s.dta = P.sb([128, NCX + NC, 16])
    s.cw = P.sb([128, 8, 5]); s.cb = P.sb([128, 8])
    for k_ in range(5):
        P.dma(s.cw[:, :, k_], DV(dr["ssd_conv_w"], k_ * 1024, [[1, 128], [128, 8]]), allow_slow_non_contiguous=True)
    P.dma(s.cb, DV(dr["ssd_conv_b"], 0, [[1, 128], [128, 8]]), allow_slow_non_contiguous=True)
    s.dtb = P.sb([128, 16]); s.ab = P.sb([128, 16])
    P.dma(s.dtb, DV(dr["ssd_dt_bias"], 0, [[0, 128], [1, 16]]))
    P.dma(s.ab, DV(dr["ssd_a_log"], 0, [[0, 128], [1, 16]]))
    P.act(s.ab, s.ab, AF.Exp)
    P.ts(s.ab, s.ab, -1.0, None, op0=ALU.mult)
    s.dcol = P.sb([128, 4])
    for hh in range(2):
        P.dma(s.dcol[hh * 64:(hh + 1) * 64, :], DV(dr["ssd_d"], hh, [[0, 64], [2, 4]]), allow_slow_non_contiguous=True)
    s.ng = P.sb([128, 4])
    P.dma(s.ng, DV(dr["ssd_norm_g"], 0, [[1, 128], [128, 4]]), allow_slow_non_contiguous=True)
    P.push()
    wx = load_w(g, "w_in", 0, 1024, C_XBC, 1024)
    wz = load_w(g, "w_in", 0, 1024, C_Z, 512)
    wdt = load_w(g, "w_in", 0, 1024, C_DT, 16)
    regions = [(0, NCX * 128, g.e_ctx, False), (NCX * 128, T, g.e_own, True)]
    W = NS + 8
    xin = P.sb([128, W])
    for j in range(8):
        P.memset(xin[:, 0:2], 0.0); P.memset(xin[:, 2 + NCX * 128:4 + NCX * 128], 0.0)
        for (s0, n, e0, is_own) in regions:
            xo = 2 + s0 + (4 if is_own else 0)
            lo, hi = (e0 - 2, e0 + n + 2) if is_own else (e0, e0 + n)
            xo_lo = xo - 2 if is_own else xo
            for (c, m) in col_tiles(lo, hi - lo):
                P.push()
                ht = load_h(g, c, m)
                ps = g.pbank()
                proj(g, ps[:, 0:m], wx, j * 128, ht, m)
                P.copy(xin[:, xo_lo + (c - lo):xo_lo + (c - lo) + m], ps[:, 0:m], eng="act")
                P.pop()
            if is_own:
                P.ts(xin[:, xo - 2:xo], xin[:, xo - 2:xo], g.flagL, None, op0=ALU.mult)
                P.ts(xin[:, xo + n:xo + n + 2], xin[:, xo + n:xo + n + 2], g.flagR, None, op0=ALU.mult)
        dst = s.xsT[:, j, :] if j < 4 else (s.bmT[:, j - 4, :] if j < 6 else s.cmT[:, j - 6, :])
        for (s0, n, e0, is_own) in regions:
            xo = 2 + s0 + (4 if is_own else 0)
            P.push()
            acc = P.sb([128, n])
            P.ts(acc, xin[:, xo - 2:xo - 2 + n], s.cw[:, j, 0:1], None, op0=ALU.mult)
            for k in range(1, 5):
                P.stt(acc, xin[:, xo - 2 + k:xo - 2 + k + n], s.cw[:, j, k:k + 1], acc, ALU.mult, ALU.add)
            P.act(dst[:, s0:s0 + n], acc, AF.Silu, bias=s.cb[:, j:j + 1])
            P.pop()
    for (s0, n, e0, is_own) in regions:
        for (c, m) in col_tiles(e0, n):
            P.push()
            ht = load_h(g, c, m)
            so = s0 + (c - e0)
            for j in range(4 if need_z else 0):
                ps = g.pbank()
                proj(g, ps[:, 0:m], wz, j * 128, ht, m)
                P.act(s.gz[:, j, so:so + m], ps[:, 0:m], AF.Silu)
            for cc in range(m // 128):
                ps = g.pbank()
                for k in range(8):
                    P.mm(ps[:, 0:16], ht[:, k, cc * 128:(cc + 1) * 128], wdt[:, k, :], start=(k == 0), stop=(k == 7))
                ci = (so + cc * 128) // 128
                P.tt(s.dt[:, ci, :], ps[:, 0:16], s.dtb, ALU.add)
            P.pop()
    P.pop()
    P.act(s.dt, s.dt, AF.Exp)
    P.act(s.dt, s.dt, AF.Ln, bias=1.0)
    P.tt(s.dta, s.dt, V(s.ab, [[0, NCX + NC], [1, 16]]), ALU.mult)


def ssd_chunk(g, c, want_y, hin_f, hin_b, ybuf=None, sdirs=(0, 1)):
    P = g.P
    s = g.ssd
    cs = slice(c * 128, (c + 1) * 128)
    ut = g.c["ut"]; lt = g.c["lt"]
    xs_tok = P.sb([128, 8, 64], BF16); bm_tok = P.sb([128, 2, 128], BF16)
    pt = g.pbank_bf()
    for b in range(4):
        P.transpose(pt[:, b * 128:(b + 1) * 128], s.xsT[:, b, cs], g.ident_bf)
    P.copy(V(xs_tok, [[1, 512]]), pt[:, 0:512], eng="act")
    pt2 = g.pbank_bf()
    for b in range(2):
        P.transpose(pt2[:, b * 128:(b + 1) * 128], s.bmT[:, b, cs], g.ident_bf)
    P.copy(V(bm_tok, [[1, 256]]), pt2[:, 0:256], eng="act")
    pc = g.pbank()
    P.mm(pc[:, 0:8], ut, s.dta[:, c, 0:8])
    P.mm(pc[:, 8:16], lt, s.dta[:, c, 8:16])
    nacum = P.sb([128, 16])
    P.ts(nacum, pc[:, 0:16], -1.0, None, op0=ALU.mult)
    acb = [None] * 4
    adirs = (0, 1) if want_y else sdirs
    for dr_ in adirs:
        m = ut if dr_ == 0 else lt
        for hq in range(2):
            rb = P.sb([128, 4, 128])
            P.tt(rb, V(m, [[0, 4], [1, 128]]), V(s.dta[:, c, dr_ * 8 + hq * 4:dr_ * 8 + hq * 4 + 4], [[1, 4], [0, 128]]), ALU.mult,
                 eng="pool")
            pa = g.pbank()
            P.mm(V(pa, [[1, 512]]), g.c["ones_f"], V(rb, [[1, 512]]))
            pas = P.sb([128, 512])
            P.copy(pas, pa, eng="act" if hq else "dve")
            acb[dr_ * 2 + hq] = pas
    tot = P.sb([128, 16])
    if len(adirs) < 2:
        P.memset(tot, 0.0)
    for dr_ in adirs:
        for hq in range(2):
            pa = acb[dr_ * 2 + hq]
            col = 127 if dr_ == 0 else 0
            P.copy(tot[:, dr_ * 8 + hq * 4:dr_ * 8 + hq * 4 + 4], V(pa, [[128, 4]], off=col))
    w = P.sb([128, 16]); cd = P.sb([128, 16])
    P.tt(w, tot, nacum, ALU.add)
    P.act(w, w, AF.Exp)
    P.tt(w, w, s.dt[:, c, :], ALU.mult)
    P.act(cd, tot, AF.Exp)
    S = [None, None]
    for dr_ in sdirs:
        xsw = P.sb([128, 8, 64], BF16)
        P.tt(xsw, xs_tok, V(w[:, dr_ * 8:dr_ * 8 + 8], [[1, 8], [0, 64]]), ALU.mult)
        pS = g.pbank()
        for h in range(8):
            P.mm(pS[:, h * 64:(h + 1) * 64], bm_tok[:, h // 4, :], xsw[:, h, :])
        Ss = P.sb([128, 512])
        P.copy(Ss, pS, eng="act")
        S[dr_] = Ss
    if not want_y:
        return S, cd, tot
    cbm = []
    for gq in range(2):
        pcb = g.pbank()
        P.mm(pcb[:, 0:128], s.bmT[:, gq, cs], s.cmT[:, gq, cs])
        cf = P.sb([128, 128]); cbk = P.sb([128, 128])
        P.tt(cf, pcb[:, 0:128], ut, ALU.mult)
        P.tt(cbk, pcb[:, 0:128], lt, ALU.mult)
        cbm.append((cf, cbk))
    ring_e = [P.sb([128, 128]) for _ in range(4)]; ring_d = [P.sb([128, 128]) for _ in range(4)]
    ring_w = [P.sb([128, 128], BF16) for _ in range(4)]; ring_c = [P.sb([128, 128], BF16) for _ in range(4)]
    ri = 0
    for pair in range(4):
        py = g.pbank()
        for hh in range(2):
            h = pair * 2 + hh
            gq = h // 4
            hq, hi4 = h // 4, h % 4
            mms = []
            for dr_ in range(2):
                pa = acb[dr_ * 2 + hq]
                ri += 1
                e1 = ring_e[ri % 4]
                P.ts(e1, pa[:, hi4 * 128:(hi4 + 1) * 128], nacum[:, dr_ * 8 + h:dr_ * 8 + h + 1], g.zero_col, op0=ALU.add, op1=ALU.min)
                P.act(e1, e1, AF.Exp)
                wt = ring_w[ri % 4]
                P.stt(wt, e1, s.dt[:, c, dr_ * 8 + h:dr_ * 8 + h + 1], cbm[gq][dr_], ALU.mult, ALU.mult)
                mms.append((xs_tok[:, h, :], wt))
                hin = hin_f if dr_ == 0 else hin_b
                if hin is not None:
                    dec = ring_d[ri % 4]
                    P.act(dec, pa[:, hi4 * 128:(hi4 + 1) * 128], AF.Exp)
                    csd = ring_c[ri % 4]
                    P.tt(csd, s.cmT[:, gq, cs], dec, ALU.mult, eng="pool")
                    mms.append((hin[:, h, :], csd))
            for i_, (l_, r_) in enumerate(mms):
                P.mm(py[hh * 64:(hh + 1) * 64, 0:128], l_, r_, start=(i_ == 0), stop=(i_ == len(mms) - 1))
        P.stt(ybuf[:, pair, :], s.xsT[:, pair, cs], s.dcol[:, pair:pair + 1], py[:, 0:128], ALU.mult, ALU.add)
    return S, cd, tot


def ssd_run(g, NCX, NC, multi=False, yb_cb=None):
    P = g.P
    s = g.ssd
    NCH = NCX + NC
    hf = P.sb([128, 512]); hb = P.sb([128, 512])
    hbf = P.sb([128, 8, 64], BF16)
    it1 = 0; it2 = 0
    for (c0, n) in ((0, NCX), (NCX, NC)):
        if c0 == 0:
            P.memset(hb, 0.0)
        elif multi:
            P.push(); tmpc = P.sb([128, 512]); ssd_carry(g, 1, hb, tmpc); P.copy(hb, tmpc); P.pop()
        for c in range(c0 + n - 1, c0 - 1, -1):
            P.copy(V(hbf, [[1, 512]]), hb)
            P.dma(DV(g.ssd_hb_d, c * 128 * 512, [[512, 128], [1, 512]]), V(hbf, [[1, 512]]))
            P.iter_push(it1, "ssd1")
            S, cd, _t = ssd_chunk(g, c, False, None, None, sdirs=(1,))
            P.tt(V(hb, [[64, 8], [1, 64]]), V(hb, [[64, 8], [1, 64]]), V(cd[:, 8:16], [[1, 8], [0, 64]]), ALU.mult)
            P.tt(hb, hb, S[1], ALU.add)
            P.iter_pop(it1, "ssd1"); it1 += 1
    hfb = P.sb([128, 8, 64], BF16); hbb = P.sb([128, 8, 64], BF16)
    ybuf = P.sb([128, 4, 128])
    for (c0, n) in ((0, NCX), (NCX, NC)):
        if c0 == 0:
            P.memset(hf, 0.0)
        elif multi:
            P.push(); tmpc = P.sb([128, 512]); ssd_carry(g, 0, hf, tmpc); P.copy(hf, tmpc); P.pop()
        for c in range(c0, c0 + n):
            P.copy(V(hfb, [[1, 512]]), hf)
            P.dma(V(hbb, [[1, 512]]), DV(g.ssd_hb_d, c * 128 * 512, [[512, 128], [1, 512]]))
            P.iter_push(it2, "ssd2")
            S, cd, _t = ssd_chunk(g, c, True, hfb, hbb, ybuf, sdirs=(0,))
            P.tt(V(hf, [[64, 8], [1, 64]]), V(hf, [[64, 8], [1, 64]]), V(cd[:, 0:8], [[1, 8], [0, 64]]), ALU.mult)
            P.tt(hf, hf, S[0], ALU.add)
            cs = slice(c * 128, (c + 1) * 128)
            yg = P.sb([128, 4, 128]); sq = P.sb([128, 4, 128], BF16)
            P.tt(yg, ybuf, s.gz[:, :, cs], ALU.mult)
            P.tt(sq, yg, yg, ALU.mult, eng="pool")
            pn = g.pbank()
            for b in range(4):
                P.mm(pn[:, 0:128], g.ones_bf, sq[:, b, :], start=(b == 0), stop=(b == 3))
            rstd = P.sb([128, 128])
            P.act(rstd, pn[:, 0:128], AF.Sqrt, scale=1.0 / 512, bias=g.eps_col)
            P.recip(rstd, rstd)
            yo = P.sb([128, 4, 128], BF16)
            for b in range(4):
                P.stt(yo[:, b, :], yg[:, b, :], s.ng[:, b:b + 1], rstd, ALU.mult, ALU.mult)
            yb_cb(c, yo)
            P.iter_pop(it2, "ssd2"); it2 += 1


def attn_run(g, NCX, NC, yc_cb):
    P = g.P
    dr = g.dram
    T = NC * 128
    NL = T + 256
    NLT = NL // 128
    P.push()
    wq = P.sb([128, 8, 512], BF16)
    d = dr["w_in"]
    for r in range(4):
        for hh, head in enumerate((r, 4 + r)):
            P.dma(wq[:, :, r * 128 + hh * 64:r * 128 + hh * 64 + 64],
                  DV(d, C_Q + head * 64, [[IN_W, 128], [128 * IN_W, 8], [1, 64]]))
    wk = load_w(g, "w_in", 0, 1024, C_K, 128)
    wv = load_w(g, "w_in", 0, 1024, C_V, 128)
    pswap = P.sb([128, 128], BF16)
    P.copy(pswap, g.c["pswap"])
    esink = P.sb([128, 4])
    for hh in range(2):
        P.dma(esink[hh * 64:(hh + 1) * 64, :], DV(dr["attn_sink"], hh * 4, [[0, 64], [1, 4]]))
    P.act(esink, esink, AF.Exp)
    mprev = P.sb([128, 128], BF16); mnext = P.sb([128, 128], BF16); mprevL = P.sb([128, 128], BF16); mnextR = P.sb([128, 128], BF16)
    P.copy(mprev, g.c["lt"]); P.copy(mnext, g.c["ut"])
    P.ts(mprevL, g.c["lt"], g.flagL, None, op0=ALU.mult)
    P.ts(mnextR, g.c["ut"], g.flagR, None, op0=ALU.mult)
    import os
    CUT = os.environ.get('ATT_CUT', '')
    if CUT == 'setup':
        P.pop(); return
    NS = (NCX + NC) * 128
    qT = P.sb([128, 4, NS], BF16)
    kT = P.sb([128, NL + 256], BF16)
    vtok = P.sb([128, NLT + 2, 128], BF16)

    def rope(dst, ps, n, ecol):
        if 'norope' in CUT:
            P.copy(dst, ps); return
        P.push()
        cs_ = P.sb([128, n]); sn_ = P.sb([128, n]); xb = P.sb([128, n], BF16); t1 = P.sb([128, n])
        if 'nodma' in CUT:
            P.memset(cs_, 1.0); P.memset(sn_, 0.0)
        else:
            P.dma(cs_, DV(dr["rope_cos"], ecol, [[NL, 128], [1, n]]))
            P.dma(sn_, DV(dr["rope_sin"], ecol, [[NL, 128], [1, n]]))
        if 'dmaonly' in CUT:
            P.tt(dst, ps, cs_, ALU.mult); P.pop(); return
        P.copy(xb, ps)
        p2 = g.pbank()
        P.mm(p2[:, 0:n], pswap, xb)
        P.tt(t1, ps, cs_, ALU.mult)
        t2 = P.sb([128, n])
        P.tt(t2, p2[:, 0:n], sn_, ALU.mult)
        P.tt(dst, t1, t2, ALU.add)
        P.pop()

    for (c, m) in col_tiles(0, NL):
        P.push()
        ht = load_h(g, c, m)
        ps = g.pbank()
        proj(g, ps[:, 0:m], wk, 0, ht, m)
        rope(kT[:, c:c + m], ps[:, 0:m], m, c)
        for cc in range(m // 128):
            if 'nov' in CUT:
                break
            pv = g.pbank()
            for k in range(8):
                P.mm(pv[:, 0:128], ht[:, k, cc * 128:(cc + 1) * 128], wv[:, k, :], start=(k == 0), stop=(k == 7))
            P.copy(vtok[:, (c // 128) + cc, :], pv[:, 0:128], eng="act")
        if 'noq' in CUT:
            P.pop(); continue
        lo = max(c, 128); hi = min(c + m, 128 + T)
        if hi > lo:
            for r in range(4):
                pq = g.pbank()
                proj(g, pq[:, 0:hi - lo], wq, r * 128, ht[:, :, lo - c:hi - c], hi - lo)
                so = NCX * 128 + (lo - 128)
                rope(qT[:, r, so:so + (hi - lo)], pq[:, 0:hi - lo], hi - lo, lo)
        P.pop()
    if 'lat' in CUT:
        P.pop(); return
    for (c, m) in col_tiles(g.e_ctx, NCX * 128):
        P.push()
        ht = load_h(g, c, m)
        so = c - g.e_ctx
        ps = g.pbank()
        proj(g, ps[:, 0:m], wk, 0, ht, m)
        P.copy(kT[:, NL + so:NL + so + m], ps[:, 0:m], eng="act")
        for cc in range(m // 128):
            pv = g.pbank()
            for k in range(8):
                P.mm(pv[:, 0:128], ht[:, k, cc * 128:(cc + 1) * 128], wv[:, k, :], start=(k == 0), stop=(k == 7))
            P.copy(vtok[:, NLT + so // 128 + cc, :], pv[:, 0:128], eng="act")
        for r in range(4):
            pq = g.pbank()
            proj(g, pq[:, 0:m], wq, r * 128, ht, m)
            P.copy(qT[:, r, so:so + m], pq[:, 0:m], eng="act")
        P.pop()
    g._nrot = 4
    po = g._pb_o; pd = g._pb_d
    import os
    for qb in range(NCX + NC):
        if os.environ.get('ATT_CUT') == 'proj':
            break
        if qb < NCX:
            tiles = [(NLT + 0, NL + 0, None), (NLT + 1, NL + 128, None)]
        else:
            n = qb - NCX
            tiles = [(n, n * 128, mprevL if n == 0 else mprev), (n + 1, (n + 1) * 128, None),
                     (n + 2, (n + 2) * 128, mnextR if n == NC - 1 else mnext),
                     (NLT + 0, NL + 0, None), (NLT + 1, NL + 128, None)]
        P.iter_push(qb, "attq")
        nt = len(tiles)
        for hk in range(2):
            h = slice(hk * 64, (hk + 1) * 64)
            for ti, (kt, kcol, mask) in enumerate(tiles):
                ps = g.pbank()
                P.mm(ps[:, 0:512], kT[h, kcol:kcol + 128], V(qT[h], [[NS, 4], [1, 128]], off=qb * 128))
                ex = P.sb([128, 4, 128], BF16)
                P.act(V(ex, [[1, 512]]), ps, AF.Exp, scale=0.125)
                if mask is not None:
                    P.tt(ex, ex, V(mask, [[0, 4], [1, 128]]), ALU.mult, eng="pool")
                P.mm(po[h, :], vtok[:, kt, h], V(ex, [[1, 512]]), start=(ti == 0), stop=(ti == nt - 1))
                P.mm(pd[h, :], g.ones_bf[:, 0:64], V(ex, [[1, 512]]), start=(ti == 0), stop=(ti == nt - 1))
        rd = P.sb([128, 4, 128])
        for r in range(4):
            P.ts(rd[:, r, :], pd[:, r * 128:(r + 1) * 128], esink[:, r:r + 1], None, op0=ALU.add)
        P.recip(V(rd, [[1, 512]]), V(rd, [[1, 512]]))
        yo = P.sb([128, 4, 128], BF16)
        P.tt(V(yo, [[1, 512]]), po[:, :], V(rd, [[1, 512]]), ALU.mult)
        yc_cb(qb, yo)
        P.iter_pop(qb, "attq")
    g._nrot = 6
    P.pop()


def rms_rstd(g, xt, n, rstd):
    P = g.P
    P.push()
    sq = P.sb([128, 8, n], BF16)
    P.act(sq, xt, AF.Square)
    ps = g.pbank()
    for k in range(8):
        P.mm(ps[:, 0:n], g.ones_bf, sq[:, k, :], start=(k == 0), stop=(k == 7))
    P.act(rstd, ps[:, 0:n], AF.Sqrt, scale=1.0 / D, bias=g.eps_col)
    P.recip(rstd, rstd)
    P.pop()


def mod_norm(g, xt, n, acol, bcol, out, v):
    P = g.P
    P.push()
    rstd = P.sb([128, n])
    rms_rstd(g, xt, n, rstd)
    tmp = P.sb([128, n])
    for k in range(8):
        P.stt(tmp, xt[:, k, :], acol[:, k, v:v + 1], rstd, ALU.mult, ALU.mult)
        P.act(out[:, k, :], tmp, AF.Identity, bias=bcol[:, k, v:v + 1])
    P.pop()


def load_mod(g):
    P = g.P
    m = P.sb([128, 48, 2])
    P.dma(m, g.dram["modT"])
    g.mod = m
    n1 = P.sb([128, 8]); n2 = P.sb([128, 8])
    lst = []
    if "norm1_g" in g.dram:
        P.dma(n1, DV(g.dram["norm1_g"], 0, [[1, 128], [128, 8]]), allow_slow_non_contiguous=True)
        g.a1 = P.sb([128, 8, 2]); lst.append((g.a1, n1, 1))
    if "norm2_g" in g.dram:
        P.dma(n2, DV(g.dram["norm2_g"], 0, [[1, 128], [128, 8]]), allow_slow_non_contiguous=True)
        g.a2 = P.sb([128, 8, 2]); lst.append((g.a2, n2, 4))
    for (a, nn, j) in lst:
        P.ts(a, m[:, j * 8:(j + 1) * 8, :], 1.0, None, op0=ALU.add)
        P.tt(a, a, V(nn, [[1, 8], [0, 2]]), ALU.mult)
    g.b1 = m[:, 0:8, :]; g.b2 = m[:, 24:32, :]
    g.g1 = m[:, 16:24, :]; g.g2 = m[:, 40:48, :]


def router_aff(g, h2f, n, wr, aff_out):
    P = g.P
    for blk in range(n // 128):
        ps = g.pbank()
        for k in range(8):
            P.mm(ps[:, 0:16], h2f[:, k, blk * 128:(blk + 1) * 128], wr[:, k, :], start=(k == 0), stop=(k == 7))
        P.push()
        mx = P.sb([128, 1]); sm = P.sb([128, 1]); ex = P.sb([128, 16])
        P.reduce(mx, ps[:, 0:16], op=ALU.max)
        P.ts(mx, mx, -1.0, None, op0=ALU.mult)
        P.act(ex, ps[:, 0:16], AF.Exp, bias=mx, accum_out=sm)
        P.recip(sm, sm)
        P.ts(aff_out[:, blk, :], ex, sm, None, op0=ALU.mult)
        P.pop()


def s_tiles(NCX, NC, step=512):
    out = [(c, m, True) for (c, m) in col_tiles(0, NCX * 128, step)]
    out += [(c, m, False) for (c, m) in col_tiles(NCX * 128, NC * 128, step)]
    return out


def s2e(g, NCX, s0, is_ctx):
    return g.e_ctx + s0 if is_ctx else g.e_own + (s0 - NCX * 128)


def merge_run(g, NCX, NC):
    P = g.P
    dr = g.dram
    NS = (NCX + NC) * 128
    P.push()
    macc = P.sb([128, 8, NS], BF16)
    for kbr in range(3):
        P.push()
        wg = load_w(g, "w_in", 0, 1024, C_G + kbr * 1024, 1024)
        wb = P.sb([128, 4, 1024], BF16)
        d = dr["w_branch"]
        if kbr < 2:
            P.dma(wb, DV(d, kbr * 512 * 1024, [[1024, 128], [128 * 1024, 4], [1, 1024]]))
        else:
            for r in range(4):
                for hh, head in enumerate((r, 4 + r)):
                    P.dma(wb[hh * 64:(hh + 1) * 64, r, :], DV(d, (2 * 512 + head * 64) * 1024, [[1024, 64], [1, 1024]]))
        yd = g.y_d[kbr]
        for it_, (s0, n, is_ctx) in enumerate(s_tiles(NCX, NC)):
            P.iter_push(it_, "mrg1")
            ht = load_h(g, s2e(g, NCX, s0, is_ctx), n)
            yt = P.sb([128, 4, n], BF16)
            P.dma(yt, DV(yd, s0, [[NS, 128], [128 * NS, 4], [1, n]]))
            for j in range(8):
                pg = g.pbank()
                proj(g, pg[:, 0:n], wg, j * 128, ht, n)
                gt = P.sb([128, n])
                P.act(gt, pg[:, 0:n], AF.Sigmoid)
                pb = g.pbank()
                for cc in range(4):
                    P.mm(pb[:, 0:n], wb[:, cc, j * 128:(j + 1) * 128], yt[:, cc, :], start=(cc == 0), stop=(cc == 3))
                if kbr == 0:
                    P.tt(macc[:, j, s0:s0 + n], gt, pb[:, 0:n], ALU.mult)
                else:
                    P.tt(gt, gt, pb[:, 0:n], ALU.mult)
                    P.tt(macc[:, j, s0:s0 + n], macc[:, j, s0:s0 + n], gt, ALU.add, eng="pool")
            P.iter_pop(it_, "mrg1")
        P.pop()
    wo = load_w(g, "w_out", 0, 1024, 0, 1024)
    wr = load_w(g, "w_router", 0, 1024, 0, 16, dtype=F32)
    for (s0, n, is_ctx) in s_tiles(NCX, NC):
        v = 1 if is_ctx else 0
        P.push()
        xt = P.sb([128, 8, n])
        xsrc = g.dram["xcT"] if is_ctx else g.dram["xT"]
        xw = NCX * 128 if is_ctx else NC * 128 + 256
        xo = s0 if is_ctx else (s0 - NCX * 128) + 128
        P.dma(xt, DV(xsrc, xo, [[xw, 128], [128 * xw, 8], [1, n]]))
        for j in range(8):
            po = g.pbank()
            for k in range(8):
                P.mm(po[:, 0:n], wo[:, k, j * 128:(j + 1) * 128], macc[:, k, s0:s0 + n], start=(k == 0), stop=(k == 7))
            P.stt(xt[:, j, :], po[:, 0:n], g.g1[:, j, v:v + 1], xt[:, j, :], ALU.mult, ALU.add)
        P.dma(DV(g.dram["x1T"], s0, [[NS, 128], [128 * NS, 8], [1, n]]), xt)
        h2 = P.sb([128, 8, n])
        mod_norm(g, xt, n, g.a2, g.b2, h2, v)
        aff = P.sb([128, n // 128, 16])
        router_aff(g, h2, n, wr, aff)
        P.dma(DV(g.dram["aff"], s0 * 16, [[16, 128], [128 * 16, n // 128], [1, 16]]), aff)
        P.pop()
    P.pop()


B_INPUTS = [("s5_lam_re", [2, 32, 64]), ("s5_lam_im", [2, 32, 64]), ("s5_log_dt", [2, 32]), ("s5_b_re", [2, 32, 64, 16]),
            ("s5_b_im", [2, 32, 64, 16]), ("s5_c_re", [2, 32, 16, 64]), ("s5_c_im", [2, 32, 16, 64]), ("s5_d", [512]),
            ("s5_b_glu", [512]), ("ssd_conv_w", [5, 1024]), ("ssd_conv_b", [1024]), ("ssd_a_log", [2, 8]),
            ("ssd_dt_bias", [2, 8]), ("ssd_d", [8]), ("ssd_norm_g", [512]), ("attn_sink", [8]), ("norm1_g", [1024]),
            ("norm2_g", [1024]), ("w_router", [1024, 16]), ("modT", [128, 48, 2]), ("flags", [128, 2])]
B_INPUTS_BF = [("w_in", [1024, IN_W]), ("s5_w_glu", [512, 512]), ("w_branch", [3 * 512, 1024]), ("w_out", [1024, 1024])]


def declare_inputs(g, lst, dt):
    for name, shp in lst:
        g.dram[name] = g.nc.dram_tensor(name, shp, dt, kind="ExternalInput").ap()


def phase0_h(g, NCX, NC):
    P = g.P
    T = NC * 128
    for it_, (c, m, is_ctx) in enumerate([(c, m, False) for (c, m) in col_tiles(0, T + 256)] + [(c, m, True) for (c, m) in col_tiles(0, NCX * 128)]):
        P.iter_push(it_, "ph0")
        xt = P.sb([128, 8, m])
        src = g.dram["xcT"] if is_ctx else g.dram["xT"]
        xw = NCX * 128 if is_ctx else T + 256
        P.dma(xt, DV(src, c, [[xw, 128], [128 * xw, 8], [1, m]]))
        ht = P.sb([128, 8, m], BF16)
        mod_norm(g, xt, m, g.a1, g.b1, ht, 1 if is_ctx else 0)
        e0 = (g.e_ctx + c) if is_ctx else c
        P.dma(DV(g.hT_d, e0, [[g.E, 128], [128 * g.E, 8], [1, m]]), ht)
        P.iter_pop(it_, "ph0")


def s5_phase(g, NCX, NC, carry_fn=None, states_cb=None, states_only=False):
    P = g.P
    dr = g.dram
    NS = (NCX + NC) * 128
    P.push()
    uT = P.sb([128, 4, NS], BF16)
    aT = P.sb([128, 4, NS], BF16)
    P.push()
    wu = load_w(g, "w_in", 0, 1024, C_U, 512)
    for (s0, n, is_ctx) in s_tiles(NCX, NC):
        P.push()
        ht = load_h(g, s2e(g, NCX, s0, is_ctx), n)
        for j in range(4):
            ps = g.pbank()
            proj(g, ps[:, 0:n], wu, j * 128, ht, n)
            P.copy(uT[:, j, s0:s0 + n], ps[:, 0:n], eng="act" if j % 2 else "dve")
        P.pop()
    P.pop()
    dcol = P.sb([128, 4]); bglu = P.sb([128, 4])
    P.dma(dcol, DV(dr["s5_d"], 0, [[1, 128], [128, 4]]), allow_slow_non_contiguous=True)
    P.dma(bglu, DV(dr["s5_b_glu"], 0, [[1, 128], [128, 4]]), allow_slow_non_contiguous=True)

    def ya_cb(b, yacc):
        P.stt(yacc, uT[:, b, :], dcol[:, b:b + 1], yacc, ALU.mult, ALU.add)
        P.act(aT[:, b, :], yacc, AF.Gelu)

    if carry_fn is None:
        carry_fn = lambda: (g.s5.fin_re[:, :, 0], g.s5.fin_im[:, :, 0])
    s5_core(g, uT, NCX, NC, carry_fn, ya_cb, states_cb, states_only)
    if states_only:
        P.pop()
        return
    wgl = load_w(g, "s5_w_glu", 0, 512, 0, 512)
    for (s0, n) in col_tiles(0, NS):
        P.push()
        yo = P.sb([128, 4, n], BF16)
        for j in range(4):
            ps = g.pbank()
            for k in range(4):
                P.mm(ps[:, 0:n], wgl[:, k, j * 128:(j + 1) * 128], aT[:, k, s0:s0 + n], start=(k == 0), stop=(k == 3))
            gt = P.sb([128, n])
            P.act(gt, ps[:, 0:n], AF.Sigmoid, bias=bglu[:, j:j + 1])
            P.tt(yo[:, j, :], gt, aT[:, j, s0:s0 + n], ALU.mult)
        P.dma(DV(g.y_d[0], s0, [[NS, 128], [128 * NS, 4], [1, n]]), yo)
        P.pop()
    P.pop()


def build_B(T, debug=False, multi=False, mode="B"):
    nc = bass.Bass("TRN2", target_bir_lowering=False)
    g = G(); g.nc = nc; g.P = Prog(nc); P = g.P
    P.init_arenas(18 * 1024, 62 * 1024)
    NCX = 2; NC = T // 128; NS = (NCX + NC) * 128
    g.E = T + 512; g.e_own = 128; g.e_ctx = T + 256
    g.dram = {}
    consts = dict(CONST_SHAPES); consts["pswap"] = [128, 128]
    declare_inputs(g, list(consts.items()), F32)
    declare_inputs(g, B_INPUTS, F32)
    declare_inputs(g, B_INPUTS_BF, BF16)
    declare_inputs(g, [("s5_vr", [128, 32, 128]), ("s5_vi", [128, 32, 128]), ("s5_w2re", [128, 32, 128]), ("s5_nw2im", [128, 32, 128]),
                       ("s5_kt", [4, 2, 2, 128, 1024])], BF16)
    declare_inputs(g, [("xT", [8, 128, T + 256]), ("xcT", [8, 128, 256]), ("rope_cos", [128, T + 256]), ("rope_sin", [128, T + 256])], F32)
    if mode == "B":
        g.dram["x1T"] = nc.dram_tensor("x1T", [8, 128, NS], F32, kind="ExternalOutput").ap()
        g.dram["aff"] = nc.dram_tensor("aff", [NS, 16], F32, kind="ExternalOutput").ap()
        if multi:
            declare_inputs(g, [("s5_fin_all", [NCORES, 128, 64]), ("ssd_fin_all", [NCORES, 2, 128, 512]), ("ssd_tot_all", [NCORES, 128, 16]),
                               ("onehot", [128, NCORES])], F32)
    else:
        g.dram["s5_fin"] = nc.dram_tensor("s5_fin", [128, 64], F32, kind="ExternalOutput").ap()
        g.dram["ssd_fin"] = nc.dram_tensor("ssd_fin", [2, 128, 512], F32, kind="ExternalOutput").ap()
        g.dram["ssd_tot"] = nc.dram_tensor("ssd_tot", [128, 16], F32, kind="ExternalOutput").ap()
    g.hT_d = nc.dram_tensor("hT_scr", [8, 128, g.E], BF16).ap()
    g.ssd_hb_d = nc.dram_tensor("ssd_hb_scr", [NCX + NC, 128, 512], BF16).ap()
    kind = "ExternalOutput" if debug else "Internal"
    g.y_d = [nc.dram_tensor(f"y{k}_scr", [4, 128, NS], BF16, kind=kind).ap() for k in range(3)]
    setup_psum(g)
    CONST_SHAPES2 = consts
    g.c = {}
    for k, shp in CONST_SHAPES2.items():
        t = P.sb(shp, F32)
        P.dma(t, g.dram[k])
        g.c[k] = t
    g.ident_bf = P.sb([128, 128], BF16); P.copy(g.ident_bf, g.c["ident"])
    g.ones_bf = P.sb([128, 128], BF16); P.memset(g.ones_bf, 1.0)
    g.zeros_bf = P.sb([128, 128], BF16); P.memset(g.zeros_bf, 0.0)
    g.c["ones_f"] = P.sb([128, 128], F32); P.memset(g.c["ones_f"], 1.0)
    g.eps_col = P.sb([128, 1]); P.memset(g.eps_col, 1e-6)
    g.zero_col = P.sb([128, 1]); P.memset(g.zero_col, 0.0)
    fl = P.sb([128, 2]); P.dma(fl, g.dram["flags"])
    g.flagL = fl[:, 0:1]; g.flagR = fl[:, 1:2]
    import os
    ph = os.environ.get("PH", "s5,ssd,att,merge").split(",")
    load_mod(g)
    phase0_h(g, NCX, NC)
    if multi and mode == "B":
        g.onehot = P.sb([128, NCORES]); P.dma(g.onehot, g.dram["onehot"])
    if mode == "A":
        def dump_fin():
            t_ = P.sb([128, 32, 2])
            P.copy(t_[:, :, 0], g.s5.fin_re[:, :, 1]); P.copy(t_[:, :, 1], g.s5.fin_im[:, :, 1])
            P.dma(g.dram["s5_fin"], V(t_, [[1, 64]]))
        s5_phase(g, NCX, NC, states_cb=dump_fin, states_only=True)
        P.push()
        ssd_prep(g, NCX, NC, need_z=False)
        ssd_local_finals(g, NCX, NC)
        P.pop()
        P.wait_all(); P.emit(); P.close()
        return nc, g
    if "s5" in ph:
        s5_phase(g, NCX, NC, carry_fn=(lambda: s5_carry_chain(g, NC)) if multi else None)
    if "ssd" in ph:
        P.push()
        ssd_prep(g, NCX, NC)
        ssd_run(g, NCX, NC, multi=multi, yb_cb=lambda c, yo: P.dma(DV(g.y_d[1], c * 128, [[NS, 128], [128 * NS, 4], [1, 128]]), yo))
        P.pop()
    if "att" in ph:
        attn_run(g, NCX, NC, lambda qb, yo: P.dma(DV(g.y_d[2], qb * 128, [[NS, 128], [128 * NS, 4], [1, 128]]), yo))
    if "merge" in ph:
        merge_run(g, NCX, NC)
    P.wait_all(); P.emit(); P.close()
    return nc, g


def topk_threshold(g, aff, nblk, cap, tau, iters=30):
    P = g.P
    P.push()
    lo = P.sb([128, 16]); mid = P.sb([128, 16]); cmp_ = P.sb([128, 16, nblk])
    cnt = P.sb([128, 16]); ge = P.sb([128, 16])
    P.memset(lo, 0.0)
    affv = V(aff, [[1, 16], [16, nblk]])
    for it in range(iters):
        cst = 2.0 ** (-(it + 1))
        P.ts(mid, lo, cst, None, op0=ALU.add)
        P.tt(cmp_, affv, V(mid, [[1, 16], [0, nblk]]), ALU.is_ge)
        P.reduce(cnt, cmp_)
        ps = g.pbank()
        P.mm(ps[:, 0:16], g.c["ones_f"], cnt)
        P.ts(ge, ps[:, 0:16], float(cap) - 0.5, cst, op0=ALU.is_ge, op1=ALU.mult)
        P.tt(lo, lo, ge, ALU.add)
    P.copy(tau, lo)
    P.pop()


C_INPUTS = [("norm2_g", [1024]), ("modT", [128, 48, 2]), ("final_norm_g", [1024]), ("ident", [128, 128])]
C_INPUTS_BF = [("w_e_gate", [16 * 1024, 1024]), ("w_e_up", [16 * 1024, 1024]), ("w_e_down", [16 * 1024, 1024])]


def build_C(T, n_total):
    nc = bass.Bass("TRN2", target_bir_lowering=False)
    g = G(); g.nc = nc; g.P = Prog(nc); P = g.P
    P.init_arenas(22 * 1024, 50 * 1024)
    NCX = 2; NC = T // 128; NS = (NCX + NC) * 128
    g.dram = {}
    declare_inputs(g, C_INPUTS, F32)
    declare_inputs(g, C_INPUTS_BF, BF16)
    declare_inputs(g, [("x1T", [8, 128, NS]), ("aff_all", [n_total, 16]), ("aff_own", [NS, 16])], F32)
    x2_d = nc.dram_tensor("x2T", [8, 128, NS], F32, kind="ExternalOutput").ap()
    fin_d = nc.dram_tensor("finT", [8, 128, NS], F32, kind="ExternalOutput").ap()
    setup_psum(g)
    g.c = {}
    g.c["ident"] = P.sb([128, 128]); P.dma(g.c["ident"], g.dram["ident"])
    g.ones_bf = P.sb([128, 128], BF16); P.memset(g.ones_bf, 1.0)
    g.c["ones_f"] = P.sb([128, 128], F32); P.memset(g.c["ones_f"], 1.0)
    g.eps_col = P.sb([128, 1]); P.memset(g.eps_col, 1e-6)
    load_mod(g)
    gfin = P.sb([128, 8]); P.dma(gfin, DV(g.dram["final_norm_g"], 0, [[1, 128], [128, 8]]), allow_slow_non_contiguous=True)
    tau = P.sb([128, 16]); tauc = P.sb([128, 16])
    nblk = n_total // 128
    P.push()
    affa = P.sb([128, nblk, 16])
    P.dma(affa, DV(g.dram["aff_all"], 0, [[16, 128], [2048, nblk], [1, 16]]))
    topk_threshold(g, affa, nblk, 2 * n_total // 16, tau)
    P.pop()
    tiles = [(c, m, True) for (c, m) in col_tiles(0, NCX * 128, 256)] + [(c, m, False) for (c, m) in col_tiles(NCX * 128, T, 512)]
    GROUP = 3
    groups = [tiles[i:i + GROUP] for i in range(0, len(tiles), GROUP)]
    affc = P.sb([128, NCX, 16])
    P.dma(affc, DV(g.dram["aff_own"], 0, [[16, 128], [2048, NCX], [1, 16]]))
    topk_threshold(g, affc, NCX, 2 * NCX * 128 // 16, tauc)
    for grp in groups:
        P.push()
        ncols = sum(n for (_, n, _) in grp)
        gs0 = grp[0][0]
        yacc = P.sb([128, 8, ncols])
        h2b = P.sb([128, 8, ncols], BF16)
        coef = P.sb([128, ncols // 128, 16])
        xres = P.sb([128, 8, ncols]) if False else None
        for (s0, n, is_ctx) in grp:
            P.push()
            o = s0 - gs0
            xt = P.sb([128, 8, n]); P.dma(xt, DV(g.dram["x1T"], s0, [[NS, 128], [128 * NS, 8], [1, n]]))
            mod_norm(g, xt, n, g.a2, g.b2, h2b[:, :, o:o + n], 1 if is_ctx else 0)
            aff = P.sb([128, n // 128, 16])
            P.dma(aff, DV(g.dram["aff_own"], s0 * 16, [[16, 128], [2048, n // 128], [1, 16]]))
            tb = V(tauc if is_ctx else tau, [[0, n // 128], [1, 16]])
            msk = P.sb([128, n // 128, 16])
            P.tt(msk, aff, tb, ALU.is_ge)
            P.tt(coef[:, o // 128:(o + n) // 128, :], aff, msk, ALU.mult)
            P.pop()
        for e in range(16):
            P.push()
            wg = load_w(g, "w_e_gate", e * 1024, 1024, 0, 1024)
            wu = load_w(g, "w_e_up", e * 1024, 1024, 0, 1024)
            wd = load_w(g, "w_e_down", e * 1024, 1024, 0, 1024)
            for (s0, n, is_ctx) in grp:
                P.push()
                o = s0 - gs0
                pcb = g.pbank()
                for blk in range(n // 128):
                    cb_ = coef[:, o // 128 + blk, e:e + 1]
                    P.mm(pcb[:, blk * 128:(blk + 1) * 128], V(cb_, [[0, 128]]), g.c["ident"])
                cbs = P.sb([128, n])
                P.copy(cbs, pcb[:, 0:n], eng="act")
                hid = P.sb([128, 8, n], BF16)
                for f in range(8):
                    pg = g.pbank(); pu = g.pbank()
                    for k in range(8):
                        P.mm(pg[:, 0:n], wg[:, k, f * 128:(f + 1) * 128], h2b[:, k, o:o + n], start=(k == 0), stop=(k == 7))
                    for k in range(8):
                        P.mm(pu[:, 0:n], wu[:, k, f * 128:(f + 1) * 128], h2b[:, k, o:o + n], start=(k == 0), stop=(k == 7))
                    sg = P.sb([128, n])
                    P.act(sg, pg[:, 0:n], AF.Silu)
                    P.tt(sg, sg, pu[:, 0:n], ALU.mult)
                    P.tt(hid[:, f, :], sg, cbs, ALU.mult, eng="pool")
                for j in range(8):
                    pd_ = g.pbank()
                    for f in range(8):
                        P.mm(pd_[:, 0:n], wd[:, f, j * 128:(j + 1) * 128], hid[:, f, :], start=(f == 0), stop=(f == 7))
                    if e == 0:
                        P.copy(yacc[:, j, o:o + n], pd_[:, 0:n], eng="act")
                    else:
                        P.tt(yacc[:, j, o:o + n], yacc[:, j, o:o + n], pd_[:, 0:n], ALU.add)
                P.pop()
            P.pop()
        for (s0, n, is_ctx) in grp:
            P.push()
            o = s0 - gs0
            v = 1 if is_ctx else 0
            xt = P.sb([128, 8, n]); P.dma(xt, DV(g.dram["x1T"], s0, [[NS, 128], [128 * NS, 8], [1, n]]))
            for j in range(8):
                P.stt(xt[:, j, :], yacc[:, j, o:o + n], g.g2[:, j, v:v + 1], xt[:, j, :], ALU.mult, ALU.add)
            P.dma(DV(x2_d, s0, [[NS, 128], [128 * NS, 8], [1, n]]), xt)
            rstd = P.sb([128, n])
            rms_rstd(g, xt, n, rstd)
            for j in range(8):
                P.stt(xt[:, j, :], xt[:, j, :], gfin[:, j:j + 1], rstd, ALU.mult, ALU.mult)
            P.dma(DV(fin_d, s0, [[NS, 128], [128 * NS, 8], [1, n]]), xt)
            P.pop()
        P.pop()
    P.wait_all(); P.emit(); P.close()
    return nc, g


NCORES = 8


def s5_carry_chain(g, NC):
    P = g.P
    s = g.s5
    NCX = 2
    dre = P.sb([128, 32]); dim_ = P.sb([128, 32]); t1 = P.sb([128, 32]); t2 = P.sb([128, 32])
    for half, last in ((slice(0, 64), NCX + NC - 1), (slice(64, 128), NCX)):
        cmul_acc(P, dre[half], dim_[half], s.aqr[half], s.aqi[half], s.apow_re[half, :, last], s.apow_im[half, :, last],
                 None, None, t1[half], t2[half])
    fin = P.sb([128, NCORES, 32, 2])
    P.dma(fin, DV(g.dram["s5_fin_all"], 0, [[64, 128], [128 * 64, NCORES], [1, 64]]))
    H = P.sb([128, NCORES + 1, 32, 2])
    P.copy(H[:, 0, :, 0], s.fin_re[:, :, 0]); P.copy(H[:, 0, :, 1], s.fin_im[:, :, 0])
    for t in range(NCORES):
        for half, m in ((slice(0, 64), t), (slice(64, 128), NCORES - 1 - t)):
            cmul_acc(P, H[half, t + 1, :, 0], H[half, t + 1, :, 1], dre[half], dim_[half], H[half, t, :, 0], H[half, t, :, 1],
                     fin[half, m, :, 0], fin[half, m, :, 1], t1[half], t2[half])
    cr = P.sb([128, 32]); ci = P.sb([128, 32])
    P.memset(cr, 0.0); P.memset(ci, 0.0)
    for t in range(NCORES):
        for half, oh in ((slice(0, 64), g.onehot[0:64, t:t + 1]), (slice(64, 128), g.onehot[64:128, NCORES - 1 - t:NCORES - t])):
            P.stt(cr[half], H[half, t, :, 0], oh, cr[half], ALU.mult, ALU.add)
            P.stt(ci[half], H[half, t, :, 1], oh, ci[half], ALU.mult, ALU.add)
    return cr, ci


def ssd_carry(g, dr_, hctx, out):
    P = g.P
    P.push()
    tot = P.sb([128, NCORES, 16])
    P.dma(tot, DV(g.dram["ssd_tot_all"], 0, [[16, 128], [128 * 16, NCORES], [1, 16]]))
    P.act(tot, tot, AF.Exp)
    H = P.sb([128, 512]); fin = P.sb([128, 512])
    P.copy(H, hctx)
    P.memset(out, 0.0)
    for t in range(NCORES):
        m = t if dr_ == 0 else NCORES - 1 - t
        P.stt(out, H, g.onehot[:, m:m + 1], out, ALU.mult, ALU.add)
        if t == NCORES - 1:
            break
        P.dma(fin, DV(g.dram["ssd_fin_all"], (m * 2 + dr_) * 128 * 512, [[512, 128], [1, 512]]))
        P.tt(V(H, [[64, 8], [1, 64]]), V(H, [[64, 8], [1, 64]]), V(tot[:, m, dr_ * 8:dr_ * 8 + 8], [[1, 8], [0, 64]]), ALU.mult)
        P.tt(H, H, fin, ALU.add)
    P.pop()


def ssd_local_finals(g, NCX, NC):
    P = g.P
    hf = P.sb([128, 512]); hb = P.sb([128, 512]); ts_ = P.sb([128, 16]); pb = P.sb([128, 8]); tmp = P.sb([128, 512])
    P.memset(hf, 0.0); P.memset(hb, 0.0); P.memset(ts_, 0.0); P.memset(pb, 1.0)
    for i in range(NC):
        c = NCX + i
        P.push()
        S, cd, tot = ssd_chunk(g, c, False, None, None)
        P.tt(V(hf, [[64, 8], [1, 64]]), V(hf, [[64, 8], [1, 64]]), V(cd[:, 0:8], [[1, 8], [0, 64]]), ALU.mult)
        P.tt(hf, hf, S[0], ALU.add)
        P.tt(V(tmp, [[64, 8], [1, 64]]), V(S[1], [[64, 8], [1, 64]]), V(pb, [[1, 8], [0, 64]]), ALU.mult)
        P.tt(hb, hb, tmp, ALU.add)
        P.tt(pb, pb, cd[:, 8:16], ALU.mult)
        P.tt(ts_, ts_, tot, ALU.add)
        P.pop()
    P.dma(g.dram["ssd_fin"][0], hf); P.dma(g.dram["ssd_fin"][1], hb); P.dma(g.dram["ssd_tot"], ts_)


W_LIST = [("w_in", 4 * 1024, IN_W), ("s5_w_glu", 4 * 512, 512), ("w_branch", 4 * 1536, 1024), ("w_out", 4 * 1024, 1024),
          ("w_e_gate", 4 * 16 * 1024, 1024), ("w_e_up", 4 * 16 * 1024, 1024), ("w_e_down", 4 * 16 * 1024, 1024)]


def build_W(wlist=W_LIST):
    nc = bass.Bass("TRN2", target_bir_lowering=False)
    g = G(); g.nc = nc; g.P = Prog(nc); P = g.P
    P.init_arenas(24 * 1024, 24 * 1024)
    setup_psum(g)
    g.dram = {}
    engs = ["dve", "act", "pool"]
    ei = 0
    for (name, rows, cols) in wlist:
        rpc = rows // NCORES
        assert rpc % 128 == 0
        src = nc.dram_tensor(name, [rpc, cols], F32, kind="ExternalInput").ap()
        dst = nc.dram_tensor(name + "_bf", [rpc, cols], BF16, kind="ExternalOutput").ap()
        rt = rpc // 128
        cc = cols
        while cc > 2048:
            cc //= 2
        rstep = max(1, 4096 // cc)
        for c0 in range(0, cols, cc):
            for r0 in range(0, rt, rstep):
                r = min(rstep, rt - r0)
                P.iter_push(ei, "wcast")
                a = P.sb([128, rstep, cc]); b = P.sb([128, rstep, cc], BF16)
                a = a[:, 0:r]; b = b[:, 0:r]
                P.dma(a, DV(src, r0 * 128 * cols + c0, [[cols, 128], [128 * cols, r], [1, cc]]))
                P.copy(b, a, eng=engs[ei % 3])
                P.dma(DV(dst, r0 * 128 * cols + c0, [[cols, 128], [128 * cols, r], [1, cc]]), b)
                P.iter_pop(ei, "wcast"); ei += 1
    g.NG = 16
    declare_inputs(g, [("s5_lam_re", [2, 16, 64]), ("s5_lam_im", [2, 16, 64]), ("s5_log_dt", [2, 16]), ("s5_b_re", [2, 16, 64, 16]),
                       ("s5_b_im", [2, 16, 64, 16]), ("s5_c_re", [2, 16, 16, 64]), ("s5_c_im", [2, 16, 16, 64]),
                       ("exl", [128, 128]), ("exr", [128, 128]), ("exv", [128, 2])], F32)
    g.c = {}
    for k_ in ("exl", "exr", "exv"):
        t_ = P.sb(CONST_SHAPES[k_], F32); P.dma(t_, g.dram[k_]); g.c[k_] = t_
    outs = {nm: nc.dram_tensor(nm, [128, 16, 128], BF16, kind="ExternalOutput").ap() for nm in ("s5_vr", "s5_vi", "s5_w2re", "s5_nw2im")}
    kt_o = nc.dram_tensor("s5_kt", [2, 2, 2, 128, 1024], BF16, kind="ExternalOutput").ap()
    P.push()
    s5_params(g)
    P.push()
    vr = P.sb([128, 16, 128], BF16); vi = P.sb([128, 16, 128], BF16)
    s5_gen_V(g, V(vr, [[128, 16], [64, 2], [1, 64]]), V(vi, [[128, 16], [64, 2], [1, 64]]))
    P.dma(outs["s5_vr"], vr); P.dma(outs["s5_vi"], vi)
    w2 = P.sb([128, 16, 128], BF16); nw2 = P.sb([128, 16, 128], BF16)
    s5_gen_E(g, g.c["exr"], w2, nw2, neg_im=True)
    P.dma(outs["s5_w2re"], w2); P.dma(outs["s5_nw2im"], nw2)
    P.pop()
    s5_kt_build(g, 2, kt_o)
    P.pop()
    wm = nc.dram_tensor("w_mod", [1024, 6144], F32, kind="ExternalInput").ap()
    bm = nc.dram_tensor("b_mod", [6144], F32, kind="ExternalInput").ap()
    cc_ = nc.dram_tensor("c2", [2, 1024], F32, kind="ExternalInput").ap()
    mo = nc.dram_tensor("modT", [128, 48, 2], F32, kind="ExternalOutput").ap()
    sc = P.sb([128, 8, 2])
    for v in range(2):
        P.dma(sc[:, :, v], DV(cc_, v * 1024, [[1, 128], [128, 8]]), allow_slow_non_contiguous=True)
    P.act(sc, sc, AF.Silu)
    bcol = P.sb([128, 48])
    P.dma(bcol, DV(bm, 0, [[1, 128], [128, 48]]), allow_slow_non_contiguous=True)
    ps = g.pbank()
    for grp in range(12):
        P.push()
        wt = P.sb([128, 8, 512])
        P.dma(wt, DV(wm, grp * 512, [[6144, 128], [128 * 6144, 8], [1, 512]]))
        for q in range(4):
            ccix = grp * 4 + q
            for k in range(8):
                P.mm(ps[:, ccix * 2:ccix * 2 + 2], wt[:, k, q * 128:(q + 1) * 128], sc[:, k, :], start=(k == 0), stop=(k == 7))
        P.pop()
    mt = P.sb([128, 48, 2])
    P.tt(mt, V(ps, [[2, 48], [1, 2]]), V(bcol, [[1, 48], [0, 2]]), ALU.add)
    P.dma(mo, mt)
    P.wait_all(); P.emit(); P.close()
    return nc, g


def setup_psum(g):
    P = g.P
    g._pb = [P.ps([128, 512], F32) for _ in range(6)]
    g._pb_o = g._pb[4][:, :]
    g._pb_d = g._pb[5][:, :]
    g._nrot = 6
    g._pbf = [P.ps([128, 1024], BF16) for _ in range(2)]
    g._pi = 0
    g._pbi = 0

    def pbank():
        t = g._pb[g._pi % g._nrot]
        g._pi += 1
        return t[:, :]

    def pbank_bf():
        h = g._pbi % 2
        g._pbi += 1
        return g._pbf[h][:, 0:512]
    g.pbank = pbank
    g.pbank_bf = pbank_bf


BF=ml_dtypes.bfloat16

def rope_tables(own_start, T, n_total):
    NL=T+256
    pos=own_start+np.arange(NL)-128
    valid=(pos>=0)&(pos<n_total)
    pos=np.where(valid,pos,0)
    d=np.arange(64); which=d//32; i=d%16; first=(d%32)<16
    inv=10000.0**(-(i.astype(np.float64))/16)
    axis=np.where(which[:,None]==0, (pos//64)[None,:], (pos%64)[None,:]).astype(np.float64)
    ang=axis*inv[:,None]
    cos=np.cos(ang); sin=np.where(first[:,None], -np.sin(ang), np.sin(ang))
    return np.tile(cos,(2,1)).astype(np.float32), np.tile(sin,(2,1)).astype(np.float32)

def pswap():
    m=np.zeros((128,128),np.float32)
    for j in range(128):
        p = j+16 if (j%32)<16 else j-16
        m[p,j]=1.0
    return m

def fm(a, ncols):
    return np.ascontiguousarray(a.T.reshape(8,128,ncols))

def consts_all():
    c=host_consts(); c["pswap"]=pswap(); return c

def modT_from(mod2):
    return np.ascontiguousarray(mod2.reshape(2,48,128).transpose(2,1,0)).astype(np.float32)


import numpy as np, ml_dtypes
import os

_CACHE = {}


def _run(nc_, maps, ids):
    if os.environ.get('ORCH_TRACE'):
        r = run_bass_kernel_spmd(nc_, maps, core_ids=ids, trace=True)
        print('exec_time_ns', r.exec_time_ns, flush=True)
        return r
    return run_bass_kernel_spmd(nc_, maps, core_ids=ids)

def _prog(key, fn):
    if key not in _CACHE:
        _CACHE[key] = fn()[0]
    return _CACHE[key]

def run_model(inputs, T, ncores=NCORES, depth=4, hook=None):
    assert ncores == NCORES
    n = ncores * T
    NS = T + 256
    f32 = lambda a: np.ascontiguousarray(np.asarray(a, dtype=np.float32))
    ncW = _prog(("W",), build_W)
    flat = {"w_in": f32(inputs["w_in"]).reshape(-1, IN_W), "s5_w_glu": f32(inputs["s5_w_glu"]).reshape(-1, 512),
            "w_branch": f32(inputs["w_branch"]).reshape(-1, 1024), "w_out": f32(inputs["w_out"]).reshape(-1, 1024),
            "w_e_gate": f32(inputs["w_e_gate"]).reshape(-1, 1024), "w_e_up": f32(inputs["w_e_up"]).reshape(-1, 1024),
            "w_e_down": f32(inputs["w_e_down"]).reshape(-1, 1024)}
    c2 = np.stack([f32(inputs["c"])[0], f32(inputs["c_ctx"])], 0)
    maps = []
    for k in range(ncores):
        m = {}
        for name, rows, cols in W_LIST:
            rpc = rows // ncores
            m[name] = np.ascontiguousarray(flat[name][k * rpc:(k + 1) * rpc])
        nl = 4
        l = k % nl
        m["w_mod"] = f32(inputs["w_mod"][l]); m["b_mod"] = f32(inputs["b_mod"][l]); m["c2"] = c2
        hf_ = k // nl
        gsl = slice(hf_ * 16, hf_ * 16 + 16)
        for nm in ("s5_lam_re", "s5_lam_im", "s5_log_dt", "s5_b_re", "s5_b_im", "s5_c_re", "s5_c_im"):
            m[nm] = np.ascontiguousarray(f32(inputs[nm][l])[:, gsl])
        hc_ = host_consts()
        for nm in ("exl", "exr", "exv"):
            m[nm] = hc_[nm]
        maps.append(m)
    res = _run(ncW, maps, list(range(ncores))).results
    wbf = {name: np.concatenate([np.asarray(res[k][name + "_bf"]) for k in range(ncores)], 0) for name, _, _ in W_LIST}
    nl = 4
    modT = [np.asarray(res[l]["modT"]) for l in range(nl)]
    s5tab = []
    for l in range(nl):
        d_ = {nm: np.concatenate([np.asarray(res[l][nm]), np.asarray(res[l + nl][nm])], 1) for nm in ("s5_vr", "s5_vi", "s5_w2re", "s5_nw2im")}
        d_["s5_kt"] = np.concatenate([np.asarray(res[l]["s5_kt"]), np.asarray(res[l + nl]["s5_kt"])], 0)
        s5tab.append(d_)
    if hook: hook("W", dict(wbf=wbf, modT=modT))
    consts = consts_all()
    ropes = [rope_tables(k * T, T, n) for k in range(ncores)]
    flags = []
    onehots = []
    for k in range(ncores):
        fl = np.ones((128, 2), np.float32)
        if k == 0: fl[:, 0] = 0
        if k == ncores - 1: fl[:, 1] = 0
        flags.append(fl)
        oh = np.zeros((128, ncores), np.float32); oh[:, k] = 1
        onehots.append(oh)
    x = f32(inputs["x"])[0]
    xc = f32(inputs["ctx"])[0]
    ncA = _prog(("A", T), lambda: build_B(T, multi=True, mode="A"))
    ncB = _prog(("B", T), lambda: build_B(T, multi=True, mode="B"))
    ncC = _prog(("C", T, n), lambda: build_C(T, n))
    fin = None
    for l in range(depth):
        lw = lambda name: f32(inputs[name][l])
        base = dict(consts)
        for name, _ in B_INPUTS:
            if name in inputs: base[name] = lw(name)
        base["modT"] = modT[l]
        base.update(s5tab[l])
        base["w_in"] = wbf["w_in"][l * 1024:(l + 1) * 1024]
        base["s5_w_glu"] = wbf["s5_w_glu"][l * 512:(l + 1) * 512]
        base["w_branch"] = wbf["w_branch"][l * 1536:(l + 1) * 1536]
        base["w_out"] = wbf["w_out"][l * 1024:(l + 1) * 1024]
        xpad = np.concatenate([np.zeros((128, 1024), np.float32), x, np.zeros((128, 1024), np.float32)], 0)
        xcT = fm(xc, 256)
        maps = []
        for k in range(ncores):
            m = dict(base)
            m["flags"] = flags[k]
            m["xT"] = fm(xpad[k * T:k * T + T + 256], T + 256)
            m["xcT"] = xcT
            m["rope_cos"], m["rope_sin"] = ropes[k]
            maps.append(m)
        ra = _run(ncA, maps, list(range(ncores))).results
        s5_fin_all = np.stack([np.asarray(ra[k]["s5_fin"]) for k in range(ncores)], 0)
        ssd_fin_all = np.stack([np.asarray(ra[k]["ssd_fin"]) for k in range(ncores)], 0)
        ssd_tot_all = np.stack([np.asarray(ra[k]["ssd_tot"]) for k in range(ncores)], 0)
        for k in range(ncores):
            maps[k]["s5_fin_all"] = s5_fin_all; maps[k]["ssd_fin_all"] = ssd_fin_all; maps[k]["ssd_tot_all"] = ssd_tot_all
            maps[k]["onehot"] = onehots[k]
        rb = _run(ncB, maps, list(range(ncores))).results
        aff_all = np.concatenate([np.asarray(rb[k]["aff"])[256:] for k in range(ncores)], 0)
        if hook: hook(("B", l), dict(rb=rb))
        cmaps = []
        for k in range(ncores):
            cm = {"ident": consts["ident"], "norm2_g": lw("norm2_g"), "modT": modT[l], "final_norm_g": f32(inputs["final_norm_g"]),
                  "w_e_gate": wbf["w_e_gate"][l * 16384:(l + 1) * 16384], "w_e_up": wbf["w_e_up"][l * 16384:(l + 1) * 16384],
                  "w_e_down": wbf["w_e_down"][l * 16384:(l + 1) * 16384],
                  "x1T": np.asarray(rb[k]["x1T"]), "aff_all": aff_all, "aff_own": np.asarray(rb[k]["aff"])}
            cmaps.append(cm)
        rc = _run(ncC, cmaps, list(range(ncores))).results
        unfm = lambda a: np.asarray(a).reshape(1024, -1).T
        x = np.concatenate([unfm(rc[k]["x2T"])[256:] for k in range(ncores)], 0)
        xc = unfm(rc[0]["x2T"])[:256]
        fin = np.concatenate([unfm(rc[k]["finT"])[256:] for k in range(ncores)], 0)
        if hook: hook(("C", l), dict(x=x, xc=xc))
    return np.ascontiguousarray(fin[None].astype(np.float32))


def kernel(**inputs):
    return run_model(inputs, 2048)
```
